# Optimizing a Trainium2 kernel written in Bass

```python
import jax, jax.numpy as jnp
from jax import lax
import numpy as np

D_MODEL = 1024
BATCH = 32
SEQ = 2048
DEPTH = 1

D_MIX = D_MODEL
D_SGU = D_MIX // 2
SGU_GROUPS = 4
SGU_GROUP_DIM = D_SGU // SGU_GROUPS
CHUNK = 128
N_HEADS = 8
HEAD_DIM = (D_MIX - D_SGU) // N_HEADS
N_KV_HEADS = 2
GQA_GROUP = N_HEADS // N_KV_HEADS
ROT_DIM = HEAD_DIM // 4
ROPE_THETA = 500000.0
IDX_HEADS = 8
IDX_DIM = 64
TOPK_MAX = 256
Q_BLOCK = 128
D_FF = 4 * D_MODEL
EPS = 1e-6
COL_SIZES = (D_SGU, D_SGU,
             N_HEADS * HEAD_DIM,
             N_KV_HEADS * HEAD_DIM,
             N_KV_HEADS * HEAD_DIM,
             IDX_HEADS * IDX_DIM,
             IDX_DIM,
             IDX_HEADS)
D_IN = sum(COL_SIZES)

kernel_name = "hybrid_sgu_dsa_block"


def rms_norm(x, g):
    x32 = x.astype(jnp.float32)
    y = x32 * lax.rsqrt(jnp.mean(x32 * x32, axis=-1, keepdims=True) + EPS)
    return (y * g.astype(jnp.float32)).astype(x.dtype)


def rope_tables(positions):
    inv_freq = ROPE_THETA ** (-jnp.arange(0, ROT_DIM, 2, dtype=jnp.float32) / ROT_DIM)
    ang = positions.astype(jnp.float32)[..., None] * inv_freq
    return jnp.cos(ang), jnp.sin(ang)


def partial_rope(x, cos, sin):
    half = ROT_DIM // 2
    c = cos[:, :, None, :].astype(x.dtype)
    s = sin[:, :, None, :].astype(x.dtype)
    x1, x2, xp = x[..., :half], x[..., half:ROT_DIM], x[..., ROT_DIM:]
    return jnp.concatenate([x1 * c - x2 * s, x2 * c + x1 * s, xp], axis=-1)


def split_cols(p):
    outs, start = [], 0
    for n in COL_SIZES:
        outs.append(p[..., start:start + n])
        start += n
    return outs


def spatial_gating(u, v, g_v, w_s, b_s):
    B, S, _ = u.shape
    v = rms_norm(v.reshape(B, S, SGU_GROUPS, SGU_GROUP_DIM), g_v.reshape(SGU_GROUPS, SGU_GROUP_DIM))
    v = v.reshape(B, S // CHUNK, CHUNK, SGU_GROUPS, SGU_GROUP_DIM)
    causal = jnp.tril(jnp.ones((CHUNK, CHUNK), dtype=w_s.dtype))
    w_m = w_s * causal[None]
    mixed = jnp.einsum('gts,bnsgc->bntgc', w_m, v) + b_s.T[None, None, :, :, None]
    return u * mixed.reshape(B, S, D_SGU)


def dsa_attention(q, k, v, q_idx, k_idx, w_idx):
    B, S = q.shape[0], q.shape[1]
    topk = min(TOPK_MAX, S // 4)
    nb = S // Q_BLOCK
    key_pos = jnp.arange(S)

    def blocks(a):
        return a.reshape((B, nb, Q_BLOCK) + a.shape[2:]).swapaxes(0, 1)

    def one_block(args):
        qb, qib, wb, start = args
        t_pos = start + jnp.arange(Q_BLOCK)
        causal = key_pos[None, :] <= t_pos[:, None]
        dots = jnp.einsum('bthd,bsd->bths', qib, k_idx,
                          preferred_element_type=jnp.float32) * (IDX_DIM ** -0.5)
        score = jnp.einsum('bth,bths->bts', wb.astype(jnp.float32) * (IDX_HEADS ** -0.5),
                           jax.nn.relu(dots))
        score = jnp.where(causal[None], score, -jnp.inf)
        _, idx = lax.top_k(score, topk)
        sel_ok = idx <= t_pos[None, :, None]
        kg = jax.vmap(lambda kb, ib: kb[ib])(k, idx)
        vg = jax.vmap(lambda vb, ib: vb[ib])(v, idx)
        qg = qb.reshape(B, Q_BLOCK, N_KV_HEADS, GQA_GROUP, HEAD_DIM)
        logits = jnp.einsum('btkgd,btjkd->btkgj', qg, kg,
                            preferred_element_type=jnp.float32) * (HEAD_DIM ** -0.5)
        logits = jnp.where(sel_ok[:, :, None, None, :], logits, -jnp.inf)
        p = jax.nn.softmax(logits, axis=-1).astype(v.dtype)
        o = jnp.einsum('btkgj,btjkd->btkgd', p, vg)
        return o.reshape(B, Q_BLOCK, N_HEADS * HEAD_DIM)

    starts = jnp.arange(nb, dtype=jnp.int32) * Q_BLOCK
    out = lax.map(one_block, (blocks(q), blocks(q_idx), blocks(w_idx), starts))
    return out.swapaxes(0, 1).reshape(B, S, N_HEADS * HEAD_DIM)


def setup_inputs(seed: int = 0) -> dict:
    key = jax.random.key(seed)
    ks = jax.random.split(key, 24)
    f32 = jnp.float32
    L, D = DEPTH, D_MODEL

    def nrm(k, shape, scale):
        return jax.random.normal(k, shape, f32) * scale

    def gain(k, shape):
        return 1.0 + 0.02 * jax.random.normal(k, shape, f32)

    x = jax.random.normal(ks[0], (BATCH, SEQ, D), f32)
    c = jax.random.normal(ks[1], (BATCH, D), f32)
    offset = jax.random.randint(ks[2], (BATCH, 1), 0, 1024, dtype=jnp.int32)
    positions = offset + jnp.arange(SEQ, dtype=jnp.int32)[None, :]
    return {
        "x": x,
        "c": c,
        "positions": positions,
        "w_ada": nrm(ks[3], (L, D, 6 * D), D ** -0.5),
        "b_ada": nrm(ks[4], (L, 6 * D), 0.02),
        "g_pre_mix": gain(ks[5], (L, D)),
        "w_in": nrm(ks[6], (L, D, D_IN), D ** -0.5),
        "g_sgu_v": gain(ks[7], (L, D_SGU)),
        "w_spatial": nrm(ks[8], (L, SGU_GROUPS, CHUNK, CHUNK), CHUNK ** -0.5),
        "b_spatial": gain(ks[9], (L, SGU_GROUPS, CHUNK)),
        "g_out_sgu": gain(ks[10], (L, D_SGU)),
        "g_out_attn": gain(ks[11], (L, N_HEADS * HEAD_DIM)),
        "w_out": nrm(ks[12], (L, D_MIX, D), D_MIX ** -0.5),
        "g_post_mix": gain(ks[13], (L, D)),
        "g_pre_ffn": gain(ks[14], (L, D)),
        "w_ff1": nrm(ks[15], (L, D, D_FF), D ** -0.5),
        "w_ff2": nrm(ks[16], (L, D_FF, D), D_FF ** -0.5),
        "g_post_ffn": gain(ks[17], (L, D)),
    }


def reference(x, c, positions, w_ada, b_ada, g_pre_mix, w_in, g_sgu_v, w_spatial, b_spatial,
              g_out_sgu, g_out_attn, w_out, g_post_mix, g_pre_ffn, w_ff1, w_ff2, g_post_ffn):
    B, S, _ = x.shape
    cos, sin = rope_tables(positions)
    c_act = jax.nn.silu(c)
    for l in range(DEPTH):
        mod = jnp.einsum('bd,de->be', c_act, w_ada[l]) + b_ada[l]
        sh1, sc1, g1, sh2, sc2, g2 = jnp.split(mod[:, None, :], 6, axis=-1)

        h = rms_norm(x, g_pre_mix[l]) * (1.0 + sc1) + sh1
        proj = jnp.einsum('bsd,de->bse', h, w_in[l])
        p_u, p_v, p_q, p_k, p_val, p_qi, p_ki, p_wi = split_cols(proj)

        z_u, z_v = jax.nn.gelu(p_u), jax.nn.gelu(p_v)
        y_a = spatial_gating(z_u, z_v, g_sgu_v[l], w_spatial[l], b_spatial[l])

        q = partial_rope(p_q.reshape(B, S, N_HEADS, HEAD_DIM), cos, sin)
        k = partial_rope(p_k.reshape(B, S, N_KV_HEADS, HEAD_DIM), cos, sin)
        v = p_val.reshape(B, S, N_KV_HEADS, HEAD_DIM)
        q_idx = partial_rope(p_qi.reshape(B, S, IDX_HEADS, IDX_DIM), cos, sin)
        k_idx = partial_rope(p_ki.reshape(B, S, 1, IDX_DIM), cos, sin)[:, :, 0, :]
        y_b = dsa_attention(q, k, v, q_idx, k_idx, p_wi)

        merged = jnp.concatenate([rms_norm(y_a, g_out_sgu[l]), rms_norm(y_b, g_out_attn[l])], axis=-1)
        o = jnp.einsum('bse,ed->bsd', merged, w_out[l])
        x = x + g1 * rms_norm(o, g_post_mix[l])

        h2 = rms_norm(x, g_pre_ffn[l]) * (1.0 + sc2) + sh2
        f = jnp.square(jax.nn.relu(jnp.einsum('bsd,df->bsf', h2, w_ff1[l])))
        f = jnp.einsum('bsf,fd->bsd', f, w_ff2[l])
        x = x + g2 * rms_norm(f, g_post_ffn[l])
    return x
```

```python
import contextlib
import math
import numpy as np
import concourse.bass as bass
import concourse.mybir as mybir
from concourse.bass_utils import run_bass_kernel_spmd

F32 = mybir.dt.float32
BF16 = mybir.dt.bfloat16
I32 = mybir.dt.int32
AF = mybir.ActivationFunctionType
ALU = mybir.AluOpType
AX = mybir.AxisListType

D = 1024
DIN = 2376
DFF = 4096
NCORES = 8
EPS = 1e-6
NIT = 18
IDX_SCALE = (64 ** -0.5) * (8 ** -0.5)
TWO_PI = 2.0 * math.pi


class StopBuild(Exception):
    pass


class Prog:
    NDMA = 32

    def __init__(self, nc, stack):
        self.nc = nc
        self.eng = {"pe": nc.tensor, "act": nc.scalar, "dve": nc.vector,
                    "pool": nc.gpsimd, "sp": nc.sync}
        self.sem = {k: stack.enter_context(nc.semaphore("c_" + k)) for k in self.eng}
        self.cnt = {k: 0 for k in self.eng}
        self.dsem = [stack.enter_context(nc.semaphore("d%d" % i)) for i in range(self.NDMA)]
        self.dval = [0] * self.NDMA
        self.dnext = 0
        self.seen = {k: {} for k in self.eng}
        self.res = {}
        self.nwaits = 0
        self.ninstr = 0

    def _wait(self, eng, dep):
        kind, key, val = dep
        if kind == "e":
            if key == "pe" and eng == "pe":
                return
            sem = self.sem[key]
            skey = key
        else:
            sem = self.dsem[key]
            skey = ("d", key)
        if self.seen[eng].get(skey, 0) >= val:
            return
        self.seen[eng][skey] = val
        self.eng[eng].wait_ge(sem, val)
        self.nwaits += 1

    def _deps(self, eng, reads, writes):
        deps = []
        for r in reads:
            st = self.res.get(r)
            if st and st["w"]:
                deps.append(st["w"])
        for w in writes:
            st = self.res.get(w)
            if st:
                if st["w"]:
                    deps.append(st["w"])
                deps.extend(st["r"])
        for d in deps:
            self._wait(eng, d)

    def _record(self, token, reads, writes):
        for r in reads:
            st = self.res.setdefault(r, {"w": None, "r": []})
            st["r"].append(token)
            if len(st["r"]) > 48:
                best = {}
                for t in st["r"]:
                    k = (t[0], t[1])
                    if k not in best or best[k][2] < t[2]:
                        best[k] = t
                st["r"] = list(best.values())
        for w in writes:
            self.res[w] = {"w": token, "r": []}

    def op(self, eng, fn, reads=(), writes=()):
        self._deps(eng, reads, writes)
        ins = fn(self.eng[eng])
        self.cnt[eng] += 1
        ins.then_inc(self.sem[eng], 1)
        token = ("e", eng, self.cnt[eng])
        self._record(token, reads, writes)
        self.ninstr += 1
        return token

    def dma(self, out, in_, reads=(), writes=(), q="sp", **kw):
        self._deps(q, reads, writes)
        i = self.dnext
        self.dnext = (self.dnext + 1) % self.NDMA
        if self.dval[i] > 0:
            self._wait(q, ("d", i, self.dval[i]))
        ins = self.eng[q].dma_start(out=out, in_=in_, **kw)
        self.dval[i] += 16
        ins.then_inc(self.dsem[i], 16)
        token = ("d", i, self.dval[i])
        self._record(token, reads, writes)
        self.ninstr += 1
        return token

    def barrier(self):
        for e in self.eng:
            for f in self.eng:
                if f != e and self.cnt[f] > 0:
                    self._wait(e, ("e", f, self.cnt[f]))
            for i in range(self.NDMA):
                if self.dval[i] > 0:
                    self._wait(e, ("d", i, self.dval[i]))
        self.res = {}

    def finish(self):
        for i in range(self.NDMA):
            if self.dval[i] > 0:
                self._wait("sp", ("d", i, self.dval[i]))
        for f in self.eng:
            if f != "sp" and self.cnt[f] > 0:
                self._wait("sp", ("e", f, self.cnt[f]))


def build_program(nc, NSEQ=4, S=2048, taps=None, stop=None):
    NT = S // 128
    NTT = NSEQ * NT
    NTOK = NSEQ * S
    TOPK = min(256, S // 4)
    KB = TOPK // 128
    taps = taps or {}

    def din(name, shape, dt=F32):
        return nc.dram_tensor(name, list(shape), dt, kind="ExternalInput").ap()

    x_d = din("x", [NTOK, D])
    cT_d = din("cT", [128, 8, NSEQ])
    pos_d = din("pos", [128, NTT], I32)
    wada_d = din("w_ada", [D, 6 * D])
    bada_d = din("b_ada", [1, 6 * D])
    win_d = din("w_in", [D, DIN])
    gpre_d = din("gpre", [128, 8])
    gpre2_d = din("gpre2", [128, 8])
    gv_d = din("gv", [1, 512])
    ws_d = din("ws", [128, 4, 128])
    bs_d = din("bs", [128, 4])
    goa_d = din("goa", [1, 512])
    gob_d = din("gob", [1, 512])
    wout_d = din("w_out", [D, D])
    gpost_d = din("gpost", [1, D])
    w1_d = din("w1", [D, DFF])
    w2_d = din("w2", [DFF, D])
    gpost2_d = din("gpost2", [1, D])
    out_d = nc.dram_tensor("out", [NTOK, D], F32, kind="ExternalOutput").ap()
    x1s_d = out_d
    tap_d = {k: nc.dram_tensor("tap_" + k, list(shp), F32, kind="ExternalOutput").ap()
             for k, shp in taps.items()}

    with contextlib.ExitStack() as gst:
        P = Prog(nc, gst)

        uid = [0]

        def mk_sb(stack):
            def sb(name, shape, dt=F32):
                uid[0] += 1
                return stack.enter_context(nc.sbuf_tensor("s%d_%s" % (uid[0], name), list(shape), dt))
            return sb

        gsb = mk_sb(gst)
        banks = [gst.enter_context(nc.psum_tensor("bank%d" % i, [128, 512], F32)) for i in range(8)]
        bkey = ["b%d" % i for i in range(8)]
        bbf = [b[:].bitcast(BF16) for b in banks]

        def V(fn, r=(), w=()):
            return P.op("dve", fn, r, w)

        def A(fn, r=(), w=()):
            return P.op("act", fn, r, w)

        def G(fn, r=(), w=()):
            return P.op("pool", fn, r, w)

        def T(fn, r=(), w=()):
            return P.op("pe", fn, r, w)

        ident = gsb("ident", [128, 128])
        identb = gsb("identb", [128, 128], BF16)
        ones_f = gsb("ones_f", [128, 128])
        zeros_f = gsb("zeros_f", [128, 128])
        NEGM = gsb("NEGM", [128, 128])
        TRIU = gsb("TRIU", [128, 128], BF16)
        ONESB = gsb("ONESB", [128, 128], BF16)
        D32 = gsb("D32", [128, 32])
        G4 = gsb("G4", [128, 4])
        zerob = gsb("zerob", [128, 260], BF16)
        P2 = gsb("P2", [128, NIT])
        mhalf = gsb("mhalf", [128, 16])
        iot = gsb("iot", [128, 8], I32)
        iof = gsb("iof", [128, 8])
        invf = gsb("invf", [128, 8])
        rs_tmp = gsb("rs_tmp", [128, 16])

        G(lambda e: e.memset(ones_f[:], 1.0), w=["ones_f"])
        G(lambda e: e.memset(zeros_f[:], 0.0), w=["zeros_f"])
        G(lambda e: e.affine_select(out=ident[:], in_=ones_f[:], pattern=[[-1, 128]], compare_op=ALU.is_equal,
                                    fill=0.0, base=0, channel_multiplier=1), r=["ones_f"], w=["ident"])
        V(lambda e: e.tensor_copy(out=identb[:], in_=ident[:]), r=["ident"], w=["identb"])
        G(lambda e: e.affine_select(out=NEGM[:], in_=zeros_f[:], pattern=[[-1, 128]], compare_op=ALU.is_ge,
                                    fill=-1.0e30, base=0, channel_multiplier=1), r=["zeros_f"], w=["NEGM"])
        G(lambda e: e.affine_select(out=TRIU[:], in_=ones_f[:], pattern=[[1, 128]], compare_op=ALU.is_ge,
                                    fill=0.0, base=0, channel_multiplier=-1), r=["ones_f"], w=["TRIU"])
        V(lambda e: e.tensor_copy(out=ONESB[:], in_=ones_f[:]), r=["ones_f"], w=["ONESB"])
        for m in range(4):
            G(lambda e, m=m: e.affine_select(out=D32[32 * m:32 * m + 32, :], in_=ones_f[32 * m:32 * m + 32, 0:32],
                                             pattern=[[-1, 32]], compare_op=ALU.is_equal, fill=0.0, base=0,
                                             channel_multiplier=1), r=["ones_f"], w=["D32"])
        G(lambda e: e.memset(G4[:], 0.0), w=["G4"])
        for g in range(4):
            G(lambda e, g=g: e.memset(G4[32 * g:32 * g + 32, g:g + 1], 1.0), r=["G4"], w=["G4"])
        G(lambda e: e.memset(zerob[:], 0.0), w=["zerob"])
        for i in range(NIT):
            G(lambda e, i=i: e.memset(P2[:, i:i + 1], 2.0 ** -(i + 1)), w=["P2"])
        G(lambda e: e.memset(mhalf[:], -0.5), w=["mhalf"])
        G(lambda e: e.iota(iot[:], pattern=[[1, 8]], base=0, channel_multiplier=0), w=["iot"])
        V(lambda e: e.tensor_copy(out=iof[:], in_=iot[:]), r=["iot"], w=["iof"])
        A(lambda e: e.activation(out=invf[:], in_=iof[:], func=AF.Exp, scale=-math.log(500000.0) / 8.0),
          r=["iof"], w=["invf"])

        def rstd(out_ap, ss_ap, n, inv_n, rk, wk, ss2_ap=None):
            tmp = rs_tmp[:, 0:n]
            if ss2_ap is not None:
                G(lambda e: e.tensor_tensor(out=tmp, in0=ss_ap, in1=ss2_ap, op=ALU.add), r=rk, w=["rs_tmp"])
                G(lambda e: e.tensor_scalar(out=tmp, in0=tmp, scalar1=inv_n, scalar2=EPS, op0=ALU.mult,
                                            op1=ALU.add), r=["rs_tmp"], w=["rs_tmp"])
            else:
                G(lambda e: e.tensor_scalar(out=tmp, in0=ss_ap, scalar1=inv_n, scalar2=EPS, op0=ALU.mult,
                                            op1=ALU.add), r=rk, w=["rs_tmp"])
            G(lambda e: e.tensor_tensor(out=out_ap, in0=tmp, in1=mhalf[:, 0:n], op=ALU.pow),
              r=["rs_tmp", "mhalf"], w=wk)

        def ck(name):
            if stop == name:
                P.finish()
                raise StopBuild()

        def tap(name, ap, rk, rows=None):
            if name in tap_d:
                dst = tap_d[name]
                P.dma(dst if rows is None else dst[rows], ap, reads=rk)

        S1T = gsb("S1T", [128, 8, NSEQ])
        sh1T = gsb("sh1T", [128, 8, NSEQ])
        S2T = gsb("S2T", [128, 8, NSEQ])
        sh2T = gsb("sh2T", [128, 8, NSEQ])
        gmod = gsb("gmod", [NSEQ, 2, D])
        sel = gsb("sel", [NSEQ, NSEQ, 128])
        gpre = gsb("gpre", [128, 8])
        gpre2 = gsb("gpre2", [128, 8])
        P.dma(gpre[:], gpre_d[:, :], writes=["gpre"])
        P.dma(gpre2[:], gpre2_d[:, :], writes=["gpre2"])

        G(lambda e: e.affine_select(out=sel[:], in_=ones_f[0:NSEQ, :].unsqueeze(1).broadcast_to([NSEQ, NSEQ, 128]),
                                    pattern=[[-1, NSEQ], [0, 128]], compare_op=ALU.is_equal, fill=0.0, base=0,
                                    channel_multiplier=1), r=["ones_f"], w=["sel"])

        with contextlib.ExitStack() as ast:
            asb = mk_sb(ast)
            Win = asb("Win", [128, 8, DIN], BF16)
            Wout = asb("Wout", [128, 8, D], BF16)
            cs = asb("cs", [128, NTT, 8])
            sn = asb("sn", [128, NTT, 8])
            gv_row = asb("gv_row", [128, 512])
            goa_row = asb("goa_row", [128, 512])
            gob_row = asb("gob_row", [128, 512])
            bcol = asb("bcol", [128, 4])
            WmT = asb("WmT", [128, 4, 128], BF16)
            P.dma(gv_row[:], gv_d[0:1, :].partition_broadcast(128), writes=["gv_row"])
            P.dma(goa_row[:], goa_d[0:1, :].partition_broadcast(128), writes=["goa_row"])
            P.dma(gob_row[:], gob_d[0:1, :].partition_broadcast(128), writes=["gob_row"])
            P.dma(bcol[:], bs_d[:, :], writes=["bcol"])

            with contextlib.ExitStack() as sst:
                ssb = mk_sb(sst)
                cTs = ssb("cTs", [128, 8, NSEQ])
                scs = ssb("scs", [128, 8, NSEQ])
                bada4 = ssb("bada4", [NSEQ, 6 * D])
                modrow = ssb("modrow", [NSEQ, 6 * D])
                gpost4 = ssb("gpost4", [NSEQ, 2, D])
                wada_st = [ssb("wada_st%d" % i, [128, 8, 512]) for i in range(2)]
                win_st = [ssb("win_st%d" % i, [128, DIN]) for i in range(2)]
                ws_sb = ssb("ws_sb", [128, 4, 128])
                wsm = ssb("wsm", [128, 4, 128])
                posi = ssb("posi", [128, NTT], I32)
                posf = ssb("posf", [128, NTT])
                ang = ssb("ang", [128, NTT * 8])
                angk = ssb("angk", [128, NTT * 8], I32)
                angf = ssb("angf", [128, NTT * 8])
                angm = ssb("angm", [128, NTT * 8])
                ang2 = ssb("ang2", [128, NTT * 8])

                P.dma(cTs[:], cT_d[:, :, :], writes=["cTs"])
                P.dma(bada4[:], bada_d[0:1, :].partition_broadcast(NSEQ), writes=["bada4"])
                P.dma(gpost4[:, 0, :], gpost_d[0:1, :].partition_broadcast(NSEQ), writes=["gpost4a"])
                P.dma(gpost4[:, 1, :], gpost2_d[0:1, :].partition_broadcast(NSEQ), writes=["gpost4b"])
                P.dma(posi[:], pos_d[:, :], writes=["posi"])
                P.dma(ws_sb[:], ws_d[:, :, :], writes=["ws_sb"])
                A(lambda e: e.activation(out=scs[:], in_=cTs[:], func=AF.Silu), r=["cTs"], w=["scs"])

                order = [2, 3, 0, 1] + list(range(4, 12))
                for n_, cb in enumerate(order):
                    st_ = wada_st[n_ % 2]
                    sk = "wada_st%d" % (n_ % 2)
                    P.dma(st_[:], wada_d[:, cb * 512:(cb + 1) * 512].rearrange("(k p) n -> p k n", p=128),
                          writes=[sk])
                    bk = n_ % 2
                    for k in range(8):
                        T(lambda e, k=k, st_=st_, bk=bk: e.matmul(banks[bk][0:NSEQ, :], lhsT=scs[:, k, :],
                                                                  rhs=st_[:, k, :], start=(k == 0), stop=(k == 7)),
                          r=["scs", sk], w=[bkey[bk]])
                    V(lambda e, bk=bk, cb=cb: e.tensor_tensor(out=modrow[:, cb * 512:(cb + 1) * 512],
                                                              in0=banks[bk][0:NSEQ, :],
                                                              in1=bada4[:, cb * 512:(cb + 1) * 512], op=ALU.add),
                      r=[bkey[bk], "bada4"], w=["modrow%d" % cb])
                allmod = ["modrow%d" % cb for cb in range(12)]
                for si, sp_ in enumerate([0, 1, 3, 4]):
                    for k in range(8):
                        c0 = (si * 8 + k) * NSEQ
                        T(lambda e, sp_=sp_, k=k, c0=c0: e.transpose(
                            out=banks[2][:, c0:c0 + NSEQ], in_=modrow[0:NSEQ, sp_ * D + k * 128:sp_ * D + (k + 1) * 128],
                            identity=ident[0:NSEQ, 0:NSEQ]), r=allmod + ["ident"], w=[bkey[2]])

                def mview(si):
                    return banks[2][:, si * 8 * NSEQ:(si + 1) * 8 * NSEQ].rearrange("p (k b) -> p k b", b=NSEQ)

                V(lambda e: e.tensor_copy(out=sh1T[:], in_=mview(0)), r=[bkey[2]], w=["sh1T"])
                V(lambda e: e.scalar_tensor_tensor(out=S1T[:], in0=mview(1), scalar=1.0,
                                                   in1=gpre[:].unsqueeze(2).broadcast_to([128, 8, NSEQ]),
                                                   op0=ALU.add, op1=ALU.mult), r=[bkey[2], "gpre"], w=["S1T"])
                V(lambda e: e.tensor_copy(out=sh2T[:], in_=mview(2)), r=[bkey[2]], w=["sh2T"])
                V(lambda e: e.scalar_tensor_tensor(out=S2T[:], in0=mview(3), scalar=1.0,
                                                   in1=gpre2[:].unsqueeze(2).broadcast_to([128, 8, NSEQ]),
                                                   op0=ALU.add, op1=ALU.mult), r=[bkey[2], "gpre2"], w=["S2T"])
                V(lambda e: e.tensor_tensor(out=gmod[:, 0, :], in0=modrow[:, 2 * D:3 * D], in1=gpost4[:, 0, :],
                                            op=ALU.mult), r=allmod + ["gpost4a"], w=["gmod0"])
                V(lambda e: e.tensor_tensor(out=gmod[:, 1, :], in0=modrow[:, 5 * D:6 * D], in1=gpost4[:, 1, :],
                                            op=ALU.mult), r=allmod + ["gpost4b"], w=["gmod1"])

                cast_engs = ["dve", "act", "pool"]
                ci = 0
                for k in range(8):
                    st_ = win_st[k % 2]
                    sk = "win_st%d" % (k % 2)
                    P.dma(st_[:], win_d[k * 128:(k + 1) * 128, :], writes=[sk])
                    for h0, h1 in ((0, 1188), (1188, DIN)):
                        eng = cast_engs[ci % 3]
                        ci += 1
                        if eng == "act":
                            A(lambda e, k=k, st_=st_, h0=h0, h1=h1: e.activation(out=Win[:, k, h0:h1], in_=st_[:, h0:h1],
                                                                              func=AF.Copy), r=[sk], w=["Win"])
                        else:
                            P.op(eng, lambda e, k=k, st_=st_, h0=h0, h1=h1: e.tensor_copy(out=Win[:, k, h0:h1],
                                                                                       in_=st_[:, h0:h1]),
                                 [sk], ["Win"])
                for k in range(8):
                    st_ = win_st[k % 2]
                    sk = "win_st%d" % (k % 2)
                    P.dma(st_[:, 0:D], wout_d[k * 128:(k + 1) * 128, :], writes=[sk])
                    eng = cast_engs[ci % 3]
                    ci += 1
                    if eng == "act":
                        A(lambda e, k=k, st_=st_: e.activation(out=Wout[:, k, :], in_=st_[:, 0:D], func=AF.Copy),
                          r=[sk], w=["Wout"])
                    else:
                        P.op(eng, lambda e, k=k, st_=st_: e.tensor_copy(out=Wout[:, k, :], in_=st_[:, 0:D]),
                             [sk], ["Wout"])
                for g in range(4):
                    G(lambda e, g=g: e.affine_select(out=wsm[:, g, :], in_=ws_sb[:, g, :], pattern=[[-1, 128]],
                                                     compare_op=ALU.is_ge, fill=0.0, base=0, channel_multiplier=1),
                      r=["ws_sb"], w=["wsm"])
                for g in range(4):
                    T(lambda e, g=g: e.transpose(out=banks[3][:, g * 128:(g + 1) * 128], in_=wsm[:, g, :],
                                                 identity=ident[:]), r=["wsm", "ident"], w=[bkey[3]])
                V(lambda e: e.tensor_copy(out=WmT[:], in_=banks[3][:, :].rearrange("p (g t) -> p g t", g=4)),
                  r=[bkey[3]], w=["WmT"])

                NA = NTT * 8
                V(lambda e: e.tensor_copy(out=posf[:], in_=posi[:]), r=["posi"], w=["posf"])
                V(lambda e: e.tensor_tensor(out=ang[:].rearrange("p (t f) -> p t f", f=8),
                                            in0=posf[:].unsqueeze(2).broadcast_to([128, NTT, 8]),
                                            in1=invf[:].unsqueeze(1).broadcast_to([128, NTT, 8]), op=ALU.mult),
                  r=["posf", "invf"], w=["ang"])

                def reduce_sin(dst, src_key, shift):
                    V(lambda e: e.tensor_scalar(out=ang2[:], in0=ang[:], scalar1=shift, scalar2=None, op0=ALU.add),
                      r=["ang"], w=["ang2"])
                    V(lambda e: e.tensor_scalar(out=angk[:], in0=ang2[:], scalar1=1.0 / TWO_PI, scalar2=None,
                                                op0=ALU.mult), r=["ang2"], w=["angk"])
                    V(lambda e: e.tensor_copy(out=angf[:], in_=angk[:]), r=["angk"], w=["angf"])
                    V(lambda e: e.scalar_tensor_tensor(out=ang2[:], in0=angf[:], scalar=-TWO_PI, in1=ang2[:],
                                                       op0=ALU.mult, op1=ALU.add), r=["angf", "ang2"], w=["ang2"])
                    V(lambda e: e.tensor_scalar(out=angm[:], in0=ang2[:], scalar1=math.pi, scalar2=-TWO_PI,
                                                op0=ALU.is_gt, op1=ALU.mult), r=["ang2"], w=["angm"])
                    V(lambda e: e.tensor_tensor(out=ang2[:], in0=ang2[:], in1=angm[:], op=ALU.add),
                      r=["ang2", "angm"], w=["ang2"])
                    V(lambda e: e.tensor_scalar(out=angm[:], in0=ang2[:], scalar1=-math.pi, scalar2=TWO_PI,
                                                op0=ALU.is_lt, op1=ALU.mult), r=["ang2"], w=["angm"])
                    V(lambda e: e.tensor_tensor(out=ang2[:], in0=ang2[:], in1=angm[:], op=ALU.add),
                      r=["ang2", "angm"], w=["ang2"])
                    V(lambda e: e.tensor_scalar(out=ang2[:], in0=ang2[:], scalar1=-3.1415925, scalar2=3.1415925,
                                                op0=ALU.max, op1=ALU.min), r=["ang2"], w=["ang2"])
                    A(lambda e: e.activation(out=dst[:].rearrange("p t f -> p (t f)"), in_=ang2[:], func=AF.Sin),
                      r=["ang2"], w=[src_key])

                reduce_sin(sn, "sn", 0.0)
                reduce_sin(cs, "cs", math.pi / 2.0)
                P.barrier()

            if stop == "setup":
                P.finish()
                return
            tap("S1T", S1T[:].rearrange("p k b -> p (k b)"), ["S1T"])
            tap("gmod", gmod[:].rearrange("b g d -> b (g d)"), ["gmod0", "gmod1"])
            tap("cs", cs[:].rearrange("p t f -> p (t f)"), ["cs"])
            tap("sn", sn[:].rearrange("p t f -> p (t f)"), ["sn"])

            qT2 = asb("qT2", [128, 4, S], BF16)
            kTd = asb("kTd", [128, 2, S], BF16)
            qiT2 = asb("qiT2", [128, S // 32, 4, 32], BF16)
            kiTd = asb("kiTd", [128, S], BF16)
            v_aug = asb("v_aug", [128, NT, 2, 65], BF16)
            w_tok = asb("w_tok", [128, NT, 8])
            mTa = asb("mTa", [128, 4, S], BF16)
            G1row = asb("G1row", [128, D])
            junkA = asb("junkA", [128, D], BF16)
            G(lambda e: e.memset(v_aug[:].rearrange("p a b c -> p (a b) c")[:, :, 64:65], 1.0), w=["v_aug"])

            for b in range(NSEQ):
                for n in range(2):
                    T(lambda e, n=n: e.matmul(banks[n][:, :], lhsT=sel[0:NSEQ, b, :],
                                              rhs=gmod[0:NSEQ, 0, n * 512:(n + 1) * 512], start=True, stop=True),
                      r=["sel", "gmod0"], w=[bkey[n]])
                    V(lambda e, n=n: e.tensor_copy(out=G1row[:, n * 512:(n + 1) * 512], in_=banks[n][:, :]),
                      r=[bkey[n]], w=["G1row"])

                with contextlib.ExitStack() as pst:
                    psb = mk_sb(pst)
                    xt = [psb("xt%d" % i, [128, D]) for i in range(2)]
                    xn = [psb("xn%d" % i, [128, D]) for i in range(2)]
                    hT = [psb("hT%d" % i, [128, 8, 128], BF16) for i in range(2)]
                    ssx = psb("ssx", [128, 2])
                    rsx = psb("rsx", [128, 2])
                    zu = [psb("zu%d" % i, [128, 512], BF16) for i in range(2)]
                    zv = psb("zv", [128, 512], BF16)
                    vn = [psb("vn%d" % i, [128, 512], BF16) for i in range(2)]
                    ssv = psb("ssv", [128, 4])
                    rsv = psb("rsv", [128, 4])
                    ya = psb("ya", [128, 512])
                    ssa = psb("ssa", [128, 1])
                    rsa = psb("rsa", [128, 1])
                    ma = psb("ma", [128, 512], BF16)
                    q_tok = [psb("q_tok%d" % i, [128, 8, 64], BF16) for i in range(2)]
                    qi_tok = [psb("qi_tok%d" % i, [128, 8, 64], BF16) for i in range(2)]
                    kd = [psb("kd%d" % i, [128, 2, 2, 64], BF16) for i in range(2)]
                    kid = [psb("kid%d" % i, [128, 2, 64], BF16) for i in range(2)]
                    rt = [psb("rt%d" % i, [128, 8, 8]) for i in range(4)]

                    def s1_load(i):
                        it = b * NT + i
                        P.dma(xt[i % 2][:], x_d[it * 128:(it + 1) * 128, :], writes=["xt%d" % (i % 2)])

                    def s1(i):
                        j = i % 2
                        A(lambda e: e.activation(out=junkA[:], in_=xt[j][:], func=AF.Square,
                                                 accum_out=ssx[:, j:j + 1]), r=["xt%d" % j], w=["junkA", "ssx%d" % j])
                        rstd(rsx[:, j:j + 1], ssx[:, j:j + 1], 1, 1.0 / D, ["ssx%d" % j], ["rsx%d" % j])
                        V(lambda e: e.tensor_scalar(out=xn[j][:], in0=xt[j][:], scalar1=rsx[:, j:j + 1], scalar2=None,
                                                    op0=ALU.mult), r=["xt%d" % j, "rsx%d" % j], w=["xn%d" % j])
                        for k in range(8):
                            T(lambda e, k=k: e.transpose(out=banks[k // 4][:, (k % 4) * 128:(k % 4 + 1) * 128],
                                                         in_=xn[j][:, k * 128:(k + 1) * 128], identity=ident[:]),
                              r=["xn%d" % j, "ident"], w=[bkey[k // 4]])
                        for k in range(8):
                            A(lambda e, k=k: e.activation(out=hT[j][:, k, :],
                                                          in_=banks[k // 4][:, (k % 4) * 128:(k % 4 + 1) * 128],
                                                          func=AF.Identity, scale=S1T[:, k, b:b + 1],
                                                          bias=sh1T[:, k, b:b + 1]),
                              r=[bkey[k // 4], "S1T", "sh1T"], w=["hT%d" % j])

                    GROUPS = [(0, 512, 2), (512, 512, 3), (1024, 512, 4), (1536, 328, 5), (1864, 512, 6)]

                    def rope(src3, src_key, dst3, dst_key, H, it):
                        c = cs[:, it, :].unsqueeze(1).broadcast_to([128, H, 8])
                        s_ = sn[:, it, :].unsqueeze(1).broadcast_to([128, H, 8])
                        x1 = src3[:, :, 0:8]
                        x2 = src3[:, :, 8:16]
                        t = [r_[:, 0:H, :] for r_ in rt]
                        V(lambda e: e.tensor_tensor(out=t[0], in0=x1, in1=c, op=ALU.mult), r=[src_key, "cs"], w=["rt0"])
                        V(lambda e: e.tensor_tensor(out=t[1], in0=x2, in1=s_, op=ALU.mult), r=[src_key, "sn"], w=["rt1"])
                        V(lambda e: e.tensor_tensor(out=dst3[:, :, 0:8], in0=t[0], in1=t[1], op=ALU.subtract),
                          r=["rt0", "rt1"], w=[dst_key])
                        V(lambda e: e.tensor_tensor(out=t[2], in0=x2, in1=c, op=ALU.mult), r=[src_key, "cs"], w=["rt2"])
                        V(lambda e: e.tensor_tensor(out=t[3], in0=x1, in1=s_, op=ALU.mult), r=[src_key, "sn"], w=["rt3"])
                        V(lambda e: e.tensor_tensor(out=dst3[:, :, 8:16], in0=t[2], in1=t[3], op=ALU.add),
                          r=["rt2", "rt3"], w=[dst_key])
                        A(lambda e: e.activation(out=dst3[:, :, 16:64], in_=src3[:, :, 16:64], func=AF.Copy),
                          r=[src_key], w=[dst_key])

                    def s2(i):
                        j = i % 2
                        it = b * NT + i
                        for (c0, n, bk) in GROUPS:
                            for k in range(8):
                                T(lambda e, k=k, c0=c0, n=n, bk=bk: e.matmul(banks[bk][:, 0:n], lhsT=hT[j][:, k, :],
                                                                             rhs=Win[:, k, c0:c0 + n], start=(k == 0),
                                                                             stop=(k == 7)),
                                  r=["hT%d" % j, "Win"], w=[bkey[bk]])
                        A(lambda e: e.activation(out=zu[j][:], in_=banks[2][:, :], func=AF.Gelu_apprx_tanh),
                          r=[bkey[2]], w=["zu%d" % j])
                        A(lambda e: e.activation(out=zv[:], in_=banks[3][:, :], func=AF.Gelu_apprx_tanh),
                          r=[bkey[3]], w=["zv"])
                        rope(banks[4][:, :].rearrange("p (h d) -> p h d", d=64), bkey[4], q_tok[j][:], "q_tok%d" % j, 8, it)
                        rope(banks[5][:, 0:128].rearrange("p (h d) -> p h d", d=64), bkey[5], kd[j][:, :, 0, :],
                             "kd%d" % j, 2, it)
                        V(lambda e: e.tensor_copy(out=kd[j][:, :, 1, :], in_=kd[j][:, :, 0, :]), r=["kd%d" % j],
                          w=["kd%d" % j])
                        V(lambda e: e.tensor_copy(out=v_aug[:, i, :, 0:64],
                                                  in_=banks[5][:, 128:256].rearrange("p (h d) -> p h d", d=64)),
                          r=[bkey[5]], w=["v_aug"])
                        rope(banks[5][:, 256:320].rearrange("p (h d) -> p h d", d=64), bkey[5], kid[j][:, 0:1, :],
                             "kid%d" % j, 1, it)
                        V(lambda e: e.tensor_copy(out=kid[j][:, 1:2, :], in_=kid[j][:, 0:1, :]), r=["kid%d" % j],
                          w=["kid%d" % j])
                        V(lambda e: e.tensor_copy(out=w_tok[:, i, :], in_=banks[5][:, 320:328]), r=[bkey[5]],
                          w=["w_tok"])
                        rope(banks[6][:, :].rearrange("p (h d) -> p h d", d=64), bkey[6], qi_tok[j][:], "qi_tok%d" % j, 8, it)
                        for g in range(4):
                            A(lambda e, g=g: e.activation(out=junkA[:, 0:128], in_=zv[:, g * 128:(g + 1) * 128],
                                                          func=AF.Square, accum_out=ssv[:, g:g + 1]),
                              r=["zv"], w=["junkA", "ssv"])
                        rstd(rsv[:, 0:4], ssv[:, 0:4], 4, 1.0 / 128, ["ssv"], ["rsv"])
                        for g in range(4):
                            V(lambda e, g=g: e.scalar_tensor_tensor(out=vn[j][:, g * 128:(g + 1) * 128],
                                                                    in0=zv[:, g * 128:(g + 1) * 128],
                                                                    scalar=rsv[:, g:g + 1],
                                                                    in1=gv_row[:, g * 128:(g + 1) * 128],
                                                                    op0=ALU.mult, op1=ALU.mult),
                              r=["zv", "rsv", "gv_row"], w=["vn%d" % j])

                    def s3a(i):
                        j = i % 2
                        ts = slice(i * 128, (i + 1) * 128)
                        qf = q_tok[j][:].rearrange("p h d -> p (h d)")
                        for c in range(4):
                            T(lambda e, c=c: e.transpose(out=bbf[0][:, c * 128:(c + 1) * 128],
                                                         in_=qf[:, c * 128:(c + 1) * 128], identity=identb[:]),
                              r=["q_tok%d" % j, "identb"], w=[bkey[0]])
                        for kv in range(2):
                            T(lambda e, kv=kv: e.transpose(out=bbf[0][:, 512 + kv * 128:512 + (kv + 1) * 128],
                                                           in_=kd[j][:, kv, :, :].rearrange("p a d -> p (a d)"),
                                                           identity=identb[:]),
                              r=["kd%d" % j, "identb"], w=[bkey[0]])
                        T(lambda e: e.transpose(out=bbf[0][:, 768:896], in_=kid[j][:].rearrange("p a d -> p (a d)"),
                                                identity=identb[:]), r=["kid%d" % j, "identb"], w=[bkey[0]])
                        V(lambda e: e.tensor_copy(out=qT2[:, :, ts],
                                                  in_=bbf[0][:, 0:512].rearrange("p (c t) -> p c t", t=128)),
                          r=[bkey[0]], w=["qT2"])
                        V(lambda e: e.tensor_copy(out=kTd[:, :, ts],
                                                  in_=bbf[0][:, 512:768].rearrange("p (c t) -> p c t", t=128)),
                          r=[bkey[0]], w=["kTd"])
                        V(lambda e: e.tensor_copy(out=kiTd[:, ts], in_=bbf[0][:, 768:896]), r=[bkey[0]], w=["kiTd"])
                        qif = qi_tok[j][:].rearrange("p h d -> p (h d)")
                        for c in range(4):
                            T(lambda e, c=c: e.transpose(out=bbf[1][:, c * 128:(c + 1) * 128],
                                                         in_=qif[:, c * 128:(c + 1) * 128], identity=identb[:]),
                              r=["qi_tok%d" % j, "identb"], w=[bkey[1]])
                        A(lambda e: e.activation(out=qiT2[:, i * 4:(i + 1) * 4, :, :],
                                                 in_=bbf[1][:, 0:512].rearrange("p (c g t) -> p g c t", c=4, g=4),
                                                 func=AF.Copy), r=[bkey[1]], w=["qiT2"])
                        for g in range(4):
                            T(lambda e, g=g: e.matmul(banks[7][:, g * 128:(g + 1) * 128], lhsT=WmT[:, g, :],
                                                      rhs=vn[j][:, g * 128:(g + 1) * 128], start=True, stop=True),
                              r=["WmT", "vn%d" % j], w=[bkey[7]])
                        for g in range(4):
                            V(lambda e, g=g: e.scalar_tensor_tensor(out=ya[:, g * 128:(g + 1) * 128],
                                                                    in0=banks[7][:, g * 128:(g + 1) * 128],
                                                                    scalar=bcol[:, g:g + 1],
                                                                    in1=zu[j][:, g * 128:(g + 1) * 128],
                                                                    op0=ALU.add, op1=ALU.mult),
                              r=[bkey[7], "bcol", "zu%d" % j], w=["ya"])
                        A(lambda e: e.activation(out=junkA[:, 0:512], in_=ya[:], func=AF.Square, accum_out=ssa[:]),
                          r=["ya"], w=["junkA", "ssa"])
                        rstd(rsa[:], ssa[:], 1, 1.0 / 512, ["ssa"], ["rsa"])
                        V(lambda e: e.scalar_tensor_tensor(out=ma[:], in0=ya[:], scalar=rsa[:, 0:1], in1=goa_row[:],
                                                           op0=ALU.mult, op1=ALU.mult),
                          r=["ya", "rsa", "goa_row"], w=["ma"])
                        if b == 0 and i == 0:
                            tap("ya", ya[:], ["ya"])

                    def s3b(i):
                        ts = slice(i * 128, (i + 1) * 128)
                        for c in range(4):
                            T(lambda e, c=c: e.transpose(out=bbf[7][:, c * 128:(c + 1) * 128],
                                                         in_=ma[:, c * 128:(c + 1) * 128], identity=identb[:]),
                              r=["ma", "identb"], w=[bkey[7]])
                        A(lambda e: e.activation(out=mTa[:, :, ts],
                                                 in_=bbf[7][:, 0:512].rearrange("p (c t) -> p c t", t=128),
                                                 func=AF.Copy), r=[bkey[7]], w=["mTa"])

                    s1_load(0)
                    if NT > 1:
                        s1_load(1)
                    s1(0)
                    for n in range(NT + 1):
                        if n - 1 >= 0:
                            s3a(n - 1)
                        if n + 1 < NT:
                            s1(n + 1)
                        if n + 2 < NT:
                            pass
                        if n < NT:
                            s2(n)
                            if n + 2 < NT:
                                s1_load(n + 2)
                        if n - 1 >= 0:
                            s3b(n - 1)
                    P.barrier()
                    if stop == "proj":
                        P.finish()
                        return

                with contextlib.ExitStack() as tst:
                    tsb = mk_sb(tst)
                    score = tsb("score", [128, S])
                    cmax = tsb("cmax", [128, 4])
                    mask = tsb("mask", [128, S], BF16)
                    maskT = tsb("maskT", [128, NT, 128], BF16)
                    rl = [tsb("rl%d" % i, [128, 512], BF16) for i in range(3)]
                    pT = [tsb("pT%d" % i, [128, 512], BF16) for i in range(4)]
                    Wsel = [tsb("Wsel%d" % i, [128, 8, 128], BF16) for i in range(2)]
                    wrep = tsb("wrep", [128, 2, 128])
                    wcol = tsb("wcol", [128, 8])
                    lo0 = tsb("lo0", [128, 1])
                    hi0 = tsb("hi0", [128, 1])
                    w0 = tsb("w0", [128, 1])
                    wh = tsb("wh", [128, NIT])
                    mid = tsb("mid", [128, 1])
                    cnt = tsb("cnt", [128, 1])
                    btmp = tsb("btmp", [128, 1])
                    rden = tsb("rden", [128, 8])
                    yb = tsb("yb", [128, 512])
                    ssb_ = tsb("ssb_", [128, 1])
                    rsb = tsb("rsb", [128, 1])
                    mb = tsb("mb", [128, 512], BF16)
                    mbT = tsb("mbT", [128, 4, 128], BF16)
                    sso = tsb("sso", [128, 2])
                    rso = tsb("rso", [128, 1])
                    ot = tsb("ot", [128, D])
                    xres = [tsb("xres%d" % i, [128, D]) for i in range(2)]
                    x1t = [tsb("x1t%d" % i, [128, D]) for i in range(2)]
                    for i in range(2):
                        G(lambda e, i=i: e.memset(Wsel[i][:], 0.0), w=["Wsel%d" % i])
                    rl_i = [0]
                    pT_i = [0]
                    D_i = [0]

                    def indexer(qb):
                        N = (qb + 1) * 128
                        nch = (N + 511) // 512
                        wi = qb % 2
                        wk = "Wsel%d" % wi
                        w2v = w_tok[:, qb, :].rearrange("p (i two) -> p i two", two=2)
                        for par in range(2):
                            V(lambda e, par=par: e.tensor_tensor(
                                out=wrep[:, par, :].rearrange("p (i t) -> p i t", t=32),
                                in0=w2v[:, :, par].unsqueeze(2).broadcast_to([128, 4, 32]),
                                in1=D32[:].unsqueeze(1).broadcast_to([128, 4, 32]), op=ALU.mult),
                              r=["w_tok", "D32"], w=["wrep"])
                        for par in range(2):
                            T(lambda e, par=par: e.matmul(banks[0][:, par * 4:(par + 1) * 4], lhsT=wrep[:, par, :],
                                                          rhs=G4[:], start=True, stop=True),
                              r=["wrep", "G4"], w=[bkey[0]])
                        V(lambda e: e.tensor_scalar(out=wcol[:], in0=banks[0][:, 0:8], scalar1=IDX_SCALE, scalar2=None,
                                                    op0=ALU.mult), r=[bkey[0]], w=["wcol"])
                        for par in range(2):
                            for g in range(4):
                                V(lambda e, par=par, g=g: e.tensor_scalar(
                                    out=Wsel[wi][:, par * 4 + g, 32 * g:32 * g + 32], in0=D32[:],
                                    scalar1=wcol[:, par * 4 + g:par * 4 + g + 1], scalar2=None, op0=ALU.mult),
                                  r=["D32", "wcol"], w=[wk])
                        for c in range(nch):
                            n = min(512, N - c * 512)
                            sbk = 2 + (c % 2)
                            first = True
                            for g in range(4):
                                for par in range(2):
                                    dbk = par
                                    ri = rl_i[0] % 3
                                    rl_i[0] += 1
                                    ps = slice(64 * par, 64 * par + 64)
                                    T(lambda e, g=g, ps=ps, dbk=dbk, n=n, c=c: e.matmul(
                                        banks[dbk][:, 0:n],
                                        lhsT=qiT2[ps, qb * 4 + g, :, :].rearrange("p c t -> p (c t)"),
                                        rhs=kiTd[ps, c * 512:c * 512 + n], start=True, stop=True),
                                      r=["qiT2", "kiTd"], w=[bkey[dbk]])
                                    A(lambda e, dbk=dbk, ri=ri, n=n: e.activation(out=rl[ri][:, 0:n],
                                                                                  in_=banks[dbk][:, 0:n],
                                                                                  func=AF.Relu),
                                      r=[bkey[dbk]], w=["rl%d" % ri])
                                    last = (g == 3 and par == 1)
                                    T(lambda e, g=g, par=par, ri=ri, n=n, sbk=sbk, first=first, last=last: e.matmul(
                                        banks[sbk][:, 0:n], lhsT=Wsel[wi][:, par * 4 + g, :], rhs=rl[ri][:, 0:n],
                                        start=first, stop=last), r=[wk, "rl%d" % ri], w=[bkey[sbk]])
                                    first = False
                            V(lambda e, c=c, n=n, sbk=sbk: e.tensor_scalar(
                                out=score[:, c * 512:c * 512 + n], in0=banks[sbk][:, 0:n], scalar1=1.0, scalar2=None,
                                op0=ALU.mult, op1=ALU.max, accum_out=cmax[:, c:c + 1]),
                              r=[bkey[sbk]], w=["score", "cmax"])

                    def topk_iter(qb):
                        N = (qb + 1) * 128
                        nch = (N + 511) // 512
                        V(lambda e: e.tensor_reduce(out=lo0[:], in_=score[:, 0:N], axis=AX.X, op=ALU.min),
                          r=["score"], w=["lo0"])
                        V(lambda e: e.tensor_reduce(out=hi0[:], in_=cmax[:, 0:nch], axis=AX.X, op=ALU.max),
                          r=["cmax"], w=["hi0"])
                        V(lambda e: e.tensor_tensor(out=score[:, qb * 128:N], in0=score[:, qb * 128:N], in1=NEGM[:],
                                                    op=ALU.add), r=["score", "NEGM"], w=["score"])
                        V(lambda e: e.tensor_tensor(out=w0[:], in0=hi0[:], in1=lo0[:], op=ALU.subtract),
                          r=["hi0", "lo0"], w=["w0"])
                        V(lambda e: e.tensor_scalar(out=wh[:], in0=P2[:], scalar1=w0[:, 0:1], scalar2=None,
                                                    op0=ALU.mult), r=["P2", "w0"], w=["wh"])
                        V(lambda e: e.tensor_tensor(out=mid[:], in0=lo0[:], in1=wh[:, 0:1], op=ALU.add),
                          r=["lo0", "wh"], w=["mid"])
                        for i in range(NIT):
                            V(lambda e: e.tensor_scalar(out=mask[:, 0:N], in0=score[:, 0:N], scalar1=mid[:, 0:1],
                                                        scalar2=None, op0=ALU.is_ge, op1=ALU.add, accum_out=cnt[:]),
                              r=["score", "mid"], w=["mask", "cnt"])
                            V(lambda e, i=i: e.scalar_tensor_tensor(out=btmp[:], in0=cnt[:], scalar=TOPK - 0.5,
                                                                    in1=wh[:, i:i + 1], op0=ALU.is_ge, op1=ALU.mult),
                              r=["cnt", "wh"], w=["btmp"])
                            nx = i + 1 if i + 1 < NIT else i
                            V(lambda e, nx=nx: e.scalar_tensor_tensor(out=mid[:], in0=mid[:], scalar=wh[:, nx:nx + 1],
                                                                      in1=btmp[:], op0=ALU.subtract, op1=ALU.add),
                              r=["mid", "wh", "btmp"], w=["mid"])
                            yield

                    def topk_finish(qb):
                        N = (qb + 1) * 128
                        if qb < KB:
                            for jj in range(qb + 1):
                                src = TRIU if jj == qb else ONESB
                                G(lambda e, jj=jj, src=src: e.tensor_copy(out=maskT[:, jj, :], in_=src[:]),
                                  r=["TRIU", "ONESB"], w=["maskT"])
                            return
                        V(lambda e: e.tensor_scalar(out=mask[:, 0:N], in0=score[:, 0:N], scalar1=mid[:, 0:1],
                                                    scalar2=None, op0=ALU.is_ge), r=["score", "mid"], w=["mask"])
                        if b == 0 and qb == NT - 1:
                            tap("score", score[:, 0:N], ["score"])
                            tap("thr", mid[:], ["mid"])
                        for jj in range(qb + 1):
                            lb = 4 + jj // 8
                            T(lambda e, jj=jj, lb=lb: e.transpose(out=bbf[lb][:, (jj % 8) * 128:(jj % 8 + 1) * 128],
                                                                  in_=mask[:, jj * 128:(jj + 1) * 128],
                                                                  identity=identb[:]),
                              r=["mask", "identb"], w=[bkey[lb]])
                        for lb in range(4, 4 + (qb + 8) // 8):
                            j0 = (lb - 4) * 8
                            j1 = min(qb + 1, j0 + 8)
                            nj = j1 - j0
                            V(lambda e, lb=lb, j0=j0, j1=j1, nj=nj: e.tensor_copy(
                                out=maskT[:, j0:j1, :],
                                in_=bbf[lb][:, 0:nj * 128].rearrange("p (j t) -> p j t", t=128)),
                              r=[bkey[lb]], w=["maskT"])

                    def attention(qb):
                        qs = slice(qb * 128, (qb + 1) * 128)
                        for kv in range(2):
                            T(lambda e, kv=kv: e.matmul(banks[6 + kv][:, 0:260], lhsT=zerob[:, 0:128],
                                                        rhs=zerob[:, 0:260], start=True, stop=False,
                                                        skip_group_check=True), r=["zerob"], w=[bkey[6 + kv]])
                        for jj in range(qb + 1):
                            ks = slice(jj * 128, (jj + 1) * 128)
                            pis = []
                            for par in range(2):
                                ps = slice(64 * par, 64 * par + 64)
                                lb = 4 + par
                                pi = pT_i[0] % 4
                                pT_i[0] += 1
                                pis.append(pi)
                                for kv in range(2):
                                    T(lambda e, ps=ps, lb=lb, kv=kv, ks=ks: e.matmul(
                                        banks[lb][:, kv * 256:(kv + 1) * 256], lhsT=kTd[ps, kv, ks],
                                        rhs=qT2[ps, 2 * kv:2 * kv + 2, qs], start=True, stop=True),
                                      r=["kTd", "qT2"], w=[bkey[lb]])
                                A(lambda e, lb=lb, pi=pi: e.activation(out=pT[pi][:], in_=banks[lb][:, :], func=AF.Exp,
                                                                       scale=0.125), r=[bkey[lb]], w=["pT%d" % pi])
                                V(lambda e, pi=pi, jj=jj: e.tensor_tensor(
                                    out=pT[pi][:].rearrange("p (h t) -> p h t", t=128),
                                    in0=pT[pi][:].rearrange("p (h t) -> p h t", t=128),
                                    in1=maskT[:, jj, :].unsqueeze(1).broadcast_to([128, 4, 128]), op=ALU.mult),
                                  r=["pT%d" % pi, "maskT"], w=["pT%d" % pi])
                            for par in range(2):
                                pi = pis[par]
                                for kv in range(2):
                                    for ii in range(2):
                                        hl = 2 * ii + par
                                        T(lambda e, ii=ii, hl=hl, kv=kv, pi=pi, jj=jj: e.matmul(
                                            banks[6 + kv][:, hl * 65:hl * 65 + 65],
                                            lhsT=pT[pi][:, (kv * 2 + ii) * 128:(kv * 2 + ii + 1) * 128],
                                            rhs=v_aug[:, jj, kv, :], start=False, stop=(jj == qb),
                                            skip_group_check=True),
                                          r=["pT%d" % pi, "v_aug"], w=[bkey[6 + kv]])
                            yield

                    def post(qb):
                        it = b * NT + qb
                        xj = qb % 2
                        for kv in range(2):
                            ov = banks[6 + kv][:, 0:260].rearrange("p (h d) -> p h d", d=65)
                            V(lambda e, kv=kv, ov=ov: e.reciprocal(out=rden[:, kv * 4:(kv + 1) * 4], in_=ov[:, :, 64]),
                              r=[bkey[6 + kv]], w=["rden"])
                            V(lambda e, kv=kv, ov=ov: e.tensor_tensor(
                                out=yb[:, kv * 256:(kv + 1) * 256].rearrange("p (h d) -> p h d", d=64),
                                in0=ov[:, :, 0:64],
                                in1=rden[:, kv * 4:(kv + 1) * 4].unsqueeze(2).broadcast_to([128, 4, 64]),
                                op=ALU.mult), r=[bkey[6 + kv], "rden"], w=["yb"])
                        if b == 0 and qb == NT - 1:
                            tap("yb", yb[:], ["yb"])
                        A(lambda e: e.activation(out=junkA[:, 0:512], in_=yb[:], func=AF.Square, accum_out=ssb_[:]),
                          r=["yb"], w=["junkA", "ssb_"])
                        rstd(rsb[:], ssb_[:], 1, 1.0 / 512, ["ssb_"], ["rsb"])
                        V(lambda e: e.scalar_tensor_tensor(out=mb[:], in0=yb[:], scalar=rsb[:, 0:1], in1=gob_row[:],
                                                           op0=ALU.mult, op1=ALU.mult),
                          r=["yb", "rsb", "gob_row"], w=["mb"])
                        for c in range(4):
                            T(lambda e, c=c: e.transpose(out=bbf[4][:, c * 128:(c + 1) * 128],
                                                         in_=mb[:, c * 128:(c + 1) * 128], identity=identb[:]),
                              r=["mb", "identb"], w=[bkey[4]])
                        A(lambda e: e.activation(out=mbT[:], in_=bbf[4][:, 0:512].rearrange("p (c t) -> p c t", t=128),
                                                 func=AF.Copy), r=[bkey[4]], w=["mbT"])
                        for n in range(2):
                            for k in range(8):
                                lhs = mTa[:, k, qb * 128:(qb + 1) * 128] if k < 4 else mbT[:, k - 4, :]
                                T(lambda e, n=n, k=k, lhs=lhs: e.matmul(banks[6 + n][:, :], lhsT=lhs,
                                                                        rhs=Wout[:, k, n * 512:(n + 1) * 512],
                                                                        start=(k == 0), stop=(k == 7)),
                                  r=["mTa", "mbT", "Wout"], w=[bkey[6 + n]])
                        for n in range(2):
                            A(lambda e, n=n: e.activation(out=junkA[:, 0:512], in_=banks[6 + n][:, :], func=AF.Square,
                                                          accum_out=sso[:, n:n + 1]),
                              r=[bkey[6 + n]], w=["junkA", "sso%d" % n])
                        rstd(rso[:], sso[:, 0:1], 1, 1.0 / D, ["sso0", "sso1"], ["rso"], ss2_ap=sso[:, 1:2])
                        for n in range(2):
                            V(lambda e, n=n: e.scalar_tensor_tensor(out=ot[:, n * 512:(n + 1) * 512],
                                                                    in0=banks[6 + n][:, :], scalar=rso[:, 0:1],
                                                                    in1=G1row[:, n * 512:(n + 1) * 512],
                                                                    op0=ALU.mult, op1=ALU.mult),
                              r=[bkey[6 + n], "rso", "G1row"], w=["ot"])
                        G(lambda e: e.tensor_tensor(out=x1t[xj][:], in0=ot[:], in1=xres[xj][:], op=ALU.add),
                          r=["ot", "xres%d" % xj], w=["x1t%d" % xj])
                        P.dma(x1s_d[it * 128:(it + 1) * 128, :], x1t[xj][:], reads=["x1t%d" % xj],
                              writes=["x1s_%d" % it])

                    def interleave(g1, g2):
                        gens = [g for g in (g1, g2) if g is not None]
                        while gens:
                            for g_ in list(gens):
                                try:
                                    next(g_)
                                except StopIteration:
                                    gens.remove(g_)

                    if 0 >= KB:
                        indexer(0)
                        interleave(topk_iter(0), None)
                    topk_finish(0)
                    ck("a_tf0")
                    for qb in range(NT):
                        it = b * NT + qb
                        P.dma(xres[qb % 2][:], x_d[it * 128:(it + 1) * 128, :], writes=["xres%d" % (qb % 2)])
                        tk = None
                        if qb + 1 < NT and qb + 1 >= KB:
                            indexer(qb + 1)
                            ck("a_idx%d" % (qb + 1))
                            tk = topk_iter(qb + 1)
                        if stop == "a_tk%d" % (qb + 1):
                            interleave(tk, None)
                            ck("a_tk%d" % (qb + 1))
                        interleave(attention(qb), tk)
                        ck("a_att%d" % qb)
                        if qb + 1 < NT:
                            topk_finish(qb + 1)
                        ck("a_tf%d" % (qb + 1))
                        post(qb)
                        ck("a_post%d" % qb)
                    P.barrier()
                    if stop == "attn":
                        P.finish()
                        return
        with contextlib.ExitStack() as bst:
            bsb = mk_sb(bst)
            W1 = bsb("W1", [128, 8, DFF], BF16)
            W2 = bsb("W2", [128, 32, D], BF16)
            wst = [bsb("wst%d" % i, [128, 2048]) for i in range(2)]
            G2row = bsb("G2row", [128, D])
            xg = [bsb("xg%d" % i, [128, D]) for i in range(4)]
            xn2 = [bsb("xn2_%d" % i, [128, D]) for i in range(1)]
            h2T = [bsb("h2T%d" % i, [128, 8, 256], BF16) for i in range(2)]
            rr = [bsb("rr%d" % i, [128, 256], BF16) for i in range(3)]
            fT = [bsb("fT%d" % i, [128, 256], BF16) for i in range(3)]
            junkB = bsb("junkB", [128, D], BF16)
            ss2 = bsb("ss2", [128, 4])
            rs2 = bsb("rs2", [128, 4])
            ssf = bsb("ssf", [128, 4])
            rsf = bsb("rsf", [128, 2])
            of = [bsb("of%d" % i, [128, D]) for i in range(2)]

            cast_engs = ["dve", "act", "pool"]
            ci = 0
            wi_ = 0
            for k in range(8):
                for hf in range(2):
                    st_ = wst[wi_ % 2]
                    sk = "wst%d" % (wi_ % 2)
                    wi_ += 1
                    P.dma(st_[:], w1_d[k * 128:(k + 1) * 128, hf * 2048:(hf + 1) * 2048], writes=[sk])
                    for q2 in range(2):
                        eng = cast_engs[ci % 3]
                        ci += 1
                        sl = slice(q2 * 1024, (q2 + 1) * 1024)
                        dl = slice(hf * 2048 + q2 * 1024, hf * 2048 + (q2 + 1) * 1024)
                        if eng == "act":
                            A(lambda e, k=k, st_=st_, sl=sl, dl=dl: e.activation(out=W1[:, k, dl], in_=st_[:, sl],
                                                                              func=AF.Copy), r=[sk], w=["W1"])
                        else:
                            P.op(eng, lambda e, k=k, st_=st_, sl=sl, dl=dl: e.tensor_copy(out=W1[:, k, dl],
                                                                                       in_=st_[:, sl]), [sk], ["W1"])
            for c2 in range(16):
                st_ = wst[wi_ % 2]
                sk = "wst%d" % (wi_ % 2)
                wi_ += 1
                P.dma(st_[:].rearrange("p (c n) -> p c n", n=D),
                      w2_d[c2 * 256:(c2 + 1) * 256, :].rearrange("(c p) n -> p c n", p=128), writes=[sk])
                for q2 in range(2):
                    eng = cast_engs[ci % 3]
                    ci += 1
                    sl = slice(q2 * 1024, (q2 + 1) * 1024)
                    if eng == "act":
                        A(lambda e, c2=c2, q2=q2, st_=st_, sl=sl: e.activation(out=W2[:, c2 * 2 + q2, :], in_=st_[:, sl],
                                                                          func=AF.Copy), r=[sk], w=["W2"])
                    else:
                        P.op(eng, lambda e, c2=c2, q2=q2, st_=st_, sl=sl: e.tensor_copy(out=W2[:, c2 * 2 + q2, :],
                                                                                   in_=st_[:, sl]), [sk], ["W2"])

            NG = NTOK // 256

            def b_load(g):
                for t in range(2):
                    it = g * 2 + t
                    xi = (g % 2) * 2 + t
                    P.dma(xg[xi][:], x1s_d[it * 128:(it + 1) * 128, :], reads=["x1s_%d" % it], writes=["xg%d" % xi])

            def b_prep(g):
                hj = g % 2
                b = (g * 256) // S
                for t in range(2):
                    xi = (g % 2) * 2 + t
                    A(lambda e, xi=xi, t=t: e.activation(out=junkB[:], in_=xg[xi][:], func=AF.Square,
                                                        accum_out=ss2[:, t:t + 1]), r=["xg%d" % xi],
                      w=["junkB", "ss2_%d" % t])
                    rstd(rs2[:, t:t + 1], ss2[:, t:t + 1], 1, 1.0 / D, ["ss2_%d" % t], ["rs2_%d" % t])
                    V(lambda e, xi=xi, t=t: e.tensor_scalar(out=xn2[0][:], in0=xg[xi][:], scalar1=rs2[:, t:t + 1],
                                                           scalar2=None, op0=ALU.mult),
                      r=["xg%d" % xi, "rs2_%d" % t], w=["xn2_0"])
                    for k in range(8):
                        T(lambda e, k=k, t=t: e.transpose(out=banks[6 + k // 4][:, (k % 4) * 128:(k % 4 + 1) * 128],
                                                          in_=xn2[0][:, k * 128:(k + 1) * 128], identity=ident[:]),
                          r=["xn2_0", "ident"], w=[bkey[6 + k // 4]])
                    for k in range(8):
                        A(lambda e, k=k, t=t: e.activation(out=h2T[hj][:, k, t * 128:(t + 1) * 128],
                                                           in_=banks[6 + k // 4][:, (k % 4) * 128:(k % 4 + 1) * 128],
                                                           func=AF.Identity, scale=S2T[:, k, b:b + 1],
                                                           bias=sh2T[:, k, b:b + 1]),
                          r=[bkey[6 + k // 4], "S2T", "sh2T"], w=["h2T%d" % hj])

            f_i = [0]

            def b_main(g):
                hj = g % 2
                b = (g * 256) // S
                if (g * 256) % S == 0:
                    for n in range(2):
                        T(lambda e, n=n: e.matmul(banks[4 + n][:, :], lhsT=sel[0:NSEQ, b, :],
                                                  rhs=gmod[0:NSEQ, 1, n * 512:(n + 1) * 512], start=True, stop=True),
                          r=["sel", "gmod1"], w=[bkey[4 + n]])
                        V(lambda e, n=n: e.tensor_copy(out=G2row[:, n * 512:(n + 1) * 512], in_=banks[4 + n][:, :]),
                          r=[bkey[4 + n]], w=["G2row"])
                for c in range(32):
                    fb = 4 + (c % 2)
                    fi = f_i[0] % 3
                    f_i[0] += 1
                    for k in range(8):
                        T(lambda e, k=k, c=c, fb=fb: e.matmul(banks[fb][:, 0:256],
                                                              lhsT=W1[:, k, c * 128:(c + 1) * 128],
                                                              rhs=h2T[hj][:, k, :], start=(k == 0), stop=(k == 7)),
                          r=["W1", "h2T%d" % hj], w=[bkey[fb]])
                    A(lambda e, fb=fb, fi=fi: e.activation(out=rr[fi][:], in_=banks[fb][:, 0:256], func=AF.Relu),
                      r=[bkey[fb]], w=["rr%d" % fi])
                    V(lambda e, fb=fb, fi=fi: e.scalar_tensor_tensor(out=fT[fi][:], in0=banks[fb][:, 0:256],
                                                                     scalar=0.0, in1=rr[fi][:], op0=ALU.max,
                                                                     op1=ALU.mult),
                      r=[bkey[fb], "rr%d" % fi], w=["fT%d" % fi])
                    for t in range(2):
                        for n in range(2):
                            ob = t * 2 + n
                            T(lambda e, t=t, n=n, ob=ob, fi=fi, c=c: e.matmul(
                                banks[ob][:, :], lhsT=fT[fi][:, t * 128:(t + 1) * 128],
                                rhs=W2[:, c, n * 512:(n + 1) * 512], start=(c == 0), stop=(c == 31)),
                              r=["fT%d" % fi, "W2"], w=[bkey[ob]])
                    if c == 20 and g + 1 < NG:
                        b_prep(g + 1)
                    if c == 4 and g + 2 < NG:
                        pass
                for t in range(2):
                    it = g * 2 + t
                    xi = (g % 2) * 2 + t
                    for n in range(2):
                        A(lambda e, t=t, n=n: e.activation(out=junkB[:, 0:512], in_=banks[t * 2 + n][:, :],
                                                           func=AF.Square, accum_out=ssf[:, t * 2 + n:t * 2 + n + 1]),
                          r=[bkey[t * 2 + n]], w=["junkB", "ssf%d" % (t * 2 + n)])
                    rstd(rsf[:, t:t + 1], ssf[:, t * 2:t * 2 + 1], 1, 1.0 / D, ["ssf%d" % (t * 2), "ssf%d" % (t * 2 + 1)],
                         ["rsf%d" % t], ss2_ap=ssf[:, t * 2 + 1:t * 2 + 2])
                    for n in range(2):
                        V(lambda e, t=t, n=n: e.scalar_tensor_tensor(out=of[t][:, n * 512:(n + 1) * 512],
                                                                     in0=banks[t * 2 + n][:, :], scalar=rsf[:, t:t + 1],
                                                                     in1=G2row[:, n * 512:(n + 1) * 512],
                                                                     op0=ALU.mult, op1=ALU.mult),
                          r=[bkey[t * 2 + n], "rsf%d" % t, "G2row"], w=["of%d" % t])
                    G(lambda e, t=t, xi=xi: e.tensor_tensor(out=of[t][:], in0=of[t][:], in1=xg[xi][:], op=ALU.add),
                      r=["of%d" % t, "xg%d" % xi], w=["of%d" % t])
                    P.dma(out_d[it * 128:(it + 1) * 128, :], of[t][:], reads=["of%d" % t])
                if g + 2 < NG:
                    b_load(g + 2)

            b_load(0)
            if NG > 1:
                b_load(1)
            b_prep(0)
            for g in range(NG):
                b_main(g)
            P.finish()
        print("program built: instrs=%d waits=%d" % (P.ninstr, P.nwaits), flush=True)


def make_core_inputs(ci, NSEQ, S, x, c, positions, w_ada, b_ada, g_pre_mix, w_in, g_sgu_v, w_spatial, b_spatial,
                     g_out_sgu, g_out_attn, w_out, g_post_mix, g_pre_ffn, w_ff1, w_ff2, g_post_ffn):
    f32 = np.float32
    bs = slice(ci * NSEQ, (ci + 1) * NSEQ)
    NT = S // 128
    xc = np.ascontiguousarray(x[bs]).reshape(NSEQ * S, D).astype(f32, copy=False)
    cc = np.asarray(c[bs], dtype=f32)
    cT = np.ascontiguousarray(cc.T.reshape(8, 128, NSEQ).transpose(1, 0, 2))
    pos = np.ascontiguousarray(np.asarray(positions[bs]).reshape(NSEQ * NT, 128).T.astype(np.int32))
    wi = np.asarray(w_in[0], dtype=f32)
    perm = np.concatenate([np.arange(0, 1792), np.arange(2304, 2376), np.arange(1792, 2304)])
    wi_p = np.ascontiguousarray(wi[:, perm])
    return {
        "x": xc, "cT": cT, "pos": pos,
        "w_ada": np.ascontiguousarray(w_ada[0], dtype=f32),
        "b_ada": np.ascontiguousarray(b_ada[0:1], dtype=f32),
        "w_in": wi_p,
        "gpre": np.ascontiguousarray(np.asarray(g_pre_mix[0], dtype=f32).reshape(8, 128).T),
        "gpre2": np.ascontiguousarray(np.asarray(g_pre_ffn[0], dtype=f32).reshape(8, 128).T),
        "gv": np.ascontiguousarray(g_sgu_v[0:1], dtype=f32),
        "ws": np.ascontiguousarray(np.asarray(w_spatial[0], dtype=f32).transpose(1, 0, 2)),
        "bs": np.ascontiguousarray(np.asarray(b_spatial[0], dtype=f32).T),
        "goa": np.ascontiguousarray(g_out_sgu[0:1], dtype=f32),
        "gob": np.ascontiguousarray(g_out_attn[0:1], dtype=f32),
        "w_out": np.ascontiguousarray(w_out[0], dtype=f32),
        "gpost": np.ascontiguousarray(g_post_mix[0:1], dtype=f32),
        "w1": np.ascontiguousarray(w_ff1[0], dtype=f32),
        "w2": np.ascontiguousarray(w_ff2[0], dtype=f32),
        "gpost2": np.ascontiguousarray(g_post_ffn[0:1], dtype=f32),
    }


def run(inputs, n_cores, NSEQ, S, taps=None, trace=False, stop=None):
    nc = bass.Bass("TRN2", target_bir_lowering=False)
    try:
        build_program(nc, NSEQ=NSEQ, S=S, taps=taps, stop=stop)
    except StopBuild:
        pass
    in_maps = [make_core_inputs(ci, NSEQ, S, **inputs) for ci in range(n_cores)]
    res = run_bass_kernel_spmd(nc, in_maps, core_ids=list(range(n_cores)), trace=trace)
    return res


def kernel(**inputs):
    inputs = {k: np.asarray(v) for k, v in inputs.items()}
    B, S, _ = inputs["x"].shape
    NSEQ = B // NCORES
    res = run(inputs, NCORES, NSEQ, S)
    outs = [np.asarray(r["out"]).reshape(NSEQ, S, D) for r in res.results]
    return np.concatenate(outs, axis=0).astype(np.float32, copy=False)
```

```python
import contextlib
import math
import numpy as np
import concourse.bass as bass
import concourse.mybir as mybir
from concourse.bass_utils import run_bass_kernel_spmd

F32 = mybir.dt.float32
BF16 = mybir.dt.bfloat16
I32 = mybir.dt.int32
AF = mybir.ActivationFunctionType
ALU = mybir.AluOpType
AX = mybir.AxisListType

D = 1024
DIN = 2376
DFF = 4096
NCORES = 8
EPS = 1e-6
NIT = 16
IDX_SCALE = (64 ** -0.5) * (8 ** -0.5)
TWO_PI = 2.0 * math.pi


class StopBuild(Exception):
    pass


class Prog:
    NDMA = 32

    def __init__(self, nc, stack):
        self.nc = nc
        self.eng = {"pe": nc.tensor, "act": nc.scalar, "dve": nc.vector,
                    "pool": nc.gpsimd, "sp": nc.sync}
        self.sem = {k: stack.enter_context(nc.semaphore("c_" + k)) for k in self.eng}
        self.cnt = {k: 0 for k in self.eng}
        self.dsem = [stack.enter_context(nc.semaphore("d%d" % i)) for i in range(self.NDMA)]
        self.dval = [0] * self.NDMA
        self.dnext = 0
        self.seen = {k: {} for k in self.eng}
        self.res = {}
        self.nwaits = 0
        self.ninstr = 0

    def _wait(self, eng, dep):
        kind, key, val = dep
        if kind == "e":
            if key == "pe" and eng == "pe":
                return
            sem = self.sem[key]
            skey = key
        else:
            sem = self.dsem[key]
            skey = ("d", key)
        if self.seen[eng].get(skey, 0) >= val:
            return
        self.seen[eng][skey] = val
        self.eng[eng].wait_ge(sem, val)
        self.nwaits += 1

    def _deps(self, eng, reads, writes):
        deps = []
        for r in reads:
            st = self.res.get(r)
            if st and st["w"]:
                deps.append(st["w"])
        for w in writes:
            st = self.res.get(w)
            if st:
                if st["w"]:
                    deps.append(st["w"])
                deps.extend(st["r"])
        for d in deps:
            self._wait(eng, d)

    def _record(self, token, reads, writes):
        for r in reads:
            st = self.res.setdefault(r, {"w": None, "r": []})
            st["r"].append(token)
            if len(st["r"]) > 48:
                best = {}
                for t in st["r"]:
                    k = (t[0], t[1])
                    if k not in best or best[k][2] < t[2]:
                        best[k] = t
                st["r"] = list(best.values())
        for w in writes:
            self.res[w] = {"w": token, "r": []}

    def op(self, eng, fn, reads=(), writes=()):
        self._deps(eng, reads, writes)
        ins = fn(self.eng[eng])
        self.cnt[eng] += 1
        ins.then_inc(self.sem[eng], 1)
        token = ("e", eng, self.cnt[eng])
        self._record(token, reads, writes)
        self.ninstr += 1
        return token

    def dma(self, out, in_, reads=(), writes=(), q="sp", **kw):
        self._deps(q, reads, writes)
        i = self.dnext
        self.dnext = (self.dnext + 1) % self.NDMA
        if self.dval[i] > 0:
            self._wait(q, ("d", i, self.dval[i]))
        ins = self.eng[q].dma_start(out=out, in_=in_, **kw)
        self.dval[i] += 16
        ins.then_inc(self.dsem[i], 16)
        token = ("d", i, self.dval[i])
        self._record(token, reads, writes)
        self.ninstr += 1
        return token

    def barrier(self):
        for e in self.eng:
            for f in self.eng:
                if f != e and self.cnt[f] > 0:
                    self._wait(e, ("e", f, self.cnt[f]))
            for i in range(self.NDMA):
                if self.dval[i] > 0:
                    self._wait(e, ("d", i, self.dval[i]))
        self.res = {}

    def finish(self):
        for i in range(self.NDMA):
            if self.dval[i] > 0:
                self._wait("sp", ("d", i, self.dval[i]))
        for f in self.eng:
            if f != "sp" and self.cnt[f] > 0:
                self._wait("sp", ("e", f, self.cnt[f]))


def build_program(nc, NSEQ=4, S=2048, taps=None, stop=None):
    NT = S // 128
    NTT = NSEQ * NT
    NTOK = NSEQ * S
    TOPK = min(256, S // 4)
    KB = TOPK // 128
    taps = taps or {}

    def din(name, shape, dt=F32):
        return nc.dram_tensor(name, list(shape), dt, kind="ExternalInput").ap()

    x_d = din("x", [NTOK, D])
    cT_d = din("cT", [128, 8, NSEQ])
    pos_d = din("pos", [128, NTT], I32)
    wada_d = din("w_ada", [D, 6 * D])
    bada_d = din("b_ada", [1, 6 * D])
    win_d = din("w_in", [D, DIN])
    gpre_d = din("gpre", [128, 8])
    gpre2_d = din("gpre2", [128, 8])
    gv_d = din("gv", [1, 512])
    ws_d = din("ws", [128, 4, 128])
    bs_d = din("bs", [128, 4])
    goa_d = din("goa", [1, 512])
    gob_d = din("gob", [1, 512])
    wout_d = din("w_out", [D, D])
    gpost_d = din("gpost", [1, D])
    w1_d = din("w1", [D, DFF])
    w2_d = din("w2", [DFF, D])
    gpost2_d = din("gpost2", [1, D])
    out_d = nc.dram_tensor("out", [NTOK, D], F32, kind="ExternalOutput").ap()
    x1s_d = out_d
    tap_d = {k: nc.dram_tensor("tap_" + k, list(shp), F32, kind="ExternalOutput").ap()
             for k, shp in taps.items()}

    with contextlib.ExitStack() as gst:
        P = Prog(nc, gst)

        uid = [0]

        def mk_sb(stack):
            def sb(name, shape, dt=F32):
                uid[0] += 1
                return stack.enter_context(nc.sbuf_tensor("s%d_%s" % (uid[0], name), list(shape), dt))
            return sb

        gsb = mk_sb(gst)
        banks = [gst.enter_context(nc.psum_tensor("bank%d" % i, [128, 512], F32)) for i in range(8)]
        bkey = ["b%d" % i for i in range(8)]
        bbf = [b[:].bitcast(BF16) for b in banks]

        def V(fn, r=(), w=()):
            return P.op("dve", fn, r, w)

        def A(fn, r=(), w=()):
            return P.op("act", fn, r, w)

        def G(fn, r=(), w=()):
            return P.op("pool", fn, r, w)

        def T(fn, r=(), w=()):
            return P.op("pe", fn, r, w)

        ident = gsb("ident", [128, 128])
        identb = gsb("identb", [128, 128], BF16)
        ones_f = gsb("ones_f", [128, 128])
        zeros_f = gsb("zeros_f", [128, 128])
        NEGM = gsb("NEGM", [128, 128])
        TRIU = gsb("TRIU", [128, 128], BF16)
        ONESB = gsb("ONESB", [128, 128], BF16)
        D32 = gsb("D32", [128, 32])
        G4 = gsb("G4", [128, 4])
        zerob = gsb("zerob", [128, 260], BF16)
        P2 = gsb("P2", [128, NIT + 1])
        mhalf = gsb("mhalf", [128, 16])
        iot = gsb("iot", [128, 8], I32)
        iof = gsb("iof", [128, 8])
        invf = gsb("invf", [128, 8])
        rs_tmp = gsb("rs_tmp", [128, 16])

        G(lambda e: e.memset(ones_f[:], 1.0), w=["ones_f"])
        G(lambda e: e.memset(zeros_f[:], 0.0), w=["zeros_f"])
        G(lambda e: e.affine_select(out=ident[:], in_=ones_f[:], pattern=[[-1, 128]], compare_op=ALU.is_equal,
                                    fill=0.0, base=0, channel_multiplier=1), r=["ones_f"], w=["ident"])
        V(lambda e: e.tensor_copy(out=identb[:], in_=ident[:]), r=["ident"], w=["identb"])
        G(lambda e: e.affine_select(out=NEGM[:], in_=zeros_f[:], pattern=[[-1, 128]], compare_op=ALU.is_ge,
                                    fill=-1.0e30, base=0, channel_multiplier=1), r=["zeros_f"], w=["NEGM"])
        G(lambda e: e.affine_select(out=TRIU[:], in_=ones_f[:], pattern=[[1, 128]], compare_op=ALU.is_ge,
                                    fill=0.0, base=0, channel_multiplier=-1), r=["ones_f"], w=["TRIU"])
        V(lambda e: e.tensor_copy(out=ONESB[:], in_=ones_f[:]), r=["ones_f"], w=["ONESB"])
        for m in range(4):
            G(lambda e, m=m: e.affine_select(out=D32[32 * m:32 * m + 32, :], in_=ones_f[32 * m:32 * m + 32, 0:32],
                                             pattern=[[-1, 32]], compare_op=ALU.is_equal, fill=0.0, base=0,
                                             channel_multiplier=1), r=["ones_f"], w=["D32"])
        G(lambda e: e.memset(G4[:], 0.0), w=["G4"])
        for g in range(4):
            G(lambda e, g=g: e.memset(G4[32 * g:32 * g + 32, g:g + 1], 1.0), r=["G4"], w=["G4"])
        G(lambda e: e.memset(zerob[:], 0.0), w=["zerob"])
        for i in range(NIT + 1):
            G(lambda e, i=i: e.memset(P2[:, i:i + 1], 2.0 ** -(i + 1)), w=["P2"])
        G(lambda e: e.memset(mhalf[:], -0.5), w=["mhalf"])
        G(lambda e: e.iota(iot[:], pattern=[[1, 8]], base=0, channel_multiplier=0), w=["iot"])
        V(lambda e: e.tensor_copy(out=iof[:], in_=iot[:]), r=["iot"], w=["iof"])
        A(lambda e: e.activation(out=invf[:], in_=iof[:], func=AF.Exp, scale=-math.log(500000.0) / 8.0),
          r=["iof"], w=["invf"])

        def rstd(out_ap, ss_ap, n, inv_n, rk, wk, ss2_ap=None):
            tmp = rs_tmp[:, 0:n]
            if ss2_ap is not None:
                G(lambda e: e.tensor_tensor(out=tmp, in0=ss_ap, in1=ss2_ap, op=ALU.add), r=rk, w=["rs_tmp"])
                G(lambda e: e.tensor_scalar(out=tmp, in0=tmp, scalar1=inv_n, scalar2=EPS, op0=ALU.mult,
                                            op1=ALU.add), r=["rs_tmp"], w=["rs_tmp"])
            else:
                G(lambda e: e.tensor_scalar(out=tmp, in0=ss_ap, scalar1=inv_n, scalar2=EPS, op0=ALU.mult,
                                            op1=ALU.add), r=rk, w=["rs_tmp"])
            G(lambda e: e.tensor_tensor(out=out_ap, in0=tmp, in1=mhalf[:, 0:n], op=ALU.pow),
              r=["rs_tmp", "mhalf"], w=wk)

        def ck(name):
            if stop == name:
                P.finish()
                raise StopBuild()

        def tap(name, ap, rk, rows=None):
            if name in tap_d:
                dst = tap_d[name]
                P.dma(dst if rows is None else dst[rows], ap, reads=rk)

        S1T = gsb("S1T", [128, 8, NSEQ])
        sh1T = gsb("sh1T", [128, 8, NSEQ])
        S2T = gsb("S2T", [128, 8, NSEQ])
        sh2T = gsb("sh2T", [128, 8, NSEQ])
        gmod = gsb("gmod", [NSEQ, 2, D])
        sel = gsb("sel", [NSEQ, NSEQ, 128])
        gpre = gsb("gpre", [128, 8])
        gpre2 = gsb("gpre2", [128, 8])
        P.dma(gpre[:], gpre_d[:, :], writes=["gpre"])
        P.dma(gpre2[:], gpre2_d[:, :], writes=["gpre2"])

        G(lambda e: e.affine_select(out=sel[:], in_=ones_f[0:NSEQ, :].unsqueeze(1).broadcast_to([NSEQ, NSEQ, 128]),
                                    pattern=[[-1, NSEQ], [0, 128]], compare_op=ALU.is_equal, fill=0.0, base=0,
                                    channel_multiplier=1), r=["ones_f"], w=["sel"])

        with contextlib.ExitStack() as ast:
            asb = mk_sb(ast)
            Win = asb("Win", [128, 8, DIN], BF16)
            Wout = asb("Wout", [128, 8, D], BF16)
            cs = asb("cs", [128, NTT, 8])
            sn = asb("sn", [128, NTT, 8])
            gv_row = asb("gv_row", [128, 512])
            goa_row = asb("goa_row", [128, 512])
            gob_row = asb("gob_row", [128, 512])
            bcol = asb("bcol", [128, 4])
            WmT = asb("WmT", [128, 4, 128], BF16)
            P.dma(gv_row[:], gv_d[0:1, :].partition_broadcast(128), writes=["gv_row"])
            P.dma(goa_row[:], goa_d[0:1, :].partition_broadcast(128), writes=["goa_row"])
            P.dma(gob_row[:], gob_d[0:1, :].partition_broadcast(128), writes=["gob_row"])
            P.dma(bcol[:], bs_d[:, :], writes=["bcol"])

            with contextlib.ExitStack() as sst:
                ssb = mk_sb(sst)
                cTs = ssb("cTs", [128, 8, NSEQ])
                scs = ssb("scs", [128, 8, NSEQ])
                bada4 = ssb("bada4", [NSEQ, 6 * D])
                modrow = ssb("modrow", [NSEQ, 6 * D])
                gpost4 = ssb("gpost4", [NSEQ, 2, D])
                wada_st = [ssb("wada_st%d" % i, [128, 8, 512]) for i in range(2)]
                win_st = [ssb("win_st%d" % i, [128, DIN]) for i in range(2)]
                ws_sb = ssb("ws_sb", [128, 4, 128])
                wsm = ssb("wsm", [128, 4, 128])
                posi = ssb("posi", [128, NTT], I32)
                posf = ssb("posf", [128, NTT])
                ang = ssb("ang", [128, NTT * 8])
                angk = ssb("angk", [128, NTT * 8], I32)
                angf = ssb("angf", [128, NTT * 8])
                angm = ssb("angm", [128, NTT * 8])
                ang2 = ssb("ang2", [128, NTT * 8])

                P.dma(cTs[:], cT_d[:, :, :], writes=["cTs"])
                P.dma(bada4[:], bada_d[0:1, :].partition_broadcast(NSEQ), writes=["bada4"])
                P.dma(gpost4[:, 0, :], gpost_d[0:1, :].partition_broadcast(NSEQ), writes=["gpost4a"])
                P.dma(gpost4[:, 1, :], gpost2_d[0:1, :].partition_broadcast(NSEQ), writes=["gpost4b"])
                P.dma(posi[:], pos_d[:, :], writes=["posi"])
                P.dma(ws_sb[:], ws_d[:, :, :], writes=["ws_sb"])
                A(lambda e: e.activation(out=scs[:], in_=cTs[:], func=AF.Silu), r=["cTs"], w=["scs"])

                order = [2, 3, 0, 1] + list(range(4, 12))
                for n_, cb in enumerate(order):
                    st_ = wada_st[n_ % 2]
                    sk = "wada_st%d" % (n_ % 2)
                    P.dma(st_[:], wada_d[:, cb * 512:(cb + 1) * 512].rearrange("(k p) n -> p k n", p=128),
                          writes=[sk])
                    bk = n_ % 2
                    for k in range(8):
                        T(lambda e, k=k, st_=st_, bk=bk: e.matmul(banks[bk][0:NSEQ, :], lhsT=scs[:, k, :],
                                                                  rhs=st_[:, k, :], start=(k == 0), stop=(k == 7)),
                          r=["scs", sk], w=[bkey[bk]])
                    V(lambda e, bk=bk, cb=cb: e.tensor_tensor(out=modrow[:, cb * 512:(cb + 1) * 512],
                                                              in0=banks[bk][0:NSEQ, :],
                                                              in1=bada4[:, cb * 512:(cb + 1) * 512], op=ALU.add),
                      r=[bkey[bk], "bada4"], w=["modrow%d" % cb])
                allmod = ["modrow%d" % cb for cb in range(12)]
                for si, sp_ in enumerate([0, 1, 3, 4]):
                    for k in range(8):
                        c0 = (si * 8 + k) * NSEQ
                        T(lambda e, sp_=sp_, k=k, c0=c0: e.transpose(
                            out=banks[2][:, c0:c0 + NSEQ], in_=modrow[0:NSEQ, sp_ * D + k * 128:sp_ * D + (k + 1) * 128],
                            identity=ident[0:NSEQ, 0:NSEQ]), r=allmod + ["ident"], w=[bkey[2]])

                def mview(si):
                    return banks[2][:, si * 8 * NSEQ:(si + 1) * 8 * NSEQ].rearrange("p (k b) -> p k b", b=NSEQ)

                V(lambda e: e.tensor_copy(out=sh1T[:], in_=mview(0)), r=[bkey[2]], w=["sh1T"])
                V(lambda e: e.scalar_tensor_tensor(out=S1T[:], in0=mview(1), scalar=1.0,
                                                   in1=gpre[:].unsqueeze(2).broadcast_to([128, 8, NSEQ]),
                                                   op0=ALU.add, op1=ALU.mult), r=[bkey[2], "gpre"], w=["S1T"])
                V(lambda e: e.tensor_copy(out=sh2T[:], in_=mview(2)), r=[bkey[2]], w=["sh2T"])
                V(lambda e: e.scalar_tensor_tensor(out=S2T[:], in0=mview(3), scalar=1.0,
                                                   in1=gpre2[:].unsqueeze(2).broadcast_to([128, 8, NSEQ]),
                                                   op0=ALU.add, op1=ALU.mult), r=[bkey[2], "gpre2"], w=["S2T"])
                V(lambda e: e.tensor_tensor(out=gmod[:, 0, :], in0=modrow[:, 2 * D:3 * D], in1=gpost4[:, 0, :],
                                            op=ALU.mult), r=allmod + ["gpost4a"], w=["gmod0"])
                V(lambda e: e.tensor_tensor(out=gmod[:, 1, :], in0=modrow[:, 5 * D:6 * D], in1=gpost4[:, 1, :],
                                            op=ALU.mult), r=allmod + ["gpost4b"], w=["gmod1"])

                cast_engs = ["dve", "act", "pool"]
                ci = 0
                for k in range(8):
                    st_ = win_st[k % 2]
                    sk = "win_st%d" % (k % 2)
                    P.dma(st_[:], win_d[k * 128:(k + 1) * 128, :], writes=[sk])
                    for h0, h1 in ((0, 1188), (1188, DIN)):
                        eng = cast_engs[ci % 3]
                        ci += 1
                        if eng == "act":
                            A(lambda e, k=k, st_=st_, h0=h0, h1=h1: e.activation(out=Win[:, k, h0:h1], in_=st_[:, h0:h1],
                                                                              func=AF.Copy), r=[sk], w=["Win"])
                        else:
                            P.op(eng, lambda e, k=k, st_=st_, h0=h0, h1=h1: e.tensor_copy(out=Win[:, k, h0:h1],
                                                                                       in_=st_[:, h0:h1]),
                                 [sk], ["Win"])
                for k in range(8):
                    st_ = win_st[k % 2]
                    sk = "win_st%d" % (k % 2)
                    P.dma(st_[:, 0:D], wout_d[k * 128:(k + 1) * 128, :], writes=[sk])
                    eng = cast_engs[ci % 3]
                    ci += 1
                    if eng == "act":
                        A(lambda e, k=k, st_=st_: e.activation(out=Wout[:, k, :], in_=st_[:, 0:D], func=AF.Copy),
                          r=[sk], w=["Wout"])
                    else:
                        P.op(eng, lambda e, k=k, st_=st_: e.tensor_copy(out=Wout[:, k, :], in_=st_[:, 0:D]),
                             [sk], ["Wout"])
                for g in range(4):
                    G(lambda e, g=g: e.affine_select(out=wsm[:, g, :], in_=ws_sb[:, g, :], pattern=[[-1, 128]],
                                                     compare_op=ALU.is_ge, fill=0.0, base=0, channel_multiplier=1),
                      r=["ws_sb"], w=["wsm"])
                for g in range(4):
                    T(lambda e, g=g: e.transpose(out=banks[3][:, g * 128:(g + 1) * 128], in_=wsm[:, g, :],
                                                 identity=ident[:]), r=["wsm", "ident"], w=[bkey[3]])
                V(lambda e: e.tensor_copy(out=WmT[:], in_=banks[3][:, :].rearrange("p (g t) -> p g t", g=4)),
                  r=[bkey[3]], w=["WmT"])

                NA = NTT * 8
                V(lambda e: e.tensor_copy(out=posf[:], in_=posi[:]), r=["posi"], w=["posf"])
                V(lambda e: e.tensor_tensor(out=ang[:].rearrange("p (t f) -> p t f", f=8),
                                            in0=posf[:].unsqueeze(2).broadcast_to([128, NTT, 8]),
                                            in1=invf[:].unsqueeze(1).broadcast_to([128, NTT, 8]), op=ALU.mult),
                  r=["posf", "invf"], w=["ang"])

                def reduce_sin(dst, src_key, shift):
                    V(lambda e: e.tensor_scalar(out=ang2[:], in0=ang[:], scalar1=shift, scalar2=None, op0=ALU.add),
                      r=["ang"], w=["ang2"])
                    V(lambda e: e.tensor_scalar(out=angk[:], in0=ang2[:], scalar1=1.0 / TWO_PI, scalar2=None,
                                                op0=ALU.mult), r=["ang2"], w=["angk"])
                    V(lambda e: e.tensor_copy(out=angf[:], in_=angk[:]), r=["angk"], w=["angf"])
                    V(lambda e: e.scalar_tensor_tensor(out=ang2[:], in0=angf[:], scalar=-TWO_PI, in1=ang2[:],
                                                       op0=ALU.mult, op1=ALU.add), r=["angf", "ang2"], w=["ang2"])
                    V(lambda e: e.tensor_scalar(out=angm[:], in0=ang2[:], scalar1=math.pi, scalar2=-TWO_PI,
                                                op0=ALU.is_gt, op1=ALU.mult), r=["ang2"], w=["angm"])
                    V(lambda e: e.tensor_tensor(out=ang2[:], in0=ang2[:], in1=angm[:], op=ALU.add),
                      r=["ang2", "angm"], w=["ang2"])
                    V(lambda e: e.tensor_scalar(out=angm[:], in0=ang2[:], scalar1=-math.pi, scalar2=TWO_PI,
                                                op0=ALU.is_lt, op1=ALU.mult), r=["ang2"], w=["angm"])
                    V(lambda e: e.tensor_tensor(out=ang2[:], in0=ang2[:], in1=angm[:], op=ALU.add),
                      r=["ang2", "angm"], w=["ang2"])
                    V(lambda e: e.tensor_scalar(out=ang2[:], in0=ang2[:], scalar1=-3.1415925, scalar2=3.1415925,
                                                op0=ALU.max, op1=ALU.min), r=["ang2"], w=["ang2"])
                    A(lambda e: e.activation(out=dst[:].rearrange("p t f -> p (t f)"), in_=ang2[:], func=AF.Sin),
                      r=["ang2"], w=[src_key])

                reduce_sin(sn, "sn", 0.0)
                reduce_sin(cs, "cs", math.pi / 2.0)
                P.barrier()

            if stop == "setup":
                P.finish()
                return
            tap("S1T", S1T[:].rearrange("p k b -> p (k b)"), ["S1T"])
            tap("gmod", gmod[:].rearrange("b g d -> b (g d)"), ["gmod0", "gmod1"])
            tap("cs", cs[:].rearrange("p t f -> p (t f)"), ["cs"])
            tap("sn", sn[:].rearrange("p t f -> p (t f)"), ["sn"])

            qT2 = asb("qT2", [128, 4, S], BF16)
            kTd = asb("kTd", [128, 2, S], BF16)
            qiT2 = asb("qiT2", [128, S // 32, 4, 32], BF16)
            kiTd = asb("kiTd", [128, S], BF16)
            v_aug = asb("v_aug", [128, NT, 2, 65], BF16)
            w_tok = asb("w_tok", [128, NT, 8])
            mTa = asb("mTa", [128, 4, S], BF16)
            G1row = asb("G1row", [128, D])
            junkA = asb("junkA", [128, D], BF16)
            G(lambda e: e.memset(v_aug[:].rearrange("p a b c -> p (a b) c")[:, :, 64:65], 1.0), w=["v_aug"])

            for b in range(NSEQ):
                for n in range(2):
                    T(lambda e, n=n: e.matmul(banks[n][:, :], lhsT=sel[0:NSEQ, b, :],
                                              rhs=gmod[0:NSEQ, 0, n * 512:(n + 1) * 512], start=True, stop=True),
                      r=["sel", "gmod0"], w=[bkey[n]])
                    V(lambda e, n=n: e.tensor_copy(out=G1row[:, n * 512:(n + 1) * 512], in_=banks[n][:, :]),
                      r=[bkey[n]], w=["G1row"])

                with contextlib.ExitStack() as pst:
                    psb = mk_sb(pst)
                    xt = [psb("xt%d" % i, [128, D]) for i in range(2)]
                    xn = [psb("xn%d" % i, [128, D]) for i in range(2)]
                    hT = [psb("hT%d" % i, [128, 8, 128], BF16) for i in range(2)]
                    ssx = psb("ssx", [128, 2])
                    rsx = psb("rsx", [128, 2])
                    zu = [psb("zu%d" % i, [128, 512], BF16) for i in range(2)]
                    zv = psb("zv", [128, 512], BF16)
                    vn = [psb("vn%d" % i, [128, 512], BF16) for i in range(2)]
                    ssv = psb("ssv", [128, 4])
                    rsv = psb("rsv", [128, 4])
                    ya = psb("ya", [128, 512])
                    ssa = psb("ssa", [128, 1])
                    rsa = psb("rsa", [128, 1])
                    ma = psb("ma", [128, 512], BF16)
                    q_tok = [psb("q_tok%d" % i, [128, 8, 64], BF16) for i in range(2)]
                    qi_tok = [psb("qi_tok%d" % i, [128, 8, 64], BF16) for i in range(2)]
                    kd = [psb("kd%d" % i, [128, 2, 2, 64], BF16) for i in range(2)]
                    kid = [psb("kid%d" % i, [128, 2, 64], BF16) for i in range(2)]
                    rt = [psb("rt%d" % i, [128, 8, 8]) for i in range(4)]

                    def s1_load(i):
                        it = b * NT + i
                        P.dma(xt[i % 2][:], x_d[it * 128:(it + 1) * 128, :], writes=["xt%d" % (i % 2)])

                    def s1(i):
                        j = i % 2
                        A(lambda e: e.activation(out=junkA[:], in_=xt[j][:], func=AF.Square,
                                                 accum_out=ssx[:, j:j + 1]), r=["xt%d" % j], w=["junkA", "ssx%d" % j])
                        rstd(rsx[:, j:j + 1], ssx[:, j:j + 1], 1, 1.0 / D, ["ssx%d" % j], ["rsx%d" % j])
                        V(lambda e: e.tensor_scalar(out=xn[j][:], in0=xt[j][:], scalar1=rsx[:, j:j + 1], scalar2=None,
                                                    op0=ALU.mult), r=["xt%d" % j, "rsx%d" % j], w=["xn%d" % j])
                        for k in range(8):
                            T(lambda e, k=k: e.transpose(out=banks[k // 4][:, (k % 4) * 128:(k % 4 + 1) * 128],
                                                         in_=xn[j][:, k * 128:(k + 1) * 128], identity=ident[:]),
                              r=["xn%d" % j, "ident"], w=[bkey[k // 4]])
                        for k in range(8):
                            A(lambda e, k=k: e.activation(out=hT[j][:, k, :],
                                                          in_=banks[k // 4][:, (k % 4) * 128:(k % 4 + 1) * 128],
                                                          func=AF.Identity, scale=S1T[:, k, b:b + 1],
                                                          bias=sh1T[:, k, b:b + 1]),
                              r=[bkey[k // 4], "S1T", "sh1T"], w=["hT%d" % j])

                    GROUPS = [(0, 512, 2), (512, 512, 3), (1024, 512, 4), (1536, 328, 5), (1864, 512, 6)]

                    def rope(src3, src_key, dst3, dst_key, H, it):
                        c = cs[:, it, :].unsqueeze(1).broadcast_to([128, H, 8])
                        s_ = sn[:, it, :].unsqueeze(1).broadcast_to([128, H, 8])
                        x1 = src3[:, :, 0:8]
                        x2 = src3[:, :, 8:16]
                        t = [r_[:, 0:H, :] for r_ in rt]
                        V(lambda e: e.tensor_tensor(out=t[0], in0=x1, in1=c, op=ALU.mult), r=[src_key, "cs"], w=["rt0"])
                        V(lambda e: e.tensor_tensor(out=t[1], in0=x2, in1=s_, op=ALU.mult), r=[src_key, "sn"], w=["rt1"])
                        V(lambda e: e.tensor_tensor(out=dst3[:, :, 0:8], in0=t[0], in1=t[1], op=ALU.subtract),
                          r=["rt0", "rt1"], w=[dst_key])
                        V(lambda e: e.tensor_tensor(out=t[2], in0=x2, in1=c, op=ALU.mult), r=[src_key, "cs"], w=["rt2"])
                        V(lambda e: e.tensor_tensor(out=t[3], in0=x1, in1=s_, op=ALU.mult), r=[src_key, "sn"], w=["rt3"])
                        V(lambda e: e.tensor_tensor(out=dst3[:, :, 8:16], in0=t[2], in1=t[3], op=ALU.add),
                          r=["rt2", "rt3"], w=[dst_key])
                        A(lambda e: e.activation(out=dst3[:, :, 16:64], in_=src3[:, :, 16:64], func=AF.Copy),
                          r=[src_key], w=[dst_key])

                    def s2(i):
                        j = i % 2
                        it = b * NT + i
                        for (c0, n, bk) in GROUPS:
                            for k in range(8):
                                T(lambda e, k=k, c0=c0, n=n, bk=bk: e.matmul(banks[bk][:, 0:n], lhsT=hT[j][:, k, :],
                                                                             rhs=Win[:, k, c0:c0 + n], start=(k == 0),
                                                                             stop=(k == 7)),
                                  r=["hT%d" % j, "Win"], w=[bkey[bk]])
                        A(lambda e: e.activation(out=zu[j][:], in_=banks[2][:, :], func=AF.Gelu_apprx_tanh),
                          r=[bkey[2]], w=["zu%d" % j])
                        A(lambda e: e.activation(out=zv[:], in_=banks[3][:, :], func=AF.Gelu_apprx_tanh),
                          r=[bkey[3]], w=["zv"])
                        rope(banks[4][:, :].rearrange("p (h d) -> p h d", d=64), bkey[4], q_tok[j][:], "q_tok%d" % j, 8, it)
                        rope(banks[5][:, 0:128].rearrange("p (h d) -> p h d", d=64), bkey[5], kd[j][:, :, 0, :],
                             "kd%d" % j, 2, it)
                        V(lambda e: e.tensor_copy(out=kd[j][:, :, 1, :], in_=kd[j][:, :, 0, :]), r=["kd%d" % j],
                          w=["kd%d" % j])
                        V(lambda e: e.tensor_copy(out=v_aug[:, i, :, 0:64],
                                                  in_=banks[5][:, 128:256].rearrange("p (h d) -> p h d", d=64)),
                          r=[bkey[5]], w=["v_aug"])
                        rope(banks[5][:, 256:320].rearrange("p (h d) -> p h d", d=64), bkey[5], kid[j][:, 0:1, :],
                             "kid%d" % j, 1, it)
                        V(lambda e: e.tensor_copy(out=kid[j][:, 1:2, :], in_=kid[j][:, 0:1, :]), r=["kid%d" % j],
                          w=["kid%d" % j])
                        V(lambda e: e.tensor_copy(out=w_tok[:, i, :], in_=banks[5][:, 320:328]), r=[bkey[5]],
                          w=["w_tok"])
                        rope(banks[6][:, :].rearrange("p (h d) -> p h d", d=64), bkey[6], qi_tok[j][:], "qi_tok%d" % j, 8, it)
                        for g in range(4):
                            A(lambda e, g=g: e.activation(out=junkA[:, 0:128], in_=zv[:, g * 128:(g + 1) * 128],
                                                          func=AF.Square, accum_out=ssv[:, g:g + 1]),
                              r=["zv"], w=["junkA", "ssv"])
                        rstd(rsv[:, 0:4], ssv[:, 0:4], 4, 1.0 / 128, ["ssv"], ["rsv"])
                        for g in range(4):
                            V(lambda e, g=g: e.scalar_tensor_tensor(out=vn[j][:, g * 128:(g + 1) * 128],
                                                                    in0=zv[:, g * 128:(g + 1) * 128],
                                                                    scalar=rsv[:, g:g + 1],
                                                                    in1=gv_row[:, g * 128:(g + 1) * 128],
                                                                    op0=ALU.mult, op1=ALU.mult),
                              r=["zv", "rsv", "gv_row"], w=["vn%d" % j])

                    def s3a(i):
                        j = i % 2
                        ts = slice(i * 128, (i + 1) * 128)
                        qf = q_tok[j][:].rearrange("p h d -> p (h d)")
                        for c in range(4):
                            T(lambda e, c=c: e.transpose(out=bbf[0][:, c * 128:(c + 1) * 128],
                                                         in_=qf[:, c * 128:(c + 1) * 128], identity=identb[:]),
                              r=["q_tok%d" % j, "identb"], w=[bkey[0]])
                        for kv in range(2):
                            T(lambda e, kv=kv: e.transpose(out=bbf[0][:, 512 + kv * 128:512 + (kv + 1) * 128],
                                                           in_=kd[j][:, kv, :, :].rearrange("p a d -> p (a d)"),
                                                           identity=identb[:]),
                              r=["kd%d" % j, "identb"], w=[bkey[0]])
                        T(lambda e: e.transpose(out=bbf[0][:, 768:896], in_=kid[j][:].rearrange("p a d -> p (a d)"),
                                                identity=identb[:]), r=["kid%d" % j, "identb"], w=[bkey[0]])
                        V(lambda e: e.tensor_copy(out=qT2[:, :, ts],
                                                  in_=bbf[0][:, 0:512].rearrange("p (c t) -> p c t", t=128)),
                          r=[bkey[0]], w=["qT2"])
                        V(lambda e: e.tensor_copy(out=kTd[:, :, ts],
                                                  in_=bbf[0][:, 512:768].rearrange("p (c t) -> p c t", t=128)),
                          r=[bkey[0]], w=["kTd"])
                        V(lambda e: e.tensor_copy(out=kiTd[:, ts], in_=bbf[0][:, 768:896]), r=[bkey[0]], w=["kiTd"])
                        qif = qi_tok[j][:].rearrange("p h d -> p (h d)")
                        for c in range(4):
                            T(lambda e, c=c: e.transpose(out=bbf[1][:, c * 128:(c + 1) * 128],
                                                         in_=qif[:, c * 128:(c + 1) * 128], identity=identb[:]),
                              r=["qi_tok%d" % j, "identb"], w=[bkey[1]])
                        A(lambda e: e.activation(out=qiT2[:, i * 4:(i + 1) * 4, :, :],
                                                 in_=bbf[1][:, 0:512].rearrange("p (c g t) -> p g c t", c=4, g=4),
                                                 func=AF.Copy), r=[bkey[1]], w=["qiT2"])
                        for g in range(4):
                            T(lambda e, g=g: e.matmul(banks[7][:, g * 128:(g + 1) * 128], lhsT=WmT[:, g, :],
                                                      rhs=vn[j][:, g * 128:(g + 1) * 128], start=True, stop=True),
                              r=["WmT", "vn%d" % j], w=[bkey[7]])
                        for g in range(4):
                            V(lambda e, g=g: e.scalar_tensor_tensor(out=ya[:, g * 128:(g + 1) * 128],
                                                                    in0=banks[7][:, g * 128:(g + 1) * 128],
                                                                    scalar=bcol[:, g:g + 1],
                                                                    in1=zu[j][:, g * 128:(g + 1) * 128],
                                                                    op0=ALU.add, op1=ALU.mult),
                              r=[bkey[7], "bcol", "zu%d" % j], w=["ya"])
                        A(lambda e: e.activation(out=junkA[:, 0:512], in_=ya[:], func=AF.Square, accum_out=ssa[:]),
                          r=["ya"], w=["junkA", "ssa"])
                        rstd(rsa[:], ssa[:], 1, 1.0 / 512, ["ssa"], ["rsa"])
                        V(lambda e: e.scalar_tensor_tensor(out=ma[:], in0=ya[:], scalar=rsa[:, 0:1], in1=goa_row[:],
                                                           op0=ALU.mult, op1=ALU.mult),
                          r=["ya", "rsa", "goa_row"], w=["ma"])
                        if b == 0 and i == 0:
                            tap("ya", ya[:], ["ya"])

                    def s3b(i):
                        ts = slice(i * 128, (i + 1) * 128)
                        for c in range(4):
                            T(lambda e, c=c: e.transpose(out=bbf[7][:, c * 128:(c + 1) * 128],
                                                         in_=ma[:, c * 128:(c + 1) * 128], identity=identb[:]),
                              r=["ma", "identb"], w=[bkey[7]])
                        A(lambda e: e.activation(out=mTa[:, :, ts],
                                                 in_=bbf[7][:, 0:512].rearrange("p (c t) -> p c t", t=128),
                                                 func=AF.Copy), r=[bkey[7]], w=["mTa"])

                    s1_load(0)
                    if NT > 1:
                        s1_load(1)
                    s1(0)
                    for n in range(NT + 1):
                        if n - 1 >= 0:
                            s3a(n - 1)
                        if n + 1 < NT:
                            s1(n + 1)
                        if n + 2 < NT:
                            pass
                        if n < NT:
                            s2(n)
                            if n + 2 < NT:
                                s1_load(n + 2)
                        if n - 1 >= 0:
                            s3b(n - 1)
                    P.barrier()
                    if stop == "proj":
                        P.finish()
                        return

                with contextlib.ExitStack() as tst:
                    tsb = mk_sb(tst)
                    score = tsb("score", [128, S])
                    cmax = tsb("cmax", [128, 4])
                    mask = tsb("mask", [128, S], BF16)
                    maskT = tsb("maskT", [128, NT, 128], BF16)
                    rl = [tsb("rl%d" % i, [128, 512], BF16) for i in range(4)]
                    pT = [tsb("pT%d" % i, [128, 512], BF16) for i in range(4)]
                    Wsel = [tsb("Wsel%d" % i, [128, 8, 128], BF16) for i in range(2)]
                    wrep = tsb("wrep", [128, 2, 128])
                    wcol = tsb("wcol", [128, 8])
                    lo0 = tsb("lo0", [128, 1])
                    hi0 = tsb("hi0", [128, 1])
                    w0 = tsb("w0", [128, 1])
                    wh = tsb("wh", [128, NIT + 1])
                    cbias = tsb("cbias", [128, NT])
                    mid = tsb("mid", [128, 1])
                    cnt = tsb("cnt", [128, 1])
                    btmp = tsb("btmp", [128, 1])
                    rden = tsb("rden", [128, 8])
                    yb = tsb("yb", [128, 512])
                    ssb_ = tsb("ssb_", [128, 1])
                    rsb = tsb("rsb", [128, 1])
                    mb = tsb("mb", [128, 512], BF16)
                    mbT = tsb("mbT", [128, 4, 128], BF16)
                    sso = tsb("sso", [128, 2])
                    rso = tsb("rso", [128, 1])
                    ot = tsb("ot", [128, D])
                    xres = [tsb("xres%d" % i, [128, D]) for i in range(2)]
                    x1t = [tsb("x1t%d" % i, [128, D]) for i in range(2)]
                    for i in range(2):
                        G(lambda e, i=i: e.memset(Wsel[i][:], 0.0), w=["Wsel%d" % i])
                    for qq in range(NT):
                        G(lambda e, qq=qq: e.memset(cbias[:, qq:qq + 1], float((qq + 1) * 128 - 2 * TOPK) + 0.5),
                          w=["cbias"])
                    rl_i = [0]
                    pT_i = [0]
                    D_i = [0]

                    def indexer(qb):
                        N = (qb + 1) * 128
                        nch = (N + 511) // 512
                        wi = qb % 2
                        wk = "Wsel%d" % wi
                        w2v = w_tok[:, qb, :].rearrange("p (i two) -> p i two", two=2)
                        for par in range(2):
                            V(lambda e, par=par: e.tensor_tensor(
                                out=wrep[:, par, :].rearrange("p (i t) -> p i t", t=32),
                                in0=w2v[:, :, par].unsqueeze(2).broadcast_to([128, 4, 32]),
                                in1=D32[:].unsqueeze(1).broadcast_to([128, 4, 32]), op=ALU.mult),
                              r=["w_tok", "D32"], w=["wrep"])
                        for par in range(2):
                            T(lambda e, par=par: e.matmul(banks[0][:, par * 4:(par + 1) * 4], lhsT=wrep[:, par, :],
                                                          rhs=G4[:], start=True, stop=True),
                              r=["wrep", "G4"], w=[bkey[0]])
                        V(lambda e: e.tensor_scalar(out=wcol[:], in0=banks[0][:, 0:8], scalar1=IDX_SCALE, scalar2=None,
                                                    op0=ALU.mult), r=[bkey[0]], w=["wcol"])
                        for par in range(2):
                            for g in range(4):
                                V(lambda e, par=par, g=g: e.tensor_scalar(
                                    out=Wsel[wi][:, par * 4 + g, 32 * g:32 * g + 32], in0=D32[:],
                                    scalar1=wcol[:, par * 4 + g:par * 4 + g + 1], scalar2=None, op0=ALU.mult),
                                  r=["D32", "wcol"], w=[wk])
                        units = []
                        for c in range(nch):
                            n = min(512, N - c * 512)
                            for g in range(4):
                                for par in range(2):
                                    units.append((c, n, g, par))

                        def dots(u):
                            c, n, g, par = u
                            ri = rl_i[0] % 4
                            rl_i[0] += 1
                            ps = slice(64 * par, 64 * par + 64)
                            T(lambda e: e.matmul(banks[par][:, 0:n],
                                                 lhsT=qiT2[ps, qb * 4 + g, :, :].rearrange("p c t -> p (c t)"),
                                                 rhs=kiTd[ps, c * 512:c * 512 + n], start=True, stop=True),
                              r=["qiT2", "kiTd"], w=[bkey[par]])
                            A(lambda e: e.activation(out=rl[ri][:, 0:n], in_=banks[par][:, 0:n], func=AF.Relu),
                              r=[bkey[par]], w=["rl%d" % ri])
                            return ri

                        def selmm(u, ri):
                            c, n, g, par = u
                            sbk = 2 + (c % 2)
                            first = (g == 0 and par == 0)
                            last = (g == 3 and par == 1)
                            T(lambda e: e.matmul(banks[sbk][:, 0:n], lhsT=Wsel[wi][:, par * 4 + g, :],
                                                 rhs=rl[ri][:, 0:n], start=first, stop=last),
                              r=[wk, "rl%d" % ri], w=[bkey[sbk]])
                            if last:
                                V(lambda e: e.tensor_scalar(out=score[:, c * 512:c * 512 + n], in0=banks[sbk][:, 0:n],
                                                            scalar1=1.0, scalar2=None, op0=ALU.mult, op1=ALU.max,
                                                            accum_out=cmax[:, c:c + 1]),
                                  r=[bkey[sbk]], w=["score", "cmax"])

                        ris = {}
                        LOOK = 2
                        for i_ in range(min(LOOK, len(units))):
                            ris[i_] = dots(units[i_])
                        for i_ in range(len(units)):
                            if i_ + LOOK < len(units):
                                ris[i_ + LOOK] = dots(units[i_ + LOOK])
                            selmm(units[i_], ris[i_])

                    def topk_iter(qb):
                        N = (qb + 1) * 128
                        nch = (N + 511) // 512
                        V(lambda e: e.tensor_reduce(out=lo0[:], in_=score[:, 0:N], axis=AX.X, op=ALU.min),
                          r=["score"], w=["lo0"])
                        V(lambda e: e.tensor_reduce(out=hi0[:], in_=cmax[:, 0:nch], axis=AX.X, op=ALU.max),
                          r=["cmax"], w=["hi0"])
                        V(lambda e: e.tensor_tensor(out=score[:, qb * 128:N], in0=score[:, qb * 128:N], in1=NEGM[:],
                                                    op=ALU.add), r=["score", "NEGM"], w=["score"])
                        V(lambda e: e.tensor_tensor(out=w0[:], in0=lo0[:], in1=hi0[:], op=ALU.subtract),
                          r=["hi0", "lo0"], w=["w0"])
                        V(lambda e: e.tensor_scalar(out=wh[:], in0=P2[:], scalar1=w0[:, 0:1], scalar2=None,
                                                    op0=ALU.mult), r=["P2", "w0"], w=["wh"])
                        V(lambda e: e.tensor_scalar(out=mid[:], in0=lo0[:], scalar1=-1.0, scalar2=wh[:, 0:1],
                                                    op0=ALU.mult, op1=ALU.add), r=["lo0", "wh"], w=["mid"])
                        for i in range(NIT):
                            A(lambda e: e.activation(out=mask[:, 0:N], in_=score[:, 0:N], func=AF.Sign,
                                                     bias=mid[:, 0:1], accum_out=cnt[:]),
                              r=["score", "mid"], w=["mask", "cnt"])
                            A(lambda e: e.activation(out=btmp[:], in_=cnt[:], func=AF.Sign,
                                                     bias=cbias[:, qb:qb + 1]), r=["cnt", "cbias"], w=["btmp"])
                            A(lambda e, i=i: e.activation(out=mid[:], in_=btmp[:], func=AF.Identity,
                                                          scale=wh[:, i + 1:i + 2], bias=mid[:, 0:1]),
                              r=["btmp", "wh", "mid"], w=["mid"])
                            yield
                        V(lambda e: e.tensor_scalar(out=mid[:], in0=mid[:], scalar1=-1.0, scalar2=wh[:, NIT:NIT + 1],
                                                    op0=ALU.mult, op1=ALU.add), r=["mid", "wh"], w=["mid"])

                    def topk_finish(qb):
                        N = (qb + 1) * 128
                        if qb < KB:
                            for jj in range(qb + 1):
                                src = TRIU if jj == qb else ONESB
                                G(lambda e, jj=jj, src=src: e.tensor_copy(out=maskT[:, jj, :], in_=src[:]),
                                  r=["TRIU", "ONESB"], w=["maskT"])
                            return
                        V(lambda e: e.tensor_scalar(out=mask[:, 0:N], in0=score[:, 0:N], scalar1=mid[:, 0:1],
                                                    scalar2=None, op0=ALU.is_ge), r=["score", "mid"], w=["mask"])
                        if b == 0 and qb == NT - 1:
                            tap("score", score[:, 0:N], ["score"])
                            tap("thr", mid[:], ["mid"])
                        for jj in range(qb + 1):
                            lb = 4 + jj // 8
                            T(lambda e, jj=jj, lb=lb: e.transpose(out=bbf[lb][:, (jj % 8) * 128:(jj % 8 + 1) * 128],
                                                                  in_=mask[:, jj * 128:(jj + 1) * 128],
                                                                  identity=identb[:]),
                              r=["mask", "identb"], w=[bkey[lb]])
                        for lb in range(4, 4 + (qb + 8) // 8):
                            j0 = (lb - 4) * 8
                            j1 = min(qb + 1, j0 + 8)
                            nj = j1 - j0
                            V(lambda e, lb=lb, j0=j0, j1=j1, nj=nj: e.tensor_copy(
                                out=maskT[:, j0:j1, :],
                                in_=bbf[lb][:, 0:nj * 128].rearrange("p (j t) -> p j t", t=128)),
                              r=[bkey[lb]], w=["maskT"])

                    def attention(qb):
                        qs = slice(qb * 128, (qb + 1) * 128)
                        for kv in range(2):
                            T(lambda e, kv=kv: e.matmul(banks[6 + kv][:, 0:260], lhsT=zerob[:, 0:128],
                                                        rhs=zerob[:, 0:260], start=True, stop=False,
                                                        skip_group_check=True), r=["zerob"], w=[bkey[6 + kv]])

                        def Lstage(jj):
                            ks = slice(jj * 128, (jj + 1) * 128)
                            pis = []
                            for par in range(2):
                                ps = slice(64 * par, 64 * par + 64)
                                lb = 4 + par
                                pi = pT_i[0] % 4
                                pT_i[0] += 1
                                pis.append(pi)
                                for kv in range(2):
                                    T(lambda e, ps=ps, lb=lb, kv=kv: e.matmul(
                                        banks[lb][:, kv * 256:(kv + 1) * 256], lhsT=kTd[ps, kv, ks],
                                        rhs=qT2[ps, 2 * kv:2 * kv + 2, qs], start=True, stop=True),
                                      r=["kTd", "qT2"], w=[bkey[lb]])
                                A(lambda e, lb=lb, pi=pi: e.activation(out=pT[pi][:], in_=banks[lb][:, :], func=AF.Exp,
                                                                       scale=0.125), r=[bkey[lb]], w=["pT%d" % pi])
                                V(lambda e, pi=pi: e.tensor_tensor(
                                    out=pT[pi][:].rearrange("p (h t) -> p h t", t=128),
                                    in0=pT[pi][:].rearrange("p (h t) -> p h t", t=128),
                                    in1=maskT[:, jj, :].unsqueeze(1).broadcast_to([128, 4, 128]), op=ALU.mult),
                                  r=["pT%d" % pi, "maskT"], w=["pT%d" % pi])
                            return pis

                        def PVstage(jj, pis):
                            for par in range(2):
                                pi = pis[par]
                                for kv in range(2):
                                    for ii in range(2):
                                        hl = 2 * ii + par
                                        T(lambda e, ii=ii, hl=hl, kv=kv, pi=pi: e.matmul(
                                            banks[6 + kv][:, hl * 65:hl * 65 + 65],
                                            lhsT=pT[pi][:, (kv * 2 + ii) * 128:(kv * 2 + ii + 1) * 128],
                                            rhs=v_aug[:, jj, kv, :], start=False, stop=(jj == qb),
                                            skip_group_check=True),
                                          r=["pT%d" % pi, "v_aug"], w=[bkey[6 + kv]])

                        nxt = Lstage(0)
                        for jj in range(qb + 1):
                            cur = nxt
                            if jj + 1 <= qb:
                                nxt = Lstage(jj + 1)
                            PVstage(jj, cur)
                            yield

                    def post(qb):
                        it = b * NT + qb
                        xj = qb % 2
                        for kv in range(2):
                            ov = banks[6 + kv][:, 0:260].rearrange("p (h d) -> p h d", d=65)
                            V(lambda e, kv=kv, ov=ov: e.reciprocal(out=rden[:, kv * 4:(kv + 1) * 4], in_=ov[:, :, 64]),
                              r=[bkey[6 + kv]], w=["rden"])
                            V(lambda e, kv=kv, ov=ov: e.tensor_tensor(
                                out=yb[:, kv * 256:(kv + 1) * 256].rearrange("p (h d) -> p h d", d=64),
                                in0=ov[:, :, 0:64],
                                in1=rden[:, kv * 4:(kv + 1) * 4].unsqueeze(2).broadcast_to([128, 4, 64]),
                                op=ALU.mult), r=[bkey[6 + kv], "rden"], w=["yb"])
                        if b == 0 and qb == NT - 1:
                            tap("yb", yb[:], ["yb"])
                        A(lambda e: e.activation(out=junkA[:, 0:512], in_=yb[:], func=AF.Square, accum_out=ssb_[:]),
                          r=["yb"], w=["junkA", "ssb_"])
                        rstd(rsb[:], ssb_[:], 1, 1.0 / 512, ["ssb_"], ["rsb"])
                        V(lambda e: e.scalar_tensor_tensor(out=mb[:], in0=yb[:], scalar=rsb[:, 0:1], in1=gob_row[:],
                                                           op0=ALU.mult, op1=ALU.mult),
                          r=["yb", "rsb", "gob_row"], w=["mb"])
                        for c in range(4):
                            T(lambda e, c=c: e.transpose(out=bbf[4][:, c * 128:(c + 1) * 128],
                                                         in_=mb[:, c * 128:(c + 1) * 128], identity=identb[:]),
                              r=["mb", "identb"], w=[bkey[4]])
                        A(lambda e: e.activation(out=mbT[:], in_=bbf[4][:, 0:512].rearrange("p (c t) -> p c t", t=128),
                                                 func=AF.Copy), r=[bkey[4]], w=["mbT"])
                        for n in range(2):
                            for k in range(8):
                                lhs = mTa[:, k, qb * 128:(qb + 1) * 128] if k < 4 else mbT[:, k - 4, :]
                                T(lambda e, n=n, k=k, lhs=lhs: e.matmul(banks[6 + n][:, :], lhsT=lhs,
                                                                        rhs=Wout[:, k, n * 512:(n + 1) * 512],
                                                                        start=(k == 0), stop=(k == 7)),
                                  r=["mTa", "mbT", "Wout"], w=[bkey[6 + n]])
                        for n in range(2):
                            A(lambda e, n=n: e.activation(out=junkA[:, 0:512], in_=banks[6 + n][:, :], func=AF.Square,
                                                          accum_out=sso[:, n:n + 1]),
                              r=[bkey[6 + n]], w=["junkA", "sso%d" % n])
                        rstd(rso[:], sso[:, 0:1], 1, 1.0 / D, ["sso0", "sso1"], ["rso"], ss2_ap=sso[:, 1:2])
                        for n in range(2):
                            V(lambda e, n=n: e.scalar_tensor_tensor(out=ot[:, n * 512:(n + 1) * 512],
                                                                    in0=banks[6 + n][:, :], scalar=rso[:, 0:1],
                                                                    in1=G1row[:, n * 512:(n + 1) * 512],
                                                                    op0=ALU.mult, op1=ALU.mult),
                              r=[bkey[6 + n], "rso", "G1row"], w=["ot"])
                        G(lambda e: e.tensor_tensor(out=x1t[xj][:], in0=ot[:], in1=xres[xj][:], op=ALU.add),
                          r=["ot", "xres%d" % xj], w=["x1t%d" % xj])
                        P.dma(x1s_d[it * 128:(it + 1) * 128, :], x1t[xj][:], reads=["x1t%d" % xj],
                              writes=["x1s_%d" % it])

                    def interleave(g1, g2):
                        gens = [g for g in (g1, g2) if g is not None]
                        while gens:
                            for g_ in list(gens):
                                try:
                                    next(g_)
                                except StopIteration:
                                    gens.remove(g_)

                    if 0 >= KB:
                        indexer(0)
                        interleave(topk_iter(0), None)
                    topk_finish(0)
                    ck("a_tf0")
                    for qb in range(NT):
                        it = b * NT + qb
                        P.dma(xres[qb % 2][:], x_d[it * 128:(it + 1) * 128, :], writes=["xres%d" % (qb % 2)])
                        tk = None
                        if qb + 1 < NT and qb + 1 >= KB:
                            indexer(qb + 1)
                            ck("a_idx%d" % (qb + 1))
                            tk = topk_iter(qb + 1)
                        if stop == "a_tk%d" % (qb + 1):
                            interleave(tk, None)
                            ck("a_tk%d" % (qb + 1))
                        interleave(attention(qb), tk)
                        ck("a_att%d" % qb)
                        if qb + 1 < NT:
                            topk_finish(qb + 1)
                        ck("a_tf%d" % (qb + 1))
                        post(qb)
                        ck("a_post%d" % qb)
                    P.barrier()
                    if stop == "attn":
                        P.finish()
                        return
        with contextlib.ExitStack() as bst:
            bsb = mk_sb(bst)
            W1 = bsb("W1", [128, 8, DFF], BF16)
            W2 = bsb("W2", [128, 32, D], BF16)
            wst = [bsb("wst%d" % i, [128, 2048]) for i in range(2)]
            G2row = bsb("G2row", [128, D])
            xg = [bsb("xg%d" % i, [128, D]) for i in range(4)]
            xn2 = [bsb("xn2_%d" % i, [128, D]) for i in range(1)]
            h2T = [bsb("h2T%d" % i, [128, 8, 256], BF16) for i in range(2)]
            rr = [bsb("rr%d" % i, [128, 256], BF16) for i in range(3)]
            fT = [bsb("fT%d" % i, [128, 256], BF16) for i in range(3)]
            junkB = bsb("junkB", [128, D], BF16)
            ss2 = bsb("ss2", [128, 4])
            rs2 = bsb("rs2", [128, 4])
            ssf = bsb("ssf", [128, 4])
            rsf = bsb("rsf", [128, 2])
            of = [bsb("of%d" % i, [128, D]) for i in range(2)]

            cast_engs = ["dve", "act", "pool"]
            ci = 0
            wi_ = 0
            for k in range(8):
                for hf in range(2):
                    st_ = wst[wi_ % 2]
                    sk = "wst%d" % (wi_ % 2)
                    wi_ += 1
                    P.dma(st_[:], w1_d[k * 128:(k + 1) * 128, hf * 2048:(hf + 1) * 2048], writes=[sk])
                    for q2 in range(2):
                        eng = cast_engs[ci % 3]
                        ci += 1
                        sl = slice(q2 * 1024, (q2 + 1) * 1024)
                        dl = slice(hf * 2048 + q2 * 1024, hf * 2048 + (q2 + 1) * 1024)
                        if eng == "act":
                            A(lambda e, k=k, st_=st_, sl=sl, dl=dl: e.activation(out=W1[:, k, dl], in_=st_[:, sl],
                                                                              func=AF.Copy), r=[sk], w=["W1"])
                        else:
                            P.op(eng, lambda e, k=k, st_=st_, sl=sl, dl=dl: e.tensor_copy(out=W1[:, k, dl],
                                                                                       in_=st_[:, sl]), [sk], ["W1"])
            for c2 in range(16):
                st_ = wst[wi_ % 2]
                sk = "wst%d" % (wi_ % 2)
                wi_ += 1
                P.dma(st_[:].rearrange("p (c n) -> p c n", n=D),
                      w2_d[c2 * 256:(c2 + 1) * 256, :].rearrange("(c p) n -> p c n", p=128), writes=[sk])
                for q2 in range(2):
                    eng = cast_engs[ci % 3]
                    ci += 1
                    sl = slice(q2 * 1024, (q2 + 1) * 1024)
                    if eng == "act":
                        A(lambda e, c2=c2, q2=q2, st_=st_, sl=sl: e.activation(out=W2[:, c2 * 2 + q2, :], in_=st_[:, sl],
                                                                          func=AF.Copy), r=[sk], w=["W2"])
                    else:
                        P.op(eng, lambda e, c2=c2, q2=q2, st_=st_, sl=sl: e.tensor_copy(out=W2[:, c2 * 2 + q2, :],
                                                                                   in_=st_[:, sl]), [sk], ["W2"])

            NG = NTOK // 256

            def b_load(g):
                for t in range(2):
                    it = g * 2 + t
                    xi = (g % 2) * 2 + t
                    P.dma(xg[xi][:], x1s_d[it * 128:(it + 1) * 128, :], reads=["x1s_%d" % it], writes=["xg%d" % xi])

            def b_prep(g):
                hj = g % 2
                b = (g * 256) // S
                for t in range(2):
                    xi = (g % 2) * 2 + t
                    A(lambda e, xi=xi, t=t: e.activation(out=junkB[:], in_=xg[xi][:], func=AF.Square,
                                                        accum_out=ss2[:, t:t + 1]), r=["xg%d" % xi],
                      w=["junkB", "ss2_%d" % t])
                    rstd(rs2[:, t:t + 1], ss2[:, t:t + 1], 1, 1.0 / D, ["ss2_%d" % t], ["rs2_%d" % t])
                    V(lambda e, xi=xi, t=t: e.tensor_scalar(out=xn2[0][:], in0=xg[xi][:], scalar1=rs2[:, t:t + 1],
                                                           scalar2=None, op0=ALU.mult),
                      r=["xg%d" % xi, "rs2_%d" % t], w=["xn2_0"])
                    for k in range(8):
                        T(lambda e, k=k, t=t: e.transpose(out=banks[6 + k // 4][:, (k % 4) * 128:(k % 4 + 1) * 128],
                                                          in_=xn2[0][:, k * 128:(k + 1) * 128], identity=ident[:]),
                          r=["xn2_0", "ident"], w=[bkey[6 + k // 4]])
                    for k in range(8):
                        A(lambda e, k=k, t=t: e.activation(out=h2T[hj][:, k, t * 128:(t + 1) * 128],
                                                           in_=banks[6 + k // 4][:, (k % 4) * 128:(k % 4 + 1) * 128],
                                                           func=AF.Identity, scale=S2T[:, k, b:b + 1],
                                                           bias=sh2T[:, k, b:b + 1]),
                          r=[bkey[6 + k // 4], "S2T", "sh2T"], w=["h2T%d" % hj])

            f_i = [0]

            def b_main(g):
                hj = g % 2
                b = (g * 256) // S
                if (g * 256) % S == 0:
                    for n in range(2):
                        T(lambda e, n=n: e.matmul(banks[4 + n][:, :], lhsT=sel[0:NSEQ, b, :],
                                                  rhs=gmod[0:NSEQ, 1, n * 512:(n + 1) * 512], start=True, stop=True),
                          r=["sel", "gmod1"], w=[bkey[4 + n]])
                        V(lambda e, n=n: e.tensor_copy(out=G2row[:, n * 512:(n + 1) * 512], in_=banks[4 + n][:, :]),
                          r=[bkey[4 + n]], w=["G2row"])
                def Fst(c):
                    fb = 4 + (c % 2)
                    fi = c % 3
                    for k in range(8):
                        T(lambda e, k=k: e.matmul(banks[fb][:, 0:256], lhsT=W1[:, k, c * 128:(c + 1) * 128],
                                                  rhs=h2T[hj][:, k, :], start=(k == 0), stop=(k == 7)),
                          r=["W1", "h2T%d" % hj], w=[bkey[fb]])
                    A(lambda e: e.activation(out=rr[fi][:], in_=banks[fb][:, 0:256], func=AF.Relu),
                      r=[bkey[fb]], w=["rr%d" % fi])
                    V(lambda e: e.scalar_tensor_tensor(out=fT[fi][:], in0=banks[fb][:, 0:256], scalar=0.0,
                                                       in1=rr[fi][:], op0=ALU.max, op1=ALU.mult),
                      r=[bkey[fb], "rr%d" % fi], w=["fT%d" % fi])

                def P2st(c):
                    fi = c % 3
                    for t in range(2):
                        for n in range(2):
                            ob = t * 2 + n
                            T(lambda e, t=t, n=n, ob=ob: e.matmul(
                                banks[ob][:, :], lhsT=fT[fi][:, t * 128:(t + 1) * 128],
                                rhs=W2[:, c, n * 512:(n + 1) * 512], start=(c == 0), stop=(c == 31)),
                              r=["fT%d" % fi, "W2"], w=[bkey[ob]])

                Fst(0)
                for c in range(32):
                    if c + 1 < 32:
                        Fst(c + 1)
                    P2st(c)
                    if c == 20 and g + 1 < NG:
                        b_prep(g + 1)
                for t in range(2):
                    it = g * 2 + t
                    xi = (g % 2) * 2 + t
                    for n in range(2):
                        A(lambda e, t=t, n=n: e.activation(out=junkB[:, 0:512], in_=banks[t * 2 + n][:, :],
                                                           func=AF.Square, accum_out=ssf[:, t * 2 + n:t * 2 + n + 1]),
                          r=[bkey[t * 2 + n]], w=["junkB", "ssf%d" % (t * 2 + n)])
                    rstd(rsf[:, t:t + 1], ssf[:, t * 2:t * 2 + 1], 1, 1.0 / D, ["ssf%d" % (t * 2), "ssf%d" % (t * 2 + 1)],
                         ["rsf%d" % t], ss2_ap=ssf[:, t * 2 + 1:t * 2 + 2])
                    for n in range(2):
                        V(lambda e, t=t, n=n: e.scalar_tensor_tensor(out=of[t][:, n * 512:(n + 1) * 512],
                                                                     in0=banks[t * 2 + n][:, :], scalar=rsf[:, t:t + 1],
                                                                     in1=G2row[:, n * 512:(n + 1) * 512],
                                                                     op0=ALU.mult, op1=ALU.mult),
                          r=[bkey[t * 2 + n], "rsf%d" % t, "G2row"], w=["of%d" % t])
                    G(lambda e, t=t, xi=xi: e.tensor_tensor(out=of[t][:], in0=of[t][:], in1=xg[xi][:], op=ALU.add),
                      r=["of%d" % t, "xg%d" % xi], w=["of%d" % t])
                    P.dma(out_d[it * 128:(it + 1) * 128, :], of[t][:], reads=["of%d" % t])
                if g + 2 < NG:
                    b_load(g + 2)

            b_load(0)
            if NG > 1:
                b_load(1)
            b_prep(0)
            for g in range(NG):
                b_main(g)
            P.finish()
        print("program built: instrs=%d waits=%d" % (P.ninstr, P.nwaits), flush=True)


def make_core_inputs(ci, NSEQ, S, x, c, positions, w_ada, b_ada, g_pre_mix, w_in, g_sgu_v, w_spatial, b_spatial,
                     g_out_sgu, g_out_attn, w_out, g_post_mix, g_pre_ffn, w_ff1, w_ff2, g_post_ffn):
    f32 = np.float32
    bs = slice(ci * NSEQ, (ci + 1) * NSEQ)
    NT = S // 128
    xc = np.ascontiguousarray(x[bs]).reshape(NSEQ * S, D).astype(f32, copy=False)
    cc = np.asarray(c[bs], dtype=f32)
    cT = np.ascontiguousarray(cc.T.reshape(8, 128, NSEQ).transpose(1, 0, 2))
    pos = np.ascontiguousarray(np.asarray(positions[bs]).reshape(NSEQ * NT, 128).T.astype(np.int32))
    wi = np.asarray(w_in[0], dtype=f32)
    perm = np.concatenate([np.arange(0, 1792), np.arange(2304, 2376), np.arange(1792, 2304)])
    wi_p = np.ascontiguousarray(wi[:, perm])
    return {
        "x": xc, "cT": cT, "pos": pos,
        "w_ada": np.ascontiguousarray(w_ada[0], dtype=f32),
        "b_ada": np.ascontiguousarray(b_ada[0:1], dtype=f32),
        "w_in": wi_p,
        "gpre": np.ascontiguousarray(np.asarray(g_pre_mix[0], dtype=f32).reshape(8, 128).T),
        "gpre2": np.ascontiguousarray(np.asarray(g_pre_ffn[0], dtype=f32).reshape(8, 128).T),
        "gv": np.ascontiguousarray(g_sgu_v[0:1], dtype=f32),
        "ws": np.ascontiguousarray(np.asarray(w_spatial[0], dtype=f32).transpose(1, 0, 2)),
        "bs": np.ascontiguousarray(np.asarray(b_spatial[0], dtype=f32).T),
        "goa": np.ascontiguousarray(g_out_sgu[0:1], dtype=f32),
        "gob": np.ascontiguousarray(g_out_attn[0:1], dtype=f32),
        "w_out": np.ascontiguousarray(w_out[0], dtype=f32),
        "gpost": np.ascontiguousarray(g_post_mix[0:1], dtype=f32),
        "w1": np.ascontiguousarray(w_ff1[0], dtype=f32),
        "w2": np.ascontiguousarray(w_ff2[0], dtype=f32),
        "gpost2": np.ascontiguousarray(g_post_ffn[0:1], dtype=f32),
    }


def run(inputs, n_cores, NSEQ, S, taps=None, trace=False, stop=None):
    nc = bass.Bass("TRN2", target_bir_lowering=False)
    try:
        build_program(nc, NSEQ=NSEQ, S=S, taps=taps, stop=stop)
    except StopBuild:
        pass
    in_maps = [make_core_inputs(ci, NSEQ, S, **inputs) for ci in range(n_cores)]
    res = run_bass_kernel_spmd(nc, in_maps, core_ids=list(range(n_cores)), trace=trace)
    return res


def kernel(**inputs):
    inputs = {k: np.asarray(v) for k, v in inputs.items()}
    B, S, _ = inputs["x"].shape
    NSEQ = B // NCORES
    res = run(inputs, NCORES, NSEQ, S)
    outs = [np.asarray(r["out"]).reshape(NSEQ, S, D) for r in res.results]
    return np.concatenate(outs, axis=0).astype(np.float32, copy=False)
```

```python
import contextlib
import math
import numpy as np
import concourse.bass as bass
import concourse.mybir as mybir
from concourse.bass_utils import run_bass_kernel_spmd

F32 = mybir.dt.float32
BF16 = mybir.dt.bfloat16
I32 = mybir.dt.int32
AF = mybir.ActivationFunctionType
ALU = mybir.AluOpType
AX = mybir.AxisListType

D = 1024
DIN = 2376
DFF = 4096
NCORES = 8
EPS = 1e-6
NIT = 16
IDX_SCALE = (64 ** -0.5) * (8 ** -0.5)
TWO_PI = 2.0 * math.pi


class StopBuild(Exception):
    pass


class Prog:
    NDMA = 32

    def __init__(self, nc, stack):
        self.nc = nc
        self.eng = {"pe": nc.tensor, "act": nc.scalar, "dve": nc.vector,
                    "pool": nc.gpsimd, "sp": nc.sync}
        self.sem = {k: stack.enter_context(nc.semaphore("c_" + k)) for k in self.eng}
        self.cnt = {k: 0 for k in self.eng}
        self.dsem = [stack.enter_context(nc.semaphore("d%d" % i)) for i in range(self.NDMA)]
        self.dval = [0] * self.NDMA
        self.dnext = 0
        self.seen = {k: {} for k in self.eng}
        self.res = {}
        self.nwaits = 0
        self.ninstr = 0

    def _wait(self, eng, dep):
        kind, key, val = dep
        if kind == "e":
            if key == "pe" and eng == "pe":
                return
            sem = self.sem[key]
            skey = key
        else:
            sem = self.dsem[key]
            skey = ("d", key)
        if self.seen[eng].get(skey, 0) >= val:
            return
        self.seen[eng][skey] = val
        self.eng[eng].wait_ge(sem, val)
        self.nwaits += 1

    def _deps(self, eng, reads, writes):
        deps = []
        for r in reads:
            st = self.res.get(r)
            if st and st["w"]:
                deps.append(st["w"])
        for w in writes:
            st = self.res.get(w)
            if st:
                if st["w"]:
                    deps.append(st["w"])
                deps.extend(st["r"])
        for d in deps:
            self._wait(eng, d)

    def _record(self, token, reads, writes):
        for r in reads:
            st = self.res.setdefault(r, {"w": None, "r": []})
            st["r"].append(token)
            if len(st["r"]) > 48:
                best = {}
                for t in st["r"]:
                    k = (t[0], t[1])
                    if k not in best or best[k][2] < t[2]:
                        best[k] = t
                st["r"] = list(best.values())
        for w in writes:
            self.res[w] = {"w": token, "r": []}

    def op(self, eng, fn, reads=(), writes=()):
        self._deps(eng, reads, writes)
        ins = fn(self.eng[eng])
        self.cnt[eng] += 1
        ins.then_inc(self.sem[eng], 1)
        token = ("e", eng, self.cnt[eng])
        self._record(token, reads, writes)
        self.ninstr += 1
        return token

    def dma(self, out, in_, reads=(), writes=(), q="sp", **kw):
        self._deps(q, reads, writes)
        i = self.dnext
        self.dnext = (self.dnext + 1) % self.NDMA
        if self.dval[i] > 0:
            self._wait(q, ("d", i, self.dval[i]))
        ins = self.eng[q].dma_start(out=out, in_=in_, **kw)
        self.dval[i] += 16
        ins.then_inc(self.dsem[i], 16)
        token = ("d", i, self.dval[i])
        self._record(token, reads, writes)
        self.ninstr += 1
        return token

    def barrier(self):
        for e in self.eng:
            for f in self.eng:
                if f != e and self.cnt[f] > 0:
                    self._wait(e, ("e", f, self.cnt[f]))
            for i in range(self.NDMA):
                if self.dval[i] > 0:
                    self._wait(e, ("d", i, self.dval[i]))
        self.res = {}

    def finish(self):
        for i in range(self.NDMA):
            if self.dval[i] > 0:
                self._wait("sp", ("d", i, self.dval[i]))
        for f in self.eng:
            if f != "sp" and self.cnt[f] > 0:
                self._wait("sp", ("e", f, self.cnt[f]))


def build_program(nc, NSEQ=4, S=2048, taps=None, stop=None):
    NT = S // 128
    NTT = NSEQ * NT
    NTOK = NSEQ * S
    TOPK = min(256, S // 4)
    KB = TOPK // 128
    taps = taps or {}

    def din(name, shape, dt=F32):
        return nc.dram_tensor(name, list(shape), dt, kind="ExternalInput").ap()

    x_d = din("x", [NTOK, D])
    cT_d = din("cT", [128, 8, NSEQ])
    pos_d = din("pos", [128, NTT], I32)
    wada_d = din("w_ada", [D, 6 * D])
    bada_d = din("b_ada", [1, 6 * D])
    win_d = din("w_in", [D, DIN])
    gpre_d = din("gpre", [128, 8])
    gpre2_d = din("gpre2", [128, 8])
    gv_d = din("gv", [1, 512])
    ws_d = din("ws", [128, 4, 128])
    bs_d = din("bs", [128, 4])
    goa_d = din("goa", [1, 512])
    gob_d = din("gob", [1, 512])
    wout_d = din("w_out", [D, D])
    gpost_d = din("gpost", [1, D])
    w1_d = din("w1", [D, DFF])
    w2_d = din("w2", [DFF, D])
    gpost2_d = din("gpost2", [1, D])
    out_d = nc.dram_tensor("out", [NTOK, D], F32, kind="ExternalOutput").ap()
    x1s_d = out_d
    tap_d = {k: nc.dram_tensor("tap_" + k, list(shp), F32, kind="ExternalOutput").ap()
             for k, shp in taps.items()}

    with contextlib.ExitStack() as gst:
        P = Prog(nc, gst)

        uid = [0]

        def mk_sb(stack):
            def sb(name, shape, dt=F32):
                uid[0] += 1
                return stack.enter_context(nc.sbuf_tensor("s%d_%s" % (uid[0], name), list(shape), dt))
            return sb

        gsb = mk_sb(gst)
        banks = [gst.enter_context(nc.psum_tensor("bank%d" % i, [128, 512], F32)) for i in range(8)]
        bkey = ["b%d" % i for i in range(8)]
        bbf = [b[:].bitcast(BF16) for b in banks]

        def V(fn, r=(), w=()):
            return P.op("dve", fn, r, w)

        def A(fn, r=(), w=()):
            return P.op("act", fn, r, w)

        def G(fn, r=(), w=()):
            return P.op("pool", fn, r, w)

        def T(fn, r=(), w=()):
            return P.op("pe", fn, r, w)

        ident = gsb("ident", [128, 128])
        identb = gsb("identb", [128, 128], BF16)
        ones_f = gsb("ones_f", [128, 128])
        zeros_f = gsb("zeros_f", [128, 128])
        NEGM = gsb("NEGM", [128, 128])
        TRIU = gsb("TRIU", [128, 128], BF16)
        ONESB = gsb("ONESB", [128, 128], BF16)
        D32 = gsb("D32", [128, 32])
        G4 = gsb("G4", [128, 4])
        zerob = gsb("zerob", [128, 260], BF16)
        P2 = gsb("P2", [128, NIT + 1])
        mhalf = gsb("mhalf", [128, 16])
        iot = gsb("iot", [128, 8], I32)
        iof = gsb("iof", [128, 8])
        invf = gsb("invf", [128, 8])
        rs_tmp = gsb("rs_tmp", [128, 16])

        G(lambda e: e.memset(ones_f[:], 1.0), w=["ones_f"])
        G(lambda e: e.memset(zeros_f[:], 0.0), w=["zeros_f"])
        G(lambda e: e.affine_select(out=ident[:], in_=ones_f[:], pattern=[[-1, 128]], compare_op=ALU.is_equal,
                                    fill=0.0, base=0, channel_multiplier=1), r=["ones_f"], w=["ident"])
        V(lambda e: e.tensor_copy(out=identb[:], in_=ident[:]), r=["ident"], w=["identb"])
        G(lambda e: e.affine_select(out=NEGM[:], in_=zeros_f[:], pattern=[[-1, 128]], compare_op=ALU.is_ge,
                                    fill=-1.0e30, base=0, channel_multiplier=1), r=["zeros_f"], w=["NEGM"])
        G(lambda e: e.affine_select(out=TRIU[:], in_=ones_f[:], pattern=[[1, 128]], compare_op=ALU.is_ge,
                                    fill=0.0, base=0, channel_multiplier=-1), r=["ones_f"], w=["TRIU"])
        V(lambda e: e.tensor_copy(out=ONESB[:], in_=ones_f[:]), r=["ones_f"], w=["ONESB"])
        for m in range(4):
            G(lambda e, m=m: e.affine_select(out=D32[32 * m:32 * m + 32, :], in_=ones_f[32 * m:32 * m + 32, 0:32],
                                             pattern=[[-1, 32]], compare_op=ALU.is_equal, fill=0.0, base=0,
                                             channel_multiplier=1), r=["ones_f"], w=["D32"])
        G(lambda e: e.memset(G4[:], 0.0), w=["G4"])
        for g in range(4):
            G(lambda e, g=g: e.memset(G4[32 * g:32 * g + 32, g:g + 1], 1.0), r=["G4"], w=["G4"])
        G(lambda e: e.memset(zerob[:], 0.0), w=["zerob"])
        for i in range(NIT + 1):
            G(lambda e, i=i: e.memset(P2[:, i:i + 1], 2.0 ** -(i + 1)), w=["P2"])
        G(lambda e: e.memset(mhalf[:], -0.5), w=["mhalf"])
        G(lambda e: e.iota(iot[:], pattern=[[1, 8]], base=0, channel_multiplier=0), w=["iot"])
        V(lambda e: e.tensor_copy(out=iof[:], in_=iot[:]), r=["iot"], w=["iof"])
        A(lambda e: e.activation(out=invf[:], in_=iof[:], func=AF.Exp, scale=-math.log(500000.0) / 8.0),
          r=["iof"], w=["invf"])

        def rstd(out_ap, ss_ap, n, inv_n, rk, wk, ss2_ap=None):
            tmp = rs_tmp[:, 0:n]
            if ss2_ap is not None:
                G(lambda e: e.tensor_tensor(out=tmp, in0=ss_ap, in1=ss2_ap, op=ALU.add), r=rk, w=["rs_tmp"])
                G(lambda e: e.tensor_scalar(out=tmp, in0=tmp, scalar1=inv_n, scalar2=EPS, op0=ALU.mult,
                                            op1=ALU.add), r=["rs_tmp"], w=["rs_tmp"])
            else:
                G(lambda e: e.tensor_scalar(out=tmp, in0=ss_ap, scalar1=inv_n, scalar2=EPS, op0=ALU.mult,
                                            op1=ALU.add), r=rk, w=["rs_tmp"])
            G(lambda e: e.tensor_tensor(out=out_ap, in0=tmp, in1=mhalf[:, 0:n], op=ALU.pow),
              r=["rs_tmp", "mhalf"], w=wk)

        def ck(name):
            if stop == name:
                P.finish()
                raise StopBuild()

        def tap(name, ap, rk, rows=None):
            if name in tap_d:
                dst = tap_d[name]
                P.dma(dst if rows is None else dst[rows], ap, reads=rk)

        S1T = gsb("S1T", [128, 8, NSEQ])
        sh1T = gsb("sh1T", [128, 8, NSEQ])
        S2T = gsb("S2T", [128, 8, NSEQ])
        sh2T = gsb("sh2T", [128, 8, NSEQ])
        gmod = gsb("gmod", [NSEQ, 2, D])
        sel = gsb("sel", [NSEQ, NSEQ, 128])
        gpre = gsb("gpre", [128, 8])
        gpre2 = gsb("gpre2", [128, 8])
        P.dma(gpre[:], gpre_d[:, :], writes=["gpre"])
        P.dma(gpre2[:], gpre2_d[:, :], writes=["gpre2"])

        G(lambda e: e.affine_select(out=sel[:], in_=ones_f[0:NSEQ, :].unsqueeze(1).broadcast_to([NSEQ, NSEQ, 128]),
                                    pattern=[[-1, NSEQ], [0, 128]], compare_op=ALU.is_equal, fill=0.0, base=0,
                                    channel_multiplier=1), r=["ones_f"], w=["sel"])

        with contextlib.ExitStack() as ast:
            asb = mk_sb(ast)
            Win = asb("Win", [128, 8, DIN], BF16)
            Wout = asb("Wout", [128, 8, D], BF16)
            cs = asb("cs", [128, NTT, 8])
            sn = asb("sn", [128, NTT, 8])
            gv_row = asb("gv_row", [128, 512])
            goa_row = asb("goa_row", [128, 512])
            gob_row = asb("gob_row", [128, 512])
            bcol = asb("bcol", [128, 4])
            WmT = asb("WmT", [128, 4, 128], BF16)
            P.dma(gv_row[:], gv_d[0:1, :].partition_broadcast(128), writes=["gv_row"])
            P.dma(goa_row[:], goa_d[0:1, :].partition_broadcast(128), writes=["goa_row"])
            P.dma(gob_row[:], gob_d[0:1, :].partition_broadcast(128), writes=["gob_row"])
            P.dma(bcol[:], bs_d[:, :], writes=["bcol"])

            with contextlib.ExitStack() as sst:
                ssb = mk_sb(sst)
                cTs = ssb("cTs", [128, 8, NSEQ])
                scs = ssb("scs", [128, 8, NSEQ])
                bada4 = ssb("bada4", [NSEQ, 6 * D])
                modrow = ssb("modrow", [NSEQ, 6 * D])
                gpost4 = ssb("gpost4", [NSEQ, 2, D])
                wada_st = [ssb("wada_st%d" % i, [128, 8, 512]) for i in range(2)]
                win_st = [ssb("win_st%d" % i, [128, DIN]) for i in range(2)]
                ws_sb = ssb("ws_sb", [128, 4, 128])
                wsm = ssb("wsm", [128, 4, 128])
                posi = ssb("posi", [128, NTT], I32)
                posf = ssb("posf", [128, NTT])
                ang = ssb("ang", [128, NTT * 8])
                angk = ssb("angk", [128, NTT * 8], I32)
                angf = ssb("angf", [128, NTT * 8])
                angm = ssb("angm", [128, NTT * 8])
                ang2 = ssb("ang2", [128, NTT * 8])

                P.dma(cTs[:], cT_d[:, :, :], writes=["cTs"])
                P.dma(bada4[:], bada_d[0:1, :].partition_broadcast(NSEQ), writes=["bada4"])
                P.dma(gpost4[:, 0, :], gpost_d[0:1, :].partition_broadcast(NSEQ), writes=["gpost4a"])
                P.dma(gpost4[:, 1, :], gpost2_d[0:1, :].partition_broadcast(NSEQ), writes=["gpost4b"])
                P.dma(posi[:], pos_d[:, :], writes=["posi"])
                P.dma(ws_sb[:], ws_d[:, :, :], writes=["ws_sb"])
                A(lambda e: e.activation(out=scs[:], in_=cTs[:], func=AF.Silu), r=["cTs"], w=["scs"])

                order = [2, 3, 0, 1] + list(range(4, 12))
                for n_, cb in enumerate(order):
                    st_ = wada_st[n_ % 2]
                    sk = "wada_st%d" % (n_ % 2)
                    P.dma(st_[:], wada_d[:, cb * 512:(cb + 1) * 512].rearrange("(k p) n -> p k n", p=128),
                          writes=[sk])
                    bk = n_ % 2
                    for k in range(8):
                        T(lambda e, k=k, st_=st_, bk=bk: e.matmul(banks[bk][0:NSEQ, :], lhsT=scs[:, k, :],
                                                                  rhs=st_[:, k, :], start=(k == 0), stop=(k == 7)),
                          r=["scs", sk], w=[bkey[bk]])
                    V(lambda e, bk=bk, cb=cb: e.tensor_tensor(out=modrow[:, cb * 512:(cb + 1) * 512],
                                                              in0=banks[bk][0:NSEQ, :],
                                                              in1=bada4[:, cb * 512:(cb + 1) * 512], op=ALU.add),
                      r=[bkey[bk], "bada4"], w=["modrow%d" % cb])
                allmod = ["modrow%d" % cb for cb in range(12)]
                for si, sp_ in enumerate([0, 1, 3, 4]):
                    for k in range(8):
                        c0 = (si * 8 + k) * NSEQ
                        T(lambda e, sp_=sp_, k=k, c0=c0: e.transpose(
                            out=banks[2][:, c0:c0 + NSEQ], in_=modrow[0:NSEQ, sp_ * D + k * 128:sp_ * D + (k + 1) * 128],
                            identity=ident[0:NSEQ, 0:NSEQ]), r=allmod + ["ident"], w=[bkey[2]])

                def mview(si):
                    return banks[2][:, si * 8 * NSEQ:(si + 1) * 8 * NSEQ].rearrange("p (k b) -> p k b", b=NSEQ)

                V(lambda e: e.tensor_copy(out=sh1T[:], in_=mview(0)), r=[bkey[2]], w=["sh1T"])
                V(lambda e: e.scalar_tensor_tensor(out=S1T[:], in0=mview(1), scalar=1.0,
                                                   in1=gpre[:].unsqueeze(2).broadcast_to([128, 8, NSEQ]),
                                                   op0=ALU.add, op1=ALU.mult), r=[bkey[2], "gpre"], w=["S1T"])
                V(lambda e: e.tensor_copy(out=sh2T[:], in_=mview(2)), r=[bkey[2]], w=["sh2T"])
                V(lambda e: e.scalar_tensor_tensor(out=S2T[:], in0=mview(3), scalar=1.0,
                                                   in1=gpre2[:].unsqueeze(2).broadcast_to([128, 8, NSEQ]),
                                                   op0=ALU.add, op1=ALU.mult), r=[bkey[2], "gpre2"], w=["S2T"])
                V(lambda e: e.tensor_tensor(out=gmod[:, 0, :], in0=modrow[:, 2 * D:3 * D], in1=gpost4[:, 0, :],
                                            op=ALU.mult), r=allmod + ["gpost4a"], w=["gmod0"])
                V(lambda e: e.tensor_tensor(out=gmod[:, 1, :], in0=modrow[:, 5 * D:6 * D], in1=gpost4[:, 1, :],
                                            op=ALU.mult), r=allmod + ["gpost4b"], w=["gmod1"])

                cast_engs = ["dve", "act", "pool"]
                ci = 0
                for k in range(8):
                    st_ = win_st[k % 2]
                    sk = "win_st%d" % (k % 2)
                    P.dma(st_[:], win_d[k * 128:(k + 1) * 128, :], writes=[sk])
                    for h0, h1 in ((0, 1188), (1188, DIN)):
                        eng = cast_engs[ci % 3]
                        ci += 1
                        if eng == "act":
                            A(lambda e, k=k, st_=st_, h0=h0, h1=h1: e.activation(out=Win[:, k, h0:h1], in_=st_[:, h0:h1],
                                                                              func=AF.Copy), r=[sk], w=["Win"])
                        else:
                            P.op(eng, lambda e, k=k, st_=st_, h0=h0, h1=h1: e.tensor_copy(out=Win[:, k, h0:h1],
                                                                                       in_=st_[:, h0:h1]),
                                 [sk], ["Win"])
                for k in range(8):
                    st_ = win_st[k % 2]
                    sk = "win_st%d" % (k % 2)
                    P.dma(st_[:, 0:D], wout_d[k * 128:(k + 1) * 128, :], writes=[sk])
                    eng = cast_engs[ci % 3]
                    ci += 1
                    if eng == "act":
                        A(lambda e, k=k, st_=st_: e.activation(out=Wout[:, k, :], in_=st_[:, 0:D], func=AF.Copy),
                          r=[sk], w=["Wout"])
                    else:
                        P.op(eng, lambda e, k=k, st_=st_: e.tensor_copy(out=Wout[:, k, :], in_=st_[:, 0:D]),
                             [sk], ["Wout"])
                for g in range(4):
                    G(lambda e, g=g: e.affine_select(out=wsm[:, g, :], in_=ws_sb[:, g, :], pattern=[[-1, 128]],
                                                     compare_op=ALU.is_ge, fill=0.0, base=0, channel_multiplier=1),
                      r=["ws_sb"], w=["wsm"])
                for g in range(4):
                    T(lambda e, g=g: e.transpose(out=banks[3][:, g * 128:(g + 1) * 128], in_=wsm[:, g, :],
                                                 identity=ident[:]), r=["wsm", "ident"], w=[bkey[3]])
                V(lambda e: e.tensor_copy(out=WmT[:], in_=banks[3][:, :].rearrange("p (g t) -> p g t", g=4)),
                  r=[bkey[3]], w=["WmT"])

                NA = NTT * 8
                V(lambda e: e.tensor_copy(out=posf[:], in_=posi[:]), r=["posi"], w=["posf"])
                V(lambda e: e.tensor_tensor(out=ang[:].rearrange("p (t f) -> p t f", f=8),
                                            in0=posf[:].unsqueeze(2).broadcast_to([128, NTT, 8]),
                                            in1=invf[:].unsqueeze(1).broadcast_to([128, NTT, 8]), op=ALU.mult),
                  r=["posf", "invf"], w=["ang"])

                def reduce_sin(dst, src_key, shift):
                    V(lambda e: e.tensor_scalar(out=ang2[:], in0=ang[:], scalar1=shift, scalar2=None, op0=ALU.add),
                      r=["ang"], w=["ang2"])
                    V(lambda e: e.tensor_scalar(out=angk[:], in0=ang2[:], scalar1=1.0 / TWO_PI, scalar2=None,
                                                op0=ALU.mult), r=["ang2"], w=["angk"])
                    V(lambda e: e.tensor_copy(out=angf[:], in_=angk[:]), r=["angk"], w=["angf"])
                    V(lambda e: e.scalar_tensor_tensor(out=ang2[:], in0=angf[:], scalar=-TWO_PI, in1=ang2[:],
                                                       op0=ALU.mult, op1=ALU.add), r=["angf", "ang2"], w=["ang2"])
                    V(lambda e: e.tensor_scalar(out=angm[:], in0=ang2[:], scalar1=math.pi, scalar2=-TWO_PI,
                                                op0=ALU.is_gt, op1=ALU.mult), r=["ang2"], w=["angm"])
                    V(lambda e: e.tensor_tensor(out=ang2[:], in0=ang2[:], in1=angm[:], op=ALU.add),
                      r=["ang2", "angm"], w=["ang2"])
                    V(lambda e: e.tensor_scalar(out=angm[:], in0=ang2[:], scalar1=-math.pi, scalar2=TWO_PI,
                                                op0=ALU.is_lt, op1=ALU.mult), r=["ang2"], w=["angm"])
                    V(lambda e: e.tensor_tensor(out=ang2[:], in0=ang2[:], in1=angm[:], op=ALU.add),
                      r=["ang2", "angm"], w=["ang2"])
                    V(lambda e: e.tensor_scalar(out=ang2[:], in0=ang2[:], scalar1=-3.1415925, scalar2=3.1415925,
                                                op0=ALU.max, op1=ALU.min), r=["ang2"], w=["ang2"])
                    A(lambda e: e.activation(out=dst[:].rearrange("p t f -> p (t f)"), in_=ang2[:], func=AF.Sin),
                      r=["ang2"], w=[src_key])

                reduce_sin(sn, "sn", 0.0)
                reduce_sin(cs, "cs", math.pi / 2.0)
                P.barrier()

            if stop == "setup":
                P.finish()
                return
            tap("S1T", S1T[:].rearrange("p k b -> p (k b)"), ["S1T"])
            tap("gmod", gmod[:].rearrange("b g d -> b (g d)"), ["gmod0", "gmod1"])
            tap("cs", cs[:].rearrange("p t f -> p (t f)"), ["cs"])
            tap("sn", sn[:].rearrange("p t f -> p (t f)"), ["sn"])

            qT2 = asb("qT2", [128, 4, S], BF16)
            kTd = asb("kTd", [128, 2, S], BF16)
            qiT2 = asb("qiT2", [128, S // 32, 4, 32], BF16)
            kiTd = asb("kiTd", [128, S], BF16)
            v_aug = asb("v_aug", [128, NT, 2, 65], BF16)
            w_tok = asb("w_tok", [128, NT, 8])
            mTa = asb("mTa", [128, 4, S], BF16)
            G1row = asb("G1row", [128, D])
            junkA = asb("junkA", [128, D], BF16)
            G(lambda e: e.memset(v_aug[:].rearrange("p a b c -> p (a b) c")[:, :, 64:65], 1.0), w=["v_aug"])

            for b in range(NSEQ):
                for n in range(2):
                    T(lambda e, n=n: e.matmul(banks[n][:, :], lhsT=sel[0:NSEQ, b, :],
                                              rhs=gmod[0:NSEQ, 0, n * 512:(n + 1) * 512], start=True, stop=True),
                      r=["sel", "gmod0"], w=[bkey[n]])
                    V(lambda e, n=n: e.tensor_copy(out=G1row[:, n * 512:(n + 1) * 512], in_=banks[n][:, :]),
                      r=[bkey[n]], w=["G1row"])

                with contextlib.ExitStack() as pst:
                    psb = mk_sb(pst)
                    xt = [psb("xt%d" % i, [128, D]) for i in range(2)]
                    xn = [psb("xn%d" % i, [128, D]) for i in range(2)]
                    hT = [psb("hT%d" % i, [128, 8, 128], BF16) for i in range(2)]
                    ssx = psb("ssx", [128, 2])
                    rsx = psb("rsx", [128, 2])
                    zu = [psb("zu%d" % i, [128, 512], BF16) for i in range(2)]
                    zv = psb("zv", [128, 512], BF16)
                    vn = [psb("vn%d" % i, [128, 512], BF16) for i in range(2)]
                    ssv = psb("ssv", [128, 4])
                    rsv = psb("rsv", [128, 4])
                    ya = psb("ya", [128, 512])
                    ssa = psb("ssa", [128, 1])
                    rsa = psb("rsa", [128, 1])
                    ma = psb("ma", [128, 512], BF16)
                    q_tok = [psb("q_tok%d" % i, [128, 8, 64], BF16) for i in range(2)]
                    qi_tok = [psb("qi_tok%d" % i, [128, 8, 64], BF16) for i in range(2)]
                    kd = [psb("kd%d" % i, [128, 2, 2, 64], BF16) for i in range(2)]
                    kid = [psb("kid%d" % i, [128, 2, 64], BF16) for i in range(2)]
                    rt = [psb("rt%d" % i, [128, 8, 8]) for i in range(4)]

                    def s1_load(i):
                        it = b * NT + i
                        P.dma(xt[i % 2][:], x_d[it * 128:(it + 1) * 128, :], writes=["xt%d" % (i % 2)])

                    def s1(i):
                        j = i % 2
                        A(lambda e: e.activation(out=junkA[:], in_=xt[j][:], func=AF.Square,
                                                 accum_out=ssx[:, j:j + 1]), r=["xt%d" % j], w=["junkA", "ssx%d" % j])
                        rstd(rsx[:, j:j + 1], ssx[:, j:j + 1], 1, 1.0 / D, ["ssx%d" % j], ["rsx%d" % j])
                        V(lambda e: e.tensor_scalar(out=xn[j][:], in0=xt[j][:], scalar1=rsx[:, j:j + 1], scalar2=None,
                                                    op0=ALU.mult), r=["xt%d" % j, "rsx%d" % j], w=["xn%d" % j])
                        for k in range(8):
                            T(lambda e, k=k: e.transpose(out=banks[k // 4][:, (k % 4) * 128:(k % 4 + 1) * 128],
                                                         in_=xn[j][:, k * 128:(k + 1) * 128], identity=ident[:]),
                              r=["xn%d" % j, "ident"], w=[bkey[k // 4]])
                        for k in range(8):
                            A(lambda e, k=k: e.activation(out=hT[j][:, k, :],
                                                          in_=banks[k // 4][:, (k % 4) * 128:(k % 4 + 1) * 128],
                                                          func=AF.Identity, scale=S1T[:, k, b:b + 1],
                                                          bias=sh1T[:, k, b:b + 1]),
                              r=[bkey[k // 4], "S1T", "sh1T"], w=["hT%d" % j])

                    GROUPS = [(0, 512, 2), (512, 512, 3), (1024, 512, 4), (1536, 328, 5), (1864, 512, 6)]

                    def rope(src3, src_key, dst3, dst_key, H, it):
                        c = cs[:, it, :].unsqueeze(1).broadcast_to([128, H, 8])
                        s_ = sn[:, it, :].unsqueeze(1).broadcast_to([128, H, 8])
                        x1 = src3[:, :, 0:8]
                        x2 = src3[:, :, 8:16]
                        t = [r_[:, 0:H, :] for r_ in rt]
                        V(lambda e: e.tensor_tensor(out=t[0], in0=x1, in1=c, op=ALU.mult), r=[src_key, "cs"], w=["rt0"])
                        V(lambda e: e.tensor_tensor(out=t[1], in0=x2, in1=s_, op=ALU.mult), r=[src_key, "sn"], w=["rt1"])
                        V(lambda e: e.tensor_tensor(out=dst3[:, :, 0:8], in0=t[0], in1=t[1], op=ALU.subtract),
                          r=["rt0", "rt1"], w=[dst_key])
                        V(lambda e: e.tensor_tensor(out=t[2], in0=x2, in1=c, op=ALU.mult), r=[src_key, "cs"], w=["rt2"])
                        V(lambda e: e.tensor_tensor(out=t[3], in0=x1, in1=s_, op=ALU.mult), r=[src_key, "sn"], w=["rt3"])
                        V(lambda e: e.tensor_tensor(out=dst3[:, :, 8:16], in0=t[2], in1=t[3], op=ALU.add),
                          r=["rt2", "rt3"], w=[dst_key])
                        A(lambda e: e.activation(out=dst3[:, :, 16:64], in_=src3[:, :, 16:64], func=AF.Copy),
                          r=[src_key], w=[dst_key])

                    def s2(i):
                        j = i % 2
                        it = b * NT + i
                        for (c0, n, bk) in GROUPS:
                            for k in range(8):
                                T(lambda e, k=k, c0=c0, n=n, bk=bk: e.matmul(banks[bk][:, 0:n], lhsT=hT[j][:, k, :],
                                                                             rhs=Win[:, k, c0:c0 + n], start=(k == 0),
                                                                             stop=(k == 7)),
                                  r=["hT%d" % j, "Win"], w=[bkey[bk]])
                        A(lambda e: e.activation(out=zu[j][:], in_=banks[2][:, :], func=AF.Gelu_apprx_tanh),
                          r=[bkey[2]], w=["zu%d" % j])
                        A(lambda e: e.activation(out=zv[:], in_=banks[3][:, :], func=AF.Gelu_apprx_tanh),
                          r=[bkey[3]], w=["zv"])
                        rope(banks[4][:, :].rearrange("p (h d) -> p h d", d=64), bkey[4], q_tok[j][:], "q_tok%d" % j, 8, it)
                        rope(banks[5][:, 0:128].rearrange("p (h d) -> p h d", d=64), bkey[5], kd[j][:, :, 0, :],
                             "kd%d" % j, 2, it)
                        V(lambda e: e.tensor_copy(out=kd[j][:, :, 1, :], in_=kd[j][:, :, 0, :]), r=["kd%d" % j],
                          w=["kd%d" % j])
                        V(lambda e: e.tensor_copy(out=v_aug[:, i, :, 0:64],
                                                  in_=banks[5][:, 128:256].rearrange("p (h d) -> p h d", d=64)),
                          r=[bkey[5]], w=["v_aug"])
                        rope(banks[5][:, 256:320].rearrange("p (h d) -> p h d", d=64), bkey[5], kid[j][:, 0:1, :],
                             "kid%d" % j, 1, it)
                        V(lambda e: e.tensor_copy(out=kid[j][:, 1:2, :], in_=kid[j][:, 0:1, :]), r=["kid%d" % j],
                          w=["kid%d" % j])
                        V(lambda e: e.tensor_copy(out=w_tok[:, i, :], in_=banks[5][:, 320:328]), r=[bkey[5]],
                          w=["w_tok"])
                        rope(banks[6][:, :].rearrange("p (h d) -> p h d", d=64), bkey[6], qi_tok[j][:], "qi_tok%d" % j, 8, it)
                        for g in range(4):
                            A(lambda e, g=g: e.activation(out=junkA[:, 0:128], in_=zv[:, g * 128:(g + 1) * 128],
                                                          func=AF.Square, accum_out=ssv[:, g:g + 1]),
                              r=["zv"], w=["junkA", "ssv"])
                        rstd(rsv[:, 0:4], ssv[:, 0:4], 4, 1.0 / 128, ["ssv"], ["rsv"])
                        for g in range(4):
                            V(lambda e, g=g: e.scalar_tensor_tensor(out=vn[j][:, g * 128:(g + 1) * 128],
                                                                    in0=zv[:, g * 128:(g + 1) * 128],
                                                                    scalar=rsv[:, g:g + 1],
                                                                    in1=gv_row[:, g * 128:(g + 1) * 128],
                                                                    op0=ALU.mult, op1=ALU.mult),
                              r=["zv", "rsv", "gv_row"], w=["vn%d" % j])

                    def s3a(i):
                        j = i % 2
                        ts = slice(i * 128, (i + 1) * 128)
                        qf = q_tok[j][:].rearrange("p h d -> p (h d)")
                        for c in range(4):
                            T(lambda e, c=c: e.transpose(out=bbf[0][:, c * 128:(c + 1) * 128],
                                                         in_=qf[:, c * 128:(c + 1) * 128], identity=identb[:]),
                              r=["q_tok%d" % j, "identb"], w=[bkey[0]])
                        for kv in range(2):
                            T(lambda e, kv=kv: e.transpose(out=bbf[0][:, 512 + kv * 128:512 + (kv + 1) * 128],
                                                           in_=kd[j][:, kv, :, :].rearrange("p a d -> p (a d)"),
                                                           identity=identb[:]),
                              r=["kd%d" % j, "identb"], w=[bkey[0]])
                        T(lambda e: e.transpose(out=bbf[0][:, 768:896], in_=kid[j][:].rearrange("p a d -> p (a d)"),
                                                identity=identb[:]), r=["kid%d" % j, "identb"], w=[bkey[0]])
                        V(lambda e: e.tensor_copy(out=qT2[:, :, ts],
                                                  in_=bbf[0][:, 0:512].rearrange("p (c t) -> p c t", t=128)),
                          r=[bkey[0]], w=["qT2"])
                        V(lambda e: e.tensor_copy(out=kTd[:, :, ts],
                                                  in_=bbf[0][:, 512:768].rearrange("p (c t) -> p c t", t=128)),
                          r=[bkey[0]], w=["kTd"])
                        V(lambda e: e.tensor_copy(out=kiTd[:, ts], in_=bbf[0][:, 768:896]), r=[bkey[0]], w=["kiTd"])
                        qif = qi_tok[j][:].rearrange("p h d -> p (h d)")
                        for c in range(4):
                            T(lambda e, c=c: e.transpose(out=bbf[1][:, c * 128:(c + 1) * 128],
                                                         in_=qif[:, c * 128:(c + 1) * 128], identity=identb[:]),
                              r=["qi_tok%d" % j, "identb"], w=[bkey[1]])
                        A(lambda e: e.activation(out=qiT2[:, i * 4:(i + 1) * 4, :, :],
                                                 in_=bbf[1][:, 0:512].rearrange("p (c g t) -> p g c t", c=4, g=4),
                                                 func=AF.Copy), r=[bkey[1]], w=["qiT2"])
                        for g in range(4):
                            T(lambda e, g=g: e.matmul(banks[7][:, g * 128:(g + 1) * 128], lhsT=WmT[:, g, :],
                                                      rhs=vn[j][:, g * 128:(g + 1) * 128], start=True, stop=True),
                              r=["WmT", "vn%d" % j], w=[bkey[7]])
                        for g in range(4):
                            V(lambda e, g=g: e.scalar_tensor_tensor(out=ya[:, g * 128:(g + 1) * 128],
                                                                    in0=banks[7][:, g * 128:(g + 1) * 128],
                                                                    scalar=bcol[:, g:g + 1],
                                                                    in1=zu[j][:, g * 128:(g + 1) * 128],
                                                                    op0=ALU.add, op1=ALU.mult),
                              r=[bkey[7], "bcol", "zu%d" % j], w=["ya"])
                        A(lambda e: e.activation(out=junkA[:, 0:512], in_=ya[:], func=AF.Square, accum_out=ssa[:]),
                          r=["ya"], w=["junkA", "ssa"])
                        rstd(rsa[:], ssa[:], 1, 1.0 / 512, ["ssa"], ["rsa"])
                        V(lambda e: e.scalar_tensor_tensor(out=ma[:], in0=ya[:], scalar=rsa[:, 0:1], in1=goa_row[:],
                                                           op0=ALU.mult, op1=ALU.mult),
                          r=["ya", "rsa", "goa_row"], w=["ma"])
                        if b == 0 and i == 0:
                            tap("ya", ya[:], ["ya"])

                    def s3b(i):
                        ts = slice(i * 128, (i + 1) * 128)
                        for c in range(4):
                            T(lambda e, c=c: e.transpose(out=bbf[7][:, c * 128:(c + 1) * 128],
                                                         in_=ma[:, c * 128:(c + 1) * 128], identity=identb[:]),
                              r=["ma", "identb"], w=[bkey[7]])
                        A(lambda e: e.activation(out=mTa[:, :, ts],
                                                 in_=bbf[7][:, 0:512].rearrange("p (c t) -> p c t", t=128),
                                                 func=AF.Copy), r=[bkey[7]], w=["mTa"])

                    s1_load(0)
                    if NT > 1:
                        s1_load(1)
                    s1(0)
                    for n in range(NT + 1):
                        if n - 1 >= 0:
                            s3a(n - 1)
                        if n + 1 < NT:
                            s1(n + 1)
                        if n + 2 < NT:
                            pass
                        if n < NT:
                            s2(n)
                            if n + 2 < NT:
                                s1_load(n + 2)
                        if n - 1 >= 0:
                            s3b(n - 1)
                    P.barrier()
                    if stop == "proj":
                        P.finish()
                        return

                with contextlib.ExitStack() as tst:
                    tsb = mk_sb(tst)
                    score = tsb("score", [128, S])
                    cmax = tsb("cmax", [128, 4])
                    mask = tsb("mask", [128, S], BF16)
                    maskT = tsb("maskT", [128, NT, 128], BF16)
                    rl = [tsb("rl%d" % i, [128, 512], BF16) for i in range(4)]
                    pT = [tsb("pT%d" % i, [128, 512], BF16) for i in range(4)]
                    Wsel = [tsb("Wsel%d" % i, [128, 8, 128], BF16) for i in range(2)]
                    wrep = tsb("wrep", [128, 2, 128])
                    wcol = tsb("wcol", [128, 8])
                    lo0 = tsb("lo0", [128, 1])
                    hi0 = tsb("hi0", [128, 1])
                    w0 = tsb("w0", [128, 1])
                    wh = tsb("wh", [128, NIT + 1])
                    cbias = tsb("cbias", [128, NT])
                    mid = tsb("mid", [128, 1])
                    cnt = tsb("cnt", [128, 1])
                    btmp = tsb("btmp", [128, 1])
                    rden = tsb("rden", [128, 8])
                    yb = tsb("yb", [128, 512])
                    ssb_ = tsb("ssb_", [128, 1])
                    rsb = tsb("rsb", [128, 1])
                    mb = tsb("mb", [128, 512], BF16)
                    mbT = tsb("mbT", [128, 4, 128], BF16)
                    sso = tsb("sso", [128, 2])
                    rso = tsb("rso", [128, 1])
                    ot = tsb("ot", [128, D])
                    xres = [tsb("xres%d" % i, [128, D]) for i in range(2)]
                    x1t = [tsb("x1t%d" % i, [128, D]) for i in range(2)]
                    for i in range(2):
                        G(lambda e, i=i: e.memset(Wsel[i][:], 0.0), w=["Wsel%d" % i])
                    for qq in range(NT):
                        G(lambda e, qq=qq: e.memset(cbias[:, qq:qq + 1], float((qq + 1) * 128 - 2 * TOPK) + 0.5),
                          w=["cbias"])
                    rl_i = [0]
                    pT_i = [0]
                    D_i = [0]

                    def indexer(qb):
                        N = (qb + 1) * 128
                        nch = (N + 511) // 512
                        wi = qb % 2
                        wk = "Wsel%d" % wi
                        w2v = w_tok[:, qb, :].rearrange("p (i two) -> p i two", two=2)
                        for par in range(2):
                            V(lambda e, par=par: e.tensor_tensor(
                                out=wrep[:, par, :].rearrange("p (i t) -> p i t", t=32),
                                in0=w2v[:, :, par].unsqueeze(2).broadcast_to([128, 4, 32]),
                                in1=D32[:].unsqueeze(1).broadcast_to([128, 4, 32]), op=ALU.mult),
                              r=["w_tok", "D32"], w=["wrep"])
                        for par in range(2):
                            T(lambda e, par=par: e.matmul(banks[0][:, par * 4:(par + 1) * 4], lhsT=wrep[:, par, :],
                                                          rhs=G4[:], start=True, stop=True),
                              r=["wrep", "G4"], w=[bkey[0]])
                        V(lambda e: e.tensor_scalar(out=wcol[:], in0=banks[0][:, 0:8], scalar1=IDX_SCALE, scalar2=None,
                                                    op0=ALU.mult), r=[bkey[0]], w=["wcol"])
                        for par in range(2):
                            for g in range(4):
                                V(lambda e, par=par, g=g: e.tensor_scalar(
                                    out=Wsel[wi][:, par * 4 + g, 32 * g:32 * g + 32], in0=D32[:],
                                    scalar1=wcol[:, par * 4 + g:par * 4 + g + 1], scalar2=None, op0=ALU.mult),
                                  r=["D32", "wcol"], w=[wk])
                        units = []
                        for c in range(nch):
                            n = min(512, N - c * 512)
                            for g in range(4):
                                for par in range(2):
                                    units.append((c, n, g, par))

                        def dots(u):
                            c, n, g, par = u
                            ri = rl_i[0] % 4
                            rl_i[0] += 1
                            ps = slice(64 * par, 64 * par + 64)
                            T(lambda e: e.matmul(banks[par][:, 0:n],
                                                 lhsT=qiT2[ps, qb * 4 + g, :, :].rearrange("p c t -> p (c t)"),
                                                 rhs=kiTd[ps, c * 512:c * 512 + n], start=True, stop=True),
                              r=["qiT2", "kiTd"], w=[bkey[par]])
                            if par == 0:
                                A(lambda e: e.activation(out=rl[ri][:, 0:n], in_=banks[par][:, 0:n], func=AF.Relu),
                                  r=[bkey[par]], w=["rl%d" % ri])
                            else:
                                V(lambda e: e.tensor_scalar(out=rl[ri][:, 0:n], in0=banks[par][:, 0:n], scalar1=0.0,
                                                            scalar2=None, op0=ALU.max), r=[bkey[par]], w=["rl%d" % ri])
                            return ri

                        def selmm(u, ri):
                            c, n, g, par = u
                            sbk = 2 + (c % 2)
                            first = (g == 0 and par == 0)
                            last = (g == 3 and par == 1)
                            T(lambda e: e.matmul(banks[sbk][:, 0:n], lhsT=Wsel[wi][:, par * 4 + g, :],
                                                 rhs=rl[ri][:, 0:n], start=first, stop=last),
                              r=[wk, "rl%d" % ri], w=[bkey[sbk]])
                            if last:
                                V(lambda e: e.tensor_scalar(out=score[:, c * 512:c * 512 + n], in0=banks[sbk][:, 0:n],
                                                            scalar1=1.0, scalar2=None, op0=ALU.mult, op1=ALU.max,
                                                            accum_out=cmax[:, c:c + 1]),
                                  r=[bkey[sbk]], w=["score", "cmax"])

                        ris = {}
                        LOOK = 2
                        for i_ in range(min(LOOK, len(units))):
                            ris[i_] = dots(units[i_])
                        for i_ in range(len(units)):
                            if i_ + LOOK < len(units):
                                ris[i_ + LOOK] = dots(units[i_ + LOOK])
                            selmm(units[i_], ris[i_])

                    def topk_iter(qb):
                        N = (qb + 1) * 128
                        nch = (N + 511) // 512
                        V(lambda e: e.tensor_reduce(out=lo0[:], in_=score[:, 0:N], axis=AX.X, op=ALU.min),
                          r=["score"], w=["lo0"])
                        V(lambda e: e.tensor_reduce(out=hi0[:], in_=cmax[:, 0:nch], axis=AX.X, op=ALU.max),
                          r=["cmax"], w=["hi0"])
                        V(lambda e: e.tensor_tensor(out=score[:, qb * 128:N], in0=score[:, qb * 128:N], in1=NEGM[:],
                                                    op=ALU.add), r=["score", "NEGM"], w=["score"])
                        V(lambda e: e.tensor_tensor(out=w0[:], in0=lo0[:], in1=hi0[:], op=ALU.subtract),
                          r=["hi0", "lo0"], w=["w0"])
                        V(lambda e: e.tensor_scalar(out=wh[:], in0=P2[:], scalar1=w0[:, 0:1], scalar2=None,
                                                    op0=ALU.mult), r=["P2", "w0"], w=["wh"])
                        V(lambda e: e.tensor_scalar(out=mid[:], in0=lo0[:], scalar1=-1.0, scalar2=wh[:, 0:1],
                                                    op0=ALU.mult, op1=ALU.add), r=["lo0", "wh"], w=["mid"])
                        for i in range(NIT):
                            A(lambda e: e.activation(out=mask[:, 0:N], in_=score[:, 0:N], func=AF.Sign,
                                                     bias=mid[:, 0:1], accum_out=cnt[:]),
                              r=["score", "mid"], w=["mask", "cnt"])
                            V(lambda e, i=i: e.scalar_tensor_tensor(out=btmp[:], in0=cnt[:],
                                                                    scalar=float(2 * TOPK - N) - 0.5,
                                                                    in1=wh[:, i:i + 1], op0=ALU.is_ge, op1=ALU.mult),
                              r=["cnt", "wh"], w=["btmp"])
                            V(lambda e, i=i: e.scalar_tensor_tensor(out=mid[:], in0=mid[:], scalar=wh[:, i + 1:i + 2],
                                                                    in1=btmp[:], op0=ALU.subtract, op1=ALU.add),
                              r=["mid", "wh", "btmp"], w=["mid"])
                            yield
                        V(lambda e: e.tensor_scalar(out=mid[:], in0=mid[:], scalar1=-1.0, scalar2=wh[:, NIT:NIT + 1],
                                                    op0=ALU.mult, op1=ALU.add), r=["mid", "wh"], w=["mid"])

                    def topk_finish(qb):
                        N = (qb + 1) * 128
                        if qb < KB:
                            for jj in range(qb + 1):
                                src = TRIU if jj == qb else ONESB
                                G(lambda e, jj=jj, src=src: e.tensor_copy(out=maskT[:, jj, :], in_=src[:]),
                                  r=["TRIU", "ONESB"], w=["maskT"])
                            return
                        V(lambda e: e.tensor_scalar(out=mask[:, 0:N], in0=score[:, 0:N], scalar1=mid[:, 0:1],
                                                    scalar2=None, op0=ALU.is_ge), r=["score", "mid"], w=["mask"])
                        if b == 0 and qb == NT - 1:
                            tap("score", score[:, 0:N], ["score"])
                            tap("thr", mid[:], ["mid"])
                        for jj in range(qb + 1):
                            lb = 4 + jj // 8
                            T(lambda e, jj=jj, lb=lb: e.transpose(out=bbf[lb][:, (jj % 8) * 128:(jj % 8 + 1) * 128],
                                                                  in_=mask[:, jj * 128:(jj + 1) * 128],
                                                                  identity=identb[:]),
                              r=["mask", "identb"], w=[bkey[lb]])
                        for lb in range(4, 4 + (qb + 8) // 8):
                            j0 = (lb - 4) * 8
                            j1 = min(qb + 1, j0 + 8)
                            nj = j1 - j0
                            V(lambda e, lb=lb, j0=j0, j1=j1, nj=nj: e.tensor_copy(
                                out=maskT[:, j0:j1, :],
                                in_=bbf[lb][:, 0:nj * 128].rearrange("p (j t) -> p j t", t=128)),
                              r=[bkey[lb]], w=["maskT"])

                    def attention(qb):
                        qs = slice(qb * 128, (qb + 1) * 128)
                        for kv in range(2):
                            T(lambda e, kv=kv: e.matmul(banks[6 + kv][:, 0:260], lhsT=zerob[:, 0:128],
                                                        rhs=zerob[:, 0:260], start=True, stop=False,
                                                        skip_group_check=True), r=["zerob"], w=[bkey[6 + kv]])

                        def Lstage(jj):
                            ks = slice(jj * 128, (jj + 1) * 128)
                            pis = []
                            for par in range(2):
                                ps = slice(64 * par, 64 * par + 64)
                                lb = 4 + par
                                pi = pT_i[0] % 4
                                pT_i[0] += 1
                                pis.append(pi)
                                for kv in range(2):
                                    T(lambda e, ps=ps, lb=lb, kv=kv: e.matmul(
                                        banks[lb][:, kv * 256:(kv + 1) * 256], lhsT=kTd[ps, kv, ks],
                                        rhs=qT2[ps, 2 * kv:2 * kv + 2, qs], start=True, stop=True),
                                      r=["kTd", "qT2"], w=[bkey[lb]])
                                A(lambda e, lb=lb, pi=pi: e.activation(out=pT[pi][:], in_=banks[lb][:, :], func=AF.Exp,
                                                                       scale=0.125), r=[bkey[lb]], w=["pT%d" % pi])
                                V(lambda e, pi=pi: e.tensor_tensor(
                                    out=pT[pi][:].rearrange("p (h t) -> p h t", t=128),
                                    in0=pT[pi][:].rearrange("p (h t) -> p h t", t=128),
                                    in1=maskT[:, jj, :].unsqueeze(1).broadcast_to([128, 4, 128]), op=ALU.mult),
                                  r=["pT%d" % pi, "maskT"], w=["pT%d" % pi])
                            return pis

                        def PVstage(jj, pis):
                            for par in range(2):
                                pi = pis[par]
                                for kv in range(2):
                                    for ii in range(2):
                                        hl = 2 * ii + par
                                        T(lambda e, ii=ii, hl=hl, kv=kv, pi=pi: e.matmul(
                                            banks[6 + kv][:, hl * 65:hl * 65 + 65],
                                            lhsT=pT[pi][:, (kv * 2 + ii) * 128:(kv * 2 + ii + 1) * 128],
                                            rhs=v_aug[:, jj, kv, :], start=False, stop=(jj == qb),
                                            skip_group_check=True),
                                          r=["pT%d" % pi, "v_aug"], w=[bkey[6 + kv]])

                        nxt = Lstage(0)
                        for jj in range(qb + 1):
                            cur = nxt
                            if jj + 1 <= qb:
                                nxt = Lstage(jj + 1)
                            PVstage(jj, cur)
                            yield

                    def post(qb):
                        it = b * NT + qb
                        xj = qb % 2
                        yield
                        for kv in range(2):
                            ov = banks[6 + kv][:, 0:260].rearrange("p (h d) -> p h d", d=65)
                            V(lambda e, kv=kv, ov=ov: e.reciprocal(out=rden[:, kv * 4:(kv + 1) * 4], in_=ov[:, :, 64]),
                              r=[bkey[6 + kv]], w=["rden"])
                            V(lambda e, kv=kv, ov=ov: e.tensor_tensor(
                                out=yb[:, kv * 256:(kv + 1) * 256].rearrange("p (h d) -> p h d", d=64),
                                in0=ov[:, :, 0:64],
                                in1=rden[:, kv * 4:(kv + 1) * 4].unsqueeze(2).broadcast_to([128, 4, 64]),
                                op=ALU.mult), r=[bkey[6 + kv], "rden"], w=["yb"])
                        if b == 0 and qb == NT - 1:
                            tap("yb", yb[:], ["yb"])
                        yield
                        A(lambda e: e.activation(out=junkA[:, 0:512], in_=yb[:], func=AF.Square, accum_out=ssb_[:]),
                          r=["yb"], w=["junkA", "ssb_"])
                        yield
                        rstd(rsb[:], ssb_[:], 1, 1.0 / 512, ["ssb_"], ["rsb"])
                        yield
                        V(lambda e: e.scalar_tensor_tensor(out=mb[:], in0=yb[:], scalar=rsb[:, 0:1], in1=gob_row[:],
                                                           op0=ALU.mult, op1=ALU.mult),
                          r=["yb", "rsb", "gob_row"], w=["mb"])
                        for c in range(4):
                            T(lambda e, c=c: e.transpose(out=bbf[4][:, c * 128:(c + 1) * 128],
                                                         in_=mb[:, c * 128:(c + 1) * 128], identity=identb[:]),
                              r=["mb", "identb"], w=[bkey[4]])
                        yield
                        A(lambda e: e.activation(out=mbT[:], in_=bbf[4][:, 0:512].rearrange("p (c t) -> p c t", t=128),
                                                 func=AF.Copy), r=[bkey[4]], w=["mbT"])
                        yield
                        for n in range(2):
                            for k in range(8):
                                if k == 4:
                                    yield
                                lhs = mTa[:, k, qb * 128:(qb + 1) * 128] if k < 4 else mbT[:, k - 4, :]
                                T(lambda e, n=n, k=k, lhs=lhs: e.matmul(banks[6 + n][:, :], lhsT=lhs,
                                                                        rhs=Wout[:, k, n * 512:(n + 1) * 512],
                                                                        start=(k == 0), stop=(k == 7)),
                                  r=["mTa", "mbT", "Wout"], w=[bkey[6 + n]])
                        for n in range(2):
                            A(lambda e, n=n: e.activation(out=junkA[:, 0:512], in_=banks[6 + n][:, :], func=AF.Square,
                                                          accum_out=sso[:, n:n + 1]),
                              r=[bkey[6 + n]], w=["junkA", "sso%d" % n])
                        yield
                        rstd(rso[:], sso[:, 0:1], 1, 1.0 / D, ["sso0", "sso1"], ["rso"], ss2_ap=sso[:, 1:2])
                        yield
                        for n in range(2):
                            V(lambda e, n=n: e.scalar_tensor_tensor(out=ot[:, n * 512:(n + 1) * 512],
                                                                    in0=banks[6 + n][:, :], scalar=rso[:, 0:1],
                                                                    in1=G1row[:, n * 512:(n + 1) * 512],
                                                                    op0=ALU.mult, op1=ALU.mult),
                              r=[bkey[6 + n], "rso", "G1row"], w=["ot"])
                        G(lambda e: e.tensor_tensor(out=x1t[xj][:], in0=ot[:], in1=xres[xj][:], op=ALU.add),
                          r=["ot", "xres%d" % xj], w=["x1t%d" % xj])
                        P.dma(x1s_d[it * 128:(it + 1) * 128, :], x1t[xj][:], reads=["x1t%d" % xj],
                              writes=["x1s_%d" % it])

                    def step(g_):
                        if g_ is None:
                            return False
                        try:
                            next(g_)
                            return True
                        except StopIteration:
                            return False

                    def interleave(g1, g2):
                        a1, a2 = g1 is not None, g2 is not None
                        while a1:
                            a1 = step(g1)
                            if a2:
                                a2 = step(g2)
                        return a2

                    def drain(g_):
                        while step(g_):
                            pass

                    if 0 >= KB:
                        indexer(0)
                        drain(topk_iter(0))
                    topk_finish(0)
                    ck("a_tf0")
                    for qb in range(NT):
                        it = b * NT + qb
                        P.dma(xres[qb % 2][:], x_d[it * 128:(it + 1) * 128, :], writes=["xres%d" % (qb % 2)])
                        tk = None
                        if qb + 1 < NT and qb + 1 >= KB:
                            indexer(qb + 1)
                            ck("a_idx%d" % (qb + 1))
                            tk = topk_iter(qb + 1)
                        alive = interleave(attention(qb), tk)
                        pg = post(qb)
                        if alive:
                            a2 = True
                            a1 = True
                            while a1 or a2:
                                if a2:
                                    a2 = step(tk)
                                if a1:
                                    a1 = step(pg)
                        else:
                            drain(pg)
                        if qb + 1 < NT:
                            topk_finish(qb + 1)
                    P.barrier()
                    if stop == "attn":
                        P.finish()
                        return
        with contextlib.ExitStack() as bst:
            bsb = mk_sb(bst)
            W1 = bsb("W1", [128, 8, DFF], BF16)
            W2 = bsb("W2", [128, 32, D], BF16)
            wst = [bsb("wst%d" % i, [128, 2048]) for i in range(2)]
            G2row = bsb("G2row", [128, D])
            xg = [bsb("xg%d" % i, [128, D]) for i in range(4)]
            xn2 = [bsb("xn2_%d" % i, [128, D]) for i in range(1)]
            h2T = [bsb("h2T%d" % i, [128, 8, 256], BF16) for i in range(2)]
            rr = [bsb("rr%d" % i, [128, 256], BF16) for i in range(3)]
            fT = [bsb("fT%d" % i, [128, 256], BF16) for i in range(3)]
            junkB = bsb("junkB", [128, D], BF16)
            ss2 = bsb("ss2", [128, 4])
            rs2 = bsb("rs2", [128, 4])
            ssf = bsb("ssf", [128, 4])
            rsf = bsb("rsf", [128, 2])
            of = [bsb("of%d" % i, [128, D]) for i in range(2)]

            cast_engs = ["dve", "act", "pool"]
            ci = 0
            wi_ = 0
            for k in range(8):
                for hf in range(2):
                    st_ = wst[wi_ % 2]
                    sk = "wst%d" % (wi_ % 2)
                    wi_ += 1
                    P.dma(st_[:], w1_d[k * 128:(k + 1) * 128, hf * 2048:(hf + 1) * 2048], writes=[sk])
                    for q2 in range(2):
                        eng = cast_engs[ci % 3]
                        ci += 1
                        sl = slice(q2 * 1024, (q2 + 1) * 1024)
                        dl = slice(hf * 2048 + q2 * 1024, hf * 2048 + (q2 + 1) * 1024)
                        if eng == "act":
                            A(lambda e, k=k, st_=st_, sl=sl, dl=dl: e.activation(out=W1[:, k, dl], in_=st_[:, sl],
                                                                              func=AF.Copy), r=[sk], w=["W1"])
                        else:
                            P.op(eng, lambda e, k=k, st_=st_, sl=sl, dl=dl: e.tensor_copy(out=W1[:, k, dl],
                                                                                       in_=st_[:, sl]), [sk], ["W1"])
            for c2 in range(16):
                st_ = wst[wi_ % 2]
                sk = "wst%d" % (wi_ % 2)
                wi_ += 1
                P.dma(st_[:].rearrange("p (c n) -> p c n", n=D),
                      w2_d[c2 * 256:(c2 + 1) * 256, :].rearrange("(c p) n -> p c n", p=128), writes=[sk])
                for q2 in range(2):
                    eng = cast_engs[ci % 3]
                    ci += 1
                    sl = slice(q2 * 1024, (q2 + 1) * 1024)
                    if eng == "act":
                        A(lambda e, c2=c2, q2=q2, st_=st_, sl=sl: e.activation(out=W2[:, c2 * 2 + q2, :], in_=st_[:, sl],
                                                                          func=AF.Copy), r=[sk], w=["W2"])
                    else:
                        P.op(eng, lambda e, c2=c2, q2=q2, st_=st_, sl=sl: e.tensor_copy(out=W2[:, c2 * 2 + q2, :],
                                                                                   in_=st_[:, sl]), [sk], ["W2"])

            NG = NTOK // 256

            def b_load(g):
                for t in range(2):
                    it = g * 2 + t
                    xi = (g % 2) * 2 + t
                    P.dma(xg[xi][:], x1s_d[it * 128:(it + 1) * 128, :], reads=["x1s_%d" % it], writes=["xg%d" % xi])

            def b_prep(g):
                hj = g % 2
                b = (g * 256) // S
                for t in range(2):
                    xi = (g % 2) * 2 + t
                    A(lambda e, xi=xi, t=t: e.activation(out=junkB[:], in_=xg[xi][:], func=AF.Square,
                                                        accum_out=ss2[:, t:t + 1]), r=["xg%d" % xi],
                      w=["junkB", "ss2_%d" % t])
                    rstd(rs2[:, t:t + 1], ss2[:, t:t + 1], 1, 1.0 / D, ["ss2_%d" % t], ["rs2_%d" % t])
                    V(lambda e, xi=xi, t=t: e.tensor_scalar(out=xn2[0][:], in0=xg[xi][:], scalar1=rs2[:, t:t + 1],
                                                           scalar2=None, op0=ALU.mult),
                      r=["xg%d" % xi, "rs2_%d" % t], w=["xn2_0"])
                    for k in range(8):
                        T(lambda e, k=k, t=t: e.transpose(out=banks[6 + k // 4][:, (k % 4) * 128:(k % 4 + 1) * 128],
                                                          in_=xn2[0][:, k * 128:(k + 1) * 128], identity=ident[:]),
                          r=["xn2_0", "ident"], w=[bkey[6 + k // 4]])
                    for k in range(8):
                        A(lambda e, k=k, t=t: e.activation(out=h2T[hj][:, k, t * 128:(t + 1) * 128],
                                                           in_=banks[6 + k // 4][:, (k % 4) * 128:(k % 4 + 1) * 128],
                                                           func=AF.Identity, scale=S2T[:, k, b:b + 1],
                                                           bias=sh2T[:, k, b:b + 1]),
                          r=[bkey[6 + k // 4], "S2T", "sh2T"], w=["h2T%d" % hj])

            f_i = [0]

            def b_main(g):
                hj = g % 2
                b = (g * 256) // S
                if (g * 256) % S == 0:
                    for n in range(2):
                        T(lambda e, n=n: e.matmul(banks[4 + n][:, :], lhsT=sel[0:NSEQ, b, :],
                                                  rhs=gmod[0:NSEQ, 1, n * 512:(n + 1) * 512], start=True, stop=True),
                          r=["sel", "gmod1"], w=[bkey[4 + n]])
                        V(lambda e, n=n: e.tensor_copy(out=G2row[:, n * 512:(n + 1) * 512], in_=banks[4 + n][:, :]),
                          r=[bkey[4 + n]], w=["G2row"])
                def Fst(c):
                    fb = 4 + (c % 2)
                    fi = c % 3
                    for k in range(8):
                        T(lambda e, k=k: e.matmul(banks[fb][:, 0:256], lhsT=W1[:, k, c * 128:(c + 1) * 128],
                                                  rhs=h2T[hj][:, k, :], start=(k == 0), stop=(k == 7)),
                          r=["W1", "h2T%d" % hj], w=[bkey[fb]])
                    A(lambda e: e.activation(out=rr[fi][:], in_=banks[fb][:, 0:256], func=AF.Relu),
                      r=[bkey[fb]], w=["rr%d" % fi])
                    V(lambda e: e.scalar_tensor_tensor(out=fT[fi][:], in0=banks[fb][:, 0:256], scalar=0.0,
                                                       in1=rr[fi][:], op0=ALU.max, op1=ALU.mult),
                      r=[bkey[fb], "rr%d" % fi], w=["fT%d" % fi])

                def P2st(c):
                    fi = c % 3
                    for t in range(2):
                        for n in range(2):
                            ob = t * 2 + n
                            T(lambda e, t=t, n=n, ob=ob: e.matmul(
                                banks[ob][:, :], lhsT=fT[fi][:, t * 128:(t + 1) * 128],
                                rhs=W2[:, c, n * 512:(n + 1) * 512], start=(c == 0), stop=(c == 31)),
                              r=["fT%d" % fi, "W2"], w=[bkey[ob]])

                Fst(0)
                for c in range(32):
                    if c + 1 < 32:
                        Fst(c + 1)
                    P2st(c)
                    if c == 20 and g + 1 < NG:
                        b_prep(g + 1)
                for t in range(2):
                    it = g * 2 + t
                    xi = (g % 2) * 2 + t
                    for n in range(2):
                        A(lambda e, t=t, n=n: e.activation(out=junkB[:, 0:512], in_=banks[t * 2 + n][:, :],
                                                           func=AF.Square, accum_out=ssf[:, t * 2 + n:t * 2 + n + 1]),
                          r=[bkey[t * 2 + n]], w=["junkB", "ssf%d" % (t * 2 + n)])
                    rstd(rsf[:, t:t + 1], ssf[:, t * 2:t * 2 + 1], 1, 1.0 / D, ["ssf%d" % (t * 2), "ssf%d" % (t * 2 + 1)],
                         ["rsf%d" % t], ss2_ap=ssf[:, t * 2 + 1:t * 2 + 2])
                    for n in range(2):
                        V(lambda e, t=t, n=n: e.scalar_tensor_tensor(out=of[t][:, n * 512:(n + 1) * 512],
                                                                     in0=banks[t * 2 + n][:, :], scalar=rsf[:, t:t + 1],
                                                                     in1=G2row[:, n * 512:(n + 1) * 512],
                                                                     op0=ALU.mult, op1=ALU.mult),
                          r=[bkey[t * 2 + n], "rsf%d" % t, "G2row"], w=["of%d" % t])
                    G(lambda e, t=t, xi=xi: e.tensor_tensor(out=of[t][:], in0=of[t][:], in1=xg[xi][:], op=ALU.add),
                      r=["of%d" % t, "xg%d" % xi], w=["of%d" % t])
                    P.dma(out_d[it * 128:(it + 1) * 128, :], of[t][:], reads=["of%d" % t])
                if g + 2 < NG:
                    b_load(g + 2)

            b_load(0)
            if NG > 1:
                b_load(1)
            b_prep(0)
            for g in range(NG):
                b_main(g)
            P.finish()
        print("program built: instrs=%d waits=%d" % (P.ninstr, P.nwaits), flush=True)


def make_core_inputs(ci, NSEQ, S, x, c, positions, w_ada, b_ada, g_pre_mix, w_in, g_sgu_v, w_spatial, b_spatial,
                     g_out_sgu, g_out_attn, w_out, g_post_mix, g_pre_ffn, w_ff1, w_ff2, g_post_ffn):
    f32 = np.float32
    bs = slice(ci * NSEQ, (ci + 1) * NSEQ)
    NT = S // 128
    xc = np.ascontiguousarray(x[bs]).reshape(NSEQ * S, D).astype(f32, copy=False)
    cc = np.asarray(c[bs], dtype=f32)
    cT = np.ascontiguousarray(cc.T.reshape(8, 128, NSEQ).transpose(1, 0, 2))
    pos = np.ascontiguousarray(np.asarray(positions[bs]).reshape(NSEQ * NT, 128).T.astype(np.int32))
    wi = np.asarray(w_in[0], dtype=f32)
    perm = np.concatenate([np.arange(0, 1792), np.arange(2304, 2376), np.arange(1792, 2304)])
    wi_p = np.ascontiguousarray(wi[:, perm])
    return {
        "x": xc, "cT": cT, "pos": pos,
        "w_ada": np.ascontiguousarray(w_ada[0], dtype=f32),
        "b_ada": np.ascontiguousarray(b_ada[0:1], dtype=f32),
        "w_in": wi_p,
        "gpre": np.ascontiguousarray(np.asarray(g_pre_mix[0], dtype=f32).reshape(8, 128).T),
        "gpre2": np.ascontiguousarray(np.asarray(g_pre_ffn[0], dtype=f32).reshape(8, 128).T),
        "gv": np.ascontiguousarray(g_sgu_v[0:1], dtype=f32),
        "ws": np.ascontiguousarray(np.asarray(w_spatial[0], dtype=f32).transpose(1, 0, 2)),
        "bs": np.ascontiguousarray(np.asarray(b_spatial[0], dtype=f32).T),
        "goa": np.ascontiguousarray(g_out_sgu[0:1], dtype=f32),
        "gob": np.ascontiguousarray(g_out_attn[0:1], dtype=f32),
        "w_out": np.ascontiguousarray(w_out[0], dtype=f32),
        "gpost": np.ascontiguousarray(g_post_mix[0:1], dtype=f32),
        "w1": np.ascontiguousarray(w_ff1[0], dtype=f32),
        "w2": np.ascontiguousarray(w_ff2[0], dtype=f32),
        "gpost2": np.ascontiguousarray(g_post_ffn[0:1], dtype=f32),
    }


def run(inputs, n_cores, NSEQ, S, taps=None, trace=False, stop=None):
    nc = bass.Bass("TRN2", target_bir_lowering=False)
    try:
        build_program(nc, NSEQ=NSEQ, S=S, taps=taps, stop=stop)
    except StopBuild:
        pass
    in_maps = [make_core_inputs(ci, NSEQ, S, **inputs) for ci in range(n_cores)]
    res = run_bass_kernel_spmd(nc, in_maps, core_ids=list(range(n_cores)), trace=trace)
    return res


def kernel(**inputs):
    inputs = {k: np.asarray(v) for k, v in inputs.items()}
    B, S, _ = inputs["x"].shape
    NSEQ = B // NCORES
    res = run(inputs, NCORES, NSEQ, S)
    outs = [np.asarray(r["out"]).reshape(NSEQ, S, D) for r in res.results]
    return np.concatenate(outs, axis=0).astype(np.float32, copy=False)
```

```python
import contextlib
import math
import numpy as np
import concourse.bass as bass
import concourse.mybir as mybir
from concourse.bass_utils import run_bass_kernel_spmd

F32 = mybir.dt.float32
BF16 = mybir.dt.bfloat16
I32 = mybir.dt.int32
AF = mybir.ActivationFunctionType
ALU = mybir.AluOpType
AX = mybir.AxisListType

D = 1024
DIN = 2376
DFF = 4096
NCORES = 8
EPS = 1e-6
NIT = 16
IDX_SCALE = (64 ** -0.5) * (8 ** -0.5)
TWO_PI = 2.0 * math.pi


class StopBuild(Exception):
    pass


class Prog:
    NDMA = 32

    def __init__(self, nc, stack):
        self.nc = nc
        self.eng = {"pe": nc.tensor, "act": nc.scalar, "dve": nc.vector,
                    "pool": nc.gpsimd, "sp": nc.sync}
        self.sem = {k: stack.enter_context(nc.semaphore("c_" + k)) for k in self.eng}
        self.cnt = {k: 0 for k in self.eng}
        self.dsem = [stack.enter_context(nc.semaphore("d%d" % i)) for i in range(self.NDMA)]
        self.dval = [0] * self.NDMA
        self.dnext = 0
        self.seen = {k: {} for k in self.eng}
        self.res = {}
        self.nwaits = 0
        self.ninstr = 0

    def _wait(self, eng, dep):
        kind, key, val = dep
        if kind == "e":
            if key == "pe" and eng == "pe":
                return
            sem = self.sem[key]
            skey = key
        else:
            sem = self.dsem[key]
            skey = ("d", key)
        if self.seen[eng].get(skey, 0) >= val:
            return
        self.seen[eng][skey] = val
        self.eng[eng].wait_ge(sem, val)
        self.nwaits += 1

    def _deps(self, eng, reads, writes):
        deps = []
        for r in reads:
            st = self.res.get(r)
            if st and st["w"]:
                deps.append(st["w"])
        for w in writes:
            st = self.res.get(w)
            if st:
                if st["w"]:
                    deps.append(st["w"])
                deps.extend(st["r"])
        for d in deps:
            self._wait(eng, d)

    def _record(self, token, reads, writes):
        for r in reads:
            st = self.res.setdefault(r, {"w": None, "r": []})
            st["r"].append(token)
            if len(st["r"]) > 48:
                best = {}
                for t in st["r"]:
                    k = (t[0], t[1])
                    if k not in best or best[k][2] < t[2]:
                        best[k] = t
                st["r"] = list(best.values())
        for w in writes:
            self.res[w] = {"w": token, "r": []}

    def op(self, eng, fn, reads=(), writes=()):
        self._deps(eng, reads, writes)
        ins = fn(self.eng[eng])
        self.cnt[eng] += 1
        ins.then_inc(self.sem[eng], 1)
        token = ("e", eng, self.cnt[eng])
        self._record(token, reads, writes)
        self.ninstr += 1
        return token

    def dma(self, out, in_, reads=(), writes=(), q="sp", **kw):
        self._deps(q, reads, writes)
        i = self.dnext
        self.dnext = (self.dnext + 1) % self.NDMA
        if self.dval[i] > 0:
            self._wait(q, ("d", i, self.dval[i]))
        ins = self.eng[q].dma_start(out=out, in_=in_, **kw)
        self.dval[i] += 16
        ins.then_inc(self.dsem[i], 16)
        token = ("d", i, self.dval[i])
        self._record(token, reads, writes)
        self.ninstr += 1
        return token

    def barrier(self):
        for e in self.eng:
            for f in self.eng:
                if f != e and self.cnt[f] > 0:
                    self._wait(e, ("e", f, self.cnt[f]))
            for i in range(self.NDMA):
                if self.dval[i] > 0:
                    self._wait(e, ("d", i, self.dval[i]))
        self.res = {}

    def finish(self):
        for i in range(self.NDMA):
            if self.dval[i] > 0:
                self._wait("sp", ("d", i, self.dval[i]))
        for f in self.eng:
            if f != "sp" and self.cnt[f] > 0:
                self._wait("sp", ("e", f, self.cnt[f]))


def build_program(nc, NSEQ=4, S=2048, taps=None, stop=None):
    NT = S // 128
    NTT = NSEQ * NT
    NTOK = NSEQ * S
    TOPK = min(256, S // 4)
    KB = TOPK // 128
    taps = taps or {}

    def din(name, shape, dt=F32):
        return nc.dram_tensor(name, list(shape), dt, kind="ExternalInput").ap()

    x_d = din("x", [NTOK, D])
    cT_d = din("cT", [128, 8, NSEQ])
    pos_d = din("pos", [128, NTT], I32)
    wada_d = din("w_ada", [D, 6 * D])
    bada_d = din("b_ada", [1, 6 * D])
    win_d = din("w_in", [D, DIN])
    gpre_d = din("gpre", [128, 8])
    gpre2_d = din("gpre2", [128, 8])
    gv_d = din("gv", [1, 512])
    ws_d = din("ws", [128, 4, 128])
    bs_d = din("bs", [128, 4])
    goa_d = din("goa", [1, 512])
    gob_d = din("gob", [1, 512])
    wout_d = din("w_out", [D, D])
    gpost_d = din("gpost", [1, D])
    w1_d = din("w1", [D, DFF])
    w2_d = din("w2", [DFF, D])
    gpost2_d = din("gpost2", [1, D])
    out_d = nc.dram_tensor("out", [NTOK, D], F32, kind="ExternalOutput").ap()
    x1s_d = out_d
    tap_d = {k: nc.dram_tensor("tap_" + k, list(shp), F32, kind="ExternalOutput").ap()
             for k, shp in taps.items()}

    with contextlib.ExitStack() as gst:
        P = Prog(nc, gst)

        uid = [0]

        def mk_sb(stack):
            def sb(name, shape, dt=F32):
                uid[0] += 1
                return stack.enter_context(nc.sbuf_tensor("s%d_%s" % (uid[0], name), list(shape), dt))
            return sb

        gsb = mk_sb(gst)
        banks = [gst.enter_context(nc.psum_tensor("bank%d" % i, [128, 512], F32)) for i in range(8)]
        bkey = ["b%d" % i for i in range(8)]
        bbf = [b[:].bitcast(BF16) for b in banks]

        def V(fn, r=(), w=()):
            return P.op("dve", fn, r, w)

        def A(fn, r=(), w=()):
            return P.op("act", fn, r, w)

        def G(fn, r=(), w=()):
            return P.op("pool", fn, r, w)

        def T(fn, r=(), w=()):
            return P.op("pe", fn, r, w)

        ident = gsb("ident", [128, 128])
        identb = gsb("identb", [128, 128], BF16)
        ones_f = gsb("ones_f", [128, 128])
        zeros_f = gsb("zeros_f", [128, 128])
        NEGM = gsb("NEGM", [128, 128])
        TRIU = gsb("TRIU", [128, 128], BF16)
        ONESB = gsb("ONESB", [128, 128], BF16)
        D32 = gsb("D32", [128, 32])
        G4 = gsb("G4", [128, 4])
        zerob = gsb("zerob", [128, 260], BF16)
        P2 = gsb("P2", [128, NIT + 1])
        mhalf = gsb("mhalf", [128, 16])
        iot = gsb("iot", [128, 8], I32)
        iof = gsb("iof", [128, 8])
        invf = gsb("invf", [128, 8])
        rs_tmp = gsb("rs_tmp", [128, 16])

        G(lambda e: e.memset(ones_f[:], 1.0), w=["ones_f"])
        G(lambda e: e.memset(zeros_f[:], 0.0), w=["zeros_f"])
        G(lambda e: e.affine_select(out=ident[:], in_=ones_f[:], pattern=[[-1, 128]], compare_op=ALU.is_equal,
                                    fill=0.0, base=0, channel_multiplier=1), r=["ones_f"], w=["ident"])
        V(lambda e: e.tensor_copy(out=identb[:], in_=ident[:]), r=["ident"], w=["identb"])
        G(lambda e: e.affine_select(out=NEGM[:], in_=zeros_f[:], pattern=[[-1, 128]], compare_op=ALU.is_ge,
                                    fill=-1.0e30, base=0, channel_multiplier=1), r=["zeros_f"], w=["NEGM"])
        G(lambda e: e.affine_select(out=TRIU[:], in_=ones_f[:], pattern=[[1, 128]], compare_op=ALU.is_ge,
                                    fill=0.0, base=0, channel_multiplier=-1), r=["ones_f"], w=["TRIU"])
        V(lambda e: e.tensor_copy(out=ONESB[:], in_=ones_f[:]), r=["ones_f"], w=["ONESB"])
        for m in range(4):
            G(lambda e, m=m: e.affine_select(out=D32[32 * m:32 * m + 32, :], in_=ones_f[32 * m:32 * m + 32, 0:32],
                                             pattern=[[-1, 32]], compare_op=ALU.is_equal, fill=0.0, base=0,
                                             channel_multiplier=1), r=["ones_f"], w=["D32"])
        G(lambda e: e.memset(G4[:], 0.0), w=["G4"])
        for g in range(4):
            G(lambda e, g=g: e.memset(G4[32 * g:32 * g + 32, g:g + 1], 1.0), r=["G4"], w=["G4"])
        G(lambda e: e.memset(zerob[:], 0.0), w=["zerob"])
        for i in range(NIT + 1):
            G(lambda e, i=i: e.memset(P2[:, i:i + 1], 2.0 ** -(i + 1)), w=["P2"])
        G(lambda e: e.memset(mhalf[:], -0.5), w=["mhalf"])
        G(lambda e: e.iota(iot[:], pattern=[[1, 8]], base=0, channel_multiplier=0), w=["iot"])
        V(lambda e: e.tensor_copy(out=iof[:], in_=iot[:]), r=["iot"], w=["iof"])
        A(lambda e: e.activation(out=invf[:], in_=iof[:], func=AF.Exp, scale=-math.log(500000.0) / 8.0),
          r=["iof"], w=["invf"])

        def rstd(out_ap, ss_ap, n, inv_n, rk, wk, ss2_ap=None):
            tmp = rs_tmp[:, 0:n]
            if ss2_ap is not None:
                G(lambda e: e.tensor_tensor(out=tmp, in0=ss_ap, in1=ss2_ap, op=ALU.add), r=rk, w=["rs_tmp"])
                G(lambda e: e.tensor_scalar(out=tmp, in0=tmp, scalar1=inv_n, scalar2=EPS, op0=ALU.mult,
                                            op1=ALU.add), r=["rs_tmp"], w=["rs_tmp"])
            else:
                G(lambda e: e.tensor_scalar(out=tmp, in0=ss_ap, scalar1=inv_n, scalar2=EPS, op0=ALU.mult,
                                            op1=ALU.add), r=rk, w=["rs_tmp"])
            G(lambda e: e.tensor_tensor(out=out_ap, in0=tmp, in1=mhalf[:, 0:n], op=ALU.pow),
              r=["rs_tmp", "mhalf"], w=wk)

        def ck(name):
            if stop == name:
                P.finish()
                raise StopBuild()

        def tap(name, ap, rk, rows=None):
            if name in tap_d:
                dst = tap_d[name]
                P.dma(dst if rows is None else dst[rows], ap, reads=rk)

        S1T = gsb("S1T", [128, 8, NSEQ])
        sh1T = gsb("sh1T", [128, 8, NSEQ])
        S2T = gsb("S2T", [128, 8, NSEQ])
        sh2T = gsb("sh2T", [128, 8, NSEQ])
        gmod = gsb("gmod", [NSEQ, 2, D])
        sel = gsb("sel", [NSEQ, NSEQ, 128])
        gpre = gsb("gpre", [128, 8])
        gpre2 = gsb("gpre2", [128, 8])
        P.dma(gpre[:], gpre_d[:, :], writes=["gpre"])
        P.dma(gpre2[:], gpre2_d[:, :], writes=["gpre2"])

        G(lambda e: e.affine_select(out=sel[:], in_=ones_f[0:NSEQ, :].unsqueeze(1).broadcast_to([NSEQ, NSEQ, 128]),
                                    pattern=[[-1, NSEQ], [0, 128]], compare_op=ALU.is_equal, fill=0.0, base=0,
                                    channel_multiplier=1), r=["ones_f"], w=["sel"])

        with contextlib.ExitStack() as ast:
            asb = mk_sb(ast)
            Win = asb("Win", [128, 8, DIN], BF16)
            Wout = asb("Wout", [128, 8, D], BF16)
            cs = asb("cs", [128, NTT, 8])
            sn = asb("sn", [128, NTT, 8])
            gv_row = asb("gv_row", [128, 512])
            goa_row = asb("goa_row", [128, 512])
            gob_row = asb("gob_row", [128, 512])
            bcol = asb("bcol", [128, 4])
            WmT = asb("WmT", [128, 4, 128], BF16)
            P.dma(gv_row[:], gv_d[0:1, :].partition_broadcast(128), writes=["gv_row"])
            P.dma(goa_row[:], goa_d[0:1, :].partition_broadcast(128), writes=["goa_row"])
            P.dma(gob_row[:], gob_d[0:1, :].partition_broadcast(128), writes=["gob_row"])
            P.dma(bcol[:], bs_d[:, :], writes=["bcol"])

            with contextlib.ExitStack() as sst:
                ssb = mk_sb(sst)
                cTs = ssb("cTs", [128, 8, NSEQ])
                scs = ssb("scs", [128, 8, NSEQ])
                bada4 = ssb("bada4", [NSEQ, 6 * D])
                modrow = ssb("modrow", [NSEQ, 6 * D])
                gpost4 = ssb("gpost4", [NSEQ, 2, D])
                wada_st = [ssb("wada_st%d" % i, [128, 8, 512]) for i in range(2)]
                win_st = [ssb("win_st%d" % i, [128, DIN]) for i in range(2)]
                ws_sb = ssb("ws_sb", [128, 4, 128])
                wsm = ssb("wsm", [128, 4, 128])
                posi = ssb("posi", [128, NTT], I32)
                posf = ssb("posf", [128, NTT])
                ang = ssb("ang", [128, NTT * 8])
                angk = ssb("angk", [128, NTT * 8], I32)
                angf = ssb("angf", [128, NTT * 8])
                angm = ssb("angm", [128, NTT * 8])
                ang2 = ssb("ang2", [128, NTT * 8])

                P.dma(cTs[:], cT_d[:, :, :], writes=["cTs"])
                P.dma(bada4[:], bada_d[0:1, :].partition_broadcast(NSEQ), writes=["bada4"])
                P.dma(gpost4[:, 0, :], gpost_d[0:1, :].partition_broadcast(NSEQ), writes=["gpost4a"])
                P.dma(gpost4[:, 1, :], gpost2_d[0:1, :].partition_broadcast(NSEQ), writes=["gpost4b"])
                P.dma(posi[:], pos_d[:, :], writes=["posi"])
                P.dma(ws_sb[:], ws_d[:, :, :], writes=["ws_sb"])
                A(lambda e: e.activation(out=scs[:], in_=cTs[:], func=AF.Silu), r=["cTs"], w=["scs"])

                order = [2, 3, 0, 1] + list(range(4, 12))
                for n_, cb in enumerate(order):
                    st_ = wada_st[n_ % 2]
                    sk = "wada_st%d" % (n_ % 2)
                    P.dma(st_[:], wada_d[:, cb * 512:(cb + 1) * 512].rearrange("(k p) n -> p k n", p=128),
                          writes=[sk])
                    bk = n_ % 2
                    for k in range(8):
                        T(lambda e, k=k, st_=st_, bk=bk: e.matmul(banks[bk][0:NSEQ, :], lhsT=scs[:, k, :],
                                                                  rhs=st_[:, k, :], start=(k == 0), stop=(k == 7)),
                          r=["scs", sk], w=[bkey[bk]])
                    V(lambda e, bk=bk, cb=cb: e.tensor_tensor(out=modrow[:, cb * 512:(cb + 1) * 512],
                                                              in0=banks[bk][0:NSEQ, :],
                                                              in1=bada4[:, cb * 512:(cb + 1) * 512], op=ALU.add),
                      r=[bkey[bk], "bada4"], w=["modrow%d" % cb])
                allmod = ["modrow%d" % cb for cb in range(12)]
                for si, sp_ in enumerate([0, 1, 3, 4]):
                    for k in range(8):
                        c0 = (si * 8 + k) * NSEQ
                        T(lambda e, sp_=sp_, k=k, c0=c0: e.transpose(
                            out=banks[2][:, c0:c0 + NSEQ], in_=modrow[0:NSEQ, sp_ * D + k * 128:sp_ * D + (k + 1) * 128],
                            identity=ident[0:NSEQ, 0:NSEQ]), r=allmod + ["ident"], w=[bkey[2]])

                def mview(si):
                    return banks[2][:, si * 8 * NSEQ:(si + 1) * 8 * NSEQ].rearrange("p (k b) -> p k b", b=NSEQ)

                V(lambda e: e.tensor_copy(out=sh1T[:], in_=mview(0)), r=[bkey[2]], w=["sh1T"])
                V(lambda e: e.scalar_tensor_tensor(out=S1T[:], in0=mview(1), scalar=1.0,
                                                   in1=gpre[:].unsqueeze(2).broadcast_to([128, 8, NSEQ]),
                                                   op0=ALU.add, op1=ALU.mult), r=[bkey[2], "gpre"], w=["S1T"])
                V(lambda e: e.tensor_copy(out=sh2T[:], in_=mview(2)), r=[bkey[2]], w=["sh2T"])
                V(lambda e: e.scalar_tensor_tensor(out=S2T[:], in0=mview(3), scalar=1.0,
                                                   in1=gpre2[:].unsqueeze(2).broadcast_to([128, 8, NSEQ]),
                                                   op0=ALU.add, op1=ALU.mult), r=[bkey[2], "gpre2"], w=["S2T"])
                V(lambda e: e.tensor_tensor(out=gmod[:, 0, :], in0=modrow[:, 2 * D:3 * D], in1=gpost4[:, 0, :],
                                            op=ALU.mult), r=allmod + ["gpost4a"], w=["gmod0"])
                V(lambda e: e.tensor_tensor(out=gmod[:, 1, :], in0=modrow[:, 5 * D:6 * D], in1=gpost4[:, 1, :],
                                            op=ALU.mult), r=allmod + ["gpost4b"], w=["gmod1"])

                cast_engs = ["dve", "act", "pool"]
                ci = 0
                for k in range(8):
                    st_ = win_st[k % 2]
                    sk = "win_st%d" % (k % 2)
                    P.dma(st_[:], win_d[k * 128:(k + 1) * 128, :], writes=[sk])
                    for h0, h1 in ((0, 1188), (1188, DIN)):
                        eng = cast_engs[ci % 3]
                        ci += 1
                        if eng == "act":
                            A(lambda e, k=k, st_=st_, h0=h0, h1=h1: e.activation(out=Win[:, k, h0:h1], in_=st_[:, h0:h1],
                                                                              func=AF.Copy), r=[sk], w=["Win"])
                        else:
                            P.op(eng, lambda e, k=k, st_=st_, h0=h0, h1=h1: e.tensor_copy(out=Win[:, k, h0:h1],
                                                                                       in_=st_[:, h0:h1]),
                                 [sk], ["Win"])
                for k in range(8):
                    st_ = win_st[k % 2]
                    sk = "win_st%d" % (k % 2)
                    P.dma(st_[:, 0:D], wout_d[k * 128:(k + 1) * 128, :], writes=[sk])
                    eng = cast_engs[ci % 3]
                    ci += 1
                    if eng == "act":
                        A(lambda e, k=k, st_=st_: e.activation(out=Wout[:, k, :], in_=st_[:, 0:D], func=AF.Copy),
                          r=[sk], w=["Wout"])
                    else:
                        P.op(eng, lambda e, k=k, st_=st_: e.tensor_copy(out=Wout[:, k, :], in_=st_[:, 0:D]),
                             [sk], ["Wout"])
                for g in range(4):
                    G(lambda e, g=g: e.affine_select(out=wsm[:, g, :], in_=ws_sb[:, g, :], pattern=[[-1, 128]],
                                                     compare_op=ALU.is_ge, fill=0.0, base=0, channel_multiplier=1),
                      r=["ws_sb"], w=["wsm"])
                for g in range(4):
                    T(lambda e, g=g: e.transpose(out=banks[3][:, g * 128:(g + 1) * 128], in_=wsm[:, g, :],
                                                 identity=ident[:]), r=["wsm", "ident"], w=[bkey[3]])
                V(lambda e: e.tensor_copy(out=WmT[:], in_=banks[3][:, :].rearrange("p (g t) -> p g t", g=4)),
                  r=[bkey[3]], w=["WmT"])

                NA = NTT * 8
                V(lambda e: e.tensor_copy(out=posf[:], in_=posi[:]), r=["posi"], w=["posf"])
                V(lambda e: e.tensor_tensor(out=ang[:].rearrange("p (t f) -> p t f", f=8),
                                            in0=posf[:].unsqueeze(2).broadcast_to([128, NTT, 8]),
                                            in1=invf[:].unsqueeze(1).broadcast_to([128, NTT, 8]), op=ALU.mult),
                  r=["posf", "invf"], w=["ang"])

                def reduce_sin(dst, src_key, shift):
                    V(lambda e: e.tensor_scalar(out=ang2[:], in0=ang[:], scalar1=shift, scalar2=None, op0=ALU.add),
                      r=["ang"], w=["ang2"])
                    V(lambda e: e.tensor_scalar(out=angk[:], in0=ang2[:], scalar1=1.0 / TWO_PI, scalar2=None,
                                                op0=ALU.mult), r=["ang2"], w=["angk"])
                    V(lambda e: e.tensor_copy(out=angf[:], in_=angk[:]), r=["angk"], w=["angf"])
                    V(lambda e: e.scalar_tensor_tensor(out=ang2[:], in0=angf[:], scalar=-TWO_PI, in1=ang2[:],
                                                       op0=ALU.mult, op1=ALU.add), r=["angf", "ang2"], w=["ang2"])
                    V(lambda e: e.tensor_scalar(out=angm[:], in0=ang2[:], scalar1=math.pi, scalar2=-TWO_PI,
                                                op0=ALU.is_gt, op1=ALU.mult), r=["ang2"], w=["angm"])
                    V(lambda e: e.tensor_tensor(out=ang2[:], in0=ang2[:], in1=angm[:], op=ALU.add),
                      r=["ang2", "angm"], w=["ang2"])
                    V(lambda e: e.tensor_scalar(out=angm[:], in0=ang2[:], scalar1=-math.pi, scalar2=TWO_PI,
                                                op0=ALU.is_lt, op1=ALU.mult), r=["ang2"], w=["angm"])
                    V(lambda e: e.tensor_tensor(out=ang2[:], in0=ang2[:], in1=angm[:], op=ALU.add),
                      r=["ang2", "angm"], w=["ang2"])
                    V(lambda e: e.tensor_scalar(out=ang2[:], in0=ang2[:], scalar1=-3.1415925, scalar2=3.1415925,
                                                op0=ALU.max, op1=ALU.min), r=["ang2"], w=["ang2"])
                    A(lambda e: e.activation(out=dst[:].rearrange("p t f -> p (t f)"), in_=ang2[:], func=AF.Sin),
                      r=["ang2"], w=[src_key])

                reduce_sin(sn, "sn", 0.0)
                reduce_sin(cs, "cs", math.pi / 2.0)
                P.barrier()

            if stop == "setup":
                P.finish()
                return
            tap("S1T", S1T[:].rearrange("p k b -> p (k b)"), ["S1T"])
            tap("gmod", gmod[:].rearrange("b g d -> b (g d)"), ["gmod0", "gmod1"])
            tap("cs", cs[:].rearrange("p t f -> p (t f)"), ["cs"])
            tap("sn", sn[:].rearrange("p t f -> p (t f)"), ["sn"])

            qT2 = asb("qT2", [128, 4, S], BF16)
            kTd = asb("kTd", [128, 2, S], BF16)
            qiT2 = asb("qiT2", [128, S // 32, 4, 32], BF16)
            kiTd = asb("kiTd", [128, S], BF16)
            v_aug = asb("v_aug", [128, NT, 2, 65], BF16)
            w_tok = asb("w_tok", [128, NT, 8])
            mTa = asb("mTa", [128, 4, S], BF16)
            G1row = asb("G1row", [128, D])
            junkA = asb("junkA", [128, D], BF16)
            G(lambda e: e.memset(v_aug[:].rearrange("p a b c -> p (a b) c")[:, :, 64:65], 1.0), w=["v_aug"])

            for b in range(NSEQ):
                for n in range(2):
                    T(lambda e, n=n: e.matmul(banks[n][:, :], lhsT=sel[0:NSEQ, b, :],
                                              rhs=gmod[0:NSEQ, 0, n * 512:(n + 1) * 512], start=True, stop=True),
                      r=["sel", "gmod0"], w=[bkey[n]])
                    V(lambda e, n=n: e.tensor_copy(out=G1row[:, n * 512:(n + 1) * 512], in_=banks[n][:, :]),
                      r=[bkey[n]], w=["G1row"])

                with contextlib.ExitStack() as pst:
                    psb = mk_sb(pst)
                    xt = [psb("xt%d" % i, [128, D]) for i in range(2)]
                    xn = [psb("xn%d" % i, [128, D]) for i in range(2)]
                    hT = [psb("hT%d" % i, [128, 8, 128], BF16) for i in range(2)]
                    ssx = psb("ssx", [128, 2])
                    rsx = psb("rsx", [128, 2])
                    zu = [psb("zu%d" % i, [128, 512], BF16) for i in range(2)]
                    zv = psb("zv", [128, 512], BF16)
                    vn = [psb("vn%d" % i, [128, 512], BF16) for i in range(2)]
                    ssv = psb("ssv", [128, 4])
                    rsv = psb("rsv", [128, 4])
                    ya = psb("ya", [128, 512])
                    ssa = psb("ssa", [128, 1])
                    rsa = psb("rsa", [128, 1])
                    ma = psb("ma", [128, 512], BF16)
                    q_tok = [psb("q_tok%d" % i, [128, 8, 64], BF16) for i in range(2)]
                    qi_tok = [psb("qi_tok%d" % i, [128, 8, 64], BF16) for i in range(2)]
                    kd = [psb("kd%d" % i, [128, 2, 2, 64], BF16) for i in range(2)]
                    kid = [psb("kid%d" % i, [128, 2, 64], BF16) for i in range(2)]
                    rt = [psb("rt%d" % i, [128, 8, 8]) for i in range(4)]

                    def s1_load(i):
                        it = b * NT + i
                        P.dma(xt[i % 2][:], x_d[it * 128:(it + 1) * 128, :], writes=["xt%d" % (i % 2)])

                    def s1(i):
                        j = i % 2
                        A(lambda e: e.activation(out=junkA[:], in_=xt[j][:], func=AF.Square,
                                                 accum_out=ssx[:, j:j + 1]), r=["xt%d" % j], w=["junkA", "ssx%d" % j])
                        rstd(rsx[:, j:j + 1], ssx[:, j:j + 1], 1, 1.0 / D, ["ssx%d" % j], ["rsx%d" % j])
                        V(lambda e: e.tensor_scalar(out=xn[j][:], in0=xt[j][:], scalar1=rsx[:, j:j + 1], scalar2=None,
                                                    op0=ALU.mult), r=["xt%d" % j, "rsx%d" % j], w=["xn%d" % j])
                        for k in range(8):
                            T(lambda e, k=k: e.transpose(out=banks[k // 4][:, (k % 4) * 128:(k % 4 + 1) * 128],
                                                         in_=xn[j][:, k * 128:(k + 1) * 128], identity=ident[:]),
                              r=["xn%d" % j, "ident"], w=[bkey[k // 4]])
                        for k in range(8):
                            A(lambda e, k=k: e.activation(out=hT[j][:, k, :],
                                                          in_=banks[k // 4][:, (k % 4) * 128:(k % 4 + 1) * 128],
                                                          func=AF.Identity, scale=S1T[:, k, b:b + 1],
                                                          bias=sh1T[:, k, b:b + 1]),
                              r=[bkey[k // 4], "S1T", "sh1T"], w=["hT%d" % j])

                    GROUPS = [(0, 512, 2), (512, 512, 3), (1024, 512, 4), (1536, 328, 5), (1864, 512, 6)]

                    def rope(src3, src_key, dst3, dst_key, H, it):
                        c = cs[:, it, :].unsqueeze(1).broadcast_to([128, H, 8])
                        s_ = sn[:, it, :].unsqueeze(1).broadcast_to([128, H, 8])
                        x1 = src3[:, :, 0:8]
                        x2 = src3[:, :, 8:16]
                        t = [r_[:, 0:H, :] for r_ in rt]
                        V(lambda e: e.tensor_tensor(out=t[0], in0=x1, in1=c, op=ALU.mult), r=[src_key, "cs"], w=["rt0"])
                        V(lambda e: e.tensor_tensor(out=t[1], in0=x2, in1=s_, op=ALU.mult), r=[src_key, "sn"], w=["rt1"])
                        V(lambda e: e.tensor_tensor(out=dst3[:, :, 0:8], in0=t[0], in1=t[1], op=ALU.subtract),
                          r=["rt0", "rt1"], w=[dst_key])
                        V(lambda e: e.tensor_tensor(out=t[2], in0=x2, in1=c, op=ALU.mult), r=[src_key, "cs"], w=["rt2"])
                        V(lambda e: e.tensor_tensor(out=t[3], in0=x1, in1=s_, op=ALU.mult), r=[src_key, "sn"], w=["rt3"])
                        V(lambda e: e.tensor_tensor(out=dst3[:, :, 8:16], in0=t[2], in1=t[3], op=ALU.add),
                          r=["rt2", "rt3"], w=[dst_key])
                        A(lambda e: e.activation(out=dst3[:, :, 16:64], in_=src3[:, :, 16:64], func=AF.Copy),
                          r=[src_key], w=[dst_key])

                    def s2(i):
                        j = i % 2
                        it = b * NT + i
                        for (c0, n, bk) in GROUPS:
                            for k in range(8):
                                T(lambda e, k=k, c0=c0, n=n, bk=bk: e.matmul(banks[bk][:, 0:n], lhsT=hT[j][:, k, :],
                                                                             rhs=Win[:, k, c0:c0 + n], start=(k == 0),
                                                                             stop=(k == 7)),
                                  r=["hT%d" % j, "Win"], w=[bkey[bk]])
                        A(lambda e: e.activation(out=zu[j][:], in_=banks[2][:, :], func=AF.Gelu_apprx_tanh),
                          r=[bkey[2]], w=["zu%d" % j])
                        A(lambda e: e.activation(out=zv[:], in_=banks[3][:, :], func=AF.Gelu_apprx_tanh),
                          r=[bkey[3]], w=["zv"])
                        rope(banks[4][:, :].rearrange("p (h d) -> p h d", d=64), bkey[4], q_tok[j][:], "q_tok%d" % j, 8, it)
                        rope(banks[5][:, 0:128].rearrange("p (h d) -> p h d", d=64), bkey[5], kd[j][:, :, 0, :],
                             "kd%d" % j, 2, it)
                        V(lambda e: e.tensor_copy(out=kd[j][:, :, 1, :], in_=kd[j][:, :, 0, :]), r=["kd%d" % j],
                          w=["kd%d" % j])
                        V(lambda e: e.tensor_copy(out=v_aug[:, i, :, 0:64],
                                                  in_=banks[5][:, 128:256].rearrange("p (h d) -> p h d", d=64)),
                          r=[bkey[5]], w=["v_aug"])
                        rope(banks[5][:, 256:320].rearrange("p (h d) -> p h d", d=64), bkey[5], kid[j][:, 0:1, :],
                             "kid%d" % j, 1, it)
                        V(lambda e: e.tensor_copy(out=kid[j][:, 1:2, :], in_=kid[j][:, 0:1, :]), r=["kid%d" % j],
                          w=["kid%d" % j])
                        V(lambda e: e.tensor_copy(out=w_tok[:, i, :], in_=banks[5][:, 320:328]), r=[bkey[5]],
                          w=["w_tok"])
                        rope(banks[6][:, :].rearrange("p (h d) -> p h d", d=64), bkey[6], qi_tok[j][:], "qi_tok%d" % j, 8, it)
                        for g in range(4):
                            A(lambda e, g=g: e.activation(out=junkA[:, 0:128], in_=zv[:, g * 128:(g + 1) * 128],
                                                          func=AF.Square, accum_out=ssv[:, g:g + 1]),
                              r=["zv"], w=["junkA", "ssv"])
                        rstd(rsv[:, 0:4], ssv[:, 0:4], 4, 1.0 / 128, ["ssv"], ["rsv"])
                        for g in range(4):
                            V(lambda e, g=g: e.scalar_tensor_tensor(out=vn[j][:, g * 128:(g + 1) * 128],
                                                                    in0=zv[:, g * 128:(g + 1) * 128],
                                                                    scalar=rsv[:, g:g + 1],
                                                                    in1=gv_row[:, g * 128:(g + 1) * 128],
                                                                    op0=ALU.mult, op1=ALU.mult),
                              r=["zv", "rsv", "gv_row"], w=["vn%d" % j])

                    def s3a(i):
                        j = i % 2
                        ts = slice(i * 128, (i + 1) * 128)
                        qf = q_tok[j][:].rearrange("p h d -> p (h d)")
                        for c in range(4):
                            T(lambda e, c=c: e.transpose(out=bbf[0][:, c * 128:(c + 1) * 128],
                                                         in_=qf[:, c * 128:(c + 1) * 128], identity=identb[:]),
                              r=["q_tok%d" % j, "identb"], w=[bkey[0]])
                        for kv in range(2):
                            T(lambda e, kv=kv: e.transpose(out=bbf[0][:, 512 + kv * 128:512 + (kv + 1) * 128],
                                                           in_=kd[j][:, kv, :, :].rearrange("p a d -> p (a d)"),
                                                           identity=identb[:]),
                              r=["kd%d" % j, "identb"], w=[bkey[0]])
                        T(lambda e: e.transpose(out=bbf[0][:, 768:896], in_=kid[j][:].rearrange("p a d -> p (a d)"),
                                                identity=identb[:]), r=["kid%d" % j, "identb"], w=[bkey[0]])
                        V(lambda e: e.tensor_copy(out=qT2[:, :, ts],
                                                  in_=bbf[0][:, 0:512].rearrange("p (c t) -> p c t", t=128)),
                          r=[bkey[0]], w=["qT2"])
                        V(lambda e: e.tensor_copy(out=kTd[:, :, ts],
                                                  in_=bbf[0][:, 512:768].rearrange("p (c t) -> p c t", t=128)),
                          r=[bkey[0]], w=["kTd"])
                        V(lambda e: e.tensor_copy(out=kiTd[:, ts], in_=bbf[0][:, 768:896]), r=[bkey[0]], w=["kiTd"])
                        qif = qi_tok[j][:].rearrange("p h d -> p (h d)")
                        for c in range(4):
                            T(lambda e, c=c: e.transpose(out=bbf[1][:, c * 128:(c + 1) * 128],
                                                         in_=qif[:, c * 128:(c + 1) * 128], identity=identb[:]),
                              r=["qi_tok%d" % j, "identb"], w=[bkey[1]])
                        A(lambda e: e.activation(out=qiT2[:, i * 4:(i + 1) * 4, :, :],
                                                 in_=bbf[1][:, 0:512].rearrange("p (c g t) -> p g c t", c=4, g=4),
                                                 func=AF.Copy), r=[bkey[1]], w=["qiT2"])
                        for g in range(4):
                            T(lambda e, g=g: e.matmul(banks[7][:, g * 128:(g + 1) * 128], lhsT=WmT[:, g, :],
                                                      rhs=vn[j][:, g * 128:(g + 1) * 128], start=True, stop=True),
                              r=["WmT", "vn%d" % j], w=[bkey[7]])
                        for g in range(4):
                            V(lambda e, g=g: e.scalar_tensor_tensor(out=ya[:, g * 128:(g + 1) * 128],
                                                                    in0=banks[7][:, g * 128:(g + 1) * 128],
                                                                    scalar=bcol[:, g:g + 1],
                                                                    in1=zu[j][:, g * 128:(g + 1) * 128],
                                                                    op0=ALU.add, op1=ALU.mult),
                              r=[bkey[7], "bcol", "zu%d" % j], w=["ya"])
                        A(lambda e: e.activation(out=junkA[:, 0:512], in_=ya[:], func=AF.Square, accum_out=ssa[:]),
                          r=["ya"], w=["junkA", "ssa"])
                        rstd(rsa[:], ssa[:], 1, 1.0 / 512, ["ssa"], ["rsa"])
                        V(lambda e: e.scalar_tensor_tensor(out=ma[:], in0=ya[:], scalar=rsa[:, 0:1], in1=goa_row[:],
                                                           op0=ALU.mult, op1=ALU.mult),
                          r=["ya", "rsa", "goa_row"], w=["ma"])
                        if b == 0 and i == 0:
                            tap("ya", ya[:], ["ya"])

                    def s3b(i):
                        ts = slice(i * 128, (i + 1) * 128)
                        for c in range(4):
                            T(lambda e, c=c: e.transpose(out=bbf[7][:, c * 128:(c + 1) * 128],
                                                         in_=ma[:, c * 128:(c + 1) * 128], identity=identb[:]),
                              r=["ma", "identb"], w=[bkey[7]])
                        A(lambda e: e.activation(out=mTa[:, :, ts],
                                                 in_=bbf[7][:, 0:512].rearrange("p (c t) -> p c t", t=128),
                                                 func=AF.Copy), r=[bkey[7]], w=["mTa"])

                    s1_load(0)
                    if NT > 1:
                        s1_load(1)
                    s1(0)
                    for n in range(NT + 2):
                        if n + 1 < NT:
                            s1(n + 1)
                        if n < NT:
                            s2(n)
                            if n + 2 < NT:
                                s1_load(n + 2)
                        if 0 <= n - 2 < NT:
                            s3b(n - 2)
                        if 0 <= n - 1 < NT:
                            s3a(n - 1)
                    P.barrier()
                    if stop == "proj":
                        P.finish()
                        return

                with contextlib.ExitStack() as tst:
                    tsb = mk_sb(tst)
                    score = tsb("score", [128, S])
                    cmax = tsb("cmax", [128, 4])
                    mask = tsb("mask", [128, S], BF16)
                    maskT = tsb("maskT", [128, NT, 128], BF16)
                    rl = [tsb("rl%d" % i, [128, 512], BF16) for i in range(4)]
                    pT = [tsb("pT%d" % i, [128, 512], BF16) for i in range(4)]
                    Wsel = [tsb("Wsel%d" % i, [128, 8, 128], BF16) for i in range(2)]
                    wrep = tsb("wrep", [128, 2, 128])
                    wcol = tsb("wcol", [128, 8])
                    lo0 = tsb("lo0", [128, 1])
                    hi0 = tsb("hi0", [128, 1])
                    w0 = tsb("w0", [128, 1])
                    wh = tsb("wh", [128, NIT + 1])
                    cbias = tsb("cbias", [128, NT])
                    mid = tsb("mid", [128, 1])
                    cnt = tsb("cnt", [128, 1])
                    btmp = tsb("btmp", [128, 1])
                    rden = tsb("rden", [128, 8])
                    yb = tsb("yb", [128, 512])
                    ssb_ = tsb("ssb_", [128, 1])
                    rsb = tsb("rsb", [128, 1])
                    mb = tsb("mb", [128, 512], BF16)
                    mbT = tsb("mbT", [128, 4, 128], BF16)
                    sso = tsb("sso", [128, 2])
                    rso = tsb("rso", [128, 1])
                    ot = tsb("ot", [128, D])
                    xres = [tsb("xres%d" % i, [128, D]) for i in range(2)]
                    x1t = [tsb("x1t%d" % i, [128, D]) for i in range(2)]
                    for i in range(2):
                        G(lambda e, i=i: e.memset(Wsel[i][:], 0.0), w=["Wsel%d" % i])
                    for qq in range(NT):
                        G(lambda e, qq=qq: e.memset(cbias[:, qq:qq + 1], float((qq + 1) * 128 - 2 * TOPK) + 0.5),
                          w=["cbias"])
                    rl_i = [0]
                    pT_i = [0]
                    D_i = [0]

                    def wsel_build(qb):
                        wi = qb % 2
                        wk = "Wsel%d" % wi
                        w2v = w_tok[:, qb, :].rearrange("p (i two) -> p i two", two=2)
                        for par in range(2):
                            V(lambda e, par=par: e.tensor_tensor(
                                out=wrep[:, par, :].rearrange("p (i t) -> p i t", t=32),
                                in0=w2v[:, :, par].unsqueeze(2).broadcast_to([128, 4, 32]),
                                in1=D32[:].unsqueeze(1).broadcast_to([128, 4, 32]), op=ALU.mult),
                              r=["w_tok", "D32"], w=["wrep"])
                        for par in range(2):
                            T(lambda e, par=par: e.matmul(banks[0][:, par * 4:(par + 1) * 4], lhsT=wrep[:, par, :],
                                                          rhs=G4[:], start=True, stop=True),
                              r=["wrep", "G4"], w=[bkey[0]])
                        V(lambda e: e.tensor_scalar(out=wcol[:], in0=banks[0][:, 0:8], scalar1=IDX_SCALE, scalar2=None,
                                                    op0=ALU.mult), r=[bkey[0]], w=["wcol"])
                        for par in range(2):
                            for g in range(4):
                                V(lambda e, par=par, g=g: e.tensor_scalar(
                                    out=Wsel[wi][:, par * 4 + g, 32 * g:32 * g + 32], in0=D32[:],
                                    scalar1=wcol[:, par * 4 + g:par * 4 + g + 1], scalar2=None, op0=ALU.mult),
                                  r=["D32", "wcol"], w=[wk])

                    def indexer(qb):
                        N = (qb + 1) * 128
                        nch = (N + 511) // 512
                        wi = qb % 2
                        wk = "Wsel%d" % wi
                        units = []
                        for c in range(nch):
                            n = min(512, N - c * 512)
                            for g in range(4):
                                for par in range(2):
                                    units.append((c, n, g, par))

                        def dots(u):
                            c, n, g, par = u
                            ri = rl_i[0] % 4
                            rl_i[0] += 1
                            ps = slice(64 * par, 64 * par + 64)
                            T(lambda e: e.matmul(banks[par][:, 0:n],
                                                 lhsT=qiT2[ps, qb * 4 + g, :, :].rearrange("p c t -> p (c t)"),
                                                 rhs=kiTd[ps, c * 512:c * 512 + n], start=True, stop=True),
                              r=["qiT2", "kiTd"], w=[bkey[par]])
                            if par == 0:
                                A(lambda e: e.activation(out=rl[ri][:, 0:n], in_=banks[par][:, 0:n], func=AF.Relu),
                                  r=[bkey[par]], w=["rl%d" % ri])
                            else:
                                V(lambda e: e.tensor_scalar(out=rl[ri][:, 0:n], in0=banks[par][:, 0:n], scalar1=0.0,
                                                            scalar2=None, op0=ALU.max), r=[bkey[par]], w=["rl%d" % ri])
                            return ri

                        def selmm(u, ri):
                            c, n, g, par = u
                            sbk = 2 + (c % 2)
                            first = (g == 0 and par == 0)
                            last = (g == 3 and par == 1)
                            T(lambda e: e.matmul(banks[sbk][:, 0:n], lhsT=Wsel[wi][:, par * 4 + g, :],
                                                 rhs=rl[ri][:, 0:n], start=first, stop=last),
                              r=[wk, "rl%d" % ri], w=[bkey[sbk]])
                            if last:
                                V(lambda e: e.tensor_scalar(out=score[:, c * 512:c * 512 + n], in0=banks[sbk][:, 0:n],
                                                            scalar1=1.0, scalar2=None, op0=ALU.mult, op1=ALU.max,
                                                            accum_out=cmax[:, c:c + 1]),
                                  r=[bkey[sbk]], w=["score", "cmax"])

                        ris = {}
                        LOOK = 2
                        for i_ in range(min(LOOK, len(units))):
                            ris[i_] = dots(units[i_])
                        for i_ in range(len(units)):
                            if i_ + LOOK < len(units):
                                ris[i_ + LOOK] = dots(units[i_ + LOOK])
                            selmm(units[i_], ris[i_])

                    def topk_iter(qb):
                        N = (qb + 1) * 128
                        nch = (N + 511) // 512
                        V(lambda e: e.tensor_reduce(out=lo0[:], in_=score[:, 0:N], axis=AX.X, op=ALU.min),
                          r=["score"], w=["lo0"])
                        V(lambda e: e.tensor_reduce(out=hi0[:], in_=cmax[:, 0:nch], axis=AX.X, op=ALU.max),
                          r=["cmax"], w=["hi0"])
                        V(lambda e: e.tensor_tensor(out=score[:, qb * 128:N], in0=score[:, qb * 128:N], in1=NEGM[:],
                                                    op=ALU.add), r=["score", "NEGM"], w=["score"])
                        V(lambda e: e.tensor_tensor(out=w0[:], in0=lo0[:], in1=hi0[:], op=ALU.subtract),
                          r=["hi0", "lo0"], w=["w0"])
                        V(lambda e: e.tensor_scalar(out=wh[:], in0=P2[:], scalar1=w0[:, 0:1], scalar2=None,
                                                    op0=ALU.mult), r=["P2", "w0"], w=["wh"])
                        V(lambda e: e.tensor_scalar(out=mid[:], in0=lo0[:], scalar1=-1.0, scalar2=wh[:, 0:1],
                                                    op0=ALU.mult, op1=ALU.add), r=["lo0", "wh"], w=["mid"])
                        for i in range(NIT):
                            A(lambda e: e.activation(out=mask[:, 0:N], in_=score[:, 0:N], func=AF.Sign,
                                                     bias=mid[:, 0:1], accum_out=cnt[:]),
                              r=["score", "mid"], w=["mask", "cnt"])
                            V(lambda e, i=i: e.scalar_tensor_tensor(out=btmp[:], in0=cnt[:],
                                                                    scalar=float(2 * TOPK - N) - 0.5,
                                                                    in1=wh[:, i:i + 1], op0=ALU.is_ge, op1=ALU.mult),
                              r=["cnt", "wh"], w=["btmp"])
                            V(lambda e, i=i: e.scalar_tensor_tensor(out=mid[:], in0=mid[:], scalar=wh[:, i + 1:i + 2],
                                                                    in1=btmp[:], op0=ALU.subtract, op1=ALU.add),
                              r=["mid", "wh", "btmp"], w=["mid"])
                            yield
                        V(lambda e: e.tensor_scalar(out=mid[:], in0=mid[:], scalar1=-1.0, scalar2=wh[:, NIT:NIT + 1],
                                                    op0=ALU.mult, op1=ALU.add), r=["mid", "wh"], w=["mid"])

                    def topk_finish(qb):
                        N = (qb + 1) * 128
                        if qb < KB:
                            for jj in range(qb + 1):
                                src = TRIU if jj == qb else ONESB
                                G(lambda e, jj=jj, src=src: e.tensor_copy(out=maskT[:, jj, :], in_=src[:]),
                                  r=["TRIU", "ONESB"], w=["maskT"])
                            return
                        V(lambda e: e.tensor_scalar(out=mask[:, 0:N], in0=score[:, 0:N], scalar1=mid[:, 0:1],
                                                    scalar2=None, op0=ALU.is_ge), r=["score", "mid"], w=["mask"])
                        if b == 0 and qb == NT - 1:
                            tap("score", score[:, 0:N], ["score"])
                            tap("thr", mid[:], ["mid"])
                        for jj in range(qb + 1):
                            lb = 4 + jj // 8
                            T(lambda e, jj=jj, lb=lb: e.transpose(out=bbf[lb][:, (jj % 8) * 128:(jj % 8 + 1) * 128],
                                                                  in_=mask[:, jj * 128:(jj + 1) * 128],
                                                                  identity=identb[:]),
                              r=["mask", "identb"], w=[bkey[lb]])
                        for lb in range(4, 4 + (qb + 8) // 8):
                            j0 = (lb - 4) * 8
                            j1 = min(qb + 1, j0 + 8)
                            nj = j1 - j0
                            V(lambda e, lb=lb, j0=j0, j1=j1, nj=nj: e.tensor_copy(
                                out=maskT[:, j0:j1, :],
                                in_=bbf[lb][:, 0:nj * 128].rearrange("p (j t) -> p j t", t=128)),
                              r=[bkey[lb]], w=["maskT"])

                    def attention(qb):
                        qs = slice(qb * 128, (qb + 1) * 128)
                        for kv in range(2):
                            T(lambda e, kv=kv: e.matmul(banks[6 + kv][:, 0:260], lhsT=zerob[:, 0:128],
                                                        rhs=zerob[:, 0:260], start=True, stop=False,
                                                        skip_group_check=True), r=["zerob"], w=[bkey[6 + kv]])

                        def Lstage(jj):
                            ks = slice(jj * 128, (jj + 1) * 128)
                            pis = []
                            for par in range(2):
                                ps = slice(64 * par, 64 * par + 64)
                                lb = 4 + par
                                pi = pT_i[0] % 4
                                pT_i[0] += 1
                                pis.append(pi)
                                for kv in range(2):
                                    T(lambda e, ps=ps, lb=lb, kv=kv: e.matmul(
                                        banks[lb][:, kv * 256:(kv + 1) * 256], lhsT=kTd[ps, kv, ks],
                                        rhs=qT2[ps, 2 * kv:2 * kv + 2, qs], start=True, stop=True),
                                      r=["kTd", "qT2"], w=[bkey[lb]])
                                A(lambda e, lb=lb, pi=pi: e.activation(out=pT[pi][:], in_=banks[lb][:, :], func=AF.Exp,
                                                                       scale=0.125), r=[bkey[lb]], w=["pT%d" % pi])
                                V(lambda e, pi=pi: e.tensor_tensor(
                                    out=pT[pi][:].rearrange("p (h t) -> p h t", t=128),
                                    in0=pT[pi][:].rearrange("p (h t) -> p h t", t=128),
                                    in1=maskT[:, jj, :].unsqueeze(1).broadcast_to([128, 4, 128]), op=ALU.mult),
                                  r=["pT%d" % pi, "maskT"], w=["pT%d" % pi])
                            return pis

                        def PVstage(jj, pis):
                            for par in range(2):
                                pi = pis[par]
                                for kv in range(2):
                                    for ii in range(2):
                                        hl = 2 * ii + par
                                        T(lambda e, ii=ii, hl=hl, kv=kv, pi=pi: e.matmul(
                                            banks[6 + kv][:, hl * 65:hl * 65 + 65],
                                            lhsT=pT[pi][:, (kv * 2 + ii) * 128:(kv * 2 + ii + 1) * 128],
                                            rhs=v_aug[:, jj, kv, :], start=False, stop=(jj == qb),
                                            skip_group_check=True),
                                          r=["pT%d" % pi, "v_aug"], w=[bkey[6 + kv]])

                        nxt = Lstage(0)
                        for jj in range(qb + 1):
                            cur = nxt
                            if jj + 1 <= qb:
                                nxt = Lstage(jj + 1)
                            PVstage(jj, cur)
                            yield

                    def post_a(qb):
                        for kv in range(2):
                            ov = banks[6 + kv][:, 0:260].rearrange("p (h d) -> p h d", d=65)
                            V(lambda e, kv=kv, ov=ov: e.reciprocal(out=rden[:, kv * 4:(kv + 1) * 4], in_=ov[:, :, 64]),
                              r=[bkey[6 + kv]], w=["rden"])
                            V(lambda e, kv=kv, ov=ov: e.tensor_tensor(
                                out=yb[:, kv * 256:(kv + 1) * 256].rearrange("p (h d) -> p h d", d=64),
                                in0=ov[:, :, 0:64],
                                in1=rden[:, kv * 4:(kv + 1) * 4].unsqueeze(2).broadcast_to([128, 4, 64]),
                                op=ALU.mult), r=[bkey[6 + kv], "rden"], w=["yb"])
                        if b == 0 and qb == NT - 1:
                            tap("yb", yb[:], ["yb"])

                    def post_b1(qb):
                        yield
                        A(lambda e: e.activation(out=junkA[:, 0:512], in_=yb[:], func=AF.Square, accum_out=ssb_[:]),
                          r=["yb"], w=["junkA", "ssb_"])
                        yield
                        rstd(rsb[:], ssb_[:], 1, 1.0 / 512, ["ssb_"], ["rsb"])
                        yield
                        V(lambda e: e.scalar_tensor_tensor(out=mb[:], in0=yb[:], scalar=rsb[:, 0:1], in1=gob_row[:],
                                                           op0=ALU.mult, op1=ALU.mult),
                          r=["yb", "rsb", "gob_row"], w=["mb"])

                    def post_b2(qb):
                        it = b * NT + qb
                        xj = qb % 2
                        for c in range(4):
                            T(lambda e, c=c: e.transpose(out=bbf[2][:, c * 128:(c + 1) * 128],
                                                         in_=mb[:, c * 128:(c + 1) * 128], identity=identb[:]),
                              r=["mb", "identb"], w=[bkey[2]])
                        A(lambda e: e.activation(out=mbT[:], in_=bbf[2][:, 0:512].rearrange("p (c t) -> p c t", t=128),
                                                 func=AF.Copy), r=[bkey[2]], w=["mbT"])
                        yield
                        for n in range(2):
                            for k in range(8):
                                lhs = mTa[:, k, qb * 128:(qb + 1) * 128] if k < 4 else mbT[:, k - 4, :]
                                T(lambda e, n=n, k=k, lhs=lhs: e.matmul(banks[2 + n][:, :], lhsT=lhs,
                                                                        rhs=Wout[:, k, n * 512:(n + 1) * 512],
                                                                        start=(k == 0), stop=(k == 7)),
                                  r=["mTa", "mbT", "Wout"], w=[bkey[2 + n]])
                        for n in range(2):
                            A(lambda e, n=n: e.activation(out=junkA[:, 0:512], in_=banks[2 + n][:, :], func=AF.Square,
                                                          accum_out=sso[:, n:n + 1]),
                              r=[bkey[2 + n]], w=["junkA", "sso%d" % n])
                        yield
                        rstd(rso[:], sso[:, 0:1], 1, 1.0 / D, ["sso0", "sso1"], ["rso"], ss2_ap=sso[:, 1:2])
                        yield
                        for n in range(2):
                            V(lambda e, n=n: e.scalar_tensor_tensor(out=ot[:, n * 512:(n + 1) * 512],
                                                                    in0=banks[2 + n][:, :], scalar=rso[:, 0:1],
                                                                    in1=G1row[:, n * 512:(n + 1) * 512],
                                                                    op0=ALU.mult, op1=ALU.mult),
                              r=[bkey[2 + n], "rso", "G1row"], w=["ot"])
                        G(lambda e: e.tensor_tensor(out=x1t[xj][:], in0=ot[:], in1=xres[xj][:], op=ALU.add),
                          r=["ot", "xres%d" % xj], w=["x1t%d" % xj])
                        P.dma(x1s_d[it * 128:(it + 1) * 128, :], x1t[xj][:], reads=["x1t%d" % xj],
                              writes=["x1s_%d" % it])

                    def step(g_):
                        if g_ is None:
                            return False
                        try:
                            next(g_)
                            return True
                        except StopIteration:
                            return False

                    def interleave(g1, g2):
                        a1, a2 = g1 is not None, g2 is not None
                        while a1:
                            a1 = step(g1)
                            if a2:
                                a2 = step(g2)
                        return a2

                    def drain(g_):
                        while step(g_):
                            pass

                    if 0 >= KB:
                        wsel_build(0)
                        indexer(0)
                        drain(topk_iter(0))
                    topk_finish(0)
                    if 1 < NT and 1 >= KB:
                        wsel_build(1)
                    pb1 = pb2 = None
                    for qb in range(NT):
                        it = b * NT + qb
                        P.dma(xres[qb % 2][:], x_d[it * 128:(it + 1) * 128, :], writes=["xres%d" % (qb % 2)])
                        tk = None
                        if qb + 1 < NT and qb + 1 >= KB:
                            indexer(qb + 1)
                            tk = topk_iter(qb + 1)
                        if qb + 2 < NT and qb + 2 >= KB:
                            wsel_build(qb + 2)
                        att = attention(qb)
                        a_att, a_tk = True, tk is not None
                        a_p1, a_p2 = pb1 is not None, pb2 is not None
                        while a_att:
                            a_att = step(att)
                            if a_tk:
                                a_tk = step(tk)
                            if a_p1:
                                a_p1 = step(pb1)
                            elif a_p2:
                                a_p2 = step(pb2)
                        if a_p1:
                            drain(pb1)
                        if a_p2:
                            drain(pb2)
                        post_a(qb)
                        pb1 = post_b1(qb)
                        pb2 = post_b2(qb)
                        a_p1 = True
                        while a_tk:
                            a_tk = step(tk)
                            if a_p1:
                                a_p1 = step(pb1)
                        if qb + 1 < NT:
                            topk_finish(qb + 1)
                    drain(pb1)
                    drain(pb2)
                    P.barrier()
                    if stop == "attn":
                        P.finish()
                        return
        with contextlib.ExitStack() as bst:
            bsb = mk_sb(bst)
            W1 = bsb("W1", [128, 8, DFF], BF16)
            W2 = bsb("W2", [128, 32, D], BF16)
            wst = [bsb("wst%d" % i, [128, 2048]) for i in range(2)]
            G2row = bsb("G2row", [128, D])
            xg = [bsb("xg%d" % i, [128, D]) for i in range(4)]
            xn2 = [bsb("xn2_%d" % i, [128, D]) for i in range(1)]
            h2T = [bsb("h2T%d" % i, [128, 8, 256], BF16) for i in range(2)]
            rr = [bsb("rr%d" % i, [128, 256], BF16) for i in range(3)]
            fT = [bsb("fT%d" % i, [128, 256], BF16) for i in range(3)]
            junkB = bsb("junkB", [128, D], BF16)
            ss2 = bsb("ss2", [128, 4])
            rs2 = bsb("rs2", [128, 4])
            ssf = bsb("ssf", [128, 4])
            rsf = bsb("rsf", [128, 2])
            of = [bsb("of%d" % i, [128, D]) for i in range(2)]

            cast_engs = ["dve", "act", "pool"]
            ci = 0
            wi_ = 0
            for k in range(8):
                for hf in range(2):
                    st_ = wst[wi_ % 2]
                    sk = "wst%d" % (wi_ % 2)
                    wi_ += 1
                    P.dma(st_[:], w1_d[k * 128:(k + 1) * 128, hf * 2048:(hf + 1) * 2048], writes=[sk])
                    for q2 in range(2):
                        eng = cast_engs[ci % 3]
                        ci += 1
                        sl = slice(q2 * 1024, (q2 + 1) * 1024)
                        dl = slice(hf * 2048 + q2 * 1024, hf * 2048 + (q2 + 1) * 1024)
                        if eng == "act":
                            A(lambda e, k=k, st_=st_, sl=sl, dl=dl: e.activation(out=W1[:, k, dl], in_=st_[:, sl],
                                                                              func=AF.Copy), r=[sk], w=["W1"])
                        else:
                            P.op(eng, lambda e, k=k, st_=st_, sl=sl, dl=dl: e.tensor_copy(out=W1[:, k, dl],
                                                                                       in_=st_[:, sl]), [sk], ["W1"])
            for c2 in range(16):
                st_ = wst[wi_ % 2]
                sk = "wst%d" % (wi_ % 2)
                wi_ += 1
                P.dma(st_[:].rearrange("p (c n) -> p c n", n=D),
                      w2_d[c2 * 256:(c2 + 1) * 256, :].rearrange("(c p) n -> p c n", p=128), writes=[sk])
                for q2 in range(2):
                    eng = cast_engs[ci % 3]
                    ci += 1
                    sl = slice(q2 * 1024, (q2 + 1) * 1024)
                    if eng == "act":
                        A(lambda e, c2=c2, q2=q2, st_=st_, sl=sl: e.activation(out=W2[:, c2 * 2 + q2, :], in_=st_[:, sl],
                                                                          func=AF.Copy), r=[sk], w=["W2"])
                    else:
                        P.op(eng, lambda e, c2=c2, q2=q2, st_=st_, sl=sl: e.tensor_copy(out=W2[:, c2 * 2 + q2, :],
                                                                                   in_=st_[:, sl]), [sk], ["W2"])

            NG = NTOK // 256

            def b_load(g):
                for t in range(2):
                    it = g * 2 + t
                    xi = (g % 2) * 2 + t
                    P.dma(xg[xi][:], x1s_d[it * 128:(it + 1) * 128, :], reads=["x1s_%d" % it], writes=["xg%d" % xi])

            def b_prep(g):
                hj = g % 2
                b = (g * 256) // S
                for t in range(2):
                    xi = (g % 2) * 2 + t
                    A(lambda e, xi=xi, t=t: e.activation(out=junkB[:], in_=xg[xi][:], func=AF.Square,
                                                        accum_out=ss2[:, t:t + 1]), r=["xg%d" % xi],
                      w=["junkB", "ss2_%d" % t])
                    rstd(rs2[:, t:t + 1], ss2[:, t:t + 1], 1, 1.0 / D, ["ss2_%d" % t], ["rs2_%d" % t])
                    V(lambda e, xi=xi, t=t: e.tensor_scalar(out=xn2[0][:], in0=xg[xi][:], scalar1=rs2[:, t:t + 1],
                                                           scalar2=None, op0=ALU.mult),
                      r=["xg%d" % xi, "rs2_%d" % t], w=["xn2_0"])
                    for k in range(8):
                        T(lambda e, k=k, t=t: e.transpose(out=banks[6 + k // 4][:, (k % 4) * 128:(k % 4 + 1) * 128],
                                                          in_=xn2[0][:, k * 128:(k + 1) * 128], identity=ident[:]),
                          r=["xn2_0", "ident"], w=[bkey[6 + k // 4]])
                    for k in range(8):
                        A(lambda e, k=k, t=t: e.activation(out=h2T[hj][:, k, t * 128:(t + 1) * 128],
                                                           in_=banks[6 + k // 4][:, (k % 4) * 128:(k % 4 + 1) * 128],
                                                           func=AF.Identity, scale=S2T[:, k, b:b + 1],
                                                           bias=sh2T[:, k, b:b + 1]),
                          r=[bkey[6 + k // 4], "S2T", "sh2T"], w=["h2T%d" % hj])

            f_i = [0]

            def b_main(g):
                hj = g % 2
                b = (g * 256) // S
                if (g * 256) % S == 0:
                    for n in range(2):
                        T(lambda e, n=n: e.matmul(banks[4 + n][:, :], lhsT=sel[0:NSEQ, b, :],
                                                  rhs=gmod[0:NSEQ, 1, n * 512:(n + 1) * 512], start=True, stop=True),
                          r=["sel", "gmod1"], w=[bkey[4 + n]])
                        V(lambda e, n=n: e.tensor_copy(out=G2row[:, n * 512:(n + 1) * 512], in_=banks[4 + n][:, :]),
                          r=[bkey[4 + n]], w=["G2row"])
                def Fst(c):
                    fb = 4 + (c % 2)
                    fi = c % 3
                    for k in range(8):
                        T(lambda e, k=k: e.matmul(banks[fb][:, 0:256], lhsT=W1[:, k, c * 128:(c + 1) * 128],
                                                  rhs=h2T[hj][:, k, :], start=(k == 0), stop=(k == 7)),
                          r=["W1", "h2T%d" % hj], w=[bkey[fb]])
                    A(lambda e: e.activation(out=rr[fi][:], in_=banks[fb][:, 0:256], func=AF.Relu),
                      r=[bkey[fb]], w=["rr%d" % fi])
                    V(lambda e: e.scalar_tensor_tensor(out=fT[fi][:], in0=banks[fb][:, 0:256], scalar=0.0,
                                                       in1=rr[fi][:], op0=ALU.max, op1=ALU.mult),
                      r=[bkey[fb], "rr%d" % fi], w=["fT%d" % fi])

                def P2st(c):
                    fi = c % 3
                    for t in range(2):
                        for n in range(2):
                            ob = t * 2 + n
                            T(lambda e, t=t, n=n, ob=ob: e.matmul(
                                banks[ob][:, :], lhsT=fT[fi][:, t * 128:(t + 1) * 128],
                                rhs=W2[:, c, n * 512:(n + 1) * 512], start=(c == 0), stop=(c == 31)),
                              r=["fT%d" % fi, "W2"], w=[bkey[ob]])

                Fst(0)
                for c in range(32):
                    if c + 1 < 32:
                        Fst(c + 1)
                    P2st(c)
                    if c == 20 and g + 1 < NG:
                        b_prep(g + 1)
                for t in range(2):
                    it = g * 2 + t
                    xi = (g % 2) * 2 + t
                    for n in range(2):
                        A(lambda e, t=t, n=n: e.activation(out=junkB[:, 0:512], in_=banks[t * 2 + n][:, :],
                                                           func=AF.Square, accum_out=ssf[:, t * 2 + n:t * 2 + n + 1]),
                          r=[bkey[t * 2 + n]], w=["junkB", "ssf%d" % (t * 2 + n)])
                    rstd(rsf[:, t:t + 1], ssf[:, t * 2:t * 2 + 1], 1, 1.0 / D, ["ssf%d" % (t * 2), "ssf%d" % (t * 2 + 1)],
                         ["rsf%d" % t], ss2_ap=ssf[:, t * 2 + 1:t * 2 + 2])
                    for n in range(2):
                        V(lambda e, t=t, n=n: e.scalar_tensor_tensor(out=of[t][:, n * 512:(n + 1) * 512],
                                                                     in0=banks[t * 2 + n][:, :], scalar=rsf[:, t:t + 1],
                                                                     in1=G2row[:, n * 512:(n + 1) * 512],
                                                                     op0=ALU.mult, op1=ALU.mult),
                          r=[bkey[t * 2 + n], "rsf%d" % t, "G2row"], w=["of%d" % t])
                    G(lambda e, t=t, xi=xi: e.tensor_tensor(out=of[t][:], in0=of[t][:], in1=xg[xi][:], op=ALU.add),
                      r=["of%d" % t, "xg%d" % xi], w=["of%d" % t])
                    P.dma(out_d[it * 128:(it + 1) * 128, :], of[t][:], reads=["of%d" % t])
                if g + 2 < NG:
                    b_load(g + 2)

            b_load(0)
            if NG > 1:
                b_load(1)
            b_prep(0)
            for g in range(NG):
                b_main(g)
            P.finish()
        print("program built: instrs=%d waits=%d" % (P.ninstr, P.nwaits), flush=True)


def make_core_inputs(ci, NSEQ, S, x, c, positions, w_ada, b_ada, g_pre_mix, w_in, g_sgu_v, w_spatial, b_spatial,
                     g_out_sgu, g_out_attn, w_out, g_post_mix, g_pre_ffn, w_ff1, w_ff2, g_post_ffn):
    f32 = np.float32
    bs = slice(ci * NSEQ, (ci + 1) * NSEQ)
    NT = S // 128
    xc = np.ascontiguousarray(x[bs]).reshape(NSEQ * S, D).astype(f32, copy=False)
    cc = np.asarray(c[bs], dtype=f32)
    cT = np.ascontiguousarray(cc.T.reshape(8, 128, NSEQ).transpose(1, 0, 2))
    pos = np.ascontiguousarray(np.asarray(positions[bs]).reshape(NSEQ * NT, 128).T.astype(np.int32))
    wi = np.asarray(w_in[0], dtype=f32)
    perm = np.concatenate([np.arange(0, 1792), np.arange(2304, 2376), np.arange(1792, 2304)])
    wi_p = np.ascontiguousarray(wi[:, perm])
    return {
        "x": xc, "cT": cT, "pos": pos,
        "w_ada": np.ascontiguousarray(w_ada[0], dtype=f32),
        "b_ada": np.ascontiguousarray(b_ada[0:1], dtype=f32),
        "w_in": wi_p,
        "gpre": np.ascontiguousarray(np.asarray(g_pre_mix[0], dtype=f32).reshape(8, 128).T),
        "gpre2": np.ascontiguousarray(np.asarray(g_pre_ffn[0], dtype=f32).reshape(8, 128).T),
        "gv": np.ascontiguousarray(g_sgu_v[0:1], dtype=f32),
        "ws": np.ascontiguousarray(np.asarray(w_spatial[0], dtype=f32).transpose(1, 0, 2)),
        "bs": np.ascontiguousarray(np.asarray(b_spatial[0], dtype=f32).T),
        "goa": np.ascontiguousarray(g_out_sgu[0:1], dtype=f32),
        "gob": np.ascontiguousarray(g_out_attn[0:1], dtype=f32),
        "w_out": np.ascontiguousarray(w_out[0], dtype=f32),
        "gpost": np.ascontiguousarray(g_post_mix[0:1], dtype=f32),
        "w1": np.ascontiguousarray(w_ff1[0], dtype=f32),
        "w2": np.ascontiguousarray(w_ff2[0], dtype=f32),
        "gpost2": np.ascontiguousarray(g_post_ffn[0:1], dtype=f32),
    }


def run(inputs, n_cores, NSEQ, S, taps=None, trace=False, stop=None):
    nc = bass.Bass("TRN2", target_bir_lowering=False)
    try:
        build_program(nc, NSEQ=NSEQ, S=S, taps=taps, stop=stop)
    except StopBuild:
        pass
    in_maps = [make_core_inputs(ci, NSEQ, S, **inputs) for ci in range(n_cores)]
    res = run_bass_kernel_spmd(nc, in_maps, core_ids=list(range(n_cores)), trace=trace)
    return res


def kernel(**inputs):
    inputs = {k: np.asarray(v) for k, v in inputs.items()}
    B, S, _ = inputs["x"].shape
    NSEQ = B // NCORES
    res = run(inputs, NCORES, NSEQ, S)
    outs = [np.asarray(r["out"]).reshape(NSEQ, S, D) for r in res.results]
    return np.concatenate(outs, axis=0).astype(np.float32, copy=False)
```

```python
import contextlib
import math
import numpy as np
import concourse.bass as bass
import concourse.mybir as mybir
from concourse.bass_utils import run_bass_kernel_spmd

F32 = mybir.dt.float32
BF16 = mybir.dt.bfloat16
I32 = mybir.dt.int32
AF = mybir.ActivationFunctionType
ALU = mybir.AluOpType
AX = mybir.AxisListType

D = 1024
DIN = 2376
DFF = 4096
NCORES = 8
EPS = 1e-6
NIT = 16
IDX_SCALE = (64 ** -0.5) * (8 ** -0.5)
TWO_PI = 2.0 * math.pi


class StopBuild(Exception):
    pass


class Prog:
    NDMA = 32

    def __init__(self, nc, stack):
        self.nc = nc
        self.eng = {"pe": nc.tensor, "act": nc.scalar, "dve": nc.vector,
                    "pool": nc.gpsimd, "sp": nc.sync}
        self.sem = {k: stack.enter_context(nc.semaphore("c_" + k)) for k in self.eng}
        self.cnt = {k: 0 for k in self.eng}
        self.dsem = [stack.enter_context(nc.semaphore("d%d" % i)) for i in range(self.NDMA)]
        self.dval = [0] * self.NDMA
        self.dnext = 0
        self.seen = {k: {} for k in self.eng}
        self.res = {}
        self.nwaits = 0
        self.ninstr = 0

    def _wait(self, eng, dep):
        kind, key, val = dep
        if kind == "e":
            if key == "pe" and eng == "pe":
                return
            sem = self.sem[key]
            skey = key
        else:
            sem = self.dsem[key]
            skey = ("d", key)
        if self.seen[eng].get(skey, 0) >= val:
            return
        self.seen[eng][skey] = val
        self.eng[eng].wait_ge(sem, val)
        self.nwaits += 1

    def _deps(self, eng, reads, writes):
        deps = []
        for r in reads:
            st = self.res.get(r)
            if st and st["w"]:
                deps.append(st["w"])
        for w in writes:
            st = self.res.get(w)
            if st:
                if st["w"]:
                    deps.append(st["w"])
                deps.extend(st["r"])
        for d in deps:
            self._wait(eng, d)

    def _record(self, token, reads, writes):
        for r in reads:
            st = self.res.setdefault(r, {"w": None, "r": []})
            st["r"].append(token)
            if len(st["r"]) > 48:
                best = {}
                for t in st["r"]:
                    k = (t[0], t[1])
                    if k not in best or best[k][2] < t[2]:
                        best[k] = t
                st["r"] = list(best.values())
        for w in writes:
            self.res[w] = {"w": token, "r": []}

    def op(self, eng, fn, reads=(), writes=()):
        self._deps(eng, reads, writes)
        ins = fn(self.eng[eng])
        self.cnt[eng] += 1
        ins.then_inc(self.sem[eng], 1)
        token = ("e", eng, self.cnt[eng])
        self._record(token, reads, writes)
        self.ninstr += 1
        return token

    def dma(self, out, in_, reads=(), writes=(), q="sp", **kw):
        self._deps(q, reads, writes)
        i = self.dnext
        self.dnext = (self.dnext + 1) % self.NDMA
        if self.dval[i] > 0:
            self._wait(q, ("d", i, self.dval[i]))
        ins = self.eng[q].dma_start(out=out, in_=in_, **kw)
        self.dval[i] += 16
        ins.then_inc(self.dsem[i], 16)
        token = ("d", i, self.dval[i])
        self._record(token, reads, writes)
        self.ninstr += 1
        return token

    def barrier(self):
        for e in self.eng:
            for f in self.eng:
                if f != e and self.cnt[f] > 0:
                    self._wait(e, ("e", f, self.cnt[f]))
            for i in range(self.NDMA):
                if self.dval[i] > 0:
                    self._wait(e, ("d", i, self.dval[i]))
        self.res = {}

    def finish(self):
        for i in range(self.NDMA):
            if self.dval[i] > 0:
                self._wait("sp", ("d", i, self.dval[i]))
        for f in self.eng:
            if f != "sp" and self.cnt[f] > 0:
                self._wait("sp", ("e", f, self.cnt[f]))


def build_program(nc, NSEQ=4, S=2048, taps=None, stop=None):
    NT = S // 128
    NTT = NSEQ * NT
    NTOK = NSEQ * S
    TOPK = min(256, S // 4)
    KB = TOPK // 128
    taps = taps or {}

    def din(name, shape, dt=F32):
        return nc.dram_tensor(name, list(shape), dt, kind="ExternalInput").ap()

    x_d = din("x", [NTOK, D])
    cT_d = din("cT", [128, 8, NSEQ])
    pos_d = din("pos", [128, NTT], I32)
    wada_d = din("w_ada", [D, 6 * D])
    bada_d = din("b_ada", [1, 6 * D])
    win_d = din("w_in", [D, DIN])
    gpre_d = din("gpre", [128, 8])
    gpre2_d = din("gpre2", [128, 8])
    gv_d = din("gv", [1, 512])
    ws_d = din("ws", [128, 4, 128])
    bs_d = din("bs", [128, 4])
    goa_d = din("goa", [1, 512])
    gob_d = din("gob", [1, 512])
    wout_d = din("w_out", [D, D])
    gpost_d = din("gpost", [1, D])
    w1_d = din("w1", [D, DFF])
    w2_d = din("w2", [DFF, D])
    gpost2_d = din("gpost2", [1, D])
    out_d = nc.dram_tensor("out", [NTOK, D], F32, kind="ExternalOutput").ap()
    x1s_d = out_d
    tap_d = {k: nc.dram_tensor("tap_" + k, list(shp), F32, kind="ExternalOutput").ap()
             for k, shp in taps.items()}

    with contextlib.ExitStack() as gst:
        P = Prog(nc, gst)

        uid = [0]

        def mk_sb(stack):
            def sb(name, shape, dt=F32):
                uid[0] += 1
                return stack.enter_context(nc.sbuf_tensor("s%d_%s" % (uid[0], name), list(shape), dt))
            return sb

        gsb = mk_sb(gst)
        banks = [gst.enter_context(nc.psum_tensor("bank%d" % i, [128, 512], F32)) for i in range(8)]
        bkey = ["b%d" % i for i in range(8)]
        bbf = [b[:].bitcast(BF16) for b in banks]

        def V(fn, r=(), w=()):
            return P.op("dve", fn, r, w)

        def A(fn, r=(), w=()):
            return P.op("act", fn, r, w)

        def G(fn, r=(), w=()):
            return P.op("pool", fn, r, w)

        def T(fn, r=(), w=()):
            return P.op("pe", fn, r, w)

        ident = gsb("ident", [128, 128])
        identb = gsb("identb", [128, 128], BF16)
        ones_f = gsb("ones_f", [128, 128])
        zeros_f = gsb("zeros_f", [128, 128])
        NEGM = gsb("NEGM", [128, 128])
        TRIU = gsb("TRIU", [128, 128], BF16)
        ONESB = gsb("ONESB", [128, 128], BF16)
        D32 = gsb("D32", [128, 32])
        G4 = gsb("G4", [128, 4])
        zerob = gsb("zerob", [128, 260], BF16)
        P2 = gsb("P2", [128, NIT + 1])
        mhalf = gsb("mhalf", [128, 16])
        iot = gsb("iot", [128, 8], I32)
        iof = gsb("iof", [128, 8])
        invf = gsb("invf", [128, 8])
        rs_tmp = gsb("rs_tmp", [128, 16])

        G(lambda e: e.memset(ones_f[:], 1.0), w=["ones_f"])
        G(lambda e: e.memset(zeros_f[:], 0.0), w=["zeros_f"])
        G(lambda e: e.affine_select(out=ident[:], in_=ones_f[:], pattern=[[-1, 128]], compare_op=ALU.is_equal,
                                    fill=0.0, base=0, channel_multiplier=1), r=["ones_f"], w=["ident"])
        V(lambda e: e.tensor_copy(out=identb[:], in_=ident[:]), r=["ident"], w=["identb"])
        G(lambda e: e.affine_select(out=NEGM[:], in_=zeros_f[:], pattern=[[-1, 128]], compare_op=ALU.is_ge,
                                    fill=-1.0e30, base=0, channel_multiplier=1), r=["zeros_f"], w=["NEGM"])
        G(lambda e: e.affine_select(out=TRIU[:], in_=ones_f[:], pattern=[[1, 128]], compare_op=ALU.is_ge,
                                    fill=0.0, base=0, channel_multiplier=-1), r=["ones_f"], w=["TRIU"])
        V(lambda e: e.tensor_copy(out=ONESB[:], in_=ones_f[:]), r=["ones_f"], w=["ONESB"])
        for m in range(4):
            G(lambda e, m=m: e.affine_select(out=D32[32 * m:32 * m + 32, :], in_=ones_f[32 * m:32 * m + 32, 0:32],
                                             pattern=[[-1, 32]], compare_op=ALU.is_equal, fill=0.0, base=0,
                                             channel_multiplier=1), r=["ones_f"], w=["D32"])
        G(lambda e: e.memset(G4[:], 0.0), w=["G4"])
        for g in range(4):
            G(lambda e, g=g: e.memset(G4[32 * g:32 * g + 32, g:g + 1], 1.0), r=["G4"], w=["G4"])
        G(lambda e: e.memset(zerob[:], 0.0), w=["zerob"])
        for i in range(NIT + 1):
            G(lambda e, i=i: e.memset(P2[:, i:i + 1], 2.0 ** -(i + 1)), w=["P2"])
        G(lambda e: e.memset(mhalf[:], -0.5), w=["mhalf"])
        G(lambda e: e.iota(iot[:], pattern=[[1, 8]], base=0, channel_multiplier=0), w=["iot"])
        V(lambda e: e.tensor_copy(out=iof[:], in_=iot[:]), r=["iot"], w=["iof"])
        A(lambda e: e.activation(out=invf[:], in_=iof[:], func=AF.Exp, scale=-math.log(500000.0) / 8.0),
          r=["iof"], w=["invf"])

        def rstd(out_ap, ss_ap, n, inv_n, rk, wk, ss2_ap=None):
            tmp = rs_tmp[:, 0:n]
            if ss2_ap is not None:
                G(lambda e: e.tensor_tensor(out=tmp, in0=ss_ap, in1=ss2_ap, op=ALU.add), r=rk, w=["rs_tmp"])
                G(lambda e: e.tensor_scalar(out=tmp, in0=tmp, scalar1=inv_n, scalar2=EPS, op0=ALU.mult,
                                            op1=ALU.add), r=["rs_tmp"], w=["rs_tmp"])
            else:
                G(lambda e: e.tensor_scalar(out=tmp, in0=ss_ap, scalar1=inv_n, scalar2=EPS, op0=ALU.mult,
                                            op1=ALU.add), r=rk, w=["rs_tmp"])
            G(lambda e: e.tensor_tensor(out=out_ap, in0=tmp, in1=mhalf[:, 0:n], op=ALU.pow),
              r=["rs_tmp", "mhalf"], w=wk)

        def ck(name):
            if stop == name:
                P.finish()
                raise StopBuild()

        def tap(name, ap, rk, rows=None):
            if name in tap_d:
                dst = tap_d[name]
                P.dma(dst if rows is None else dst[rows], ap, reads=rk)

        S1T = gsb("S1T", [128, 8, NSEQ])
        sh1T = gsb("sh1T", [128, 8, NSEQ])
        S2T = gsb("S2T", [128, 8, NSEQ])
        sh2T = gsb("sh2T", [128, 8, NSEQ])
        gmod = gsb("gmod", [NSEQ, 2, D])
        sel = gsb("sel", [NSEQ, NSEQ, 128])
        gpre = gsb("gpre", [128, 8])
        gpre2 = gsb("gpre2", [128, 8])
        P.dma(gpre[:], gpre_d[:, :], writes=["gpre"])
        P.dma(gpre2[:], gpre2_d[:, :], writes=["gpre2"])

        G(lambda e: e.affine_select(out=sel[:], in_=ones_f[0:NSEQ, :].unsqueeze(1).broadcast_to([NSEQ, NSEQ, 128]),
                                    pattern=[[-1, NSEQ], [0, 128]], compare_op=ALU.is_equal, fill=0.0, base=0,
                                    channel_multiplier=1), r=["ones_f"], w=["sel"])

        with contextlib.ExitStack() as ast:
            asb = mk_sb(ast)
            Win = asb("Win", [128, 8, DIN], BF16)
            Wout = asb("Wout", [128, 8, D], BF16)
            cs = asb("cs", [128, NTT, 8])
            sn = asb("sn", [128, NTT, 8])
            gv_row = asb("gv_row", [128, 512])
            goa_row = asb("goa_row", [128, 512])
            gob_row = asb("gob_row", [128, 512])
            bcol = asb("bcol", [128, 4])
            WmT = asb("WmT", [128, 4, 128], BF16)
            P.dma(gv_row[:], gv_d[0:1, :].partition_broadcast(128), writes=["gv_row"])
            P.dma(goa_row[:], goa_d[0:1, :].partition_broadcast(128), writes=["goa_row"])
            P.dma(gob_row[:], gob_d[0:1, :].partition_broadcast(128), writes=["gob_row"])
            P.dma(bcol[:], bs_d[:, :], writes=["bcol"])

            with contextlib.ExitStack() as sst:
                ssb = mk_sb(sst)
                cTs = ssb("cTs", [128, 8, NSEQ])
                scs = ssb("scs", [128, 8, NSEQ])
                bada4 = ssb("bada4", [NSEQ, 6 * D])
                modrow = ssb("modrow", [NSEQ, 6 * D])
                gpost4 = ssb("gpost4", [NSEQ, 2, D])
                wada_st = [ssb("wada_st%d" % i, [128, 8, 512]) for i in range(2)]
                win_st = [ssb("win_st%d" % i, [128, DIN]) for i in range(2)]
                ws_sb = ssb("ws_sb", [128, 4, 128])
                wsm = ssb("wsm", [128, 4, 128])
                posi = ssb("posi", [128, NTT], I32)
                posf = ssb("posf", [128, NTT])
                ang = ssb("ang", [128, NTT * 8])
                angk = ssb("angk", [128, NTT * 8], I32)
                angf = ssb("angf", [128, NTT * 8])
                angm = ssb("angm", [128, NTT * 8])
                ang2 = ssb("ang2", [128, NTT * 8])

                P.dma(cTs[:], cT_d[:, :, :], writes=["cTs"])
                P.dma(bada4[:], bada_d[0:1, :].partition_broadcast(NSEQ), writes=["bada4"])
                P.dma(gpost4[:, 0, :], gpost_d[0:1, :].partition_broadcast(NSEQ), writes=["gpost4a"])
                P.dma(gpost4[:, 1, :], gpost2_d[0:1, :].partition_broadcast(NSEQ), writes=["gpost4b"])
                P.dma(posi[:], pos_d[:, :], writes=["posi"])
                P.dma(ws_sb[:], ws_d[:, :, :], writes=["ws_sb"])
                A(lambda e: e.activation(out=scs[:], in_=cTs[:], func=AF.Silu), r=["cTs"], w=["scs"])

                order = [2, 3, 0, 1] + list(range(4, 12))
                for n_, cb in enumerate(order):
                    st_ = wada_st[n_ % 2]
                    sk = "wada_st%d" % (n_ % 2)
                    P.dma(st_[:], wada_d[:, cb * 512:(cb + 1) * 512].rearrange("(k p) n -> p k n", p=128),
                          writes=[sk])
                    bk = n_ % 2
                    for k in range(8):
                        T(lambda e, k=k, st_=st_, bk=bk: e.matmul(banks[bk][0:NSEQ, :], lhsT=scs[:, k, :],
                                                                  rhs=st_[:, k, :], start=(k == 0), stop=(k == 7)),
                          r=["scs", sk], w=[bkey[bk]])
                    V(lambda e, bk=bk, cb=cb: e.tensor_tensor(out=modrow[:, cb * 512:(cb + 1) * 512],
                                                              in0=banks[bk][0:NSEQ, :],
                                                              in1=bada4[:, cb * 512:(cb + 1) * 512], op=ALU.add),
                      r=[bkey[bk], "bada4"], w=["modrow%d" % cb])
                allmod = ["modrow%d" % cb for cb in range(12)]
                for si, sp_ in enumerate([0, 1, 3, 4]):
                    for k in range(8):
                        c0 = (si * 8 + k) * NSEQ
                        T(lambda e, sp_=sp_, k=k, c0=c0: e.transpose(
                            out=banks[2][:, c0:c0 + NSEQ], in_=modrow[0:NSEQ, sp_ * D + k * 128:sp_ * D + (k + 1) * 128],
                            identity=ident[0:NSEQ, 0:NSEQ]), r=allmod + ["ident"], w=[bkey[2]])

                def mview(si):
                    return banks[2][:, si * 8 * NSEQ:(si + 1) * 8 * NSEQ].rearrange("p (k b) -> p k b", b=NSEQ)

                V(lambda e: e.tensor_copy(out=sh1T[:], in_=mview(0)), r=[bkey[2]], w=["sh1T"])
                V(lambda e: e.scalar_tensor_tensor(out=S1T[:], in0=mview(1), scalar=1.0,
                                                   in1=gpre[:].unsqueeze(2).broadcast_to([128, 8, NSEQ]),
                                                   op0=ALU.add, op1=ALU.mult), r=[bkey[2], "gpre"], w=["S1T"])
                V(lambda e: e.tensor_copy(out=sh2T[:], in_=mview(2)), r=[bkey[2]], w=["sh2T"])
                V(lambda e: e.scalar_tensor_tensor(out=S2T[:], in0=mview(3), scalar=1.0,
                                                   in1=gpre2[:].unsqueeze(2).broadcast_to([128, 8, NSEQ]),
                                                   op0=ALU.add, op1=ALU.mult), r=[bkey[2], "gpre2"], w=["S2T"])
                V(lambda e: e.tensor_tensor(out=gmod[:, 0, :], in0=modrow[:, 2 * D:3 * D], in1=gpost4[:, 0, :],
                                            op=ALU.mult), r=allmod + ["gpost4a"], w=["gmod0"])
                V(lambda e: e.tensor_tensor(out=gmod[:, 1, :], in0=modrow[:, 5 * D:6 * D], in1=gpost4[:, 1, :],
                                            op=ALU.mult), r=allmod + ["gpost4b"], w=["gmod1"])

                cast_engs = ["dve", "act", "pool"]
                ci = 0
                for k in range(8):
                    st_ = win_st[k % 2]
                    sk = "win_st%d" % (k % 2)
                    P.dma(st_[:], win_d[k * 128:(k + 1) * 128, :], writes=[sk])
                    for h0, h1 in ((0, 1188), (1188, DIN)):
                        eng = cast_engs[ci % 3]
                        ci += 1
                        if eng == "act":
                            A(lambda e, k=k, st_=st_, h0=h0, h1=h1: e.activation(out=Win[:, k, h0:h1], in_=st_[:, h0:h1],
                                                                              func=AF.Copy), r=[sk], w=["Win"])
                        else:
                            P.op(eng, lambda e, k=k, st_=st_, h0=h0, h1=h1: e.tensor_copy(out=Win[:, k, h0:h1],
                                                                                       in_=st_[:, h0:h1]),
                                 [sk], ["Win"])
                for k in range(8):
                    st_ = win_st[k % 2]
                    sk = "win_st%d" % (k % 2)
                    P.dma(st_[:, 0:D], wout_d[k * 128:(k + 1) * 128, :], writes=[sk])
                    eng = cast_engs[ci % 3]
                    ci += 1
                    if eng == "act":
                        A(lambda e, k=k, st_=st_: e.activation(out=Wout[:, k, :], in_=st_[:, 0:D], func=AF.Copy),
                          r=[sk], w=["Wout"])
                    else:
                        P.op(eng, lambda e, k=k, st_=st_: e.tensor_copy(out=Wout[:, k, :], in_=st_[:, 0:D]),
                             [sk], ["Wout"])
                for g in range(4):
                    G(lambda e, g=g: e.affine_select(out=wsm[:, g, :], in_=ws_sb[:, g, :], pattern=[[-1, 128]],
                                                     compare_op=ALU.is_ge, fill=0.0, base=0, channel_multiplier=1),
                      r=["ws_sb"], w=["wsm"])
                for g in range(4):
                    T(lambda e, g=g: e.transpose(out=banks[3][:, g * 128:(g + 1) * 128], in_=wsm[:, g, :],
                                                 identity=ident[:]), r=["wsm", "ident"], w=[bkey[3]])
                V(lambda e: e.tensor_copy(out=WmT[:], in_=banks[3][:, :].rearrange("p (g t) -> p g t", g=4)),
                  r=[bkey[3]], w=["WmT"])

                NA = NTT * 8
                V(lambda e: e.tensor_copy(out=posf[:], in_=posi[:]), r=["posi"], w=["posf"])
                V(lambda e: e.tensor_tensor(out=ang[:].rearrange("p (t f) -> p t f", f=8),
                                            in0=posf[:].unsqueeze(2).broadcast_to([128, NTT, 8]),
                                            in1=invf[:].unsqueeze(1).broadcast_to([128, NTT, 8]), op=ALU.mult),
                  r=["posf", "invf"], w=["ang"])

                def reduce_sin(dst, src_key, shift):
                    V(lambda e: e.tensor_scalar(out=ang2[:], in0=ang[:], scalar1=shift, scalar2=None, op0=ALU.add),
                      r=["ang"], w=["ang2"])
                    V(lambda e: e.tensor_scalar(out=angk[:], in0=ang2[:], scalar1=1.0 / TWO_PI, scalar2=None,
                                                op0=ALU.mult), r=["ang2"], w=["angk"])
                    V(lambda e: e.tensor_copy(out=angf[:], in_=angk[:]), r=["angk"], w=["angf"])
                    V(lambda e: e.scalar_tensor_tensor(out=ang2[:], in0=angf[:], scalar=-TWO_PI, in1=ang2[:],
                                                       op0=ALU.mult, op1=ALU.add), r=["angf", "ang2"], w=["ang2"])
                    V(lambda e: e.tensor_scalar(out=angm[:], in0=ang2[:], scalar1=math.pi, scalar2=-TWO_PI,
                                                op0=ALU.is_gt, op1=ALU.mult), r=["ang2"], w=["angm"])
                    V(lambda e: e.tensor_tensor(out=ang2[:], in0=ang2[:], in1=angm[:], op=ALU.add),
                      r=["ang2", "angm"], w=["ang2"])
                    V(lambda e: e.tensor_scalar(out=angm[:], in0=ang2[:], scalar1=-math.pi, scalar2=TWO_PI,
                                                op0=ALU.is_lt, op1=ALU.mult), r=["ang2"], w=["angm"])
                    V(lambda e: e.tensor_tensor(out=ang2[:], in0=ang2[:], in1=angm[:], op=ALU.add),
                      r=["ang2", "angm"], w=["ang2"])
                    V(lambda e: e.tensor_scalar(out=ang2[:], in0=ang2[:], scalar1=-3.1415925, scalar2=3.1415925,
                                                op0=ALU.max, op1=ALU.min), r=["ang2"], w=["ang2"])
                    A(lambda e: e.activation(out=dst[:].rearrange("p t f -> p (t f)"), in_=ang2[:], func=AF.Sin),
                      r=["ang2"], w=[src_key])

                reduce_sin(sn, "sn", 0.0)
                reduce_sin(cs, "cs", math.pi / 2.0)
                P.barrier()

            if stop == "setup":
                P.finish()
                return
            tap("S1T", S1T[:].rearrange("p k b -> p (k b)"), ["S1T"])
            tap("gmod", gmod[:].rearrange("b g d -> b (g d)"), ["gmod0", "gmod1"])
            tap("cs", cs[:].rearrange("p t f -> p (t f)"), ["cs"])
            tap("sn", sn[:].rearrange("p t f -> p (t f)"), ["sn"])

            qT2 = asb("qT2", [128, 4, S], BF16)
            kTz = [asb("kTz%d" % i, [128, 2, S], BF16) for i in range(2)]
            kiTz = [asb("kiTz%d" % i, [128, S], BF16) for i in range(2)]
            qiT2 = asb("qiT2", [128, S // 32, 4, 32], BF16)
            v_aug = asb("v_aug", [128, NT, 2, 65], BF16)
            w_tok = asb("w_tok", [128, NT, 8])
            mTa = asb("mTa", [128, 4, S], BF16)
            G1row = asb("G1row", [128, D])
            junkA = asb("junkA", [128, D], BF16)
            G(lambda e: e.memset(v_aug[:].rearrange("p a b c -> p (a b) c")[:, :, 64:65], 1.0), w=["v_aug"])
            G(lambda e: e.memset(kTz[0][64:128, :, :], 0.0), w=["kTd"])
            G(lambda e: e.memset(kTz[1][0:64, :, :], 0.0), w=["kTd"])
            G(lambda e: e.memset(kiTz[0][64:128, :], 0.0), w=["kiTd"])
            G(lambda e: e.memset(kiTz[1][0:64, :], 0.0), w=["kiTd"])

            for b in range(NSEQ):
                for n in range(2):
                    T(lambda e, n=n: e.matmul(banks[n][:, :], lhsT=sel[0:NSEQ, b, :],
                                              rhs=gmod[0:NSEQ, 0, n * 512:(n + 1) * 512], start=True, stop=True),
                      r=["sel", "gmod0"], w=[bkey[n]])
                    V(lambda e, n=n: e.tensor_copy(out=G1row[:, n * 512:(n + 1) * 512], in_=banks[n][:, :]),
                      r=[bkey[n]], w=["G1row"])

                with contextlib.ExitStack() as pst:
                    psb = mk_sb(pst)
                    xt = [psb("xt%d" % i, [128, D]) for i in range(2)]
                    xn = [psb("xn%d" % i, [128, D]) for i in range(2)]
                    hT = [psb("hT%d" % i, [128, 8, 128], BF16) for i in range(2)]
                    ssx = psb("ssx", [128, 2])
                    rsx = psb("rsx", [128, 2])
                    zu = [psb("zu%d" % i, [128, 512], BF16) for i in range(2)]
                    zv = psb("zv", [128, 512], BF16)
                    vn = [psb("vn%d" % i, [128, 512], BF16) for i in range(2)]
                    ssv = psb("ssv", [128, 4])
                    rsv = psb("rsv", [128, 4])
                    ya = psb("ya", [128, 512])
                    ssa = psb("ssa", [128, 1])
                    rsa = psb("rsa", [128, 1])
                    ma = psb("ma", [128, 512], BF16)
                    q_tok = [psb("q_tok%d" % i, [128, 8, 64], BF16) for i in range(2)]
                    qi_tok = [psb("qi_tok%d" % i, [128, 8, 64], BF16) for i in range(2)]
                    kd = [psb("kd%d" % i, [128, 2, 2, 64], BF16) for i in range(2)]
                    kid = [psb("kid%d" % i, [128, 2, 64], BF16) for i in range(2)]
                    rt = [psb("rt%d" % i, [128, 8, 8]) for i in range(4)]

                    def s1_load(i):
                        it = b * NT + i
                        P.dma(xt[i % 2][:], x_d[it * 128:(it + 1) * 128, :], writes=["xt%d" % (i % 2)])

                    def s1_pre(i):
                        j = i % 2
                        A(lambda e: e.activation(out=junkA[:], in_=xt[j][:], func=AF.Square,
                                                 accum_out=ssx[:, j:j + 1]), r=["xt%d" % j], w=["junkA", "ssx%d" % j])
                        rstd(rsx[:, j:j + 1], ssx[:, j:j + 1], 1, 1.0 / D, ["ssx%d" % j], ["rsx%d" % j])
                        V(lambda e: e.tensor_scalar(out=xn[j][:], in0=xt[j][:], scalar1=rsx[:, j:j + 1], scalar2=None,
                                                    op0=ALU.mult), r=["xt%d" % j, "rsx%d" % j], w=["xn%d" % j])

                    def s1_post(i):
                        j = i % 2
                        for k in range(8):
                            T(lambda e, k=k: e.transpose(out=banks[k // 4][:, (k % 4) * 128:(k % 4 + 1) * 128],
                                                         in_=xn[j][:, k * 128:(k + 1) * 128], identity=ident[:]),
                              r=["xn%d" % j, "ident"], w=[bkey[k // 4]])
                        for k in range(8):
                            A(lambda e, k=k: e.activation(out=hT[j][:, k, :],
                                                          in_=banks[k // 4][:, (k % 4) * 128:(k % 4 + 1) * 128],
                                                          func=AF.Identity, scale=S1T[:, k, b:b + 1],
                                                          bias=sh1T[:, k, b:b + 1]),
                              r=[bkey[k // 4], "S1T", "sh1T"], w=["hT%d" % j])

                    GROUPS = [(0, 512, 2), (512, 512, 3), (1024, 512, 4), (1536, 328, 5), (1864, 512, 6)]

                    def rope(src3, src_key, dst3, dst_key, H, it):
                        c = cs[:, it, :].unsqueeze(1).broadcast_to([128, H, 8])
                        s_ = sn[:, it, :].unsqueeze(1).broadcast_to([128, H, 8])
                        x1 = src3[:, :, 0:8]
                        x2 = src3[:, :, 8:16]
                        t = [r_[:, 0:H, :] for r_ in rt]
                        V(lambda e: e.tensor_tensor(out=t[0], in0=x1, in1=c, op=ALU.mult), r=[src_key, "cs"], w=["rt0"])
                        V(lambda e: e.tensor_tensor(out=t[1], in0=x2, in1=s_, op=ALU.mult), r=[src_key, "sn"], w=["rt1"])
                        V(lambda e: e.tensor_tensor(out=dst3[:, :, 0:8], in0=t[0], in1=t[1], op=ALU.subtract),
                          r=["rt0", "rt1"], w=[dst_key])
                        V(lambda e: e.tensor_tensor(out=t[2], in0=x2, in1=c, op=ALU.mult), r=[src_key, "cs"], w=["rt2"])
                        V(lambda e: e.tensor_tensor(out=t[3], in0=x1, in1=s_, op=ALU.mult), r=[src_key, "sn"], w=["rt3"])
                        V(lambda e: e.tensor_tensor(out=dst3[:, :, 8:16], in0=t[2], in1=t[3], op=ALU.add),
                          r=["rt2", "rt3"], w=[dst_key])
                        A(lambda e: e.activation(out=dst3[:, :, 16:64], in_=src3[:, :, 16:64], func=AF.Copy),
                          r=[src_key], w=[dst_key])

                    def s2(i):
                        j = i % 2
                        it = b * NT + i
                        for (c0, n, bk) in GROUPS:
                            for k in range(8):
                                T(lambda e, k=k, c0=c0, n=n, bk=bk: e.matmul(banks[bk][:, 0:n], lhsT=hT[j][:, k, :],
                                                                             rhs=Win[:, k, c0:c0 + n], start=(k == 0),
                                                                             stop=(k == 7)),
                                  r=["hT%d" % j, "Win"], w=[bkey[bk]])
                        A(lambda e: e.activation(out=zu[j][:], in_=banks[2][:, :], func=AF.Gelu_apprx_tanh),
                          r=[bkey[2]], w=["zu%d" % j])
                        A(lambda e: e.activation(out=zv[:], in_=banks[3][:, :], func=AF.Gelu_apprx_tanh),
                          r=[bkey[3]], w=["zv"])
                        rope(banks[4][:, :].rearrange("p (h d) -> p h d", d=64), bkey[4], q_tok[j][:], "q_tok%d" % j, 8, it)
                        rope(banks[5][:, 0:128].rearrange("p (h d) -> p h d", d=64), bkey[5], kd[j][:, :, 0, :],
                             "kd%d" % j, 2, it)
                        V(lambda e: e.tensor_copy(out=kd[j][:, :, 1, :], in_=kd[j][:, :, 0, :]), r=["kd%d" % j],
                          w=["kd%d" % j])
                        V(lambda e: e.tensor_copy(out=v_aug[:, i, :, 0:64],
                                                  in_=banks[5][:, 128:256].rearrange("p (h d) -> p h d", d=64)),
                          r=[bkey[5]], w=["v_aug"])
                        rope(banks[5][:, 256:320].rearrange("p (h d) -> p h d", d=64), bkey[5], kid[j][:, 0:1, :],
                             "kid%d" % j, 1, it)
                        V(lambda e: e.tensor_copy(out=kid[j][:, 1:2, :], in_=kid[j][:, 0:1, :]), r=["kid%d" % j],
                          w=["kid%d" % j])
                        V(lambda e: e.tensor_copy(out=w_tok[:, i, :], in_=banks[5][:, 320:328]), r=[bkey[5]],
                          w=["w_tok"])
                        rope(banks[6][:, :].rearrange("p (h d) -> p h d", d=64), bkey[6], qi_tok[j][:], "qi_tok%d" % j, 8, it)
                        for g in range(4):
                            A(lambda e, g=g: e.activation(out=junkA[:, 0:128], in_=zv[:, g * 128:(g + 1) * 128],
                                                          func=AF.Square, accum_out=ssv[:, g:g + 1]),
                              r=["zv"], w=["junkA", "ssv"])
                        rstd(rsv[:, 0:4], ssv[:, 0:4], 4, 1.0 / 128, ["ssv"], ["rsv"])
                        for g in range(4):
                            V(lambda e, g=g: e.scalar_tensor_tensor(out=vn[j][:, g * 128:(g + 1) * 128],
                                                                    in0=zv[:, g * 128:(g + 1) * 128],
                                                                    scalar=rsv[:, g:g + 1],
                                                                    in1=gv_row[:, g * 128:(g + 1) * 128],
                                                                    op0=ALU.mult, op1=ALU.mult),
                              r=["zv", "rsv", "gv_row"], w=["vn%d" % j])

                    def s3a(i):
                        j = i % 2
                        ts = slice(i * 128, (i + 1) * 128)
                        qf = q_tok[j][:].rearrange("p h d -> p (h d)")
                        for c in range(4):
                            T(lambda e, c=c: e.transpose(out=bbf[0][:, c * 128:(c + 1) * 128],
                                                         in_=qf[:, c * 128:(c + 1) * 128], identity=identb[:]),
                              r=["q_tok%d" % j, "identb"], w=[bkey[0]])
                        for kv in range(2):
                            T(lambda e, kv=kv: e.transpose(out=bbf[0][:, 512 + kv * 128:512 + (kv + 1) * 128],
                                                           in_=kd[j][:, kv, :, :].rearrange("p a d -> p (a d)"),
                                                           identity=identb[:]),
                              r=["kd%d" % j, "identb"], w=[bkey[0]])
                        T(lambda e: e.transpose(out=bbf[0][:, 768:896], in_=kid[j][:].rearrange("p a d -> p (a d)"),
                                                identity=identb[:]), r=["kid%d" % j, "identb"], w=[bkey[0]])
                        V(lambda e: e.tensor_copy(out=qT2[:, :, ts],
                                                  in_=bbf[0][:, 0:512].rearrange("p (c t) -> p c t", t=128)),
                          r=[bkey[0]], w=["qT2"])
                        for par in range(2):
                            ps = slice(64 * par, 64 * par + 64)
                            V(lambda e, par=par, ps=ps: e.tensor_copy(
                                out=kTz[par][ps, :, ts],
                                in_=bbf[0][ps, 512:768].rearrange("p (c t) -> p c t", t=128)),
                              r=[bkey[0]], w=["kTd"])
                            V(lambda e, par=par, ps=ps: e.tensor_copy(out=kiTz[par][ps, ts], in_=bbf[0][ps, 768:896]),
                              r=[bkey[0]], w=["kiTd"])
                        qif = qi_tok[j][:].rearrange("p h d -> p (h d)")
                        for c in range(4):
                            T(lambda e, c=c: e.transpose(out=bbf[1][:, c * 128:(c + 1) * 128],
                                                         in_=qif[:, c * 128:(c + 1) * 128], identity=identb[:]),
                              r=["qi_tok%d" % j, "identb"], w=[bkey[1]])
                        A(lambda e: e.activation(out=qiT2[:, i * 4:(i + 1) * 4, :, :],
                                                 in_=bbf[1][:, 0:512].rearrange("p (c g t) -> p g c t", c=4, g=4),
                                                 func=AF.Copy), r=[bkey[1]], w=["qiT2"])
                        for g in range(4):
                            T(lambda e, g=g: e.matmul(banks[7][:, g * 128:(g + 1) * 128], lhsT=WmT[:, g, :],
                                                      rhs=vn[j][:, g * 128:(g + 1) * 128], start=True, stop=True),
                              r=["WmT", "vn%d" % j], w=[bkey[7]])
                        for g in range(4):
                            V(lambda e, g=g: e.scalar_tensor_tensor(out=ya[:, g * 128:(g + 1) * 128],
                                                                    in0=banks[7][:, g * 128:(g + 1) * 128],
                                                                    scalar=bcol[:, g:g + 1],
                                                                    in1=zu[j][:, g * 128:(g + 1) * 128],
                                                                    op0=ALU.add, op1=ALU.mult),
                              r=[bkey[7], "bcol", "zu%d" % j], w=["ya"])
                        A(lambda e: e.activation(out=junkA[:, 0:512], in_=ya[:], func=AF.Square, accum_out=ssa[:]),
                          r=["ya"], w=["junkA", "ssa"])
                        rstd(rsa[:], ssa[:], 1, 1.0 / 512, ["ssa"], ["rsa"])
                        V(lambda e: e.scalar_tensor_tensor(out=ma[:], in0=ya[:], scalar=rsa[:, 0:1], in1=goa_row[:],
                                                           op0=ALU.mult, op1=ALU.mult),
                          r=["ya", "rsa", "goa_row"], w=["ma"])
                        if b == 0 and i == 0:
                            tap("ya", ya[:], ["ya"])

                    def s3b(i):
                        ts = slice(i * 128, (i + 1) * 128)
                        for c in range(4):
                            T(lambda e, c=c: e.transpose(out=bbf[7][:, c * 128:(c + 1) * 128],
                                                         in_=ma[:, c * 128:(c + 1) * 128], identity=identb[:]),
                              r=["ma", "identb"], w=[bkey[7]])
                        A(lambda e: e.activation(out=mTa[:, :, ts],
                                                 in_=bbf[7][:, 0:512].rearrange("p (c t) -> p c t", t=128),
                                                 func=AF.Copy), r=[bkey[7]], w=["mTa"])

                    s1_load(0)
                    if NT > 1:
                        s1_load(1)
                    s1_pre(0)
                    s1_post(0)
                    if NT > 2:
                        s1_load(2)
                    if NT > 1:
                        s1_pre(1)
                        s1_post(1)
                    for n in range(NT + 2):
                        if n + 2 < NT:
                            s1_pre(n + 2)
                            if n + 3 < NT:
                                s1_load(n + 3)
                        if n < NT:
                            s2(n)
                        if 0 <= n - 2 < NT:
                            s3b(n - 2)
                        if 0 <= n - 1 < NT:
                            s3a(n - 1)
                        if n + 2 < NT:
                            s1_post(n + 2)
                    P.barrier()
                    if stop == "proj":
                        P.finish()
                        return

                with contextlib.ExitStack() as tst:
                    tsb = mk_sb(tst)
                    score = tsb("score", [128, S])
                    cmax = tsb("cmax", [128, 4])
                    mask = tsb("mask", [128, S], BF16)
                    maskT = tsb("maskT", [128, NT, 128], BF16)
                    rl = [tsb("rl%d" % i, [128, 512], BF16) for i in range(4)]
                    pT = [tsb("pT%d" % i, [128, 512], BF16) for i in range(4)]
                    Wsel = [tsb("Wsel%d" % i, [128, 8, 128], BF16) for i in range(2)]
                    wrep = tsb("wrep", [128, 2, 128])
                    wcol = tsb("wcol", [128, 8])
                    lo0 = tsb("lo0", [128, 1])
                    hi0 = tsb("hi0", [128, 1])
                    w0 = tsb("w0", [128, 1])
                    wh = tsb("wh", [128, NIT + 1])
                    cbias = tsb("cbias", [128, NT])
                    mid = tsb("mid", [128, 1])
                    cnt = tsb("cnt", [128, 1])
                    btmp = tsb("btmp", [128, 1])
                    rden = tsb("rden", [128, 8])
                    yb = tsb("yb", [128, 512])
                    ssb_ = tsb("ssb_", [128, 1])
                    rsb = tsb("rsb", [128, 1])
                    mb = tsb("mb", [128, 512], BF16)
                    mbT = tsb("mbT", [128, 4, 128], BF16)
                    sso = tsb("sso", [128, 2])
                    rso = tsb("rso", [128, 1])
                    ot = tsb("ot", [128, D])
                    xres = [tsb("xres%d" % i, [128, D]) for i in range(2)]
                    for i in range(2):
                        G(lambda e, i=i: e.memset(Wsel[i][:], 0.0), w=["Wsel%d" % i])
                    for qq in range(NT):
                        G(lambda e, qq=qq: e.memset(cbias[:, qq:qq + 1], float((qq + 1) * 128 - 2 * TOPK) + 0.5),
                          w=["cbias"])
                    rl_i = [0]
                    pT_i = [0]
                    D_i = [0]

                    def wsel_build(qb):
                        wi = qb % 2
                        wk = "Wsel%d" % wi
                        w2v = w_tok[:, qb, :].rearrange("p (i two) -> p i two", two=2)
                        for par in range(2):
                            V(lambda e, par=par: e.tensor_tensor(
                                out=wrep[:, par, :].rearrange("p (i t) -> p i t", t=32),
                                in0=w2v[:, :, par].unsqueeze(2).broadcast_to([128, 4, 32]),
                                in1=D32[:].unsqueeze(1).broadcast_to([128, 4, 32]), op=ALU.mult),
                              r=["w_tok", "D32"], w=["wrep"])
                        for par in range(2):
                            T(lambda e, par=par: e.matmul(banks[0][:, par * 4:(par + 1) * 4], lhsT=wrep[:, par, :],
                                                          rhs=G4[:], start=True, stop=True),
                              r=["wrep", "G4"], w=[bkey[0]])
                        V(lambda e: e.tensor_scalar(out=wcol[:], in0=banks[0][:, 0:8], scalar1=IDX_SCALE, scalar2=None,
                                                    op0=ALU.mult), r=[bkey[0]], w=["wcol"])
                        for par in range(2):
                            for g in range(4):
                                V(lambda e, par=par, g=g: e.tensor_scalar(
                                    out=Wsel[wi][:, par * 4 + g, 32 * g:32 * g + 32], in0=D32[:],
                                    scalar1=wcol[:, par * 4 + g:par * 4 + g + 1], scalar2=None, op0=ALU.mult),
                                  r=["D32", "wcol"], w=[wk])

                    def indexer(qb):
                        N = (qb + 1) * 128
                        nch = (N + 511) // 512
                        wi = qb % 2
                        wk = "Wsel%d" % wi
                        units = []
                        for c in range(nch):
                            n = min(512, N - c * 512)
                            for g in range(4):
                                for par in range(2):
                                    units.append((c, n, g, par))

                        def dots(u):
                            c, n, g, par = u
                            ri = rl_i[0] % 4
                            rl_i[0] += 1
                            ps = slice(64 * par, 64 * par + 64)
                            T(lambda e: e.matmul(banks[par][:, 0:n],
                                                 lhsT=qiT2[:, qb * 4 + g, :, :].rearrange("p c t -> p (c t)"),
                                                 rhs=kiTz[par][:, c * 512:c * 512 + n], start=True, stop=True),
                              r=["qiT2", "kiTd"], w=[bkey[par]])
                            if par == 0:
                                A(lambda e: e.activation(out=rl[ri][:, 0:n], in_=banks[par][:, 0:n], func=AF.Relu),
                                  r=[bkey[par]], w=["rl%d" % ri])
                            else:
                                V(lambda e: e.tensor_scalar(out=rl[ri][:, 0:n], in0=banks[par][:, 0:n], scalar1=0.0,
                                                            scalar2=None, op0=ALU.max), r=[bkey[par]], w=["rl%d" % ri])
                            return ri

                        def selmm(u, ri):
                            c, n, g, par = u
                            sbk = 2 + (c % 2)
                            first = (g == 0 and par == 0)
                            last = (g == 3 and par == 1)
                            T(lambda e: e.matmul(banks[sbk][:, 0:n], lhsT=Wsel[wi][:, par * 4 + g, :],
                                                 rhs=rl[ri][:, 0:n], start=first, stop=last),
                              r=[wk, "rl%d" % ri], w=[bkey[sbk]])
                            if last:
                                V(lambda e: e.tensor_scalar(out=score[:, c * 512:c * 512 + n], in0=banks[sbk][:, 0:n],
                                                            scalar1=1.0, scalar2=None, op0=ALU.mult, op1=ALU.max,
                                                            accum_out=cmax[:, c:c + 1]),
                                  r=[bkey[sbk]], w=["score", "cmax"])

                        ris = {}
                        LOOK = 2
                        for i_ in range(min(LOOK, len(units))):
                            ris[i_] = dots(units[i_])
                        for i_ in range(len(units)):
                            if i_ + LOOK < len(units):
                                ris[i_ + LOOK] = dots(units[i_ + LOOK])
                            selmm(units[i_], ris[i_])

                    def topk_iter(qb):
                        N = (qb + 1) * 128
                        nch = (N + 511) // 512
                        V(lambda e: e.tensor_reduce(out=lo0[:], in_=score[:, 0:N], axis=AX.X, op=ALU.min),
                          r=["score"], w=["lo0"])
                        V(lambda e: e.tensor_reduce(out=hi0[:], in_=cmax[:, 0:nch], axis=AX.X, op=ALU.max),
                          r=["cmax"], w=["hi0"])
                        V(lambda e: e.tensor_tensor(out=score[:, qb * 128:N], in0=score[:, qb * 128:N], in1=NEGM[:],
                                                    op=ALU.add), r=["score", "NEGM"], w=["score"])
                        V(lambda e: e.tensor_tensor(out=w0[:], in0=lo0[:], in1=hi0[:], op=ALU.subtract),
                          r=["hi0", "lo0"], w=["w0"])
                        V(lambda e: e.tensor_scalar(out=wh[:], in0=P2[:], scalar1=w0[:, 0:1], scalar2=None,
                                                    op0=ALU.mult), r=["P2", "w0"], w=["wh"])
                        V(lambda e: e.tensor_scalar(out=mid[:], in0=lo0[:], scalar1=-1.0, scalar2=wh[:, 0:1],
                                                    op0=ALU.mult, op1=ALU.add), r=["lo0", "wh"], w=["mid"])
                        for i in range(NIT):
                            A(lambda e: e.activation(out=mask[:, 0:N], in_=score[:, 0:N], func=AF.Sign,
                                                     bias=mid[:, 0:1], accum_out=cnt[:]),
                              r=["score", "mid"], w=["mask", "cnt"])
                            V(lambda e, i=i: e.scalar_tensor_tensor(out=btmp[:], in0=cnt[:],
                                                                    scalar=float(2 * TOPK - N) - 0.5,
                                                                    in1=wh[:, i:i + 1], op0=ALU.is_ge, op1=ALU.mult),
                              r=["cnt", "wh"], w=["btmp"])
                            V(lambda e, i=i: e.scalar_tensor_tensor(out=mid[:], in0=mid[:], scalar=wh[:, i + 1:i + 2],
                                                                    in1=btmp[:], op0=ALU.subtract, op1=ALU.add),
                              r=["mid", "wh", "btmp"], w=["mid"])
                            yield
                        V(lambda e: e.tensor_scalar(out=mid[:], in0=mid[:], scalar1=-1.0, scalar2=wh[:, NIT:NIT + 1],
                                                    op0=ALU.mult, op1=ALU.add), r=["mid", "wh"], w=["mid"])

                    def topk_finish(qb):
                        N = (qb + 1) * 128
                        if qb < KB:
                            for jj in range(qb + 1):
                                src = TRIU if jj == qb else ONESB
                                G(lambda e, jj=jj, src=src: e.tensor_copy(out=maskT[:, jj, :], in_=src[:]),
                                  r=["TRIU", "ONESB"], w=["maskT"])
                            return
                        V(lambda e: e.tensor_scalar(out=mask[:, 0:N], in0=score[:, 0:N], scalar1=mid[:, 0:1],
                                                    scalar2=None, op0=ALU.is_ge), r=["score", "mid"], w=["mask"])
                        if b == 0 and qb == NT - 1:
                            tap("score", score[:, 0:N], ["score"])
                            tap("thr", mid[:], ["mid"])
                        for jj in range(qb + 1):
                            lb = 4 + jj // 8
                            T(lambda e, jj=jj, lb=lb: e.transpose(out=bbf[lb][:, (jj % 8) * 128:(jj % 8 + 1) * 128],
                                                                  in_=mask[:, jj * 128:(jj + 1) * 128],
                                                                  identity=identb[:]),
                              r=["mask", "identb"], w=[bkey[lb]])
                        for lb in range(4, 4 + (qb + 8) // 8):
                            j0 = (lb - 4) * 8
                            j1 = min(qb + 1, j0 + 8)
                            nj = j1 - j0
                            V(lambda e, lb=lb, j0=j0, j1=j1, nj=nj: e.tensor_copy(
                                out=maskT[:, j0:j1, :],
                                in_=bbf[lb][:, 0:nj * 128].rearrange("p (j t) -> p j t", t=128)),
                              r=[bkey[lb]], w=["maskT"])

                    def attention(qb):
                        qs = slice(qb * 128, (qb + 1) * 128)
                        for kv in range(2):
                            T(lambda e, kv=kv: e.matmul(banks[6 + kv][:, 0:260], lhsT=zerob[:, 0:128],
                                                        rhs=zerob[:, 0:260], start=True, stop=False,
                                                        skip_group_check=True), r=["zerob"], w=[bkey[6 + kv]])

                        def Lstage(jj):
                            ks = slice(jj * 128, (jj + 1) * 128)
                            pis = []
                            for par in range(2):
                                ps = slice(64 * par, 64 * par + 64)
                                lb = 4 + par
                                pi = pT_i[0] % 4
                                pT_i[0] += 1
                                pis.append(pi)
                                for kv in range(2):
                                    T(lambda e, ps=ps, lb=lb, kv=kv, par=par: e.matmul(
                                        banks[lb][:, kv * 256:(kv + 1) * 256], lhsT=kTz[par][:, kv, ks],
                                        rhs=qT2[:, 2 * kv:2 * kv + 2, qs], start=True, stop=True),
                                      r=["kTd", "qT2"], w=[bkey[lb]])
                                A(lambda e, lb=lb, pi=pi: e.activation(out=pT[pi][:], in_=banks[lb][:, :], func=AF.Exp,
                                                                       scale=0.125), r=[bkey[lb]], w=["pT%d" % pi])
                                V(lambda e, pi=pi: e.tensor_tensor(
                                    out=pT[pi][:].rearrange("p (h t) -> p h t", t=128),
                                    in0=pT[pi][:].rearrange("p (h t) -> p h t", t=128),
                                    in1=maskT[:, jj, :].unsqueeze(1).broadcast_to([128, 4, 128]), op=ALU.mult),
                                  r=["pT%d" % pi, "maskT"], w=["pT%d" % pi])
                            return pis

                        def PVstage(jj, pis):
                            for par in range(2):
                                pi = pis[par]
                                for kv in range(2):
                                    for ii in range(2):
                                        hl = 2 * ii + par
                                        T(lambda e, ii=ii, hl=hl, kv=kv, pi=pi: e.matmul(
                                            banks[6 + kv][:, hl * 65:hl * 65 + 65],
                                            lhsT=pT[pi][:, (kv * 2 + ii) * 128:(kv * 2 + ii + 1) * 128],
                                            rhs=v_aug[:, jj, kv, :], start=False, stop=(jj == qb),
                                            skip_group_check=True),
                                          r=["pT%d" % pi, "v_aug"], w=[bkey[6 + kv]])

                        nxt = Lstage(0)
                        for jj in range(qb + 1):
                            cur = nxt
                            if jj + 1 <= qb:
                                nxt = Lstage(jj + 1)
                            PVstage(jj, cur)
                            yield

                    def post_a(qb):
                        for kv in range(2):
                            ov = banks[6 + kv][:, 0:260].rearrange("p (h d) -> p h d", d=65)
                            V(lambda e, kv=kv, ov=ov: e.reciprocal(out=rden[:, kv * 4:(kv + 1) * 4], in_=ov[:, :, 64]),
                              r=[bkey[6 + kv]], w=["rden"])
                            V(lambda e, kv=kv, ov=ov: e.tensor_tensor(
                                out=yb[:, kv * 256:(kv + 1) * 256].rearrange("p (h d) -> p h d", d=64),
                                in0=ov[:, :, 0:64],
                                in1=rden[:, kv * 4:(kv + 1) * 4].unsqueeze(2).broadcast_to([128, 4, 64]),
                                op=ALU.mult), r=[bkey[6 + kv], "rden"], w=["yb"])
                        if b == 0 and qb == NT - 1:
                            tap("yb", yb[:], ["yb"])

                    def post_b1(qb):
                        yield
                        A(lambda e: e.activation(out=junkA[:, 0:512], in_=yb[:], func=AF.Square, accum_out=ssb_[:]),
                          r=["yb"], w=["junkA", "ssb_"])
                        yield
                        rstd(rsb[:], ssb_[:], 1, 1.0 / 512, ["ssb_"], ["rsb"])
                        yield
                        V(lambda e: e.scalar_tensor_tensor(out=mb[:], in0=yb[:], scalar=rsb[:, 0:1], in1=gob_row[:],
                                                           op0=ALU.mult, op1=ALU.mult),
                          r=["yb", "rsb", "gob_row"], w=["mb"])

                    def post_b2(qb):
                        it = b * NT + qb
                        xj = qb % 2
                        for c in range(4):
                            T(lambda e, c=c: e.transpose(out=bbf[2][:, c * 128:(c + 1) * 128],
                                                         in_=mb[:, c * 128:(c + 1) * 128], identity=identb[:]),
                              r=["mb", "identb"], w=[bkey[2]])
                        A(lambda e: e.activation(out=mbT[:], in_=bbf[2][:, 0:512].rearrange("p (c t) -> p c t", t=128),
                                                 func=AF.Copy), r=[bkey[2]], w=["mbT"])
                        yield
                        for n in range(2):
                            for k in range(8):
                                lhs = mTa[:, k, qb * 128:(qb + 1) * 128] if k < 4 else mbT[:, k - 4, :]
                                T(lambda e, n=n, k=k, lhs=lhs: e.matmul(banks[2 + n][:, :], lhsT=lhs,
                                                                        rhs=Wout[:, k, n * 512:(n + 1) * 512],
                                                                        start=(k == 0), stop=(k == 7)),
                                  r=["mTa", "mbT", "Wout"], w=[bkey[2 + n]])
                        for n in range(2):
                            A(lambda e, n=n: e.activation(out=junkA[:, 0:512], in_=banks[2 + n][:, :], func=AF.Square,
                                                          accum_out=sso[:, n:n + 1]),
                              r=[bkey[2 + n]], w=["junkA", "sso%d" % n])
                        yield
                        rstd(rso[:], sso[:, 0:1], 1, 1.0 / D, ["sso0", "sso1"], ["rso"], ss2_ap=sso[:, 1:2])
                        yield
                        for n in range(2):
                            V(lambda e, n=n: e.scalar_tensor_tensor(out=ot[:, n * 512:(n + 1) * 512],
                                                                    in0=banks[2 + n][:, :], scalar=rso[:, 0:1],
                                                                    in1=G1row[:, n * 512:(n + 1) * 512],
                                                                    op0=ALU.mult, op1=ALU.mult),
                              r=[bkey[2 + n], "rso", "G1row"], w=["ot"])
                        G(lambda e: e.tensor_tensor(out=ot[:], in0=ot[:], in1=xres[xj][:], op=ALU.add),
                          r=["ot", "xres%d" % xj], w=["ot"])
                        P.dma(x1s_d[it * 128:(it + 1) * 128, :], ot[:], reads=["ot"],
                              writes=["x1s_%d" % it])

                    def step(g_):
                        if g_ is None:
                            return False
                        try:
                            next(g_)
                            return True
                        except StopIteration:
                            return False

                    def interleave(g1, g2):
                        a1, a2 = g1 is not None, g2 is not None
                        while a1:
                            a1 = step(g1)
                            if a2:
                                a2 = step(g2)
                        return a2

                    def drain(g_):
                        while step(g_):
                            pass

                    if 0 >= KB:
                        wsel_build(0)
                        indexer(0)
                        drain(topk_iter(0))
                    topk_finish(0)
                    if 1 < NT and 1 >= KB:
                        wsel_build(1)
                    pb1 = pb2 = None
                    for qb in range(NT):
                        it = b * NT + qb
                        P.dma(xres[qb % 2][:], x_d[it * 128:(it + 1) * 128, :], writes=["xres%d" % (qb % 2)])
                        tk = None
                        if qb + 1 < NT and qb + 1 >= KB:
                            indexer(qb + 1)
                            tk = topk_iter(qb + 1)
                        if qb + 2 < NT and qb + 2 >= KB:
                            wsel_build(qb + 2)
                        att = attention(qb)
                        a_att, a_tk = True, tk is not None
                        a_p1, a_p2 = pb1 is not None, pb2 is not None
                        nstep = 0
                        while a_att:
                            a_att = step(att)
                            nstep += 1
                            if a_tk and (nstep >= 2 or qb + 1 < 3):
                                a_tk = step(tk)
                            if a_p1:
                                a_p1 = step(pb1)
                            elif a_p2:
                                a_p2 = step(pb2)
                        if a_p1:
                            drain(pb1)
                        if a_p2:
                            drain(pb2)
                        post_a(qb)
                        pb1 = post_b1(qb)
                        pb2 = post_b2(qb)
                        a_p1 = True
                        while a_tk:
                            a_tk = step(tk)
                            if a_p1:
                                a_p1 = step(pb1)
                        if qb + 1 < NT:
                            topk_finish(qb + 1)
                    drain(pb1)
                    drain(pb2)
                    P.barrier()
                    if stop == "attn":
                        P.finish()
                        return
        with contextlib.ExitStack() as bst:
            bsb = mk_sb(bst)
            W1 = bsb("W1", [128, 8, DFF], BF16)
            W2 = bsb("W2", [128, 32, D], BF16)
            wst = [bsb("wst%d" % i, [128, 2048]) for i in range(2)]
            G2row = bsb("G2row", [128, D])
            xg = [bsb("xg%d" % i, [128, D]) for i in range(4)]
            xn2 = [bsb("xn2_%d" % i, [128, D]) for i in range(1)]
            h2T = [bsb("h2T%d" % i, [128, 8, 256], BF16) for i in range(2)]
            rr = [bsb("rr%d" % i, [128, 256], BF16) for i in range(3)]
            fT = [bsb("fT%d" % i, [128, 256], BF16) for i in range(3)]
            junkB = bsb("junkB", [128, D], BF16)
            ss2 = bsb("ss2", [128, 4])
            rs2 = bsb("rs2", [128, 4])
            ssf = bsb("ssf", [128, 4])
            rsf = bsb("rsf", [128, 2])
            of = [bsb("of%d" % i, [128, D]) for i in range(2)]

            cast_engs = ["dve", "act", "pool"]
            ci = 0
            wi_ = 0
            for k in range(8):
                for hf in range(2):
                    st_ = wst[wi_ % 2]
                    sk = "wst%d" % (wi_ % 2)
                    wi_ += 1
                    P.dma(st_[:], w1_d[k * 128:(k + 1) * 128, hf * 2048:(hf + 1) * 2048], writes=[sk])
                    for q2 in range(2):
                        eng = cast_engs[ci % 3]
                        ci += 1
                        sl = slice(q2 * 1024, (q2 + 1) * 1024)
                        dl = slice(hf * 2048 + q2 * 1024, hf * 2048 + (q2 + 1) * 1024)
                        if eng == "act":
                            A(lambda e, k=k, st_=st_, sl=sl, dl=dl: e.activation(out=W1[:, k, dl], in_=st_[:, sl],
                                                                              func=AF.Copy), r=[sk], w=["W1"])
                        else:
                            P.op(eng, lambda e, k=k, st_=st_, sl=sl, dl=dl: e.tensor_copy(out=W1[:, k, dl],
                                                                                       in_=st_[:, sl]), [sk], ["W1"])
            for c2 in range(16):
                st_ = wst[wi_ % 2]
                sk = "wst%d" % (wi_ % 2)
                wi_ += 1
                P.dma(st_[:].rearrange("p (c n) -> p c n", n=D),
                      w2_d[c2 * 256:(c2 + 1) * 256, :].rearrange("(c p) n -> p c n", p=128), writes=[sk])
                for q2 in range(2):
                    eng = cast_engs[ci % 3]
                    ci += 1
                    sl = slice(q2 * 1024, (q2 + 1) * 1024)
                    if eng == "act":
                        A(lambda e, c2=c2, q2=q2, st_=st_, sl=sl: e.activation(out=W2[:, c2 * 2 + q2, :], in_=st_[:, sl],
                                                                          func=AF.Copy), r=[sk], w=["W2"])
                    else:
                        P.op(eng, lambda e, c2=c2, q2=q2, st_=st_, sl=sl: e.tensor_copy(out=W2[:, c2 * 2 + q2, :],
                                                                                   in_=st_[:, sl]), [sk], ["W2"])

            NG = NTOK // 256

            def b_load(g):
                for t in range(2):
                    it = g * 2 + t
                    xi = (g % 2) * 2 + t
                    P.dma(xg[xi][:], x1s_d[it * 128:(it + 1) * 128, :], reads=["x1s_%d" % it], writes=["xg%d" % xi])

            def b_prep(g):
                hj = g % 2
                b = (g * 256) // S
                for t in range(2):
                    xi = (g % 2) * 2 + t
                    A(lambda e, xi=xi, t=t: e.activation(out=junkB[:], in_=xg[xi][:], func=AF.Square,
                                                        accum_out=ss2[:, t:t + 1]), r=["xg%d" % xi],
                      w=["junkB", "ss2_%d" % t])
                    rstd(rs2[:, t:t + 1], ss2[:, t:t + 1], 1, 1.0 / D, ["ss2_%d" % t], ["rs2_%d" % t])
                    V(lambda e, xi=xi, t=t: e.tensor_scalar(out=xn2[0][:], in0=xg[xi][:], scalar1=rs2[:, t:t + 1],
                                                           scalar2=None, op0=ALU.mult),
                      r=["xg%d" % xi, "rs2_%d" % t], w=["xn2_0"])
                    for k in range(8):
                        T(lambda e, k=k, t=t: e.transpose(out=banks[6 + k // 4][:, (k % 4) * 128:(k % 4 + 1) * 128],
                                                          in_=xn2[0][:, k * 128:(k + 1) * 128], identity=ident[:]),
                          r=["xn2_0", "ident"], w=[bkey[6 + k // 4]])
                    for k in range(8):
                        A(lambda e, k=k, t=t: e.activation(out=h2T[hj][:, k, t * 128:(t + 1) * 128],
                                                           in_=banks[6 + k // 4][:, (k % 4) * 128:(k % 4 + 1) * 128],
                                                           func=AF.Identity, scale=S2T[:, k, b:b + 1],
                                                           bias=sh2T[:, k, b:b + 1]),
                          r=[bkey[6 + k // 4], "S2T", "sh2T"], w=["h2T%d" % hj])

            f_i = [0]

            def b_main(g):
                hj = g % 2
                b = (g * 256) // S
                if (g * 256) % S == 0:
                    for n in range(2):
                        T(lambda e, n=n: e.matmul(banks[4 + n][:, :], lhsT=sel[0:NSEQ, b, :],
                                                  rhs=gmod[0:NSEQ, 1, n * 512:(n + 1) * 512], start=True, stop=True),
                          r=["sel", "gmod1"], w=[bkey[4 + n]])
                        V(lambda e, n=n: e.tensor_copy(out=G2row[:, n * 512:(n + 1) * 512], in_=banks[4 + n][:, :]),
                          r=[bkey[4 + n]], w=["G2row"])
                def Fst(c):
                    fb = 4 + (c % 2)
                    fi = c % 3
                    for k in range(8):
                        T(lambda e, k=k: e.matmul(banks[fb][:, 0:256], lhsT=W1[:, k, c * 128:(c + 1) * 128],
                                                  rhs=h2T[hj][:, k, :], start=(k == 0), stop=(k == 7)),
                          r=["W1", "h2T%d" % hj], w=[bkey[fb]])
                    A(lambda e: e.activation(out=rr[fi][:], in_=banks[fb][:, 0:256], func=AF.Relu),
                      r=[bkey[fb]], w=["rr%d" % fi])
                    V(lambda e: e.scalar_tensor_tensor(out=fT[fi][:], in0=banks[fb][:, 0:256], scalar=0.0,
                                                       in1=rr[fi][:], op0=ALU.max, op1=ALU.mult),
                      r=[bkey[fb], "rr%d" % fi], w=["fT%d" % fi])

                def P2st(c):
                    fi = c % 3
                    for t in range(2):
                        for n in range(2):
                            ob = t * 2 + n
                            T(lambda e, t=t, n=n, ob=ob: e.matmul(
                                banks[ob][:, :], lhsT=fT[fi][:, t * 128:(t + 1) * 128],
                                rhs=W2[:, c, n * 512:(n + 1) * 512], start=(c == 0), stop=(c == 31)),
                              r=["fT%d" % fi, "W2"], w=[bkey[ob]])

                Fst(0)
                for c in range(32):
                    if c + 1 < 32:
                        Fst(c + 1)
                    P2st(c)
                    if c == 20 and g + 1 < NG:
                        b_prep(g + 1)
                for t in range(2):
                    it = g * 2 + t
                    xi = (g % 2) * 2 + t
                    for n in range(2):
                        A(lambda e, t=t, n=n: e.activation(out=junkB[:, 0:512], in_=banks[t * 2 + n][:, :],
                                                           func=AF.Square, accum_out=ssf[:, t * 2 + n:t * 2 + n + 1]),
                          r=[bkey[t * 2 + n]], w=["junkB", "ssf%d" % (t * 2 + n)])
                    rstd(rsf[:, t:t + 1], ssf[:, t * 2:t * 2 + 1], 1, 1.0 / D, ["ssf%d" % (t * 2), "ssf%d" % (t * 2 + 1)],
                         ["rsf%d" % t], ss2_ap=ssf[:, t * 2 + 1:t * 2 + 2])
                    for n in range(2):
                        V(lambda e, t=t, n=n: e.scalar_tensor_tensor(out=of[t][:, n * 512:(n + 1) * 512],
                                                                     in0=banks[t * 2 + n][:, :], scalar=rsf[:, t:t + 1],
                                                                     in1=G2row[:, n * 512:(n + 1) * 512],
                                                                     op0=ALU.mult, op1=ALU.mult),
                          r=[bkey[t * 2 + n], "rsf%d" % t, "G2row"], w=["of%d" % t])
                    G(lambda e, t=t, xi=xi: e.tensor_tensor(out=of[t][:], in0=of[t][:], in1=xg[xi][:], op=ALU.add),
                      r=["of%d" % t, "xg%d" % xi], w=["of%d" % t])
                    P.dma(out_d[it * 128:(it + 1) * 128, :], of[t][:], reads=["of%d" % t])
                if g + 2 < NG:
                    b_load(g + 2)

            b_load(0)
            if NG > 1:
                b_load(1)
            b_prep(0)
            for g in range(NG):
                b_main(g)
            P.finish()
        print("program built: instrs=%d waits=%d" % (P.ninstr, P.nwaits), flush=True)


def make_core_inputs(ci, NSEQ, S, x, c, positions, w_ada, b_ada, g_pre_mix, w_in, g_sgu_v, w_spatial, b_spatial,
                     g_out_sgu, g_out_attn, w_out, g_post_mix, g_pre_ffn, w_ff1, w_ff2, g_post_ffn):
    f32 = np.float32
    bs = slice(ci * NSEQ, (ci + 1) * NSEQ)
    NT = S // 128
    xc = np.ascontiguousarray(x[bs]).reshape(NSEQ * S, D).astype(f32, copy=False)
    cc = np.asarray(c[bs], dtype=f32)
    cT = np.ascontiguousarray(cc.T.reshape(8, 128, NSEQ).transpose(1, 0, 2))
    pos = np.ascontiguousarray(np.asarray(positions[bs]).reshape(NSEQ * NT, 128).T.astype(np.int32))
    wi = np.asarray(w_in[0], dtype=f32)
    perm = np.concatenate([np.arange(0, 1792), np.arange(2304, 2376), np.arange(1792, 2304)])
    wi_p = np.ascontiguousarray(wi[:, perm])
    return {
        "x": xc, "cT": cT, "pos": pos,
        "w_ada": np.ascontiguousarray(w_ada[0], dtype=f32),
        "b_ada": np.ascontiguousarray(b_ada[0:1], dtype=f32),
        "w_in": wi_p,
        "gpre": np.ascontiguousarray(np.asarray(g_pre_mix[0], dtype=f32).reshape(8, 128).T),
        "gpre2": np.ascontiguousarray(np.asarray(g_pre_ffn[0], dtype=f32).reshape(8, 128).T),
        "gv": np.ascontiguousarray(g_sgu_v[0:1], dtype=f32),
        "ws": np.ascontiguousarray(np.asarray(w_spatial[0], dtype=f32).transpose(1, 0, 2)),
        "bs": np.ascontiguousarray(np.asarray(b_spatial[0], dtype=f32).T),
        "goa": np.ascontiguousarray(g_out_sgu[0:1], dtype=f32),
        "gob": np.ascontiguousarray(g_out_attn[0:1], dtype=f32),
        "w_out": np.ascontiguousarray(w_out[0], dtype=f32),
        "gpost": np.ascontiguousarray(g_post_mix[0:1], dtype=f32),
        "w1": np.ascontiguousarray(w_ff1[0], dtype=f32),
        "w2": np.ascontiguousarray(w_ff2[0], dtype=f32),
        "gpost2": np.ascontiguousarray(g_post_ffn[0:1], dtype=f32),
    }


def run(inputs, n_cores, NSEQ, S, taps=None, trace=False, stop=None):
    nc = bass.Bass("TRN2", target_bir_lowering=False)
    try:
        build_program(nc, NSEQ=NSEQ, S=S, taps=taps, stop=stop)
    except StopBuild:
        pass
    in_maps = [make_core_inputs(ci, NSEQ, S, **inputs) for ci in range(n_cores)]
    res = run_bass_kernel_spmd(nc, in_maps, core_ids=list(range(n_cores)), trace=trace)
    return res


def kernel(**inputs):
    inputs = {k: np.asarray(v) for k, v in inputs.items()}
    B, S, _ = inputs["x"].shape
    NSEQ = B // NCORES
    res = run(inputs, NCORES, NSEQ, S)
    outs = [np.asarray(r["out"]).reshape(NSEQ, S, D) for r in res.results]
    return np.concatenate(outs, axis=0).astype(np.float32, copy=False)
```

```python
import contextlib
import math
import numpy as np
import concourse.bass as bass
import concourse.mybir as mybir
from concourse.bass_utils import run_bass_kernel_spmd

F32 = mybir.dt.float32
BF16 = mybir.dt.bfloat16
I32 = mybir.dt.int32
AF = mybir.ActivationFunctionType
ALU = mybir.AluOpType
AX = mybir.AxisListType

D = 1024
DIN = 2376
DFF = 4096
NCORES = 8
EPS = 1e-6
NIT = 16
IDX_SCALE = (64 ** -0.5) * (8 ** -0.5)
TWO_PI = 2.0 * math.pi


class StopBuild(Exception):
    pass


class Prog:
    NDMA = 32

    def __init__(self, nc, stack):
        self.nc = nc
        self.eng = {"pe": nc.tensor, "act": nc.scalar, "dve": nc.vector,
                    "pool": nc.gpsimd, "sp": nc.sync}
        self.sem = {k: stack.enter_context(nc.semaphore("c_" + k)) for k in self.eng}
        self.cnt = {k: 0 for k in self.eng}
        self.dsem = [stack.enter_context(nc.semaphore("d%d" % i)) for i in range(self.NDMA)]
        self.dval = [0] * self.NDMA
        self.dnext = 0
        self.seen = {k: {} for k in self.eng}
        self.res = {}
        self.nwaits = 0
        self.ninstr = 0

    def _wait(self, eng, dep):
        kind, key, val = dep
        if kind == "e":
            if key == "pe" and eng == "pe":
                return
            sem = self.sem[key]
            skey = key
        else:
            sem = self.dsem[key]
            skey = ("d", key)
        if self.seen[eng].get(skey, 0) >= val:
            return
        self.seen[eng][skey] = val
        self.eng[eng].wait_ge(sem, val)
        self.nwaits += 1

    def _deps(self, eng, reads, writes):
        deps = []
        for r in reads:
            st = self.res.get(r)
            if st and st["w"]:
                deps.append(st["w"])
        for w in writes:
            st = self.res.get(w)
            if st:
                if st["w"]:
                    deps.append(st["w"])
                deps.extend(st["r"])
        for d in deps:
            self._wait(eng, d)

    def _record(self, token, reads, writes):
        for r in reads:
            st = self.res.setdefault(r, {"w": None, "r": []})
            st["r"].append(token)
            if len(st["r"]) > 48:
                best = {}
                for t in st["r"]:
                    k = (t[0], t[1])
                    if k not in best or best[k][2] < t[2]:
                        best[k] = t
                st["r"] = list(best.values())
        for w in writes:
            self.res[w] = {"w": token, "r": []}

    def op(self, eng, fn, reads=(), writes=()):
        self._deps(eng, reads, writes)
        ins = fn(self.eng[eng])
        self.cnt[eng] += 1
        ins.then_inc(self.sem[eng], 1)
        token = ("e", eng, self.cnt[eng])
        self._record(token, reads, writes)
        self.ninstr += 1
        return token

    def dma(self, out, in_, reads=(), writes=(), q="sp", **kw):
        self._deps(q, reads, writes)
        i = self.dnext
        self.dnext = (self.dnext + 1) % self.NDMA
        if self.dval[i] > 0:
            self._wait(q, ("d", i, self.dval[i]))
        ins = self.eng[q].dma_start(out=out, in_=in_, **kw)
        self.dval[i] += 16
        ins.then_inc(self.dsem[i], 16)
        token = ("d", i, self.dval[i])
        self._record(token, reads, writes)
        self.ninstr += 1
        return token

    def barrier(self):
        for e in self.eng:
            for f in self.eng:
                if f != e and self.cnt[f] > 0:
                    self._wait(e, ("e", f, self.cnt[f]))
            for i in range(self.NDMA):
                if self.dval[i] > 0:
                    self._wait(e, ("d", i, self.dval[i]))
        self.res = {}

    def finish(self):
        for i in range(self.NDMA):
            if self.dval[i] > 0:
                self._wait("sp", ("d", i, self.dval[i]))
        for f in self.eng:
            if f != "sp" and self.cnt[f] > 0:
                self._wait("sp", ("e", f, self.cnt[f]))


def build_program(nc, NSEQ=4, S=2048, taps=None, stop=None):
    NT = S // 128
    NTT = NSEQ * NT
    NTOK = NSEQ * S
    TOPK = min(256, S // 4)
    KB = TOPK // 128
    taps = taps or {}

    def din(name, shape, dt=F32):
        return nc.dram_tensor(name, list(shape), dt, kind="ExternalInput").ap()

    x_d = din("x", [NTOK, D])
    cT_d = din("cT", [128, 8, NSEQ])
    pos_d = din("pos", [128, NTT], I32)
    wada_d = din("w_ada", [D, 6 * D])
    bada_d = din("b_ada", [1, 6 * D])
    win_d = din("w_in", [D, DIN])
    gpre_d = din("gpre", [128, 8])
    gpre2_d = din("gpre2", [128, 8])
    gv_d = din("gv", [1, 512])
    ws_d = din("ws", [128, 4, 128])
    bs_d = din("bs", [128, 4])
    goa_d = din("goa", [1, 512])
    gob_d = din("gob", [1, 512])
    wout_d = din("w_out", [D, D])
    gpost_d = din("gpost", [1, D])
    w1_d = din("w1", [D, DFF])
    w2_d = din("w2", [DFF, D])
    gpost2_d = din("gpost2", [1, D])
    out_d = nc.dram_tensor("out", [NTOK, D], F32, kind="ExternalOutput").ap()
    x1s_d = out_d
    tap_d = {k: nc.dram_tensor("tap_" + k, list(shp), F32, kind="ExternalOutput").ap()
             for k, shp in taps.items()}

    with contextlib.ExitStack() as gst:
        P = Prog(nc, gst)

        uid = [0]

        def mk_sb(stack):
            def sb(name, shape, dt=F32):
                uid[0] += 1
                return stack.enter_context(nc.sbuf_tensor("s%d_%s" % (uid[0], name), list(shape), dt))
            return sb

        gsb = mk_sb(gst)
        banks = [gst.enter_context(nc.psum_tensor("bank%d" % i, [128, 512], F32)) for i in range(8)]
        bkey = ["b%d" % i for i in range(8)]
        bbf = [b[:].bitcast(BF16) for b in banks]

        def V(fn, r=(), w=()):
            return P.op("dve", fn, r, w)

        def A(fn, r=(), w=()):
            return P.op("act", fn, r, w)

        def G(fn, r=(), w=()):
            return P.op("pool", fn, r, w)

        def T(fn, r=(), w=()):
            return P.op("pe", fn, r, w)

        ident = gsb("ident", [128, 128])
        identb = gsb("identb", [128, 128], BF16)
        ones_f = gsb("ones_f", [128, 128])
        zeros_f = gsb("zeros_f", [128, 128])
        NEGM = gsb("NEGM", [128, 128])
        TRIU = gsb("TRIU", [128, 128], BF16)
        ONESB = gsb("ONESB", [128, 128], BF16)
        D32 = gsb("D32", [128, 32])
        G4 = gsb("G4", [128, 4])
        zerob = gsb("zerob", [128, 260], BF16)
        P2 = gsb("P2", [128, NIT + 1])
        mhalf = gsb("mhalf", [128, 16])
        iot = gsb("iot", [128, 8], I32)
        iof = gsb("iof", [128, 8])
        invf = gsb("invf", [128, 8])
        rs_tmp = gsb("rs_tmp", [128, 16])

        G(lambda e: e.memset(ones_f[:], 1.0), w=["ones_f"])
        G(lambda e: e.memset(zeros_f[:], 0.0), w=["zeros_f"])
        G(lambda e: e.affine_select(out=ident[:], in_=ones_f[:], pattern=[[-1, 128]], compare_op=ALU.is_equal,
                                    fill=0.0, base=0, channel_multiplier=1), r=["ones_f"], w=["ident"])
        V(lambda e: e.tensor_copy(out=identb[:], in_=ident[:]), r=["ident"], w=["identb"])
        G(lambda e: e.affine_select(out=NEGM[:], in_=zeros_f[:], pattern=[[-1, 128]], compare_op=ALU.is_ge,
                                    fill=-1.0e30, base=0, channel_multiplier=1), r=["zeros_f"], w=["NEGM"])
        G(lambda e: e.affine_select(out=TRIU[:], in_=ones_f[:], pattern=[[1, 128]], compare_op=ALU.is_ge,
                                    fill=0.0, base=0, channel_multiplier=-1), r=["ones_f"], w=["TRIU"])
        V(lambda e: e.tensor_copy(out=ONESB[:], in_=ones_f[:]), r=["ones_f"], w=["ONESB"])
        for m in range(4):
            G(lambda e, m=m: e.affine_select(out=D32[32 * m:32 * m + 32, :], in_=ones_f[32 * m:32 * m + 32, 0:32],
                                             pattern=[[-1, 32]], compare_op=ALU.is_equal, fill=0.0, base=0,
                                             channel_multiplier=1), r=["ones_f"], w=["D32"])
        G(lambda e: e.memset(G4[:], 0.0), w=["G4"])
        for g in range(4):
            G(lambda e, g=g: e.memset(G4[32 * g:32 * g + 32, g:g + 1], 1.0), r=["G4"], w=["G4"])
        G(lambda e: e.memset(zerob[:], 0.0), w=["zerob"])
        for i in range(NIT + 1):
            G(lambda e, i=i: e.memset(P2[:, i:i + 1], 2.0 ** -(i + 1)), w=["P2"])
        G(lambda e: e.memset(mhalf[:], -0.5), w=["mhalf"])
        G(lambda e: e.iota(iot[:], pattern=[[1, 8]], base=0, channel_multiplier=0), w=["iot"])
        V(lambda e: e.tensor_copy(out=iof[:], in_=iot[:]), r=["iot"], w=["iof"])
        A(lambda e: e.activation(out=invf[:], in_=iof[:], func=AF.Exp, scale=-math.log(500000.0) / 8.0),
          r=["iof"], w=["invf"])

        def rstd(out_ap, ss_ap, n, inv_n, rk, wk, ss2_ap=None):
            tmp = rs_tmp[:, 0:n]
            if ss2_ap is not None:
                G(lambda e: e.tensor_tensor(out=tmp, in0=ss_ap, in1=ss2_ap, op=ALU.add), r=rk, w=["rs_tmp"])
                G(lambda e: e.tensor_scalar(out=tmp, in0=tmp, scalar1=inv_n, scalar2=EPS, op0=ALU.mult,
                                            op1=ALU.add), r=["rs_tmp"], w=["rs_tmp"])
            else:
                G(lambda e: e.tensor_scalar(out=tmp, in0=ss_ap, scalar1=inv_n, scalar2=EPS, op0=ALU.mult,
                                            op1=ALU.add), r=rk, w=["rs_tmp"])
            G(lambda e: e.tensor_tensor(out=out_ap, in0=tmp, in1=mhalf[:, 0:n], op=ALU.pow),
              r=["rs_tmp", "mhalf"], w=wk)

        def ck(name):
            if stop == name:
                P.finish()
                raise StopBuild()

        def tap(name, ap, rk, rows=None):
            if name in tap_d:
                dst = tap_d[name]
                P.dma(dst if rows is None else dst[rows], ap, reads=rk)

        S1T = gsb("S1T", [128, 8, NSEQ])
        sh1T = gsb("sh1T", [128, 8, NSEQ])
        S2T = gsb("S2T", [128, 8, NSEQ])
        sh2T = gsb("sh2T", [128, 8, NSEQ])
        gmod = gsb("gmod", [NSEQ, 2, D])
        sel = gsb("sel", [NSEQ, NSEQ, 128])
        gpre = gsb("gpre", [128, 8])
        gpre2 = gsb("gpre2", [128, 8])
        P.dma(gpre[:], gpre_d[:, :], writes=["gpre"])
        P.dma(gpre2[:], gpre2_d[:, :], writes=["gpre2"])

        G(lambda e: e.affine_select(out=sel[:], in_=ones_f[0:NSEQ, :].unsqueeze(1).broadcast_to([NSEQ, NSEQ, 128]),
                                    pattern=[[-1, NSEQ], [0, 128]], compare_op=ALU.is_equal, fill=0.0, base=0,
                                    channel_multiplier=1), r=["ones_f"], w=["sel"])

        with contextlib.ExitStack() as ast:
            asb = mk_sb(ast)
            Win = asb("Win", [128, 8, DIN], BF16)
            Wout = asb("Wout", [128, 8, D], BF16)
            cs = asb("cs", [128, NTT, 8])
            sn = asb("sn", [128, NTT, 8])
            gv_row = asb("gv_row", [128, 512])
            goa_row = asb("goa_row", [128, 512])
            gob_row = asb("gob_row", [128, 512])
            bcol = asb("bcol", [128, 4])
            WmT = asb("WmT", [128, 4, 128], BF16)
            P.dma(gv_row[:], gv_d[0:1, :].partition_broadcast(128), writes=["gv_row"])
            P.dma(goa_row[:], goa_d[0:1, :].partition_broadcast(128), writes=["goa_row"])
            P.dma(gob_row[:], gob_d[0:1, :].partition_broadcast(128), writes=["gob_row"])
            P.dma(bcol[:], bs_d[:, :], writes=["bcol"])

            with contextlib.ExitStack() as sst:
                ssb = mk_sb(sst)
                cTs = ssb("cTs", [128, 8, NSEQ])
                scs = ssb("scs", [128, 8, NSEQ])
                bada4 = ssb("bada4", [NSEQ, 6 * D])
                modrow = ssb("modrow", [NSEQ, 6 * D])
                gpost4 = ssb("gpost4", [NSEQ, 2, D])
                wada_st = [ssb("wada_st%d" % i, [128, 8, 512]) for i in range(2)]
                win_st = [ssb("win_st%d" % i, [128, DIN]) for i in range(2)]
                ws_sb = ssb("ws_sb", [128, 4, 128])
                wsm = ssb("wsm", [128, 4, 128])
                posi = ssb("posi", [128, NTT], I32)
                posf = ssb("posf", [128, NTT])
                ang = ssb("ang", [128, NTT * 8])
                angk = ssb("angk", [128, NTT * 8], I32)
                angf = ssb("angf", [128, NTT * 8])
                angm = ssb("angm", [128, NTT * 8])
                ang2 = ssb("ang2", [128, NTT * 8])

                P.dma(cTs[:], cT_d[:, :, :], writes=["cTs"])
                P.dma(bada4[:], bada_d[0:1, :].partition_broadcast(NSEQ), writes=["bada4"])
                P.dma(gpost4[:, 0, :], gpost_d[0:1, :].partition_broadcast(NSEQ), writes=["gpost4a"])
                P.dma(gpost4[:, 1, :], gpost2_d[0:1, :].partition_broadcast(NSEQ), writes=["gpost4b"])
                P.dma(posi[:], pos_d[:, :], writes=["posi"])
                P.dma(ws_sb[:], ws_d[:, :, :], writes=["ws_sb"])
                A(lambda e: e.activation(out=scs[:], in_=cTs[:], func=AF.Silu), r=["cTs"], w=["scs"])

                order = [2, 3, 0, 1] + list(range(4, 12))
                for n_, cb in enumerate(order):
                    st_ = wada_st[n_ % 2]
                    sk = "wada_st%d" % (n_ % 2)
                    P.dma(st_[:], wada_d[:, cb * 512:(cb + 1) * 512].rearrange("(k p) n -> p k n", p=128),
                          writes=[sk])
                    bk = n_ % 2
                    for k in range(8):
                        T(lambda e, k=k, st_=st_, bk=bk: e.matmul(banks[bk][0:NSEQ, :], lhsT=scs[:, k, :],
                                                                  rhs=st_[:, k, :], start=(k == 0), stop=(k == 7)),
                          r=["scs", sk], w=[bkey[bk]])
                    V(lambda e, bk=bk, cb=cb: e.tensor_tensor(out=modrow[:, cb * 512:(cb + 1) * 512],
                                                              in0=banks[bk][0:NSEQ, :],
                                                              in1=bada4[:, cb * 512:(cb + 1) * 512], op=ALU.add),
                      r=[bkey[bk], "bada4"], w=["modrow%d" % cb])
                allmod = ["modrow%d" % cb for cb in range(12)]
                for si, sp_ in enumerate([0, 1, 3, 4]):
                    for k in range(8):
                        c0 = (si * 8 + k) * NSEQ
                        T(lambda e, sp_=sp_, k=k, c0=c0: e.transpose(
                            out=banks[2][:, c0:c0 + NSEQ], in_=modrow[0:NSEQ, sp_ * D + k * 128:sp_ * D + (k + 1) * 128],
                            identity=ident[0:NSEQ, 0:NSEQ]), r=allmod + ["ident"], w=[bkey[2]])

                def mview(si):
                    return banks[2][:, si * 8 * NSEQ:(si + 1) * 8 * NSEQ].rearrange("p (k b) -> p k b", b=NSEQ)

                V(lambda e: e.tensor_copy(out=sh1T[:], in_=mview(0)), r=[bkey[2]], w=["sh1T"])
                V(lambda e: e.scalar_tensor_tensor(out=S1T[:], in0=mview(1), scalar=1.0,
                                                   in1=gpre[:].unsqueeze(2).broadcast_to([128, 8, NSEQ]),
                                                   op0=ALU.add, op1=ALU.mult), r=[bkey[2], "gpre"], w=["S1T"])
                V(lambda e: e.tensor_copy(out=sh2T[:], in_=mview(2)), r=[bkey[2]], w=["sh2T"])
                V(lambda e: e.scalar_tensor_tensor(out=S2T[:], in0=mview(3), scalar=1.0,
                                                   in1=gpre2[:].unsqueeze(2).broadcast_to([128, 8, NSEQ]),
                                                   op0=ALU.add, op1=ALU.mult), r=[bkey[2], "gpre2"], w=["S2T"])
                V(lambda e: e.tensor_tensor(out=gmod[:, 0, :], in0=modrow[:, 2 * D:3 * D], in1=gpost4[:, 0, :],
                                            op=ALU.mult), r=allmod + ["gpost4a"], w=["gmod0"])
                V(lambda e: e.tensor_tensor(out=gmod[:, 1, :], in0=modrow[:, 5 * D:6 * D], in1=gpost4[:, 1, :],
                                            op=ALU.mult), r=allmod + ["gpost4b"], w=["gmod1"])

                cast_engs = ["dve", "act", "pool"]
                ci = 0
                for k in range(8):
                    st_ = win_st[k % 2]
                    sk = "win_st%d" % (k % 2)
                    P.dma(st_[:], win_d[k * 128:(k + 1) * 128, :], writes=[sk])
                    for h0, h1 in ((0, 1188), (1188, DIN)):
                        eng = cast_engs[ci % 3]
                        ci += 1
                        if eng == "act":
                            A(lambda e, k=k, st_=st_, h0=h0, h1=h1: e.activation(out=Win[:, k, h0:h1], in_=st_[:, h0:h1],
                                                                              func=AF.Copy), r=[sk], w=["Win"])
                        else:
                            P.op(eng, lambda e, k=k, st_=st_, h0=h0, h1=h1: e.tensor_copy(out=Win[:, k, h0:h1],
                                                                                       in_=st_[:, h0:h1]),
                                 [sk], ["Win"])
                for k in range(8):
                    st_ = win_st[k % 2]
                    sk = "win_st%d" % (k % 2)
                    P.dma(st_[:, 0:D], wout_d[k * 128:(k + 1) * 128, :], writes=[sk])
                    eng = cast_engs[ci % 3]
                    ci += 1
                    if eng == "act":
                        A(lambda e, k=k, st_=st_: e.activation(out=Wout[:, k, :], in_=st_[:, 0:D], func=AF.Copy),
                          r=[sk], w=["Wout"])
                    else:
                        P.op(eng, lambda e, k=k, st_=st_: e.tensor_copy(out=Wout[:, k, :], in_=st_[:, 0:D]),
                             [sk], ["Wout"])
                for g in range(4):
                    G(lambda e, g=g: e.affine_select(out=wsm[:, g, :], in_=ws_sb[:, g, :], pattern=[[-1, 128]],
                                                     compare_op=ALU.is_ge, fill=0.0, base=0, channel_multiplier=1),
                      r=["ws_sb"], w=["wsm"])
                for g in range(4):
                    T(lambda e, g=g: e.transpose(out=banks[3][:, g * 128:(g + 1) * 128], in_=wsm[:, g, :],
                                                 identity=ident[:]), r=["wsm", "ident"], w=[bkey[3]])
                V(lambda e: e.tensor_copy(out=WmT[:], in_=banks[3][:, :].rearrange("p (g t) -> p g t", g=4)),
                  r=[bkey[3]], w=["WmT"])

                NA = NTT * 8
                V(lambda e: e.tensor_copy(out=posf[:], in_=posi[:]), r=["posi"], w=["posf"])
                V(lambda e: e.tensor_tensor(out=ang[:].rearrange("p (t f) -> p t f", f=8),
                                            in0=posf[:].unsqueeze(2).broadcast_to([128, NTT, 8]),
                                            in1=invf[:].unsqueeze(1).broadcast_to([128, NTT, 8]), op=ALU.mult),
                  r=["posf", "invf"], w=["ang"])

                def reduce_sin(dst, src_key, shift):
                    V(lambda e: e.tensor_scalar(out=ang2[:], in0=ang[:], scalar1=shift, scalar2=None, op0=ALU.add),
                      r=["ang"], w=["ang2"])
                    V(lambda e: e.tensor_scalar(out=angk[:], in0=ang2[:], scalar1=1.0 / TWO_PI, scalar2=None,
                                                op0=ALU.mult), r=["ang2"], w=["angk"])
                    V(lambda e: e.tensor_copy(out=angf[:], in_=angk[:]), r=["angk"], w=["angf"])
                    V(lambda e: e.scalar_tensor_tensor(out=ang2[:], in0=angf[:], scalar=-TWO_PI, in1=ang2[:],
                                                       op0=ALU.mult, op1=ALU.add), r=["angf", "ang2"], w=["ang2"])
                    V(lambda e: e.tensor_scalar(out=angm[:], in0=ang2[:], scalar1=math.pi, scalar2=-TWO_PI,
                                                op0=ALU.is_gt, op1=ALU.mult), r=["ang2"], w=["angm"])
                    V(lambda e: e.tensor_tensor(out=ang2[:], in0=ang2[:], in1=angm[:], op=ALU.add),
                      r=["ang2", "angm"], w=["ang2"])
                    V(lambda e: e.tensor_scalar(out=angm[:], in0=ang2[:], scalar1=-math.pi, scalar2=TWO_PI,
                                                op0=ALU.is_lt, op1=ALU.mult), r=["ang2"], w=["angm"])
                    V(lambda e: e.tensor_tensor(out=ang2[:], in0=ang2[:], in1=angm[:], op=ALU.add),
                      r=["ang2", "angm"], w=["ang2"])
                    V(lambda e: e.tensor_scalar(out=ang2[:], in0=ang2[:], scalar1=-3.1415925, scalar2=3.1415925,
                                                op0=ALU.max, op1=ALU.min), r=["ang2"], w=["ang2"])
                    A(lambda e: e.activation(out=dst[:].rearrange("p t f -> p (t f)"), in_=ang2[:], func=AF.Sin),
                      r=["ang2"], w=[src_key])

                reduce_sin(sn, "sn", 0.0)
                reduce_sin(cs, "cs", math.pi / 2.0)
                P.barrier()

            if stop == "setup":
                P.finish()
                return
            tap("S1T", S1T[:].rearrange("p k b -> p (k b)"), ["S1T"])
            tap("gmod", gmod[:].rearrange("b g d -> b (g d)"), ["gmod0", "gmod1"])
            tap("cs", cs[:].rearrange("p t f -> p (t f)"), ["cs"])
            tap("sn", sn[:].rearrange("p t f -> p (t f)"), ["sn"])

            qT2 = asb("qT2", [128, 4, S], BF16)
            kTz = [asb("kTz%d" % i, [128, 2, S], BF16) for i in range(2)]
            kiTz = [asb("kiTz%d" % i, [128, S], BF16) for i in range(2)]
            qiT2 = asb("qiT2", [128, S // 32, 4, 32], BF16)
            v_aug = asb("v_aug", [128, NT, 2, 65], BF16)
            w_tok = asb("w_tok", [128, NT, 8])
            mTa = asb("mTa", [128, 4, S], BF16)
            G1row = asb("G1row", [128, D])
            junkA = asb("junkA", [128, D], BF16)
            G(lambda e: e.memset(v_aug[:].rearrange("p a b c -> p (a b) c")[:, :, 64:65], 1.0), w=["v_aug"])
            G(lambda e: e.memset(kTz[0][64:128, :, :], 0.0), w=["kTd"])
            G(lambda e: e.memset(kTz[1][0:64, :, :], 0.0), w=["kTd"])
            G(lambda e: e.memset(kiTz[0][64:128, :], 0.0), w=["kiTd"])
            G(lambda e: e.memset(kiTz[1][0:64, :], 0.0), w=["kiTd"])

            for b in range(NSEQ):
                for n in range(2):
                    T(lambda e, n=n: e.matmul(banks[n][:, :], lhsT=sel[0:NSEQ, b, :],
                                              rhs=gmod[0:NSEQ, 0, n * 512:(n + 1) * 512], start=True, stop=True),
                      r=["sel", "gmod0"], w=[bkey[n]])
                    V(lambda e, n=n: e.tensor_copy(out=G1row[:, n * 512:(n + 1) * 512], in_=banks[n][:, :]),
                      r=[bkey[n]], w=["G1row"])

                with contextlib.ExitStack() as pst:
                    psb = mk_sb(pst)
                    xt = [psb("xt%d" % i, [128, D]) for i in range(2)]
                    xn = [psb("xn%d" % i, [128, D]) for i in range(2)]
                    hT = [psb("hT%d" % i, [128, 8, 128], BF16) for i in range(2)]
                    ssx = psb("ssx", [128, 2])
                    rsx = psb("rsx", [128, 2])
                    zu = [psb("zu%d" % i, [128, 512], BF16) for i in range(2)]
                    zv = psb("zv", [128, 512], BF16)
                    vn = [psb("vn%d" % i, [128, 512], BF16) for i in range(2)]
                    ssv = psb("ssv", [128, 4])
                    rsv = psb("rsv", [128, 4])
                    ya = psb("ya", [128, 512])
                    ssa = psb("ssa", [128, 1])
                    rsa = psb("rsa", [128, 1])
                    ma = psb("ma", [128, 512], BF16)
                    q_tok = [psb("q_tok%d" % i, [128, 8, 64], BF16) for i in range(2)]
                    qi_tok = [psb("qi_tok%d" % i, [128, 8, 64], BF16) for i in range(2)]
                    kd = [psb("kd%d" % i, [128, 2, 2, 64], BF16) for i in range(2)]
                    kid = [psb("kid%d" % i, [128, 2, 64], BF16) for i in range(2)]
                    rt = [psb("rt%d" % i, [128, 8, 8]) for i in range(4)]

                    def s1_load(i):
                        it = b * NT + i
                        P.dma(xt[i % 2][:], x_d[it * 128:(it + 1) * 128, :], writes=["xt%d" % (i % 2)])

                    def s1_pre(i):
                        j = i % 2
                        A(lambda e: e.activation(out=junkA[:], in_=xt[j][:], func=AF.Square,
                                                 accum_out=ssx[:, j:j + 1]), r=["xt%d" % j], w=["junkA", "ssx%d" % j])
                        rstd(rsx[:, j:j + 1], ssx[:, j:j + 1], 1, 1.0 / D, ["ssx%d" % j], ["rsx%d" % j])
                        V(lambda e: e.tensor_scalar(out=xn[j][:], in0=xt[j][:], scalar1=rsx[:, j:j + 1], scalar2=None,
                                                    op0=ALU.mult), r=["xt%d" % j, "rsx%d" % j], w=["xn%d" % j])

                    def s1_post(i):
                        j = i % 2
                        for k in range(8):
                            T(lambda e, k=k: e.transpose(out=banks[k // 4][:, (k % 4) * 128:(k % 4 + 1) * 128],
                                                         in_=xn[j][:, k * 128:(k + 1) * 128], identity=ident[:]),
                              r=["xn%d" % j, "ident"], w=[bkey[k // 4]])
                        for k in range(8):
                            A(lambda e, k=k: e.activation(out=hT[j][:, k, :],
                                                          in_=banks[k // 4][:, (k % 4) * 128:(k % 4 + 1) * 128],
                                                          func=AF.Identity, scale=S1T[:, k, b:b + 1],
                                                          bias=sh1T[:, k, b:b + 1]),
                              r=[bkey[k // 4], "S1T", "sh1T"], w=["hT%d" % j])

                    GROUPS = [(0, 512, 2), (512, 512, 3), (1024, 512, 4), (1536, 328, 5), (1864, 512, 6)]

                    def rope(src3, src_key, dst3, dst_key, H, it):
                        c = cs[:, it, :].unsqueeze(1).broadcast_to([128, H, 8])
                        s_ = sn[:, it, :].unsqueeze(1).broadcast_to([128, H, 8])
                        x1 = src3[:, :, 0:8]
                        x2 = src3[:, :, 8:16]
                        t = [r_[:, 0:H, :] for r_ in rt]
                        V(lambda e: e.tensor_tensor(out=t[0], in0=x1, in1=c, op=ALU.mult), r=[src_key, "cs"], w=["rt0"])
                        V(lambda e: e.tensor_tensor(out=t[1], in0=x2, in1=s_, op=ALU.mult), r=[src_key, "sn"], w=["rt1"])
                        V(lambda e: e.tensor_tensor(out=dst3[:, :, 0:8], in0=t[0], in1=t[1], op=ALU.subtract),
                          r=["rt0", "rt1"], w=[dst_key])
                        V(lambda e: e.tensor_tensor(out=t[2], in0=x2, in1=c, op=ALU.mult), r=[src_key, "cs"], w=["rt2"])
                        V(lambda e: e.tensor_tensor(out=t[3], in0=x1, in1=s_, op=ALU.mult), r=[src_key, "sn"], w=["rt3"])
                        V(lambda e: e.tensor_tensor(out=dst3[:, :, 8:16], in0=t[2], in1=t[3], op=ALU.add),
                          r=["rt2", "rt3"], w=[dst_key])
                        A(lambda e: e.activation(out=dst3[:, :, 16:64], in_=src3[:, :, 16:64], func=AF.Copy),
                          r=[src_key], w=[dst_key])

                    def s2(i):
                        j = i % 2
                        it = b * NT + i
                        for (c0, n, bk) in GROUPS:
                            for k in range(8):
                                T(lambda e, k=k, c0=c0, n=n, bk=bk: e.matmul(banks[bk][:, 0:n], lhsT=hT[j][:, k, :],
                                                                             rhs=Win[:, k, c0:c0 + n], start=(k == 0),
                                                                             stop=(k == 7)),
                                  r=["hT%d" % j, "Win"], w=[bkey[bk]])
                        A(lambda e: e.activation(out=zu[j][:], in_=banks[2][:, :], func=AF.Gelu_apprx_tanh),
                          r=[bkey[2]], w=["zu%d" % j])
                        A(lambda e: e.activation(out=zv[:], in_=banks[3][:, :], func=AF.Gelu_apprx_tanh),
                          r=[bkey[3]], w=["zv"])
                        rope(banks[4][:, :].rearrange("p (h d) -> p h d", d=64), bkey[4], q_tok[j][:], "q_tok%d" % j, 8, it)
                        rope(banks[5][:, 0:128].rearrange("p (h d) -> p h d", d=64), bkey[5], kd[j][:, :, 0, :],
                             "kd%d" % j, 2, it)
                        V(lambda e: e.tensor_copy(out=kd[j][:, :, 1, :], in_=kd[j][:, :, 0, :]), r=["kd%d" % j],
                          w=["kd%d" % j])
                        V(lambda e: e.tensor_copy(out=v_aug[:, i, :, 0:64],
                                                  in_=banks[5][:, 128:256].rearrange("p (h d) -> p h d", d=64)),
                          r=[bkey[5]], w=["v_aug"])
                        rope(banks[5][:, 256:320].rearrange("p (h d) -> p h d", d=64), bkey[5], kid[j][:, 0:1, :],
                             "kid%d" % j, 1, it)
                        V(lambda e: e.tensor_copy(out=kid[j][:, 1:2, :], in_=kid[j][:, 0:1, :]), r=["kid%d" % j],
                          w=["kid%d" % j])
                        V(lambda e: e.tensor_copy(out=w_tok[:, i, :], in_=banks[5][:, 320:328]), r=[bkey[5]],
                          w=["w_tok"])
                        rope(banks[6][:, :].rearrange("p (h d) -> p h d", d=64), bkey[6], qi_tok[j][:], "qi_tok%d" % j, 8, it)
                        for g in range(4):
                            A(lambda e, g=g: e.activation(out=junkA[:, 0:128], in_=zv[:, g * 128:(g + 1) * 128],
                                                          func=AF.Square, accum_out=ssv[:, g:g + 1]),
                              r=["zv"], w=["junkA", "ssv"])
                        rstd(rsv[:, 0:4], ssv[:, 0:4], 4, 1.0 / 128, ["ssv"], ["rsv"])
                        for g in range(4):
                            V(lambda e, g=g: e.scalar_tensor_tensor(out=vn[j][:, g * 128:(g + 1) * 128],
                                                                    in0=zv[:, g * 128:(g + 1) * 128],
                                                                    scalar=rsv[:, g:g + 1],
                                                                    in1=gv_row[:, g * 128:(g + 1) * 128],
                                                                    op0=ALU.mult, op1=ALU.mult),
                              r=["zv", "rsv", "gv_row"], w=["vn%d" % j])

                    def s3a(i):
                        j = i % 2
                        ts = slice(i * 128, (i + 1) * 128)
                        qf = q_tok[j][:].rearrange("p h d -> p (h d)")
                        for c in range(4):
                            T(lambda e, c=c: e.transpose(out=bbf[0][:, c * 128:(c + 1) * 128],
                                                         in_=qf[:, c * 128:(c + 1) * 128], identity=identb[:]),
                              r=["q_tok%d" % j, "identb"], w=[bkey[0]])
                        for kv in range(2):
                            T(lambda e, kv=kv: e.transpose(out=bbf[0][:, 512 + kv * 128:512 + (kv + 1) * 128],
                                                           in_=kd[j][:, kv, :, :].rearrange("p a d -> p (a d)"),
                                                           identity=identb[:]),
                              r=["kd%d" % j, "identb"], w=[bkey[0]])
                        T(lambda e: e.transpose(out=bbf[0][:, 768:896], in_=kid[j][:].rearrange("p a d -> p (a d)"),
                                                identity=identb[:]), r=["kid%d" % j, "identb"], w=[bkey[0]])
                        V(lambda e: e.tensor_copy(out=qT2[:, :, ts],
                                                  in_=bbf[0][:, 0:512].rearrange("p (c t) -> p c t", t=128)),
                          r=[bkey[0]], w=["qT2"])
                        for par in range(2):
                            ps = slice(64 * par, 64 * par + 64)
                            V(lambda e, par=par, ps=ps: e.tensor_copy(
                                out=kTz[par][ps, :, ts],
                                in_=bbf[0][ps, 512:768].rearrange("p (c t) -> p c t", t=128)),
                              r=[bkey[0]], w=["kTd"])
                            V(lambda e, par=par, ps=ps: e.tensor_copy(out=kiTz[par][ps, ts], in_=bbf[0][ps, 768:896]),
                              r=[bkey[0]], w=["kiTd"])
                        qif = qi_tok[j][:].rearrange("p h d -> p (h d)")
                        for c in range(4):
                            T(lambda e, c=c: e.transpose(out=bbf[1][:, c * 128:(c + 1) * 128],
                                                         in_=qif[:, c * 128:(c + 1) * 128], identity=identb[:]),
                              r=["qi_tok%d" % j, "identb"], w=[bkey[1]])
                        A(lambda e: e.activation(out=qiT2[:, i * 4:(i + 1) * 4, :, :],
                                                 in_=bbf[1][:, 0:512].rearrange("p (c g t) -> p g c t", c=4, g=4),
                                                 func=AF.Copy), r=[bkey[1]], w=["qiT2"])
                        for g in range(4):
                            T(lambda e, g=g: e.matmul(banks[7][:, g * 128:(g + 1) * 128], lhsT=WmT[:, g, :],
                                                      rhs=vn[j][:, g * 128:(g + 1) * 128], start=True, stop=True),
                              r=["WmT", "vn%d" % j], w=[bkey[7]])
                        for g in range(4):
                            V(lambda e, g=g: e.scalar_tensor_tensor(out=ya[:, g * 128:(g + 1) * 128],
                                                                    in0=banks[7][:, g * 128:(g + 1) * 128],
                                                                    scalar=bcol[:, g:g + 1],
                                                                    in1=zu[j][:, g * 128:(g + 1) * 128],
                                                                    op0=ALU.add, op1=ALU.mult),
                              r=[bkey[7], "bcol", "zu%d" % j], w=["ya"])
                        A(lambda e: e.activation(out=junkA[:, 0:512], in_=ya[:], func=AF.Square, accum_out=ssa[:]),
                          r=["ya"], w=["junkA", "ssa"])
                        rstd(rsa[:], ssa[:], 1, 1.0 / 512, ["ssa"], ["rsa"])
                        V(lambda e: e.scalar_tensor_tensor(out=ma[:], in0=ya[:], scalar=rsa[:, 0:1], in1=goa_row[:],
                                                           op0=ALU.mult, op1=ALU.mult),
                          r=["ya", "rsa", "goa_row"], w=["ma"])
                        if b == 0 and i == 0:
                            tap("ya", ya[:], ["ya"])

                    def s3b(i):
                        ts = slice(i * 128, (i + 1) * 128)
                        for c in range(4):
                            T(lambda e, c=c: e.transpose(out=bbf[7][:, c * 128:(c + 1) * 128],
                                                         in_=ma[:, c * 128:(c + 1) * 128], identity=identb[:]),
                              r=["ma", "identb"], w=[bkey[7]])
                        A(lambda e: e.activation(out=mTa[:, :, ts],
                                                 in_=bbf[7][:, 0:512].rearrange("p (c t) -> p c t", t=128),
                                                 func=AF.Copy), r=[bkey[7]], w=["mTa"])

                    s1_load(0)
                    if NT > 1:
                        s1_load(1)
                    s1_pre(0)
                    s1_post(0)
                    if NT > 2:
                        s1_load(2)
                    if NT > 1:
                        s1_pre(1)
                        s1_post(1)
                    for n in range(NT + 2):
                        if n + 2 < NT:
                            s1_pre(n + 2)
                            if n + 3 < NT:
                                s1_load(n + 3)
                        if n < NT:
                            s2(n)
                        if 0 <= n - 2 < NT:
                            s3b(n - 2)
                        if 0 <= n - 1 < NT:
                            s3a(n - 1)
                        if n + 2 < NT:
                            s1_post(n + 2)
                    P.barrier()
                    if stop == "proj":
                        P.finish()
                        return

                with contextlib.ExitStack() as tst:
                    tsb = mk_sb(tst)
                    score = tsb("score", [128, S])
                    cmax = tsb("cmax", [128, 4])
                    mask = tsb("mask", [128, S], BF16)
                    maskT = tsb("maskT", [128, NT, 128], BF16)
                    rl = [tsb("rl%d" % i, [128, 512], BF16) for i in range(4)]
                    pT = [tsb("pT%d" % i, [128, 512], BF16) for i in range(4)]
                    Wsel = [tsb("Wsel%d" % i, [128, 8, 128], BF16) for i in range(2)]
                    wrep = tsb("wrep", [128, 2, 128])
                    wcol = tsb("wcol", [128, 8])
                    lo0 = tsb("lo0", [128, 1])
                    hi0 = tsb("hi0", [128, 1])
                    w0 = tsb("w0", [128, 1])
                    wh = tsb("wh", [128, NIT + 1])
                    cbias = tsb("cbias", [128, NT])
                    mid = tsb("mid", [128, 1])
                    cnt = tsb("cnt", [128, 1])
                    btmp = tsb("btmp", [128, 1])
                    rden = tsb("rden", [128, 8])
                    yb = tsb("yb", [128, 512])
                    ssb_ = tsb("ssb_", [128, 1])
                    rsb = tsb("rsb", [128, 1])
                    mb = tsb("mb", [128, 512], BF16)
                    mbT = tsb("mbT", [128, 4, 128], BF16)
                    sso = tsb("sso", [128, 2])
                    rso = tsb("rso", [128, 1])
                    ot = tsb("ot", [128, D])
                    xres = [tsb("xres%d" % i, [128, D]) for i in range(2)]
                    for i in range(2):
                        G(lambda e, i=i: e.memset(Wsel[i][:], 0.0), w=["Wsel%d" % i])
                    for qq in range(NT):
                        G(lambda e, qq=qq: e.memset(cbias[:, qq:qq + 1], float((qq + 1) * 128 - 2 * TOPK) + 0.5),
                          w=["cbias"])
                    rl_i = [0]
                    pT_i = [0]
                    D_i = [0]

                    def wsel_build(qb):
                        wi = qb % 2
                        wk = "Wsel%d" % wi
                        w2v = w_tok[:, qb, :].rearrange("p (i two) -> p i two", two=2)
                        for par in range(2):
                            V(lambda e, par=par: e.tensor_tensor(
                                out=wrep[:, par, :].rearrange("p (i t) -> p i t", t=32),
                                in0=w2v[:, :, par].unsqueeze(2).broadcast_to([128, 4, 32]),
                                in1=D32[:].unsqueeze(1).broadcast_to([128, 4, 32]), op=ALU.mult),
                              r=["w_tok", "D32"], w=["wrep"])
                        for par in range(2):
                            T(lambda e, par=par: e.matmul(banks[0][:, par * 4:(par + 1) * 4], lhsT=wrep[:, par, :],
                                                          rhs=G4[:], start=True, stop=True),
                              r=["wrep", "G4"], w=[bkey[0]])
                        V(lambda e: e.tensor_scalar(out=wcol[:], in0=banks[0][:, 0:8], scalar1=IDX_SCALE, scalar2=None,
                                                    op0=ALU.mult), r=[bkey[0]], w=["wcol"])
                        for par in range(2):
                            for g in range(4):
                                V(lambda e, par=par, g=g: e.tensor_scalar(
                                    out=Wsel[wi][:, par * 4 + g, 32 * g:32 * g + 32], in0=D32[:],
                                    scalar1=wcol[:, par * 4 + g:par * 4 + g + 1], scalar2=None, op0=ALU.mult),
                                  r=["D32", "wcol"], w=[wk])

                    def indexer(qb):
                        N = (qb + 1) * 128
                        nch = (N + 511) // 512
                        wi = qb % 2
                        wk = "Wsel%d" % wi
                        units = []
                        for c in range(nch):
                            n = min(512, N - c * 512)
                            for g in range(4):
                                for par in range(2):
                                    units.append((c, n, g, par))

                        DBK = [0, 1, 3]

                        def dots(u):
                            c, n, g, par = u
                            ri = rl_i[0] % 4
                            dbk = DBK[D_i[0] % 3]
                            D_i[0] += 1
                            rl_i[0] += 1
                            T(lambda e: e.matmul(banks[dbk][:, 0:n],
                                                 lhsT=qiT2[:, qb * 4 + g, :, :].rearrange("p c t -> p (c t)"),
                                                 rhs=kiTz[par][:, c * 512:c * 512 + n], start=True, stop=True),
                              r=["qiT2", "kiTd"], w=[bkey[dbk]])
                            if par == 0:
                                A(lambda e: e.activation(out=rl[ri][:, 0:n], in_=banks[dbk][:, 0:n], func=AF.Relu),
                                  r=[bkey[dbk]], w=["rl%d" % ri])
                            else:
                                V(lambda e: e.tensor_scalar(out=rl[ri][:, 0:n], in0=banks[dbk][:, 0:n], scalar1=0.0,
                                                            scalar2=None, op0=ALU.max), r=[bkey[dbk]], w=["rl%d" % ri])
                            return ri

                        def selmm(u, ri):
                            c, n, g, par = u
                            sbk = 2
                            first = (g == 0 and par == 0)
                            last = (g == 3 and par == 1)
                            T(lambda e: e.matmul(banks[sbk][:, 0:n], lhsT=Wsel[wi][:, par * 4 + g, :],
                                                 rhs=rl[ri][:, 0:n], start=first, stop=last),
                              r=[wk, "rl%d" % ri], w=[bkey[sbk]])
                            if last:
                                V(lambda e: e.tensor_scalar(out=score[:, c * 512:c * 512 + n], in0=banks[sbk][:, 0:n],
                                                            scalar1=1.0, scalar2=None, op0=ALU.mult, op1=ALU.max,
                                                            accum_out=cmax[:, c:c + 1]),
                                  r=[bkey[sbk]], w=["score", "cmax"])

                        ris = {}
                        LOOK = 3
                        for i_ in range(min(LOOK, len(units))):
                            ris[i_] = dots(units[i_])
                        for i_ in range(len(units)):
                            if i_ + LOOK < len(units):
                                ris[i_ + LOOK] = dots(units[i_ + LOOK])
                            selmm(units[i_], ris[i_])

                    def topk_iter(qb):
                        N = (qb + 1) * 128
                        nch = (N + 511) // 512
                        V(lambda e: e.tensor_reduce(out=lo0[:], in_=score[:, 0:N], axis=AX.X, op=ALU.min),
                          r=["score"], w=["lo0"])
                        V(lambda e: e.tensor_reduce(out=hi0[:], in_=cmax[:, 0:nch], axis=AX.X, op=ALU.max),
                          r=["cmax"], w=["hi0"])
                        V(lambda e: e.tensor_tensor(out=score[:, qb * 128:N], in0=score[:, qb * 128:N], in1=NEGM[:],
                                                    op=ALU.add), r=["score", "NEGM"], w=["score"])
                        V(lambda e: e.tensor_tensor(out=w0[:], in0=lo0[:], in1=hi0[:], op=ALU.subtract),
                          r=["hi0", "lo0"], w=["w0"])
                        V(lambda e: e.tensor_scalar(out=wh[:], in0=P2[:], scalar1=w0[:, 0:1], scalar2=None,
                                                    op0=ALU.mult), r=["P2", "w0"], w=["wh"])
                        V(lambda e: e.tensor_scalar(out=mid[:], in0=lo0[:], scalar1=-1.0, scalar2=wh[:, 0:1],
                                                    op0=ALU.mult, op1=ALU.add), r=["lo0", "wh"], w=["mid"])
                        for i in range(NIT):
                            A(lambda e: e.activation(out=mask[:, 0:N], in_=score[:, 0:N], func=AF.Sign,
                                                     bias=mid[:, 0:1], accum_out=cnt[:]),
                              r=["score", "mid"], w=["mask", "cnt"])
                            V(lambda e, i=i: e.scalar_tensor_tensor(out=btmp[:], in0=cnt[:],
                                                                    scalar=float(2 * TOPK - N) - 0.5,
                                                                    in1=wh[:, i:i + 1], op0=ALU.is_ge, op1=ALU.mult),
                              r=["cnt", "wh"], w=["btmp"])
                            V(lambda e, i=i: e.scalar_tensor_tensor(out=mid[:], in0=mid[:], scalar=wh[:, i + 1:i + 2],
                                                                    in1=btmp[:], op0=ALU.subtract, op1=ALU.add),
                              r=["mid", "wh", "btmp"], w=["mid"])
                            yield
                        V(lambda e: e.tensor_scalar(out=mid[:], in0=mid[:], scalar1=-1.0, scalar2=wh[:, NIT:NIT + 1],
                                                    op0=ALU.mult, op1=ALU.add), r=["mid", "wh"], w=["mid"])

                    def topk_finish(qb):
                        N = (qb + 1) * 128
                        if qb < KB:
                            for jj in range(qb + 1):
                                src = TRIU if jj == qb else ONESB
                                G(lambda e, jj=jj, src=src: e.tensor_copy(out=maskT[:, jj, :], in_=src[:]),
                                  r=["TRIU", "ONESB"], w=["maskT"])
                            return
                        V(lambda e: e.tensor_scalar(out=mask[:, 0:N], in0=score[:, 0:N], scalar1=mid[:, 0:1],
                                                    scalar2=None, op0=ALU.is_ge), r=["score", "mid"], w=["mask"])
                        if b == 0 and qb == NT - 1:
                            tap("score", score[:, 0:N], ["score"])
                            tap("thr", mid[:], ["mid"])
                        for jj in range(qb + 1):
                            lb = 4 + jj // 8
                            T(lambda e, jj=jj, lb=lb: e.transpose(out=bbf[lb][:, (jj % 8) * 128:(jj % 8 + 1) * 128],
                                                                  in_=mask[:, jj * 128:(jj + 1) * 128],
                                                                  identity=identb[:]),
                              r=["mask", "identb"], w=[bkey[lb]])
                        for lb in range(4, 4 + (qb + 8) // 8):
                            j0 = (lb - 4) * 8
                            j1 = min(qb + 1, j0 + 8)
                            nj = j1 - j0
                            V(lambda e, lb=lb, j0=j0, j1=j1, nj=nj: e.tensor_copy(
                                out=maskT[:, j0:j1, :],
                                in_=bbf[lb][:, 0:nj * 128].rearrange("p (j t) -> p j t", t=128)),
                              r=[bkey[lb]], w=["maskT"])

                    def attention(qb):
                        qs = slice(qb * 128, (qb + 1) * 128)
                        for kv in range(2):
                            T(lambda e, kv=kv: e.matmul(banks[6 + kv][:, 0:260], lhsT=zerob[:, 0:128],
                                                        rhs=zerob[:, 0:260], start=True, stop=False,
                                                        skip_group_check=True), r=["zerob"], w=[bkey[6 + kv]])

                        def Lstage(jj):
                            ks = slice(jj * 128, (jj + 1) * 128)
                            pis = []
                            for par in range(2):
                                ps = slice(64 * par, 64 * par + 64)
                                lb = 4 + par
                                pi = pT_i[0] % 4
                                pT_i[0] += 1
                                pis.append(pi)
                                for kv in range(2):
                                    T(lambda e, ps=ps, lb=lb, kv=kv, par=par: e.matmul(
                                        banks[lb][:, kv * 256:(kv + 1) * 256], lhsT=kTz[par][:, kv, ks],
                                        rhs=qT2[:, 2 * kv:2 * kv + 2, qs], start=True, stop=True),
                                      r=["kTd", "qT2"], w=[bkey[lb]])
                                A(lambda e, lb=lb, pi=pi: e.activation(out=pT[pi][:], in_=banks[lb][:, :], func=AF.Exp,
                                                                       scale=0.125), r=[bkey[lb]], w=["pT%d" % pi])
                                V(lambda e, pi=pi: e.tensor_tensor(
                                    out=pT[pi][:].rearrange("p (h t) -> p h t", t=128),
                                    in0=pT[pi][:].rearrange("p (h t) -> p h t", t=128),
                                    in1=maskT[:, jj, :].unsqueeze(1).broadcast_to([128, 4, 128]), op=ALU.mult),
                                  r=["pT%d" % pi, "maskT"], w=["pT%d" % pi])
                            return pis

                        def PVstage(jj, pis):
                            for par in range(2):
                                pi = pis[par]
                                for kv in range(2):
                                    for ii in range(2):
                                        hl = 2 * ii + par
                                        T(lambda e, ii=ii, hl=hl, kv=kv, pi=pi: e.matmul(
                                            banks[6 + kv][:, hl * 65:hl * 65 + 65],
                                            lhsT=pT[pi][:, (kv * 2 + ii) * 128:(kv * 2 + ii + 1) * 128],
                                            rhs=v_aug[:, jj, kv, :], start=False, stop=(jj == qb),
                                            skip_group_check=True),
                                          r=["pT%d" % pi, "v_aug"], w=[bkey[6 + kv]])

                        nxt = Lstage(0)
                        for jj in range(qb + 1):
                            cur = nxt
                            if jj + 1 <= qb:
                                nxt = Lstage(jj + 1)
                            PVstage(jj, cur)
                            yield

                    def post_a(qb):
                        for kv in range(2):
                            ov = banks[6 + kv][:, 0:260].rearrange("p (h d) -> p h d", d=65)
                            V(lambda e, kv=kv, ov=ov: e.reciprocal(out=rden[:, kv * 4:(kv + 1) * 4], in_=ov[:, :, 64]),
                              r=[bkey[6 + kv]], w=["rden"])
                            V(lambda e, kv=kv, ov=ov: e.tensor_tensor(
                                out=yb[:, kv * 256:(kv + 1) * 256].rearrange("p (h d) -> p h d", d=64),
                                in0=ov[:, :, 0:64],
                                in1=rden[:, kv * 4:(kv + 1) * 4].unsqueeze(2).broadcast_to([128, 4, 64]),
                                op=ALU.mult), r=[bkey[6 + kv], "rden"], w=["yb"])
                        if b == 0 and qb == NT - 1:
                            tap("yb", yb[:], ["yb"])

                    def post_b1(qb):
                        yield
                        A(lambda e: e.activation(out=junkA[:, 0:512], in_=yb[:], func=AF.Square, accum_out=ssb_[:]),
                          r=["yb"], w=["junkA", "ssb_"])
                        yield
                        rstd(rsb[:], ssb_[:], 1, 1.0 / 512, ["ssb_"], ["rsb"])
                        yield
                        V(lambda e: e.scalar_tensor_tensor(out=mb[:], in0=yb[:], scalar=rsb[:, 0:1], in1=gob_row[:],
                                                           op0=ALU.mult, op1=ALU.mult),
                          r=["yb", "rsb", "gob_row"], w=["mb"])

                    def post_b2(qb):
                        it = b * NT + qb
                        xj = qb % 2
                        for c in range(4):
                            T(lambda e, c=c: e.transpose(out=bbf[2][:, c * 128:(c + 1) * 128],
                                                         in_=mb[:, c * 128:(c + 1) * 128], identity=identb[:]),
                              r=["mb", "identb"], w=[bkey[2]])
                        A(lambda e: e.activation(out=mbT[:], in_=bbf[2][:, 0:512].rearrange("p (c t) -> p c t", t=128),
                                                 func=AF.Copy), r=[bkey[2]], w=["mbT"])
                        yield
                        for n in range(2):
                            for k in range(8):
                                lhs = mTa[:, k, qb * 128:(qb + 1) * 128] if k < 4 else mbT[:, k - 4, :]
                                T(lambda e, n=n, k=k, lhs=lhs: e.matmul(banks[2 + n][:, :], lhsT=lhs,
                                                                        rhs=Wout[:, k, n * 512:(n + 1) * 512],
                                                                        start=(k == 0), stop=(k == 7)),
                                  r=["mTa", "mbT", "Wout"], w=[bkey[2 + n]])
                        for n in range(2):
                            A(lambda e, n=n: e.activation(out=junkA[:, 0:512], in_=banks[2 + n][:, :], func=AF.Square,
                                                          accum_out=sso[:, n:n + 1]),
                              r=[bkey[2 + n]], w=["junkA", "sso%d" % n])
                        yield
                        rstd(rso[:], sso[:, 0:1], 1, 1.0 / D, ["sso0", "sso1"], ["rso"], ss2_ap=sso[:, 1:2])
                        yield
                        for n in range(2):
                            V(lambda e, n=n: e.scalar_tensor_tensor(out=ot[:, n * 512:(n + 1) * 512],
                                                                    in0=banks[2 + n][:, :], scalar=rso[:, 0:1],
                                                                    in1=G1row[:, n * 512:(n + 1) * 512],
                                                                    op0=ALU.mult, op1=ALU.mult),
                              r=[bkey[2 + n], "rso", "G1row"], w=["ot"])
                        G(lambda e: e.tensor_tensor(out=ot[:], in0=ot[:], in1=xres[xj][:], op=ALU.add),
                          r=["ot", "xres%d" % xj], w=["ot"])
                        P.dma(x1s_d[it * 128:(it + 1) * 128, :], ot[:], reads=["ot"],
                              writes=["x1s_%d" % it])

                    def step(g_):
                        if g_ is None:
                            return False
                        try:
                            next(g_)
                            return True
                        except StopIteration:
                            return False

                    def interleave(g1, g2):
                        a1, a2 = g1 is not None, g2 is not None
                        while a1:
                            a1 = step(g1)
                            if a2:
                                a2 = step(g2)
                        return a2

                    def drain(g_):
                        while step(g_):
                            pass

                    if 0 >= KB:
                        wsel_build(0)
                        indexer(0)
                        drain(topk_iter(0))
                    topk_finish(0)
                    if 1 < NT and 1 >= KB:
                        wsel_build(1)
                    pb1 = pb2 = None
                    for qb in range(NT):
                        it = b * NT + qb
                        P.dma(xres[qb % 2][:], x_d[it * 128:(it + 1) * 128, :], writes=["xres%d" % (qb % 2)])
                        tk = None
                        if qb + 1 < NT and qb + 1 >= KB:
                            indexer(qb + 1)
                            tk = topk_iter(qb + 1)
                        if qb + 2 < NT and qb + 2 >= KB:
                            wsel_build(qb + 2)
                        att = attention(qb)
                        a_att, a_tk = True, tk is not None
                        a_p1, a_p2 = pb1 is not None, pb2 is not None
                        nstep = 0
                        while a_att:
                            a_att = step(att)
                            nstep += 1
                            if a_tk and (nstep >= 2 or qb + 1 < 3):
                                a_tk = step(tk)
                            if a_p1:
                                a_p1 = step(pb1)
                            elif a_p2:
                                a_p2 = step(pb2)
                        if a_p1:
                            drain(pb1)
                        if a_p2:
                            drain(pb2)
                        post_a(qb)
                        pb1 = post_b1(qb)
                        pb2 = post_b2(qb)
                        a_p1 = True
                        while a_tk:
                            a_tk = step(tk)
                            if a_p1:
                                a_p1 = step(pb1)
                        if qb + 1 < NT:
                            topk_finish(qb + 1)
                    drain(pb1)
                    drain(pb2)
                    P.barrier()
                    if stop == "attn":
                        P.finish()
                        return
        with contextlib.ExitStack() as bst:
            bsb = mk_sb(bst)
            W1 = bsb("W1", [128, 8, DFF], BF16)
            W2 = bsb("W2", [128, 32, D], BF16)
            wst = [bsb("wst%d" % i, [128, 2048]) for i in range(2)]
            G2row = bsb("G2row", [128, D])
            xg = [bsb("xg%d" % i, [128, D]) for i in range(4)]
            xn2 = [bsb("xn2_%d" % i, [128, D]) for i in range(1)]
            h2T = [bsb("h2T%d" % i, [128, 8, 256], BF16) for i in range(2)]
            rr = [bsb("rr%d" % i, [128, 256], BF16) for i in range(3)]
            fT = [bsb("fT%d" % i, [128, 256], BF16) for i in range(3)]
            junkB = bsb("junkB", [128, D], BF16)
            ss2 = bsb("ss2", [128, 4])
            rs2 = bsb("rs2", [128, 4])
            ssf = bsb("ssf", [128, 4])
            rsf = bsb("rsf", [128, 2])
            of = [bsb("of%d" % i, [128, D]) for i in range(2)]

            cast_engs = ["dve", "act"]
            ci = 0
            wi_ = 0
            for k in range(8):
                for hf in range(2):
                    st_ = wst[wi_ % 2]
                    sk = "wst%d" % (wi_ % 2)
                    wi_ += 1
                    P.dma(st_[:], w1_d[k * 128:(k + 1) * 128, hf * 2048:(hf + 1) * 2048], writes=[sk])
                    for q2 in range(2):
                        eng = cast_engs[ci % 2]
                        ci += 1
                        sl = slice(q2 * 1024, (q2 + 1) * 1024)
                        dl = slice(hf * 2048 + q2 * 1024, hf * 2048 + (q2 + 1) * 1024)
                        if eng == "act":
                            A(lambda e, k=k, st_=st_, sl=sl, dl=dl: e.activation(out=W1[:, k, dl], in_=st_[:, sl],
                                                                              func=AF.Copy), r=[sk], w=["W1"])
                        else:
                            P.op(eng, lambda e, k=k, st_=st_, sl=sl, dl=dl: e.tensor_copy(out=W1[:, k, dl],
                                                                                       in_=st_[:, sl]), [sk], ["W1"])
            for c2 in range(16):
                st_ = wst[wi_ % 2]
                sk = "wst%d" % (wi_ % 2)
                wi_ += 1
                P.dma(st_[:].rearrange("p (c n) -> p c n", n=D),
                      w2_d[c2 * 256:(c2 + 1) * 256, :].rearrange("(c p) n -> p c n", p=128), writes=[sk])
                for q2 in range(2):
                    eng = cast_engs[ci % 2]
                    ci += 1
                    sl = slice(q2 * 1024, (q2 + 1) * 1024)
                    if eng == "act":
                        A(lambda e, c2=c2, q2=q2, st_=st_, sl=sl: e.activation(out=W2[:, c2 * 2 + q2, :], in_=st_[:, sl],
                                                                          func=AF.Copy), r=[sk], w=["W2"])
                    else:
                        P.op(eng, lambda e, c2=c2, q2=q2, st_=st_, sl=sl: e.tensor_copy(out=W2[:, c2 * 2 + q2, :],
                                                                                   in_=st_[:, sl]), [sk], ["W2"])

            NG = NTOK // 256

            def b_load(g):
                for t in range(2):
                    it = g * 2 + t
                    xi = (g % 2) * 2 + t
                    P.dma(xg[xi][:], x1s_d[it * 128:(it + 1) * 128, :], reads=["x1s_%d" % it], writes=["xg%d" % xi])

            def b_prep(g):
                hj = g % 2
                b = (g * 256) // S
                for t in range(2):
                    xi = (g % 2) * 2 + t
                    A(lambda e, xi=xi, t=t: e.activation(out=junkB[:], in_=xg[xi][:], func=AF.Square,
                                                        accum_out=ss2[:, t:t + 1]), r=["xg%d" % xi],
                      w=["junkB", "ss2_%d" % t])
                    rstd(rs2[:, t:t + 1], ss2[:, t:t + 1], 1, 1.0 / D, ["ss2_%d" % t], ["rs2_%d" % t])
                    V(lambda e, xi=xi, t=t: e.tensor_scalar(out=xn2[0][:], in0=xg[xi][:], scalar1=rs2[:, t:t + 1],
                                                           scalar2=None, op0=ALU.mult),
                      r=["xg%d" % xi, "rs2_%d" % t], w=["xn2_0"])
                    for k in range(8):
                        T(lambda e, k=k, t=t: e.transpose(out=banks[6 + k // 4][:, (k % 4) * 128:(k % 4 + 1) * 128],
                                                          in_=xn2[0][:, k * 128:(k + 1) * 128], identity=ident[:]),
                          r=["xn2_0", "ident"], w=[bkey[6 + k // 4]])
                    for k in range(8):
                        A(lambda e, k=k, t=t: e.activation(out=h2T[hj][:, k, t * 128:(t + 1) * 128],
                                                           in_=banks[6 + k // 4][:, (k % 4) * 128:(k % 4 + 1) * 128],
                                                           func=AF.Identity, scale=S2T[:, k, b:b + 1],
                                                           bias=sh2T[:, k, b:b + 1]),
                          r=[bkey[6 + k // 4], "S2T", "sh2T"], w=["h2T%d" % hj])

            f_i = [0]

            def b_main(g):
                hj = g % 2
                b = (g * 256) // S
                if (g * 256) % S == 0:
                    for n in range(2):
                        T(lambda e, n=n: e.matmul(banks[4 + n][:, :], lhsT=sel[0:NSEQ, b, :],
                                                  rhs=gmod[0:NSEQ, 1, n * 512:(n + 1) * 512], start=True, stop=True),
                          r=["sel", "gmod1"], w=[bkey[4 + n]])
                        V(lambda e, n=n: e.tensor_copy(out=G2row[:, n * 512:(n + 1) * 512], in_=banks[4 + n][:, :]),
                          r=[bkey[4 + n]], w=["G2row"])
                def Fst(c):
                    fb = 4 + (c % 2)
                    fi = c % 3
                    for k in range(8):
                        T(lambda e, k=k: e.matmul(banks[fb][:, 0:256], lhsT=W1[:, k, c * 128:(c + 1) * 128],
                                                  rhs=h2T[hj][:, k, :], start=(k == 0), stop=(k == 7)),
                          r=["W1", "h2T%d" % hj], w=[bkey[fb]])
                    A(lambda e: e.activation(out=rr[fi][:], in_=banks[fb][:, 0:256], func=AF.Relu),
                      r=[bkey[fb]], w=["rr%d" % fi])
                    V(lambda e: e.scalar_tensor_tensor(out=fT[fi][:], in0=banks[fb][:, 0:256], scalar=0.0,
                                                       in1=rr[fi][:], op0=ALU.max, op1=ALU.mult),
                      r=[bkey[fb], "rr%d" % fi], w=["fT%d" % fi])

                def P2st(c):
                    fi = c % 3
                    for t in range(2):
                        for n in range(2):
                            ob = t * 2 + n
                            T(lambda e, t=t, n=n, ob=ob: e.matmul(
                                banks[ob][:, :], lhsT=fT[fi][:, t * 128:(t + 1) * 128],
                                rhs=W2[:, c, n * 512:(n + 1) * 512], start=(c == 0), stop=(c == 31)),
                              r=["fT%d" % fi, "W2"], w=[bkey[ob]])

                Fst(0)
                for c in range(32):
                    if c + 1 < 32:
                        Fst(c + 1)
                    P2st(c)
                    if c == 20 and g + 1 < NG:
                        b_prep(g + 1)
                for t in range(2):
                    it = g * 2 + t
                    xi = (g % 2) * 2 + t
                    for n in range(2):
                        A(lambda e, t=t, n=n: e.activation(out=junkB[:, 0:512], in_=banks[t * 2 + n][:, :],
                                                           func=AF.Square, accum_out=ssf[:, t * 2 + n:t * 2 + n + 1]),
                          r=[bkey[t * 2 + n]], w=["junkB", "ssf%d" % (t * 2 + n)])
                    rstd(rsf[:, t:t + 1], ssf[:, t * 2:t * 2 + 1], 1, 1.0 / D, ["ssf%d" % (t * 2), "ssf%d" % (t * 2 + 1)],
                         ["rsf%d" % t], ss2_ap=ssf[:, t * 2 + 1:t * 2 + 2])
                    for n in range(2):
                        V(lambda e, t=t, n=n: e.scalar_tensor_tensor(out=of[t][:, n * 512:(n + 1) * 512],
                                                                     in0=banks[t * 2 + n][:, :], scalar=rsf[:, t:t + 1],
                                                                     in1=G2row[:, n * 512:(n + 1) * 512],
                                                                     op0=ALU.mult, op1=ALU.mult),
                          r=[bkey[t * 2 + n], "rsf%d" % t, "G2row"], w=["of%d" % t])
                    G(lambda e, t=t, xi=xi: e.tensor_tensor(out=of[t][:], in0=of[t][:], in1=xg[xi][:], op=ALU.add),
                      r=["of%d" % t, "xg%d" % xi], w=["of%d" % t])
                    P.dma(out_d[it * 128:(it + 1) * 128, :], of[t][:], reads=["of%d" % t])
                if g + 2 < NG:
                    b_load(g + 2)

            b_load(0)
            if NG > 1:
                b_load(1)
            b_prep(0)
            for g in range(NG):
                b_main(g)
            P.finish()
        print("program built: instrs=%d waits=%d" % (P.ninstr, P.nwaits), flush=True)


def make_core_inputs(ci, NSEQ, S, x, c, positions, w_ada, b_ada, g_pre_mix, w_in, g_sgu_v, w_spatial, b_spatial,
                     g_out_sgu, g_out_attn, w_out, g_post_mix, g_pre_ffn, w_ff1, w_ff2, g_post_ffn):
    f32 = np.float32
    bs = slice(ci * NSEQ, (ci + 1) * NSEQ)
    NT = S // 128
    xc = np.ascontiguousarray(x[bs]).reshape(NSEQ * S, D).astype(f32, copy=False)
    cc = np.asarray(c[bs], dtype=f32)
    cT = np.ascontiguousarray(cc.T.reshape(8, 128, NSEQ).transpose(1, 0, 2))
    pos = np.ascontiguousarray(np.asarray(positions[bs]).reshape(NSEQ * NT, 128).T.astype(np.int32))
    wi = np.asarray(w_in[0], dtype=f32)
    perm = np.concatenate([np.arange(0, 1792), np.arange(2304, 2376), np.arange(1792, 2304)])
    wi_p = np.ascontiguousarray(wi[:, perm])
    return {
        "x": xc, "cT": cT, "pos": pos,
        "w_ada": np.ascontiguousarray(w_ada[0], dtype=f32),
        "b_ada": np.ascontiguousarray(b_ada[0:1], dtype=f32),
        "w_in": wi_p,
        "gpre": np.ascontiguousarray(np.asarray(g_pre_mix[0], dtype=f32).reshape(8, 128).T),
        "gpre2": np.ascontiguousarray(np.asarray(g_pre_ffn[0], dtype=f32).reshape(8, 128).T),
        "gv": np.ascontiguousarray(g_sgu_v[0:1], dtype=f32),
        "ws": np.ascontiguousarray(np.asarray(w_spatial[0], dtype=f32).transpose(1, 0, 2)),
        "bs": np.ascontiguousarray(np.asarray(b_spatial[0], dtype=f32).T),
        "goa": np.ascontiguousarray(g_out_sgu[0:1], dtype=f32),
        "gob": np.ascontiguousarray(g_out_attn[0:1], dtype=f32),
        "w_out": np.ascontiguousarray(w_out[0], dtype=f32),
        "gpost": np.ascontiguousarray(g_post_mix[0:1], dtype=f32),
        "w1": np.ascontiguousarray(w_ff1[0], dtype=f32),
        "w2": np.ascontiguousarray(w_ff2[0], dtype=f32),
        "gpost2": np.ascontiguousarray(g_post_ffn[0:1], dtype=f32),
    }


def run(inputs, n_cores, NSEQ, S, taps=None, trace=False, stop=None):
    nc = bass.Bass("TRN2", target_bir_lowering=False)
    try:
        build_program(nc, NSEQ=NSEQ, S=S, taps=taps, stop=stop)
    except StopBuild:
        pass
    in_maps = [make_core_inputs(ci, NSEQ, S, **inputs) for ci in range(n_cores)]
    res = run_bass_kernel_spmd(nc, in_maps, core_ids=list(range(n_cores)), trace=trace)
    return res


def kernel(**inputs):
    inputs = {k: np.asarray(v) for k, v in inputs.items()}
    B, S, _ = inputs["x"].shape
    NSEQ = B // NCORES
    res = run(inputs, NCORES, NSEQ, S)
    outs = [np.asarray(r["out"]).reshape(NSEQ, S, D) for r in res.results]
    return np.concatenate(outs, axis=0).astype(np.float32, copy=False)
```

```python
import contextlib
import math
import numpy as np
import concourse.bass as bass
import concourse.mybir as mybir
from concourse.bass_utils import run_bass_kernel_spmd

F32 = mybir.dt.float32
BF16 = mybir.dt.bfloat16
I32 = mybir.dt.int32
AF = mybir.ActivationFunctionType
ALU = mybir.AluOpType
AX = mybir.AxisListType

D = 1024
DIN = 2376
DFF = 4096
NCORES = 8
EPS = 1e-6
NIT = 16
IDX_SCALE = (64 ** -0.5) * (8 ** -0.5)
TWO_PI = 2.0 * math.pi


class StopBuild(Exception):
    pass


class Prog:
    NDMA = 32

    def __init__(self, nc, stack):
        self.nc = nc
        self.eng = {"pe": nc.tensor, "act": nc.scalar, "dve": nc.vector,
                    "pool": nc.gpsimd, "sp": nc.sync}
        self.sem = {k: stack.enter_context(nc.semaphore("c_" + k)) for k in self.eng}
        self.cnt = {k: 0 for k in self.eng}
        self.dsem = [stack.enter_context(nc.semaphore("d%d" % i)) for i in range(self.NDMA)]
        self.dval = [0] * self.NDMA
        self.dnext = 0
        self.seen = {k: {} for k in self.eng}
        self.res = {}
        self.nwaits = 0
        self.ninstr = 0

    def _wait(self, eng, dep):
        kind, key, val = dep
        if kind == "e":
            if key == "pe" and eng == "pe":
                return
            sem = self.sem[key]
            skey = key
        else:
            sem = self.dsem[key]
            skey = ("d", key)
        if self.seen[eng].get(skey, 0) >= val:
            return
        self.seen[eng][skey] = val
        self.eng[eng].wait_ge(sem, val)
        self.nwaits += 1

    def _deps(self, eng, reads, writes):
        deps = []
        for r in reads:
            st = self.res.get(r)
            if st and st["w"]:
                deps.append(st["w"])
        for w in writes:
            st = self.res.get(w)
            if st:
                if st["w"]:
                    deps.append(st["w"])
                deps.extend(st["r"])
        for d in deps:
            self._wait(eng, d)

    def _record(self, token, reads, writes):
        for r in reads:
            st = self.res.setdefault(r, {"w": None, "r": []})
            st["r"].append(token)
            if len(st["r"]) > 48:
                best = {}
                for t in st["r"]:
                    k = (t[0], t[1])
                    if k not in best or best[k][2] < t[2]:
                        best[k] = t
                st["r"] = list(best.values())
        for w in writes:
            self.res[w] = {"w": token, "r": []}

    def op(self, eng, fn, reads=(), writes=()):
        self._deps(eng, reads, writes)
        ins = fn(self.eng[eng])
        self.cnt[eng] += 1
        ins.then_inc(self.sem[eng], 1)
        token = ("e", eng, self.cnt[eng])
        self._record(token, reads, writes)
        self.ninstr += 1
        return token

    def dma(self, out, in_, reads=(), writes=(), q="sp", **kw):
        self._deps(q, reads, writes)
        i = self.dnext
        self.dnext = (self.dnext + 1) % self.NDMA
        if self.dval[i] > 0:
            self._wait(q, ("d", i, self.dval[i]))
        ins = self.eng[q].dma_start(out=out, in_=in_, **kw)
        self.dval[i] += 16
        ins.then_inc(self.dsem[i], 16)
        token = ("d", i, self.dval[i])
        self._record(token, reads, writes)
        self.ninstr += 1
        return token

    def barrier(self):
        for e in self.eng:
            for f in self.eng:
                if f != e and self.cnt[f] > 0:
                    self._wait(e, ("e", f, self.cnt[f]))
            for i in range(self.NDMA):
                if self.dval[i] > 0:
                    self._wait(e, ("d", i, self.dval[i]))
        self.res = {}

    def finish(self):
        for i in range(self.NDMA):
            if self.dval[i] > 0:
                self._wait("sp", ("d", i, self.dval[i]))
        for f in self.eng:
            if f != "sp" and self.cnt[f] > 0:
                self._wait("sp", ("e", f, self.cnt[f]))


def build_program(nc, NSEQ=4, S=2048, taps=None, stop=None):
    NT = S // 128
    NTT = NSEQ * NT
    NTOK = NSEQ * S
    TOPK = min(256, S // 4)
    KB = TOPK // 128
    taps = taps or {}

    def din(name, shape, dt=F32):
        return nc.dram_tensor(name, list(shape), dt, kind="ExternalInput").ap()

    x_d = din("x", [NTOK, D])
    cT_d = din("cT", [128, 8, NSEQ])
    pos_d = din("pos", [128, NTT], I32)
    wada_d = din("w_ada", [D, 6 * D])
    bada_d = din("b_ada", [1, 6 * D])
    win_d = din("w_in", [D, DIN])
    gpre_d = din("gpre", [128, 8])
    gpre2_d = din("gpre2", [128, 8])
    gv_d = din("gv", [1, 512])
    ws_d = din("ws", [128, 4, 128])
    bs_d = din("bs", [128, 4])
    goa_d = din("goa", [1, 512])
    gob_d = din("gob", [1, 512])
    wout_d = din("w_out", [D, D])
    gpost_d = din("gpost", [1, D])
    w1_d = din("w1", [D, DFF])
    w2_d = din("w2", [DFF, D])
    gpost2_d = din("gpost2", [1, D])
    out_d = nc.dram_tensor("out", [NTOK, D], F32, kind="ExternalOutput").ap()
    x1s_d = out_d
    tap_d = {k: nc.dram_tensor("tap_" + k, list(shp), F32, kind="ExternalOutput").ap()
             for k, shp in taps.items()}

    with contextlib.ExitStack() as gst:
        P = Prog(nc, gst)

        uid = [0]

        def mk_sb(stack):
            def sb(name, shape, dt=F32):
                uid[0] += 1
                return stack.enter_context(nc.sbuf_tensor("s%d_%s" % (uid[0], name), list(shape), dt))
            return sb

        gsb = mk_sb(gst)
        banks = [gst.enter_context(nc.psum_tensor("bank%d" % i, [128, 512], F32)) for i in range(8)]
        bkey = ["b%d" % i for i in range(8)]
        bbf = [b[:].bitcast(BF16) for b in banks]

        def V(fn, r=(), w=()):
            return P.op("dve", fn, r, w)

        def A(fn, r=(), w=()):
            return P.op("act", fn, r, w)

        def G(fn, r=(), w=()):
            return P.op("pool", fn, r, w)

        def T(fn, r=(), w=()):
            return P.op("pe", fn, r, w)

        ident = gsb("ident", [128, 128])
        identb = gsb("identb", [128, 128], BF16)
        ones_f = gsb("ones_f", [128, 128])
        zeros_f = gsb("zeros_f", [128, 128])
        NEGM = gsb("NEGM", [128, 128])
        TRIU = gsb("TRIU", [128, 128], BF16)
        ONESB = gsb("ONESB", [128, 128], BF16)
        D32 = gsb("D32", [128, 32])
        G4 = gsb("G4", [128, 4])
        zerob = gsb("zerob", [128, 260], BF16)
        P2 = gsb("P2", [128, NIT + 1])
        mhalf = gsb("mhalf", [128, 16])
        iot = gsb("iot", [128, 8], I32)
        iof = gsb("iof", [128, 8])
        invf = gsb("invf", [128, 8])
        rs_tmp = gsb("rs_tmp", [128, 16])

        G(lambda e: e.memset(ones_f[:], 1.0), w=["ones_f"])
        G(lambda e: e.memset(zeros_f[:], 0.0), w=["zeros_f"])
        G(lambda e: e.affine_select(out=ident[:], in_=ones_f[:], pattern=[[-1, 128]], compare_op=ALU.is_equal,
                                    fill=0.0, base=0, channel_multiplier=1), r=["ones_f"], w=["ident"])
        V(lambda e: e.tensor_copy(out=identb[:], in_=ident[:]), r=["ident"], w=["identb"])
        G(lambda e: e.affine_select(out=NEGM[:], in_=zeros_f[:], pattern=[[-1, 128]], compare_op=ALU.is_ge,
                                    fill=-1.0e30, base=0, channel_multiplier=1), r=["zeros_f"], w=["NEGM"])
        G(lambda e: e.affine_select(out=TRIU[:], in_=ones_f[:], pattern=[[1, 128]], compare_op=ALU.is_ge,
                                    fill=0.0, base=0, channel_multiplier=-1), r=["ones_f"], w=["TRIU"])
        V(lambda e: e.tensor_copy(out=ONESB[:], in_=ones_f[:]), r=["ones_f"], w=["ONESB"])
        for m in range(4):
            G(lambda e, m=m: e.affine_select(out=D32[32 * m:32 * m + 32, :], in_=ones_f[32 * m:32 * m + 32, 0:32],
                                             pattern=[[-1, 32]], compare_op=ALU.is_equal, fill=0.0, base=0,
                                             channel_multiplier=1), r=["ones_f"], w=["D32"])
        G(lambda e: e.memset(G4[:], 0.0), w=["G4"])
        for g in range(4):
            G(lambda e, g=g: e.memset(G4[32 * g:32 * g + 32, g:g + 1], 1.0), r=["G4"], w=["G4"])
        G(lambda e: e.memset(zerob[:], 0.0), w=["zerob"])
        for i in range(NIT + 1):
            G(lambda e, i=i: e.memset(P2[:, i:i + 1], 2.0 ** -(i + 1)), w=["P2"])
        G(lambda e: e.memset(mhalf[:], -0.5), w=["mhalf"])
        G(lambda e: e.iota(iot[:], pattern=[[1, 8]], base=0, channel_multiplier=0), w=["iot"])
        V(lambda e: e.tensor_copy(out=iof[:], in_=iot[:]), r=["iot"], w=["iof"])
        A(lambda e: e.activation(out=invf[:], in_=iof[:], func=AF.Exp, scale=-math.log(500000.0) / 8.0),
          r=["iof"], w=["invf"])

        def rstd(out_ap, ss_ap, n, inv_n, rk, wk, ss2_ap=None):
            tmp = rs_tmp[:, 0:n]
            if ss2_ap is not None:
                G(lambda e: e.tensor_tensor(out=tmp, in0=ss_ap, in1=ss2_ap, op=ALU.add), r=rk, w=["rs_tmp"])
                G(lambda e: e.tensor_scalar(out=tmp, in0=tmp, scalar1=inv_n, scalar2=EPS, op0=ALU.mult,
                                            op1=ALU.add), r=["rs_tmp"], w=["rs_tmp"])
            else:
                G(lambda e: e.tensor_scalar(out=tmp, in0=ss_ap, scalar1=inv_n, scalar2=EPS, op0=ALU.mult,
                                            op1=ALU.add), r=rk, w=["rs_tmp"])
            G(lambda e: e.tensor_tensor(out=out_ap, in0=tmp, in1=mhalf[:, 0:n], op=ALU.pow),
              r=["rs_tmp", "mhalf"], w=wk)

        def ck(name):
            if stop == name:
                P.finish()
                raise StopBuild()

        def tap(name, ap, rk, rows=None):
            if name in tap_d:
                dst = tap_d[name]
                P.dma(dst if rows is None else dst[rows], ap, reads=rk)

        S1T = gsb("S1T", [128, 8, NSEQ])
        sh1T = gsb("sh1T", [128, 8, NSEQ])
        S2T = gsb("S2T", [128, 8, NSEQ])
        sh2T = gsb("sh2T", [128, 8, NSEQ])
        gmod = gsb("gmod", [NSEQ, 2, D])
        sel = gsb("sel", [NSEQ, NSEQ, 128])
        gpre = gsb("gpre", [128, 8])
        gpre2 = gsb("gpre2", [128, 8])
        P.dma(gpre[:], gpre_d[:, :], writes=["gpre"])
        P.dma(gpre2[:], gpre2_d[:, :], writes=["gpre2"])

        G(lambda e: e.affine_select(out=sel[:], in_=ones_f[0:NSEQ, :].unsqueeze(1).broadcast_to([NSEQ, NSEQ, 128]),
                                    pattern=[[-1, NSEQ], [0, 128]], compare_op=ALU.is_equal, fill=0.0, base=0,
                                    channel_multiplier=1), r=["ones_f"], w=["sel"])

        with contextlib.ExitStack() as ast:
            asb = mk_sb(ast)
            Win = asb("Win", [128, 8, DIN], BF16)
            Wout = asb("Wout", [128, 8, D], BF16)
            cs = asb("cs", [128, NTT, 8])
            sn = asb("sn", [128, NTT, 8])
            gv_row = asb("gv_row", [128, 512])
            goa_row = asb("goa_row", [128, 512])
            gob_row = asb("gob_row", [128, 512])
            bcol = asb("bcol", [128, 4])
            WmT = asb("WmT", [128, 4, 128], BF16)
            P.dma(gv_row[:], gv_d[0:1, :].partition_broadcast(128), writes=["gv_row"])
            P.dma(goa_row[:], goa_d[0:1, :].partition_broadcast(128), writes=["goa_row"])
            P.dma(gob_row[:], gob_d[0:1, :].partition_broadcast(128), writes=["gob_row"])
            P.dma(bcol[:], bs_d[:, :], writes=["bcol"])

            with contextlib.ExitStack() as sst:
                ssb = mk_sb(sst)
                cTs = ssb("cTs", [128, 8, NSEQ])
                scs = ssb("scs", [128, 8, NSEQ])
                bada4 = ssb("bada4", [NSEQ, 6 * D])
                modrow = ssb("modrow", [NSEQ, 6 * D])
                gpost4 = ssb("gpost4", [NSEQ, 2, D])
                wada_st = [ssb("wada_st%d" % i, [128, 8, 512]) for i in range(2)]
                win_st = [ssb("win_st%d" % i, [128, DIN]) for i in range(2)]
                ws_sb = ssb("ws_sb", [128, 4, 128])
                wsm = ssb("wsm", [128, 4, 128])
                posi = ssb("posi", [128, NTT], I32)
                posf = ssb("posf", [128, NTT])
                ang = ssb("ang", [128, NTT * 8])
                angk = ssb("angk", [128, NTT * 8], I32)
                angf = ssb("angf", [128, NTT * 8])
                angm = ssb("angm", [128, NTT * 8])
                ang2 = ssb("ang2", [128, NTT * 8])

                P.dma(cTs[:], cT_d[:, :, :], writes=["cTs"])
                P.dma(bada4[:], bada_d[0:1, :].partition_broadcast(NSEQ), writes=["bada4"])
                P.dma(gpost4[:, 0, :], gpost_d[0:1, :].partition_broadcast(NSEQ), writes=["gpost4a"])
                P.dma(gpost4[:, 1, :], gpost2_d[0:1, :].partition_broadcast(NSEQ), writes=["gpost4b"])
                P.dma(posi[:], pos_d[:, :], writes=["posi"])
                P.dma(ws_sb[:], ws_d[:, :, :], writes=["ws_sb"])
                A(lambda e: e.activation(out=scs[:], in_=cTs[:], func=AF.Silu), r=["cTs"], w=["scs"])

                order = [2, 3, 0, 1] + list(range(4, 12))
                for n_, cb in enumerate(order):
                    st_ = wada_st[n_ % 2]
                    sk = "wada_st%d" % (n_ % 2)
                    P.dma(st_[:], wada_d[:, cb * 512:(cb + 1) * 512].rearrange("(k p) n -> p k n", p=128),
                          writes=[sk])
                    bk = n_ % 2
                    for k in range(8):
                        T(lambda e, k=k, st_=st_, bk=bk: e.matmul(banks[bk][0:NSEQ, :], lhsT=scs[:, k, :],
                                                                  rhs=st_[:, k, :], start=(k == 0), stop=(k == 7)),
                          r=["scs", sk], w=[bkey[bk]])
                    V(lambda e, bk=bk, cb=cb: e.tensor_tensor(out=modrow[:, cb * 512:(cb + 1) * 512],
                                                              in0=banks[bk][0:NSEQ, :],
                                                              in1=bada4[:, cb * 512:(cb + 1) * 512], op=ALU.add),
                      r=[bkey[bk], "bada4"], w=["modrow%d" % cb])
                allmod = ["modrow%d" % cb for cb in range(12)]
                for si, sp_ in enumerate([0, 1, 3, 4]):
                    for k in range(8):
                        c0 = (si * 8 + k) * NSEQ
                        T(lambda e, sp_=sp_, k=k, c0=c0: e.transpose(
                            out=banks[2][:, c0:c0 + NSEQ], in_=modrow[0:NSEQ, sp_ * D + k * 128:sp_ * D + (k + 1) * 128],
                            identity=ident[0:NSEQ, 0:NSEQ]), r=allmod + ["ident"], w=[bkey[2]])

                def mview(si):
                    return banks[2][:, si * 8 * NSEQ:(si + 1) * 8 * NSEQ].rearrange("p (k b) -> p k b", b=NSEQ)

                V(lambda e: e.tensor_copy(out=sh1T[:], in_=mview(0)), r=[bkey[2]], w=["sh1T"])
                V(lambda e: e.scalar_tensor_tensor(out=S1T[:], in0=mview(1), scalar=1.0,
                                                   in1=gpre[:].unsqueeze(2).broadcast_to([128, 8, NSEQ]),
                                                   op0=ALU.add, op1=ALU.mult), r=[bkey[2], "gpre"], w=["S1T"])
                V(lambda e: e.tensor_copy(out=sh2T[:], in_=mview(2)), r=[bkey[2]], w=["sh2T"])
                V(lambda e: e.scalar_tensor_tensor(out=S2T[:], in0=mview(3), scalar=1.0,
                                                   in1=gpre2[:].unsqueeze(2).broadcast_to([128, 8, NSEQ]),
                                                   op0=ALU.add, op1=ALU.mult), r=[bkey[2], "gpre2"], w=["S2T"])
                V(lambda e: e.tensor_tensor(out=gmod[:, 0, :], in0=modrow[:, 2 * D:3 * D], in1=gpost4[:, 0, :],
                                            op=ALU.mult), r=allmod + ["gpost4a"], w=["gmod0"])
                V(lambda e: e.tensor_tensor(out=gmod[:, 1, :], in0=modrow[:, 5 * D:6 * D], in1=gpost4[:, 1, :],
                                            op=ALU.mult), r=allmod + ["gpost4b"], w=["gmod1"])

                cast_engs = ["dve", "act", "pool"]
                ci = 0
                for k in range(8):
                    st_ = win_st[k % 2]
                    sk = "win_st%d" % (k % 2)
                    P.dma(st_[:], win_d[k * 128:(k + 1) * 128, :], writes=[sk])
                    for h0, h1 in ((0, 1188), (1188, DIN)):
                        eng = cast_engs[ci % 3]
                        ci += 1
                        if eng == "act":
                            A(lambda e, k=k, st_=st_, h0=h0, h1=h1: e.activation(out=Win[:, k, h0:h1], in_=st_[:, h0:h1],
                                                                              func=AF.Copy), r=[sk], w=["Win"])
                        else:
                            P.op(eng, lambda e, k=k, st_=st_, h0=h0, h1=h1: e.tensor_copy(out=Win[:, k, h0:h1],
                                                                                       in_=st_[:, h0:h1]),
                                 [sk], ["Win"])
                for k in range(8):
                    st_ = win_st[k % 2]
                    sk = "win_st%d" % (k % 2)
                    P.dma(st_[:, 0:D], wout_d[k * 128:(k + 1) * 128, :], writes=[sk])
                    eng = cast_engs[ci % 3]
                    ci += 1
                    if eng == "act":
                        A(lambda e, k=k, st_=st_: e.activation(out=Wout[:, k, :], in_=st_[:, 0:D], func=AF.Copy),
                          r=[sk], w=["Wout"])
                    else:
                        P.op(eng, lambda e, k=k, st_=st_: e.tensor_copy(out=Wout[:, k, :], in_=st_[:, 0:D]),
                             [sk], ["Wout"])
                for g in range(4):
                    G(lambda e, g=g: e.affine_select(out=wsm[:, g, :], in_=ws_sb[:, g, :], pattern=[[-1, 128]],
                                                     compare_op=ALU.is_ge, fill=0.0, base=0, channel_multiplier=1),
                      r=["ws_sb"], w=["wsm"])
                for g in range(4):
                    T(lambda e, g=g: e.transpose(out=banks[3][:, g * 128:(g + 1) * 128], in_=wsm[:, g, :],
                                                 identity=ident[:]), r=["wsm", "ident"], w=[bkey[3]])
                V(lambda e: e.tensor_copy(out=WmT[:], in_=banks[3][:, :].rearrange("p (g t) -> p g t", g=4)),
                  r=[bkey[3]], w=["WmT"])

                NA = NTT * 8
                V(lambda e: e.tensor_copy(out=posf[:], in_=posi[:]), r=["posi"], w=["posf"])
                V(lambda e: e.tensor_tensor(out=ang[:].rearrange("p (t f) -> p t f", f=8),
                                            in0=posf[:].unsqueeze(2).broadcast_to([128, NTT, 8]),
                                            in1=invf[:].unsqueeze(1).broadcast_to([128, NTT, 8]), op=ALU.mult),
                  r=["posf", "invf"], w=["ang"])

                def reduce_sin(dst, src_key, shift):
                    V(lambda e: e.tensor_scalar(out=ang2[:], in0=ang[:], scalar1=shift, scalar2=None, op0=ALU.add),
                      r=["ang"], w=["ang2"])
                    V(lambda e: e.tensor_scalar(out=angk[:], in0=ang2[:], scalar1=1.0 / TWO_PI, scalar2=None,
                                                op0=ALU.mult), r=["ang2"], w=["angk"])
                    V(lambda e: e.tensor_copy(out=angf[:], in_=angk[:]), r=["angk"], w=["angf"])
                    V(lambda e: e.scalar_tensor_tensor(out=ang2[:], in0=angf[:], scalar=-TWO_PI, in1=ang2[:],
                                                       op0=ALU.mult, op1=ALU.add), r=["angf", "ang2"], w=["ang2"])
                    V(lambda e: e.tensor_scalar(out=angm[:], in0=ang2[:], scalar1=math.pi, scalar2=-TWO_PI,
                                                op0=ALU.is_gt, op1=ALU.mult), r=["ang2"], w=["angm"])
                    V(lambda e: e.tensor_tensor(out=ang2[:], in0=ang2[:], in1=angm[:], op=ALU.add),
                      r=["ang2", "angm"], w=["ang2"])
                    V(lambda e: e.tensor_scalar(out=angm[:], in0=ang2[:], scalar1=-math.pi, scalar2=TWO_PI,
                                                op0=ALU.is_lt, op1=ALU.mult), r=["ang2"], w=["angm"])
                    V(lambda e: e.tensor_tensor(out=ang2[:], in0=ang2[:], in1=angm[:], op=ALU.add),
                      r=["ang2", "angm"], w=["ang2"])
                    V(lambda e: e.tensor_scalar(out=ang2[:], in0=ang2[:], scalar1=-3.1415925, scalar2=3.1415925,
                                                op0=ALU.max, op1=ALU.min), r=["ang2"], w=["ang2"])
                    A(lambda e: e.activation(out=dst[:].rearrange("p t f -> p (t f)"), in_=ang2[:], func=AF.Sin),
                      r=["ang2"], w=[src_key])

                reduce_sin(sn, "sn", 0.0)
                reduce_sin(cs, "cs", math.pi / 2.0)
                P.barrier()

            if stop == "setup":
                P.finish()
                return
            tap("S1T", S1T[:].rearrange("p k b -> p (k b)"), ["S1T"])
            tap("gmod", gmod[:].rearrange("b g d -> b (g d)"), ["gmod0", "gmod1"])
            tap("cs", cs[:].rearrange("p t f -> p (t f)"), ["cs"])
            tap("sn", sn[:].rearrange("p t f -> p (t f)"), ["sn"])

            qT2 = asb("qT2", [128, 4, S], BF16)
            kTz = [asb("kTz%d" % i, [128, 2, S], BF16) for i in range(2)]
            kiTz = [asb("kiTz%d" % i, [128, S], BF16) for i in range(2)]
            qiT2 = asb("qiT2", [128, S // 32, 4, 32], BF16)
            v_aug = asb("v_aug", [128, NT, 2, 65], BF16)
            w_tok = asb("w_tok", [128, NT, 8])
            mTa = asb("mTa", [128, 4, S], BF16)
            G1row = asb("G1row", [128, D])
            junkA = asb("junkA", [128, D], BF16)
            G(lambda e: e.memset(v_aug[:].rearrange("p a b c -> p (a b) c")[:, :, 64:65], 1.0), w=["v_aug"])
            G(lambda e: e.memset(kTz[0][64:128, :, :], 0.0), w=["kTd"])
            G(lambda e: e.memset(kTz[1][0:64, :, :], 0.0), w=["kTd"])
            G(lambda e: e.memset(kiTz[0][64:128, :], 0.0), w=["kiTd"])
            G(lambda e: e.memset(kiTz[1][0:64, :], 0.0), w=["kiTd"])

            for b in range(NSEQ):
                for n in range(2):
                    T(lambda e, n=n: e.matmul(banks[n][:, :], lhsT=sel[0:NSEQ, b, :],
                                              rhs=gmod[0:NSEQ, 0, n * 512:(n + 1) * 512], start=True, stop=True),
                      r=["sel", "gmod0"], w=[bkey[n]])
                    V(lambda e, n=n: e.tensor_copy(out=G1row[:, n * 512:(n + 1) * 512], in_=banks[n][:, :]),
                      r=[bkey[n]], w=["G1row"])

                with contextlib.ExitStack() as pst:
                    psb = mk_sb(pst)
                    xt = [psb("xt%d" % i, [128, D]) for i in range(2)]
                    xn = [psb("xn%d" % i, [128, D]) for i in range(2)]
                    hT = [psb("hT%d" % i, [128, 8, 128], BF16) for i in range(2)]
                    ssx = psb("ssx", [128, 2])
                    rsx = psb("rsx", [128, 2])
                    zu = [psb("zu%d" % i, [128, 512], BF16) for i in range(2)]
                    zv = psb("zv", [128, 512], BF16)
                    vn = [psb("vn%d" % i, [128, 512], BF16) for i in range(2)]
                    ssv = psb("ssv", [128, 4])
                    rsv = psb("rsv", [128, 4])
                    ya = psb("ya", [128, 512])
                    ssa = psb("ssa", [128, 1])
                    rsa = psb("rsa", [128, 1])
                    ma = psb("ma", [128, 512], BF16)
                    q_tok = [psb("q_tok%d" % i, [128, 8, 64], BF16) for i in range(2)]
                    qi_tok = [psb("qi_tok%d" % i, [128, 8, 64], BF16) for i in range(2)]
                    kd = [psb("kd%d" % i, [128, 2, 2, 64], BF16) for i in range(2)]
                    kid = [psb("kid%d" % i, [128, 2, 64], BF16) for i in range(2)]
                    rt = [psb("rt%d" % i, [128, 8, 8]) for i in range(4)]

                    def s1_load(i):
                        it = b * NT + i
                        P.dma(xt[i % 2][:], x_d[it * 128:(it + 1) * 128, :], writes=["xt%d" % (i % 2)])

                    def s1_pre(i):
                        j = i % 2
                        A(lambda e: e.activation(out=junkA[:], in_=xt[j][:], func=AF.Square,
                                                 accum_out=ssx[:, j:j + 1]), r=["xt%d" % j], w=["junkA", "ssx%d" % j])
                        rstd(rsx[:, j:j + 1], ssx[:, j:j + 1], 1, 1.0 / D, ["ssx%d" % j], ["rsx%d" % j])
                        V(lambda e: e.tensor_scalar(out=xn[j][:], in0=xt[j][:], scalar1=rsx[:, j:j + 1], scalar2=None,
                                                    op0=ALU.mult), r=["xt%d" % j, "rsx%d" % j], w=["xn%d" % j])

                    def s1_post(i):
                        j = i % 2
                        for k in range(8):
                            T(lambda e, k=k: e.transpose(out=banks[k // 4][:, (k % 4) * 128:(k % 4 + 1) * 128],
                                                         in_=xn[j][:, k * 128:(k + 1) * 128], identity=ident[:]),
                              r=["xn%d" % j, "ident"], w=[bkey[k // 4]])
                        for k in range(8):
                            A(lambda e, k=k: e.activation(out=hT[j][:, k, :],
                                                          in_=banks[k // 4][:, (k % 4) * 128:(k % 4 + 1) * 128],
                                                          func=AF.Identity, scale=S1T[:, k, b:b + 1],
                                                          bias=sh1T[:, k, b:b + 1]),
                              r=[bkey[k // 4], "S1T", "sh1T"], w=["hT%d" % j])

                    GROUPS = [(0, 512, 2), (512, 512, 3), (1024, 512, 4), (1536, 328, 5), (1864, 512, 6)]

                    def rope(src3, src_key, dst3, dst_key, H, it):
                        c = cs[:, it, :].unsqueeze(1).broadcast_to([128, H, 8])
                        s_ = sn[:, it, :].unsqueeze(1).broadcast_to([128, H, 8])
                        x1 = src3[:, :, 0:8]
                        x2 = src3[:, :, 8:16]
                        t = [r_[:, 0:H, :] for r_ in rt]
                        V(lambda e: e.tensor_tensor(out=t[0], in0=x1, in1=c, op=ALU.mult), r=[src_key, "cs"], w=["rt0"])
                        V(lambda e: e.tensor_tensor(out=t[1], in0=x2, in1=s_, op=ALU.mult), r=[src_key, "sn"], w=["rt1"])
                        V(lambda e: e.tensor_tensor(out=dst3[:, :, 0:8], in0=t[0], in1=t[1], op=ALU.subtract),
                          r=["rt0", "rt1"], w=[dst_key])
                        V(lambda e: e.tensor_tensor(out=t[2], in0=x2, in1=c, op=ALU.mult), r=[src_key, "cs"], w=["rt2"])
                        V(lambda e: e.tensor_tensor(out=t[3], in0=x1, in1=s_, op=ALU.mult), r=[src_key, "sn"], w=["rt3"])
                        V(lambda e: e.tensor_tensor(out=dst3[:, :, 8:16], in0=t[2], in1=t[3], op=ALU.add),
                          r=["rt2", "rt3"], w=[dst_key])
                        A(lambda e: e.activation(out=dst3[:, :, 16:64], in_=src3[:, :, 16:64], func=AF.Copy),
                          r=[src_key], w=[dst_key])

                    def s2(i):
                        j = i % 2
                        it = b * NT + i
                        for (c0, n, bk) in GROUPS:
                            for k in range(8):
                                T(lambda e, k=k, c0=c0, n=n, bk=bk: e.matmul(banks[bk][:, 0:n], lhsT=hT[j][:, k, :],
                                                                             rhs=Win[:, k, c0:c0 + n], start=(k == 0),
                                                                             stop=(k == 7)),
                                  r=["hT%d" % j, "Win"], w=[bkey[bk]])
                        A(lambda e: e.activation(out=zu[j][:], in_=banks[2][:, :], func=AF.Gelu_apprx_tanh),
                          r=[bkey[2]], w=["zu%d" % j])
                        A(lambda e: e.activation(out=zv[:], in_=banks[3][:, :], func=AF.Gelu_apprx_tanh),
                          r=[bkey[3]], w=["zv"])
                        rope(banks[4][:, :].rearrange("p (h d) -> p h d", d=64), bkey[4], q_tok[j][:], "q_tok%d" % j, 8, it)
                        rope(banks[5][:, 0:128].rearrange("p (h d) -> p h d", d=64), bkey[5], kd[j][:, :, 0, :],
                             "kd%d" % j, 2, it)
                        V(lambda e: e.tensor_copy(out=kd[j][:, :, 1, :], in_=kd[j][:, :, 0, :]), r=["kd%d" % j],
                          w=["kd%d" % j])
                        V(lambda e: e.tensor_copy(out=v_aug[:, i, :, 0:64],
                                                  in_=banks[5][:, 128:256].rearrange("p (h d) -> p h d", d=64)),
                          r=[bkey[5]], w=["v_aug"])
                        rope(banks[5][:, 256:320].rearrange("p (h d) -> p h d", d=64), bkey[5], kid[j][:, 0:1, :],
                             "kid%d" % j, 1, it)
                        V(lambda e: e.tensor_copy(out=kid[j][:, 1:2, :], in_=kid[j][:, 0:1, :]), r=["kid%d" % j],
                          w=["kid%d" % j])
                        V(lambda e: e.tensor_copy(out=w_tok[:, i, :], in_=banks[5][:, 320:328]), r=[bkey[5]],
                          w=["w_tok"])
                        rope(banks[6][:, :].rearrange("p (h d) -> p h d", d=64), bkey[6], qi_tok[j][:], "qi_tok%d" % j, 8, it)
                        for g in range(4):
                            A(lambda e, g=g: e.activation(out=junkA[:, 0:128], in_=zv[:, g * 128:(g + 1) * 128],
                                                          func=AF.Square, accum_out=ssv[:, g:g + 1]),
                              r=["zv"], w=["junkA", "ssv"])
                        rstd(rsv[:, 0:4], ssv[:, 0:4], 4, 1.0 / 128, ["ssv"], ["rsv"])
                        for g in range(4):
                            V(lambda e, g=g: e.scalar_tensor_tensor(out=vn[j][:, g * 128:(g + 1) * 128],
                                                                    in0=zv[:, g * 128:(g + 1) * 128],
                                                                    scalar=rsv[:, g:g + 1],
                                                                    in1=gv_row[:, g * 128:(g + 1) * 128],
                                                                    op0=ALU.mult, op1=ALU.mult),
                              r=["zv", "rsv", "gv_row"], w=["vn%d" % j])

                    def s3a(i):
                        j = i % 2
                        ts = slice(i * 128, (i + 1) * 128)
                        qf = q_tok[j][:].rearrange("p h d -> p (h d)")
                        for c in range(4):
                            T(lambda e, c=c: e.transpose(out=bbf[0][:, c * 128:(c + 1) * 128],
                                                         in_=qf[:, c * 128:(c + 1) * 128], identity=identb[:]),
                              r=["q_tok%d" % j, "identb"], w=[bkey[0]])
                        for kv in range(2):
                            T(lambda e, kv=kv: e.transpose(out=bbf[0][:, 512 + kv * 128:512 + (kv + 1) * 128],
                                                           in_=kd[j][:, kv, :, :].rearrange("p a d -> p (a d)"),
                                                           identity=identb[:]),
                              r=["kd%d" % j, "identb"], w=[bkey[0]])
                        T(lambda e: e.transpose(out=bbf[0][:, 768:896], in_=kid[j][:].rearrange("p a d -> p (a d)"),
                                                identity=identb[:]), r=["kid%d" % j, "identb"], w=[bkey[0]])
                        V(lambda e: e.tensor_copy(out=qT2[:, :, ts],
                                                  in_=bbf[0][:, 0:512].rearrange("p (c t) -> p c t", t=128)),
                          r=[bkey[0]], w=["qT2"])
                        for par in range(2):
                            ps = slice(64 * par, 64 * par + 64)
                            V(lambda e, par=par, ps=ps: e.tensor_copy(
                                out=kTz[par][ps, :, ts],
                                in_=bbf[0][ps, 512:768].rearrange("p (c t) -> p c t", t=128)),
                              r=[bkey[0]], w=["kTd"])
                            V(lambda e, par=par, ps=ps: e.tensor_copy(out=kiTz[par][ps, ts], in_=bbf[0][ps, 768:896]),
                              r=[bkey[0]], w=["kiTd"])
                        qif = qi_tok[j][:].rearrange("p h d -> p (h d)")
                        for c in range(4):
                            T(lambda e, c=c: e.transpose(out=bbf[1][:, c * 128:(c + 1) * 128],
                                                         in_=qif[:, c * 128:(c + 1) * 128], identity=identb[:]),
                              r=["qi_tok%d" % j, "identb"], w=[bkey[1]])
                        A(lambda e: e.activation(out=qiT2[:, i * 4:(i + 1) * 4, :, :],
                                                 in_=bbf[1][:, 0:512].rearrange("p (c g t) -> p g c t", c=4, g=4),
                                                 func=AF.Copy), r=[bkey[1]], w=["qiT2"])
                        for g in range(4):
                            T(lambda e, g=g: e.matmul(banks[7][:, g * 128:(g + 1) * 128], lhsT=WmT[:, g, :],
                                                      rhs=vn[j][:, g * 128:(g + 1) * 128], start=True, stop=True),
                              r=["WmT", "vn%d" % j], w=[bkey[7]])
                        for g in range(4):
                            V(lambda e, g=g: e.scalar_tensor_tensor(out=ya[:, g * 128:(g + 1) * 128],
                                                                    in0=banks[7][:, g * 128:(g + 1) * 128],
                                                                    scalar=bcol[:, g:g + 1],
                                                                    in1=zu[j][:, g * 128:(g + 1) * 128],
                                                                    op0=ALU.add, op1=ALU.mult),
                              r=[bkey[7], "bcol", "zu%d" % j], w=["ya"])
                        A(lambda e: e.activation(out=junkA[:, 0:512], in_=ya[:], func=AF.Square, accum_out=ssa[:]),
                          r=["ya"], w=["junkA", "ssa"])
                        rstd(rsa[:], ssa[:], 1, 1.0 / 512, ["ssa"], ["rsa"])
                        V(lambda e: e.scalar_tensor_tensor(out=ma[:], in0=ya[:], scalar=rsa[:, 0:1], in1=goa_row[:],
                                                           op0=ALU.mult, op1=ALU.mult),
                          r=["ya", "rsa", "goa_row"], w=["ma"])
                        if b == 0 and i == 0:
                            tap("ya", ya[:], ["ya"])

                    def s3b(i):
                        ts = slice(i * 128, (i + 1) * 128)
                        for c in range(4):
                            T(lambda e, c=c: e.transpose(out=bbf[7][:, c * 128:(c + 1) * 128],
                                                         in_=ma[:, c * 128:(c + 1) * 128], identity=identb[:]),
                              r=["ma", "identb"], w=[bkey[7]])
                        A(lambda e: e.activation(out=mTa[:, :, ts],
                                                 in_=bbf[7][:, 0:512].rearrange("p (c t) -> p c t", t=128),
                                                 func=AF.Copy), r=[bkey[7]], w=["mTa"])

                    s1_load(0)
                    if NT > 1:
                        s1_load(1)
                    s1_pre(0)
                    s1_post(0)
                    if NT > 2:
                        s1_load(2)
                    if NT > 1:
                        s1_pre(1)
                        s1_post(1)
                    for n in range(NT + 2):
                        if n + 2 < NT:
                            s1_pre(n + 2)
                            if n + 3 < NT:
                                s1_load(n + 3)
                        if n < NT:
                            s2(n)
                        if 0 <= n - 2 < NT:
                            s3b(n - 2)
                        if 0 <= n - 1 < NT:
                            s3a(n - 1)
                        if n + 2 < NT:
                            s1_post(n + 2)
                    P.barrier()
                    if stop == "proj":
                        P.finish()
                        return

                with contextlib.ExitStack() as tst:
                    tsb = mk_sb(tst)
                    score = tsb("score", [128, S])
                    cmax = tsb("cmax", [128, 4])
                    mask = tsb("mask", [128, S], BF16)
                    maskT = tsb("maskT", [128, NT, 128], BF16)
                    rl = [tsb("rl%d" % i, [128, 512], BF16) for i in range(4)]
                    pT = [tsb("pT%d" % i, [128, 512], BF16) for i in range(4)]
                    Wsel = [tsb("Wsel%d" % i, [128, 8, 128], BF16) for i in range(2)]
                    wrep = tsb("wrep", [128, 2, 128])
                    wcol = tsb("wcol", [128, 8])
                    lo0 = tsb("lo0", [128, 1])
                    hi0 = tsb("hi0", [128, 1])
                    w0 = tsb("w0", [128, 1])
                    wh = tsb("wh", [128, NIT + 1])
                    cbias = tsb("cbias", [128, NT])
                    mid = tsb("mid", [128, 1])
                    cnt = tsb("cnt", [128, 1])
                    btmp = tsb("btmp", [128, 1])
                    rden = tsb("rden", [128, 8])
                    yb = tsb("yb", [128, 512])
                    ssb_ = tsb("ssb_", [128, 1])
                    rsb = tsb("rsb", [128, 1])
                    mb = tsb("mb", [128, 512], BF16)
                    mbT = tsb("mbT", [128, 4, 128], BF16)
                    sso = tsb("sso", [128, 2])
                    rso = tsb("rso", [128, 1])
                    ot = tsb("ot", [128, D])
                    xres = [tsb("xres%d" % i, [128, D]) for i in range(2)]
                    for i in range(2):
                        G(lambda e, i=i: e.memset(Wsel[i][:], 0.0), w=["Wsel%d" % i])
                    for qq in range(NT):
                        G(lambda e, qq=qq: e.memset(cbias[:, qq:qq + 1], float((qq + 1) * 128 - 2 * TOPK) + 0.5),
                          w=["cbias"])
                    rl_i = [0]
                    pT_i = [0]
                    D_i = [0]

                    def wsel_build(qb):
                        wi = qb % 2
                        wk = "Wsel%d" % wi
                        w2v = w_tok[:, qb, :].rearrange("p (i two) -> p i two", two=2)
                        for par in range(2):
                            V(lambda e, par=par: e.tensor_tensor(
                                out=wrep[:, par, :].rearrange("p (i t) -> p i t", t=32),
                                in0=w2v[:, :, par].unsqueeze(2).broadcast_to([128, 4, 32]),
                                in1=D32[:].unsqueeze(1).broadcast_to([128, 4, 32]), op=ALU.mult),
                              r=["w_tok", "D32"], w=["wrep"])
                        for par in range(2):
                            T(lambda e, par=par: e.matmul(banks[0][:, par * 4:(par + 1) * 4], lhsT=wrep[:, par, :],
                                                          rhs=G4[:], start=True, stop=True),
                              r=["wrep", "G4"], w=[bkey[0]])
                        V(lambda e: e.tensor_scalar(out=wcol[:], in0=banks[0][:, 0:8], scalar1=IDX_SCALE, scalar2=None,
                                                    op0=ALU.mult), r=[bkey[0]], w=["wcol"])
                        for par in range(2):
                            for g in range(4):
                                V(lambda e, par=par, g=g: e.tensor_scalar(
                                    out=Wsel[wi][:, par * 4 + g, 32 * g:32 * g + 32], in0=D32[:],
                                    scalar1=wcol[:, par * 4 + g:par * 4 + g + 1], scalar2=None, op0=ALU.mult),
                                  r=["D32", "wcol"], w=[wk])

                    def indexer(qb):
                        N = (qb + 1) * 128
                        nch = (N + 511) // 512
                        wi = qb % 2
                        wk = "Wsel%d" % wi
                        units = []
                        for c in range(nch):
                            n = min(512, N - c * 512)
                            for g in range(4):
                                for par in range(2):
                                    units.append((c, n, g, par))

                        DBK = [0, 1, 3]

                        def dots(u):
                            c, n, g, par = u
                            ri = rl_i[0] % 4
                            dbk = DBK[D_i[0] % 3]
                            D_i[0] += 1
                            rl_i[0] += 1
                            T(lambda e: e.matmul(banks[dbk][:, 0:n],
                                                 lhsT=qiT2[:, qb * 4 + g, :, :].rearrange("p c t -> p (c t)"),
                                                 rhs=kiTz[par][:, c * 512:c * 512 + n], start=True, stop=True),
                              r=["qiT2", "kiTd"], w=[bkey[dbk]])
                            if par == 0:
                                A(lambda e: e.activation(out=rl[ri][:, 0:n], in_=banks[dbk][:, 0:n], func=AF.Relu),
                                  r=[bkey[dbk]], w=["rl%d" % ri])
                            else:
                                V(lambda e: e.tensor_scalar(out=rl[ri][:, 0:n], in0=banks[dbk][:, 0:n], scalar1=0.0,
                                                            scalar2=None, op0=ALU.max), r=[bkey[dbk]], w=["rl%d" % ri])
                            return ri

                        def selmm(u, ri):
                            c, n, g, par = u
                            sbk = 2
                            first = (g == 0 and par == 0)
                            last = (g == 3 and par == 1)
                            T(lambda e: e.matmul(banks[sbk][:, 0:n], lhsT=Wsel[wi][:, par * 4 + g, :],
                                                 rhs=rl[ri][:, 0:n], start=first, stop=last),
                              r=[wk, "rl%d" % ri], w=[bkey[sbk]])
                            if last:
                                V(lambda e: e.tensor_scalar(out=score[:, c * 512:c * 512 + n], in0=banks[sbk][:, 0:n],
                                                            scalar1=1.0, scalar2=None, op0=ALU.mult, op1=ALU.max,
                                                            accum_out=cmax[:, c:c + 1]),
                                  r=[bkey[sbk]], w=["score", "cmax"])

                        ris = {}
                        LOOK = 3
                        for i_ in range(min(LOOK, len(units))):
                            ris[i_] = dots(units[i_])
                        for i_ in range(len(units)):
                            if i_ + LOOK < len(units):
                                ris[i_ + LOOK] = dots(units[i_ + LOOK])
                            selmm(units[i_], ris[i_])

                    def topk_iter(qb):
                        N = (qb + 1) * 128
                        nch = (N + 511) // 512
                        V(lambda e: e.tensor_reduce(out=lo0[:], in_=score[:, 0:N], axis=AX.X, op=ALU.min),
                          r=["score"], w=["lo0"])
                        V(lambda e: e.tensor_reduce(out=hi0[:], in_=cmax[:, 0:nch], axis=AX.X, op=ALU.max),
                          r=["cmax"], w=["hi0"])
                        V(lambda e: e.tensor_tensor(out=score[:, qb * 128:N], in0=score[:, qb * 128:N], in1=NEGM[:],
                                                    op=ALU.add), r=["score", "NEGM"], w=["score"])
                        V(lambda e: e.tensor_tensor(out=w0[:], in0=lo0[:], in1=hi0[:], op=ALU.subtract),
                          r=["hi0", "lo0"], w=["w0"])
                        V(lambda e: e.tensor_scalar(out=wh[:], in0=P2[:], scalar1=w0[:, 0:1], scalar2=None,
                                                    op0=ALU.mult), r=["P2", "w0"], w=["wh"])
                        V(lambda e: e.tensor_scalar(out=mid[:], in0=lo0[:], scalar1=-1.0, scalar2=wh[:, 0:1],
                                                    op0=ALU.mult, op1=ALU.add), r=["lo0", "wh"], w=["mid"])
                        for i in range(NIT):
                            A(lambda e: e.activation(out=mask[:, 0:N], in_=score[:, 0:N], func=AF.Sign,
                                                     bias=mid[:, 0:1], accum_out=cnt[:]),
                              r=["score", "mid"], w=["mask", "cnt"])
                            V(lambda e, i=i: e.scalar_tensor_tensor(out=btmp[:], in0=cnt[:],
                                                                    scalar=float(2 * TOPK - N) - 0.5,
                                                                    in1=wh[:, i:i + 1], op0=ALU.is_ge, op1=ALU.mult),
                              r=["cnt", "wh"], w=["btmp"])
                            V(lambda e, i=i: e.scalar_tensor_tensor(out=mid[:], in0=mid[:], scalar=wh[:, i + 1:i + 2],
                                                                    in1=btmp[:], op0=ALU.subtract, op1=ALU.add),
                              r=["mid", "wh", "btmp"], w=["mid"])
                            yield
                        V(lambda e: e.tensor_scalar(out=mid[:], in0=mid[:], scalar1=-1.0, scalar2=wh[:, NIT:NIT + 1],
                                                    op0=ALU.mult, op1=ALU.add), r=["mid", "wh"], w=["mid"])

                    def topk_finish(qb):
                        N = (qb + 1) * 128
                        if qb < KB:
                            for jj in range(qb + 1):
                                src = TRIU if jj == qb else ONESB
                                G(lambda e, jj=jj, src=src: e.tensor_copy(out=maskT[:, jj, :], in_=src[:]),
                                  r=["TRIU", "ONESB"], w=["maskT"])
                            return
                        V(lambda e: e.tensor_scalar(out=mask[:, 0:N], in0=score[:, 0:N], scalar1=mid[:, 0:1],
                                                    scalar2=None, op0=ALU.is_ge), r=["score", "mid"], w=["mask"])
                        if b == 0 and qb == NT - 1:
                            tap("score", score[:, 0:N], ["score"])
                            tap("thr", mid[:], ["mid"])
                        for jj in range(qb + 1):
                            lb = 4 + jj // 8
                            T(lambda e, jj=jj, lb=lb: e.transpose(out=bbf[lb][:, (jj % 8) * 128:(jj % 8 + 1) * 128],
                                                                  in_=mask[:, jj * 128:(jj + 1) * 128],
                                                                  identity=identb[:]),
                              r=["mask", "identb"], w=[bkey[lb]])
                        for lb in range(4, 4 + (qb + 8) // 8):
                            j0 = (lb - 4) * 8
                            j1 = min(qb + 1, j0 + 8)
                            nj = j1 - j0
                            V(lambda e, lb=lb, j0=j0, j1=j1, nj=nj: e.tensor_copy(
                                out=maskT[:, j0:j1, :],
                                in_=bbf[lb][:, 0:nj * 128].rearrange("p (j t) -> p j t", t=128)),
                              r=[bkey[lb]], w=["maskT"])

                    def attention(qb):
                        qs = slice(qb * 128, (qb + 1) * 128)
                        for kv in range(2):
                            T(lambda e, kv=kv: e.matmul(banks[6 + kv][:, 0:260], lhsT=zerob[:, 0:128],
                                                        rhs=zerob[:, 0:260], start=True, stop=False,
                                                        skip_group_check=True), r=["zerob"], w=[bkey[6 + kv]])

                        def Lstage(jj):
                            ks = slice(jj * 128, (jj + 1) * 128)
                            pis = []
                            for par in range(2):
                                ps = slice(64 * par, 64 * par + 64)
                                lb = 4 + par
                                pi = pT_i[0] % 4
                                pT_i[0] += 1
                                pis.append(pi)
                                for kv in range(2):
                                    T(lambda e, ps=ps, lb=lb, kv=kv, par=par: e.matmul(
                                        banks[lb][:, kv * 256:(kv + 1) * 256], lhsT=kTz[par][:, kv, ks],
                                        rhs=qT2[:, 2 * kv:2 * kv + 2, qs], start=True, stop=True),
                                      r=["kTd", "qT2"], w=[bkey[lb]])
                                A(lambda e, lb=lb, pi=pi: e.activation(out=pT[pi][:], in_=banks[lb][:, :], func=AF.Exp,
                                                                       scale=0.125), r=[bkey[lb]], w=["pT%d" % pi])
                                V(lambda e, pi=pi: e.tensor_tensor(
                                    out=pT[pi][:].rearrange("p (h t) -> p h t", t=128),
                                    in0=pT[pi][:].rearrange("p (h t) -> p h t", t=128),
                                    in1=maskT[:, jj, :].unsqueeze(1).broadcast_to([128, 4, 128]), op=ALU.mult),
                                  r=["pT%d" % pi, "maskT"], w=["pT%d" % pi])
                            return pis

                        def PVstage(jj, pis):
                            for par in range(2):
                                pi = pis[par]
                                for kv in range(2):
                                    for ii in range(2):
                                        hl = 2 * ii + par
                                        T(lambda e, ii=ii, hl=hl, kv=kv, pi=pi: e.matmul(
                                            banks[6 + kv][:, hl * 65:hl * 65 + 65],
                                            lhsT=pT[pi][:, (kv * 2 + ii) * 128:(kv * 2 + ii + 1) * 128],
                                            rhs=v_aug[:, jj, kv, :], start=False, stop=(jj == qb),
                                            skip_group_check=True),
                                          r=["pT%d" % pi, "v_aug"], w=[bkey[6 + kv]])

                        nxt = Lstage(0)
                        for jj in range(qb + 1):
                            cur = nxt
                            if jj + 1 <= qb:
                                nxt = Lstage(jj + 1)
                            PVstage(jj, cur)
                            yield

                    def post_a(qb):
                        for kv in range(2):
                            ov = banks[6 + kv][:, 0:260].rearrange("p (h d) -> p h d", d=65)
                            V(lambda e, kv=kv, ov=ov: e.reciprocal(out=rden[:, kv * 4:(kv + 1) * 4], in_=ov[:, :, 64]),
                              r=[bkey[6 + kv]], w=["rden"])
                            V(lambda e, kv=kv, ov=ov: e.tensor_tensor(
                                out=yb[:, kv * 256:(kv + 1) * 256].rearrange("p (h d) -> p h d", d=64),
                                in0=ov[:, :, 0:64],
                                in1=rden[:, kv * 4:(kv + 1) * 4].unsqueeze(2).broadcast_to([128, 4, 64]),
                                op=ALU.mult), r=[bkey[6 + kv], "rden"], w=["yb"])
                        if b == 0 and qb == NT - 1:
                            tap("yb", yb[:], ["yb"])

                    def post_b1(qb):
                        yield
                        A(lambda e: e.activation(out=junkA[:, 0:512], in_=yb[:], func=AF.Square, accum_out=ssb_[:]),
                          r=["yb"], w=["junkA", "ssb_"])
                        yield
                        rstd(rsb[:], ssb_[:], 1, 1.0 / 512, ["ssb_"], ["rsb"])
                        yield
                        V(lambda e: e.scalar_tensor_tensor(out=mb[:], in0=yb[:], scalar=rsb[:, 0:1], in1=gob_row[:],
                                                           op0=ALU.mult, op1=ALU.mult),
                          r=["yb", "rsb", "gob_row"], w=["mb"])

                    def post_b2(qb):
                        it = b * NT + qb
                        xj = qb % 2
                        for c in range(4):
                            T(lambda e, c=c: e.transpose(out=bbf[2][:, c * 128:(c + 1) * 128],
                                                         in_=mb[:, c * 128:(c + 1) * 128], identity=identb[:]),
                              r=["mb", "identb"], w=[bkey[2]])
                        A(lambda e: e.activation(out=mbT[:], in_=bbf[2][:, 0:512].rearrange("p (c t) -> p c t", t=128),
                                                 func=AF.Copy), r=[bkey[2]], w=["mbT"])
                        yield
                        for n in range(2):
                            for k in range(8):
                                lhs = mTa[:, k, qb * 128:(qb + 1) * 128] if k < 4 else mbT[:, k - 4, :]
                                T(lambda e, n=n, k=k, lhs=lhs: e.matmul(banks[2 + n][:, :], lhsT=lhs,
                                                                        rhs=Wout[:, k, n * 512:(n + 1) * 512],
                                                                        start=(k == 0), stop=(k == 7)),
                                  r=["mTa", "mbT", "Wout"], w=[bkey[2 + n]])
                        for n in range(2):
                            A(lambda e, n=n: e.activation(out=junkA[:, 0:512], in_=banks[2 + n][:, :], func=AF.Square,
                                                          accum_out=sso[:, n:n + 1]),
                              r=[bkey[2 + n]], w=["junkA", "sso%d" % n])
                        yield
                        rstd(rso[:], sso[:, 0:1], 1, 1.0 / D, ["sso0", "sso1"], ["rso"], ss2_ap=sso[:, 1:2])
                        yield
                        for n in range(2):
                            V(lambda e, n=n: e.scalar_tensor_tensor(out=ot[:, n * 512:(n + 1) * 512],
                                                                    in0=banks[2 + n][:, :], scalar=rso[:, 0:1],
                                                                    in1=G1row[:, n * 512:(n + 1) * 512],
                                                                    op0=ALU.mult, op1=ALU.mult),
                              r=[bkey[2 + n], "rso", "G1row"], w=["ot"])
                        G(lambda e: e.tensor_tensor(out=ot[:], in0=ot[:], in1=xres[xj][:], op=ALU.add),
                          r=["ot", "xres%d" % xj], w=["ot"])
                        P.dma(x1s_d[it * 128:(it + 1) * 128, :], ot[:], reads=["ot"],
                              writes=["x1s_%d" % it])

                    def step(g_):
                        if g_ is None:
                            return False
                        try:
                            next(g_)
                            return True
                        except StopIteration:
                            return False

                    def interleave(g1, g2):
                        a1, a2 = g1 is not None, g2 is not None
                        while a1:
                            a1 = step(g1)
                            if a2:
                                a2 = step(g2)
                        return a2

                    def drain(g_):
                        while step(g_):
                            pass

                    if 0 >= KB:
                        wsel_build(0)
                        indexer(0)
                        drain(topk_iter(0))
                    topk_finish(0)
                    if 1 < NT and 1 >= KB:
                        wsel_build(1)
                    pb1 = pb2 = None
                    for qb in range(NT):
                        it = b * NT + qb
                        P.dma(xres[qb % 2][:], x_d[it * 128:(it + 1) * 128, :], writes=["xres%d" % (qb % 2)])
                        tk = None
                        if qb + 1 < NT and qb + 1 >= KB:
                            indexer(qb + 1)
                            tk = topk_iter(qb + 1)
                        if qb + 2 < NT and qb + 2 >= KB:
                            wsel_build(qb + 2)
                        att = attention(qb)
                        a_att, a_tk = True, tk is not None
                        a_p1, a_p2 = pb1 is not None, pb2 is not None
                        nstep = 0
                        while a_att:
                            a_att = step(att)
                            nstep += 1
                            if a_tk and (nstep >= 2 or qb + 1 < 3):
                                a_tk = step(tk)
                            if a_p1:
                                a_p1 = step(pb1)
                            elif a_p2:
                                a_p2 = step(pb2)
                        if a_p1:
                            drain(pb1)
                        if a_p2:
                            drain(pb2)
                        post_a(qb)
                        pb1 = post_b1(qb)
                        pb2 = post_b2(qb)
                        a_p1 = True
                        while a_tk:
                            a_tk = step(tk)
                            if a_p1:
                                a_p1 = step(pb1)
                        if qb + 1 < NT:
                            topk_finish(qb + 1)
                    drain(pb1)
                    drain(pb2)
                    P.barrier()
                    if stop == "attn":
                        P.finish()
                        return
        with contextlib.ExitStack() as bst:
            bsb = mk_sb(bst)
            W1 = bsb("W1", [128, 8, DFF], BF16)
            W2 = bsb("W2", [128, 32, D], BF16)
            wst = [bsb("wst%d" % i, [128, 2048]) for i in range(2)]
            G2row = bsb("G2row", [128, D])
            xg = [bsb("xg%d" % i, [128, D]) for i in range(4)]
            xn2 = [bsb("xn2_%d" % i, [128, D]) for i in range(1)]
            h2T = [bsb("h2T%d" % i, [128, 8, 256], BF16) for i in range(2)]
            rr = [bsb("rr%d" % i, [128, 256], BF16) for i in range(3)]
            fT = [bsb("fT%d" % i, [128, 256], BF16) for i in range(3)]
            junkB = bsb("junkB", [128, D], BF16)
            ss2 = bsb("ss2", [128, 4])
            rs2 = bsb("rs2", [128, 4])
            ssf = bsb("ssf", [128, 4])
            rsf = bsb("rsf", [128, 2])
            of = [bsb("of%d" % i, [128, D]) for i in range(2)]

            cast_engs = ["dve", "act"]
            ci = 0
            wi_ = 0
            for k in range(8):
                for hf in range(2):
                    st_ = wst[wi_ % 2]
                    sk = "wst%d" % (wi_ % 2)
                    wi_ += 1
                    P.dma(st_[:], w1_d[k * 128:(k + 1) * 128, hf * 2048:(hf + 1) * 2048], writes=[sk])
                    for q2 in range(2):
                        eng = cast_engs[ci % 2]
                        ci += 1
                        sl = slice(q2 * 1024, (q2 + 1) * 1024)
                        dl = slice(hf * 2048 + q2 * 1024, hf * 2048 + (q2 + 1) * 1024)
                        if eng == "act":
                            A(lambda e, k=k, st_=st_, sl=sl, dl=dl: e.activation(out=W1[:, k, dl], in_=st_[:, sl],
                                                                              func=AF.Copy), r=[sk], w=["W1"])
                        else:
                            P.op(eng, lambda e, k=k, st_=st_, sl=sl, dl=dl: e.tensor_copy(out=W1[:, k, dl],
                                                                                       in_=st_[:, sl]), [sk], ["W1"])
            for c2 in range(16):
                st_ = wst[wi_ % 2]
                sk = "wst%d" % (wi_ % 2)
                wi_ += 1
                P.dma(st_[:].rearrange("p (c n) -> p c n", n=D),
                      w2_d[c2 * 256:(c2 + 1) * 256, :].rearrange("(c p) n -> p c n", p=128), writes=[sk])
                for q2 in range(2):
                    eng = cast_engs[ci % 2]
                    ci += 1
                    sl = slice(q2 * 1024, (q2 + 1) * 1024)
                    if eng == "act":
                        A(lambda e, c2=c2, q2=q2, st_=st_, sl=sl: e.activation(out=W2[:, c2 * 2 + q2, :], in_=st_[:, sl],
                                                                          func=AF.Copy), r=[sk], w=["W2"])
                    else:
                        P.op(eng, lambda e, c2=c2, q2=q2, st_=st_, sl=sl: e.tensor_copy(out=W2[:, c2 * 2 + q2, :],
                                                                                   in_=st_[:, sl]), [sk], ["W2"])

            NG = NTOK // 256

            def b_load(g):
                for t in range(2):
                    it = g * 2 + t
                    xi = (g % 2) * 2 + t
                    P.dma(xg[xi][:], x1s_d[it * 128:(it + 1) * 128, :], reads=["x1s_%d" % it], writes=["xg%d" % xi])

            def b_prep_stats(g, t):
                xi = (g % 2) * 2 + t
                A(lambda e: e.activation(out=junkB[:], in_=xg[xi][:], func=AF.Square, accum_out=ss2[:, t:t + 1]),
                  r=["xg%d" % xi], w=["junkB", "ss2_%d" % t])
                rstd(rs2[:, t:t + 1], ss2[:, t:t + 1], 1, 1.0 / D, ["ss2_%d" % t], ["rs2_%d" % t])
                V(lambda e: e.tensor_scalar(out=xn2[0][:], in0=xg[xi][:], scalar1=rs2[:, t:t + 1], scalar2=None,
                                            op0=ALU.mult), r=["xg%d" % xi, "rs2_%d" % t], w=["xn2_0"])

            def b_prep_pe(g, t):
                hj = g % 2
                b = (g * 256) // S
                for k in range(8):
                    T(lambda e, k=k: e.transpose(out=banks[6 + k // 4][:, (k % 4) * 128:(k % 4 + 1) * 128],
                                                 in_=xn2[0][:, k * 128:(k + 1) * 128], identity=ident[:]),
                      r=["xn2_0", "ident"], w=[bkey[6 + k // 4]])
                for k in range(8):
                    A(lambda e, k=k: e.activation(out=h2T[hj][:, k, t * 128:(t + 1) * 128],
                                                  in_=banks[6 + k // 4][:, (k % 4) * 128:(k % 4 + 1) * 128],
                                                  func=AF.Identity, scale=S2T[:, k, b:b + 1],
                                                  bias=sh2T[:, k, b:b + 1]),
                      r=[bkey[6 + k // 4], "S2T", "sh2T"], w=["h2T%d" % hj])

            def b_prep(g):
                for t in range(2):
                    b_prep_stats(g, t)
                    b_prep_pe(g, t)

            f_i = [0]

            def b_main(g):
                hj = g % 2
                b = (g * 256) // S
                if (g * 256) % S == 0:
                    for n in range(2):
                        T(lambda e, n=n: e.matmul(banks[4 + n][:, :], lhsT=sel[0:NSEQ, b, :],
                                                  rhs=gmod[0:NSEQ, 1, n * 512:(n + 1) * 512], start=True, stop=True),
                          r=["sel", "gmod1"], w=[bkey[4 + n]])
                        V(lambda e, n=n: e.tensor_copy(out=G2row[:, n * 512:(n + 1) * 512], in_=banks[4 + n][:, :]),
                          r=[bkey[4 + n]], w=["G2row"])
                def Fst(c):
                    fb = 4 + (c % 2)
                    fi = c % 3
                    for k in range(8):
                        T(lambda e, k=k: e.matmul(banks[fb][:, 0:256], lhsT=W1[:, k, c * 128:(c + 1) * 128],
                                                  rhs=h2T[hj][:, k, :], start=(k == 0), stop=(k == 7)),
                          r=["W1", "h2T%d" % hj], w=[bkey[fb]])
                    A(lambda e: e.activation(out=rr[fi][:], in_=banks[fb][:, 0:256], func=AF.Relu),
                      r=[bkey[fb]], w=["rr%d" % fi])
                    V(lambda e: e.scalar_tensor_tensor(out=fT[fi][:], in0=banks[fb][:, 0:256], scalar=0.0,
                                                       in1=rr[fi][:], op0=ALU.max, op1=ALU.mult),
                      r=[bkey[fb], "rr%d" % fi], w=["fT%d" % fi])

                def P2st(c):
                    fi = c % 3
                    for t in range(2):
                        for n in range(2):
                            ob = t * 2 + n
                            T(lambda e, t=t, n=n, ob=ob: e.matmul(
                                banks[ob][:, :], lhsT=fT[fi][:, t * 128:(t + 1) * 128],
                                rhs=W2[:, c, n * 512:(n + 1) * 512], start=(c == 0), stop=(c == 31)),
                              r=["fT%d" % fi, "W2"], w=[bkey[ob]])

                Fst(0)
                for c in range(32):
                    if c + 1 < 32:
                        Fst(c + 1)
                    P2st(c)
                    if g + 1 < NG:
                        if c == 6:
                            b_prep_stats(g + 1, 0)
                        elif c == 13:
                            b_prep_pe(g + 1, 0)
                        elif c == 15:
                            b_prep_stats(g + 1, 1)
                        elif c == 22:
                            b_prep_pe(g + 1, 1)
                for t in range(2):
                    it = g * 2 + t
                    xi = (g % 2) * 2 + t
                    for n in range(2):
                        A(lambda e, t=t, n=n: e.activation(out=junkB[:, 0:512], in_=banks[t * 2 + n][:, :],
                                                           func=AF.Square, accum_out=ssf[:, t * 2 + n:t * 2 + n + 1]),
                          r=[bkey[t * 2 + n]], w=["junkB", "ssf%d" % (t * 2 + n)])
                    rstd(rsf[:, t:t + 1], ssf[:, t * 2:t * 2 + 1], 1, 1.0 / D, ["ssf%d" % (t * 2), "ssf%d" % (t * 2 + 1)],
                         ["rsf%d" % t], ss2_ap=ssf[:, t * 2 + 1:t * 2 + 2])
                    for n in range(2):
                        V(lambda e, t=t, n=n: e.scalar_tensor_tensor(out=of[t][:, n * 512:(n + 1) * 512],
                                                                     in0=banks[t * 2 + n][:, :], scalar=rsf[:, t:t + 1],
                                                                     in1=G2row[:, n * 512:(n + 1) * 512],
                                                                     op0=ALU.mult, op1=ALU.mult),
                          r=[bkey[t * 2 + n], "rsf%d" % t, "G2row"], w=["of%d" % t])
                    G(lambda e, t=t, xi=xi: e.tensor_tensor(out=of[t][:], in0=of[t][:], in1=xg[xi][:], op=ALU.add),
                      r=["of%d" % t, "xg%d" % xi], w=["of%d" % t])
                    P.dma(out_d[it * 128:(it + 1) * 128, :], of[t][:], reads=["of%d" % t])
                if g + 2 < NG:
                    b_load(g + 2)

            b_load(0)
            if NG > 1:
                b_load(1)
            b_prep(0)
            for g in range(NG):
                b_main(g)
            P.finish()
        print("program built: instrs=%d waits=%d" % (P.ninstr, P.nwaits), flush=True)


def make_core_inputs(ci, NSEQ, S, x, c, positions, w_ada, b_ada, g_pre_mix, w_in, g_sgu_v, w_spatial, b_spatial,
                     g_out_sgu, g_out_attn, w_out, g_post_mix, g_pre_ffn, w_ff1, w_ff2, g_post_ffn):
    f32 = np.float32
    bs = slice(ci * NSEQ, (ci + 1) * NSEQ)
    NT = S // 128
    xc = np.ascontiguousarray(x[bs]).reshape(NSEQ * S, D).astype(f32, copy=False)
    cc = np.asarray(c[bs], dtype=f32)
    cT = np.ascontiguousarray(cc.T.reshape(8, 128, NSEQ).transpose(1, 0, 2))
    pos = np.ascontiguousarray(np.asarray(positions[bs]).reshape(NSEQ * NT, 128).T.astype(np.int32))
    wi = np.asarray(w_in[0], dtype=f32)
    perm = np.concatenate([np.arange(0, 1792), np.arange(2304, 2376), np.arange(1792, 2304)])
    wi_p = np.ascontiguousarray(wi[:, perm])
    return {
        "x": xc, "cT": cT, "pos": pos,
        "w_ada": np.ascontiguousarray(w_ada[0], dtype=f32),
        "b_ada": np.ascontiguousarray(b_ada[0:1], dtype=f32),
        "w_in": wi_p,
        "gpre": np.ascontiguousarray(np.asarray(g_pre_mix[0], dtype=f32).reshape(8, 128).T),
        "gpre2": np.ascontiguousarray(np.asarray(g_pre_ffn[0], dtype=f32).reshape(8, 128).T),
        "gv": np.ascontiguousarray(g_sgu_v[0:1], dtype=f32),
        "ws": np.ascontiguousarray(np.asarray(w_spatial[0], dtype=f32).transpose(1, 0, 2)),
        "bs": np.ascontiguousarray(np.asarray(b_spatial[0], dtype=f32).T),
        "goa": np.ascontiguousarray(g_out_sgu[0:1], dtype=f32),
        "gob": np.ascontiguousarray(g_out_attn[0:1], dtype=f32),
        "w_out": np.ascontiguousarray(w_out[0], dtype=f32),
        "gpost": np.ascontiguousarray(g_post_mix[0:1], dtype=f32),
        "w1": np.ascontiguousarray(w_ff1[0], dtype=f32),
        "w2": np.ascontiguousarray(w_ff2[0], dtype=f32),
        "gpost2": np.ascontiguousarray(g_post_ffn[0:1], dtype=f32),
    }


def run(inputs, n_cores, NSEQ, S, taps=None, trace=False, stop=None):
    nc = bass.Bass("TRN2", target_bir_lowering=False)
    try:
        build_program(nc, NSEQ=NSEQ, S=S, taps=taps, stop=stop)
    except StopBuild:
        pass
    in_maps = [make_core_inputs(ci, NSEQ, S, **inputs) for ci in range(n_cores)]
    res = run_bass_kernel_spmd(nc, in_maps, core_ids=list(range(n_cores)), trace=trace)
    return res


def kernel(**inputs):
    inputs = {k: np.asarray(v) for k, v in inputs.items()}
    B, S, _ = inputs["x"].shape
    NSEQ = B // NCORES
    res = run(inputs, NCORES, NSEQ, S)
    outs = [np.asarray(r["out"]).reshape(NSEQ, S, D) for r in res.results]
    return np.concatenate(outs, axis=0).astype(np.float32, copy=False)
```

```python
import contextlib
import math
import numpy as np
import concourse.bass as bass
import concourse.mybir as mybir
from concourse.bass_utils import run_bass_kernel_spmd

F32 = mybir.dt.float32
BF16 = mybir.dt.bfloat16
I32 = mybir.dt.int32
AF = mybir.ActivationFunctionType
ALU = mybir.AluOpType
AX = mybir.AxisListType

D = 1024
DIN = 2376
DFF = 4096
NCORES = 8
EPS = 1e-6
NIT = 16
IDX_SCALE = (64 ** -0.5) * (8 ** -0.5)
TWO_PI = 2.0 * math.pi


class StopBuild(Exception):
    pass


class Prog:
    NDMA = 32

    def __init__(self, nc, stack):
        self.nc = nc
        self.eng = {"pe": nc.tensor, "act": nc.scalar, "dve": nc.vector,
                    "pool": nc.gpsimd, "sp": nc.sync}
        self.sem = {k: stack.enter_context(nc.semaphore("c_" + k)) for k in self.eng}
        self.cnt = {k: 0 for k in self.eng}
        self.dsem = [stack.enter_context(nc.semaphore("d%d" % i)) for i in range(self.NDMA)]
        self.dval = [0] * self.NDMA
        self.dnext = 0
        self.seen = {k: {} for k in self.eng}
        self.res = {}
        self.nwaits = 0
        self.ninstr = 0

    def _wait(self, eng, dep):
        kind, key, val = dep
        if kind == "e":
            if key == "pe" and eng == "pe":
                return
            sem = self.sem[key]
            skey = key
        else:
            sem = self.dsem[key]
            skey = ("d", key)
        if self.seen[eng].get(skey, 0) >= val:
            return
        self.seen[eng][skey] = val
        self.eng[eng].wait_ge(sem, val)
        self.nwaits += 1

    def _deps(self, eng, reads, writes):
        deps = []
        for r in reads:
            st = self.res.get(r)
            if st and st["w"]:
                deps.append(st["w"])
        for w in writes:
            st = self.res.get(w)
            if st:
                if st["w"]:
                    deps.append(st["w"])
                deps.extend(st["r"])
        for d in deps:
            self._wait(eng, d)

    def _record(self, token, reads, writes):
        for r in reads:
            st = self.res.setdefault(r, {"w": None, "r": []})
            st["r"].append(token)
            if len(st["r"]) > 48:
                best = {}
                for t in st["r"]:
                    k = (t[0], t[1])
                    if k not in best or best[k][2] < t[2]:
                        best[k] = t
                st["r"] = list(best.values())
        for w in writes:
            self.res[w] = {"w": token, "r": []}

    def op(self, eng, fn, reads=(), writes=()):
        self._deps(eng, reads, writes)
        ins = fn(self.eng[eng])
        self.cnt[eng] += 1
        ins.then_inc(self.sem[eng], 1)
        token = ("e", eng, self.cnt[eng])
        self._record(token, reads, writes)
        self.ninstr += 1
        return token

    def dma(self, out, in_, reads=(), writes=(), q="sp", **kw):
        self._deps(q, reads, writes)
        i = self.dnext
        self.dnext = (self.dnext + 1) % self.NDMA
        if self.dval[i] > 0:
            self._wait(q, ("d", i, self.dval[i]))
        ins = self.eng[q].dma_start(out=out, in_=in_, **kw)
        self.dval[i] += 16
        ins.then_inc(self.dsem[i], 16)
        token = ("d", i, self.dval[i])
        self._record(token, reads, writes)
        self.ninstr += 1
        return token

    def barrier(self):
        for e in self.eng:
            for f in self.eng:
                if f != e and self.cnt[f] > 0:
                    self._wait(e, ("e", f, self.cnt[f]))
            for i in range(self.NDMA):
                if self.dval[i] > 0:
                    self._wait(e, ("d", i, self.dval[i]))
        self.res = {}

    def finish(self):
        for i in range(self.NDMA):
            if self.dval[i] > 0:
                self._wait("sp", ("d", i, self.dval[i]))
        for f in self.eng:
            if f != "sp" and self.cnt[f] > 0:
                self._wait("sp", ("e", f, self.cnt[f]))


def build_program(nc, NSEQ=4, S=2048, taps=None, stop=None):
    NT = S // 128
    NTT = NSEQ * NT
    NTOK = NSEQ * S
    TOPK = min(256, S // 4)
    KB = TOPK // 128
    taps = taps or {}

    def din(name, shape, dt=F32):
        return nc.dram_tensor(name, list(shape), dt, kind="ExternalInput").ap()

    x_d = din("x", [NTOK, D])
    cT_d = din("cT", [128, 8, NSEQ])
    pos_d = din("pos", [128, NTT], I32)
    wada_d = din("w_ada", [D, 6 * D])
    bada_d = din("b_ada", [1, 6 * D])
    win_d = din("w_in", [D, DIN])
    gpre_d = din("gpre", [128, 8])
    gpre2_d = din("gpre2", [128, 8])
    gv_d = din("gv", [1, 512])
    ws_d = din("ws", [128, 4, 128])
    bs_d = din("bs", [128, 4])
    goa_d = din("goa", [1, 512])
    gob_d = din("gob", [1, 512])
    wout_d = din("w_out", [D, D])
    gpost_d = din("gpost", [1, D])
    w1_d = din("w1", [D, DFF])
    w2_d = din("w2", [DFF, D])
    gpost2_d = din("gpost2", [1, D])
    out_d = nc.dram_tensor("out", [NTOK, D], F32, kind="ExternalOutput").ap()
    x1s_d = out_d
    tap_d = {k: nc.dram_tensor("tap_" + k, list(shp), F32, kind="ExternalOutput").ap()
             for k, shp in taps.items()}

    with contextlib.ExitStack() as gst:
        P = Prog(nc, gst)

        uid = [0]

        def mk_sb(stack):
            def sb(name, shape, dt=F32):
                uid[0] += 1
                return stack.enter_context(nc.sbuf_tensor("s%d_%s" % (uid[0], name), list(shape), dt))
            return sb

        gsb = mk_sb(gst)
        banks = [gst.enter_context(nc.psum_tensor("bank%d" % i, [128, 512], F32)) for i in range(8)]
        bkey = ["b%d" % i for i in range(8)]
        bbf = [b[:].bitcast(BF16) for b in banks]

        def V(fn, r=(), w=()):
            return P.op("dve", fn, r, w)

        def A(fn, r=(), w=()):
            return P.op("act", fn, r, w)

        def G(fn, r=(), w=()):
            return P.op("pool", fn, r, w)

        def T(fn, r=(), w=()):
            return P.op("pe", fn, r, w)

        ident = gsb("ident", [128, 128])
        identb = gsb("identb", [128, 128], BF16)
        ones_f = gsb("ones_f", [128, 128])
        zeros_f = gsb("zeros_f", [128, 128])
        NEGM = gsb("NEGM", [128, 128])
        TRIU = gsb("TRIU", [128, 128], BF16)
        ONESB = gsb("ONESB", [128, 128], BF16)
        D32 = gsb("D32", [128, 32])
        G4 = gsb("G4", [128, 4])
        zerob = gsb("zerob", [128, 260], BF16)
        P2 = gsb("P2", [128, NIT + 1])
        mhalf = gsb("mhalf", [128, 16])
        iot = gsb("iot", [128, 8], I32)
        iof = gsb("iof", [128, 8])
        invf = gsb("invf", [128, 8])
        rs_tmp = gsb("rs_tmp", [128, 16])

        G(lambda e: e.memset(ones_f[:], 1.0), w=["ones_f"])
        G(lambda e: e.memset(zeros_f[:], 0.0), w=["zeros_f"])
        G(lambda e: e.affine_select(out=ident[:], in_=ones_f[:], pattern=[[-1, 128]], compare_op=ALU.is_equal,
                                    fill=0.0, base=0, channel_multiplier=1), r=["ones_f"], w=["ident"])
        V(lambda e: e.tensor_copy(out=identb[:], in_=ident[:]), r=["ident"], w=["identb"])
        G(lambda e: e.affine_select(out=NEGM[:], in_=zeros_f[:], pattern=[[-1, 128]], compare_op=ALU.is_ge,
                                    fill=-1.0e30, base=0, channel_multiplier=1), r=["zeros_f"], w=["NEGM"])
        G(lambda e: e.affine_select(out=TRIU[:], in_=ones_f[:], pattern=[[1, 128]], compare_op=ALU.is_ge,
                                    fill=0.0, base=0, channel_multiplier=-1), r=["ones_f"], w=["TRIU"])
        V(lambda e: e.tensor_copy(out=ONESB[:], in_=ones_f[:]), r=["ones_f"], w=["ONESB"])
        for m in range(4):
            G(lambda e, m=m: e.affine_select(out=D32[32 * m:32 * m + 32, :], in_=ones_f[32 * m:32 * m + 32, 0:32],
                                             pattern=[[-1, 32]], compare_op=ALU.is_equal, fill=0.0, base=0,
                                             channel_multiplier=1), r=["ones_f"], w=["D32"])
        G(lambda e: e.memset(G4[:], 0.0), w=["G4"])
        for g in range(4):
            G(lambda e, g=g: e.memset(G4[32 * g:32 * g + 32, g:g + 1], 1.0), r=["G4"], w=["G4"])
        G(lambda e: e.memset(zerob[:], 0.0), w=["zerob"])
        for i in range(NIT + 1):
            G(lambda e, i=i: e.memset(P2[:, i:i + 1], 2.0 ** -(i + 1)), w=["P2"])
        G(lambda e: e.memset(mhalf[:], -0.5), w=["mhalf"])
        G(lambda e: e.iota(iot[:], pattern=[[1, 8]], base=0, channel_multiplier=0), w=["iot"])
        V(lambda e: e.tensor_copy(out=iof[:], in_=iot[:]), r=["iot"], w=["iof"])
        A(lambda e: e.activation(out=invf[:], in_=iof[:], func=AF.Exp, scale=-math.log(500000.0) / 8.0),
          r=["iof"], w=["invf"])

        def rstd(out_ap, ss_ap, n, inv_n, rk, wk, ss2_ap=None):
            tmp = rs_tmp[:, 0:n]
            if ss2_ap is not None:
                G(lambda e: e.tensor_tensor(out=tmp, in0=ss_ap, in1=ss2_ap, op=ALU.add), r=rk, w=["rs_tmp"])
                G(lambda e: e.tensor_scalar(out=tmp, in0=tmp, scalar1=inv_n, scalar2=EPS, op0=ALU.mult,
                                            op1=ALU.add), r=["rs_tmp"], w=["rs_tmp"])
            else:
                G(lambda e: e.tensor_scalar(out=tmp, in0=ss_ap, scalar1=inv_n, scalar2=EPS, op0=ALU.mult,
                                            op1=ALU.add), r=rk, w=["rs_tmp"])
            G(lambda e: e.tensor_tensor(out=out_ap, in0=tmp, in1=mhalf[:, 0:n], op=ALU.pow),
              r=["rs_tmp", "mhalf"], w=wk)

        def ck(name):
            if stop == name:
                P.finish()
                raise StopBuild()

        def tap(name, ap, rk, rows=None):
            if name in tap_d:
                dst = tap_d[name]
                P.dma(dst if rows is None else dst[rows], ap, reads=rk)

        S1T = gsb("S1T", [128, 8, NSEQ])
        sh1T = gsb("sh1T", [128, 8, NSEQ])
        S2T = gsb("S2T", [128, 8, NSEQ])
        sh2T = gsb("sh2T", [128, 8, NSEQ])
        gmod = gsb("gmod", [NSEQ, 2, D])
        sel = gsb("sel", [NSEQ, NSEQ, 128])
        gpre = gsb("gpre", [128, 8])
        gpre2 = gsb("gpre2", [128, 8])
        P.dma(gpre[:], gpre_d[:, :], writes=["gpre"])
        P.dma(gpre2[:], gpre2_d[:, :], writes=["gpre2"])

        G(lambda e: e.affine_select(out=sel[:], in_=ones_f[0:NSEQ, :].unsqueeze(1).broadcast_to([NSEQ, NSEQ, 128]),
                                    pattern=[[-1, NSEQ], [0, 128]], compare_op=ALU.is_equal, fill=0.0, base=0,
                                    channel_multiplier=1), r=["ones_f"], w=["sel"])

        with contextlib.ExitStack() as ast:
            asb = mk_sb(ast)
            Win = asb("Win", [128, 8, DIN], BF16)
            Wout = asb("Wout", [128, 8, D], BF16)
            cs = asb("cs", [128, NTT, 8])
            sn = asb("sn", [128, NTT, 8])
            gv_row = asb("gv_row", [128, 512])
            goa_row = asb("goa_row", [128, 512])
            gob_row = asb("gob_row", [128, 512])
            bcol = asb("bcol", [128, 4])
            WmT = asb("WmT", [128, 4, 128], BF16)
            P.dma(gv_row[:], gv_d[0:1, :].partition_broadcast(128), writes=["gv_row"])
            P.dma(goa_row[:], goa_d[0:1, :].partition_broadcast(128), writes=["goa_row"])
            P.dma(gob_row[:], gob_d[0:1, :].partition_broadcast(128), writes=["gob_row"])
            P.dma(bcol[:], bs_d[:, :], writes=["bcol"])

            with contextlib.ExitStack() as sst:
                ssb = mk_sb(sst)
                cTs = ssb("cTs", [128, 8, NSEQ])
                scs = ssb("scs", [128, 8, NSEQ])
                bada4 = ssb("bada4", [NSEQ, 6 * D])
                modrow = ssb("modrow", [NSEQ, 6 * D])
                gpost4 = ssb("gpost4", [NSEQ, 2, D])
                wada_st = [ssb("wada_st%d" % i, [128, 8, 512]) for i in range(2)]
                win_st = [ssb("win_st%d" % i, [128, DIN]) for i in range(2)]
                ws_sb = ssb("ws_sb", [128, 4, 128])
                wsm = ssb("wsm", [128, 4, 128])
                posi = ssb("posi", [128, NTT], I32)
                posf = ssb("posf", [128, NTT])
                ang = ssb("ang", [128, NTT * 8])
                angk = ssb("angk", [128, NTT * 8], I32)
                angf = ssb("angf", [128, NTT * 8])
                angm = ssb("angm", [128, NTT * 8])
                ang2 = ssb("ang2", [128, NTT * 8])

                P.dma(cTs[:], cT_d[:, :, :], writes=["cTs"])
                P.dma(bada4[:], bada_d[0:1, :].partition_broadcast(NSEQ), writes=["bada4"])
                P.dma(gpost4[:, 0, :], gpost_d[0:1, :].partition_broadcast(NSEQ), writes=["gpost4a"])
                P.dma(gpost4[:, 1, :], gpost2_d[0:1, :].partition_broadcast(NSEQ), writes=["gpost4b"])
                P.dma(posi[:], pos_d[:, :], writes=["posi"])
                P.dma(ws_sb[:], ws_d[:, :, :], writes=["ws_sb"])
                A(lambda e: e.activation(out=scs[:], in_=cTs[:], func=AF.Silu), r=["cTs"], w=["scs"])

                order = [2, 3, 0, 1] + list(range(4, 12))
                for n_, cb in enumerate(order):
                    st_ = wada_st[n_ % 2]
                    sk = "wada_st%d" % (n_ % 2)
                    P.dma(st_[:], wada_d[:, cb * 512:(cb + 1) * 512].rearrange("(k p) n -> p k n", p=128),
                          writes=[sk])
                    bk = n_ % 2
                    for k in range(8):
                        T(lambda e, k=k, st_=st_, bk=bk: e.matmul(banks[bk][0:NSEQ, :], lhsT=scs[:, k, :],
                                                                  rhs=st_[:, k, :], start=(k == 0), stop=(k == 7)),
                          r=["scs", sk], w=[bkey[bk]])
                    V(lambda e, bk=bk, cb=cb: e.tensor_tensor(out=modrow[:, cb * 512:(cb + 1) * 512],
                                                              in0=banks[bk][0:NSEQ, :],
                                                              in1=bada4[:, cb * 512:(cb + 1) * 512], op=ALU.add),
                      r=[bkey[bk], "bada4"], w=["modrow%d" % cb])
                allmod = ["modrow%d" % cb for cb in range(12)]
                for si, sp_ in enumerate([0, 1, 3, 4]):
                    for k in range(8):
                        c0 = (si * 8 + k) * NSEQ
                        T(lambda e, sp_=sp_, k=k, c0=c0: e.transpose(
                            out=banks[2][:, c0:c0 + NSEQ], in_=modrow[0:NSEQ, sp_ * D + k * 128:sp_ * D + (k + 1) * 128],
                            identity=ident[0:NSEQ, 0:NSEQ]), r=allmod + ["ident"], w=[bkey[2]])

                def mview(si):
                    return banks[2][:, si * 8 * NSEQ:(si + 1) * 8 * NSEQ].rearrange("p (k b) -> p k b", b=NSEQ)

                V(lambda e: e.tensor_copy(out=sh1T[:], in_=mview(0)), r=[bkey[2]], w=["sh1T"])
                V(lambda e: e.scalar_tensor_tensor(out=S1T[:], in0=mview(1), scalar=1.0,
                                                   in1=gpre[:].unsqueeze(2).broadcast_to([128, 8, NSEQ]),
                                                   op0=ALU.add, op1=ALU.mult), r=[bkey[2], "gpre"], w=["S1T"])
                V(lambda e: e.tensor_copy(out=sh2T[:], in_=mview(2)), r=[bkey[2]], w=["sh2T"])
                V(lambda e: e.scalar_tensor_tensor(out=S2T[:], in0=mview(3), scalar=1.0,
                                                   in1=gpre2[:].unsqueeze(2).broadcast_to([128, 8, NSEQ]),
                                                   op0=ALU.add, op1=ALU.mult), r=[bkey[2], "gpre2"], w=["S2T"])
                V(lambda e: e.tensor_tensor(out=gmod[:, 0, :], in0=modrow[:, 2 * D:3 * D], in1=gpost4[:, 0, :],
                                            op=ALU.mult), r=allmod + ["gpost4a"], w=["gmod0"])
                V(lambda e: e.tensor_tensor(out=gmod[:, 1, :], in0=modrow[:, 5 * D:6 * D], in1=gpost4[:, 1, :],
                                            op=ALU.mult), r=allmod + ["gpost4b"], w=["gmod1"])

                cast_engs = ["dve", "act", "pool"]
                ci = 0
                for k in range(8):
                    st_ = win_st[k % 2]
                    sk = "win_st%d" % (k % 2)
                    P.dma(st_[:], win_d[k * 128:(k + 1) * 128, :], writes=[sk])
                    for h0, h1 in ((0, 1188), (1188, DIN)):
                        eng = cast_engs[ci % 3]
                        ci += 1
                        if eng == "act":
                            A(lambda e, k=k, st_=st_, h0=h0, h1=h1: e.activation(out=Win[:, k, h0:h1], in_=st_[:, h0:h1],
                                                                              func=AF.Copy), r=[sk], w=["Win"])
                        else:
                            P.op(eng, lambda e, k=k, st_=st_, h0=h0, h1=h1: e.tensor_copy(out=Win[:, k, h0:h1],
                                                                                       in_=st_[:, h0:h1]),
                                 [sk], ["Win"])
                for k in range(8):
                    st_ = win_st[k % 2]
                    sk = "win_st%d" % (k % 2)
                    P.dma(st_[:, 0:D], wout_d[k * 128:(k + 1) * 128, :], writes=[sk])
                    eng = cast_engs[ci % 3]
                    ci += 1
                    if eng == "act":
                        A(lambda e, k=k, st_=st_: e.activation(out=Wout[:, k, :], in_=st_[:, 0:D], func=AF.Copy),
                          r=[sk], w=["Wout"])
                    else:
                        P.op(eng, lambda e, k=k, st_=st_: e.tensor_copy(out=Wout[:, k, :], in_=st_[:, 0:D]),
                             [sk], ["Wout"])
                for g in range(4):
                    G(lambda e, g=g: e.affine_select(out=wsm[:, g, :], in_=ws_sb[:, g, :], pattern=[[-1, 128]],
                                                     compare_op=ALU.is_ge, fill=0.0, base=0, channel_multiplier=1),
                      r=["ws_sb"], w=["wsm"])
                for g in range(4):
                    T(lambda e, g=g: e.transpose(out=banks[3][:, g * 128:(g + 1) * 128], in_=wsm[:, g, :],
                                                 identity=ident[:]), r=["wsm", "ident"], w=[bkey[3]])
                V(lambda e: e.tensor_copy(out=WmT[:], in_=banks[3][:, :].rearrange("p (g t) -> p g t", g=4)),
                  r=[bkey[3]], w=["WmT"])

                NA = NTT * 8
                V(lambda e: e.tensor_copy(out=posf[:], in_=posi[:]), r=["posi"], w=["posf"])
                V(lambda e: e.tensor_tensor(out=ang[:].rearrange("p (t f) -> p t f", f=8),
                                            in0=posf[:].unsqueeze(2).broadcast_to([128, NTT, 8]),
                                            in1=invf[:].unsqueeze(1).broadcast_to([128, NTT, 8]), op=ALU.mult),
                  r=["posf", "invf"], w=["ang"])

                def reduce_sin(dst, src_key, shift):
                    V(lambda e: e.tensor_scalar(out=ang2[:], in0=ang[:], scalar1=shift, scalar2=None, op0=ALU.add),
                      r=["ang"], w=["ang2"])
                    V(lambda e: e.tensor_scalar(out=angk[:], in0=ang2[:], scalar1=1.0 / TWO_PI, scalar2=None,
                                                op0=ALU.mult), r=["ang2"], w=["angk"])
                    V(lambda e: e.tensor_copy(out=angf[:], in_=angk[:]), r=["angk"], w=["angf"])
                    V(lambda e: e.scalar_tensor_tensor(out=ang2[:], in0=angf[:], scalar=-TWO_PI, in1=ang2[:],
                                                       op0=ALU.mult, op1=ALU.add), r=["angf", "ang2"], w=["ang2"])
                    V(lambda e: e.tensor_scalar(out=angm[:], in0=ang2[:], scalar1=math.pi, scalar2=-TWO_PI,
                                                op0=ALU.is_gt, op1=ALU.mult), r=["ang2"], w=["angm"])
                    V(lambda e: e.tensor_tensor(out=ang2[:], in0=ang2[:], in1=angm[:], op=ALU.add),
                      r=["ang2", "angm"], w=["ang2"])
                    V(lambda e: e.tensor_scalar(out=angm[:], in0=ang2[:], scalar1=-math.pi, scalar2=TWO_PI,
                                                op0=ALU.is_lt, op1=ALU.mult), r=["ang2"], w=["angm"])
                    V(lambda e: e.tensor_tensor(out=ang2[:], in0=ang2[:], in1=angm[:], op=ALU.add),
                      r=["ang2", "angm"], w=["ang2"])
                    V(lambda e: e.tensor_scalar(out=ang2[:], in0=ang2[:], scalar1=-3.1415925, scalar2=3.1415925,
                                                op0=ALU.max, op1=ALU.min), r=["ang2"], w=["ang2"])
                    A(lambda e: e.activation(out=dst[:].rearrange("p t f -> p (t f)"), in_=ang2[:], func=AF.Sin),
                      r=["ang2"], w=[src_key])

                reduce_sin(sn, "sn", 0.0)
                reduce_sin(cs, "cs", math.pi / 2.0)
                P.barrier()

            if stop == "setup":
                P.finish()
                return
            tap("S1T", S1T[:].rearrange("p k b -> p (k b)"), ["S1T"])
            tap("gmod", gmod[:].rearrange("b g d -> b (g d)"), ["gmod0", "gmod1"])
            tap("cs", cs[:].rearrange("p t f -> p (t f)"), ["cs"])
            tap("sn", sn[:].rearrange("p t f -> p (t f)"), ["sn"])

            qT2 = asb("qT2", [128, 4, S], BF16)
            kTz = [asb("kTz%d" % i, [128, 2, S], BF16) for i in range(2)]
            kiTz = [asb("kiTz%d" % i, [128, S], BF16) for i in range(2)]
            qiT2 = asb("qiT2", [128, S // 32, 4, 32], BF16)
            v_aug = asb("v_aug", [128, NT, 2, 65], BF16)
            w_tok = asb("w_tok", [128, NT, 8])
            mTa = asb("mTa", [128, 4, S], BF16)
            G1row = asb("G1row", [128, D])
            junkA = asb("junkA", [128, D], BF16)
            G(lambda e: e.memset(v_aug[:].rearrange("p a b c -> p (a b) c")[:, :, 64:65], 1.0), w=["v_aug"])
            G(lambda e: e.memset(kTz[0][64:128, :, :], 0.0), w=["kTd"])
            G(lambda e: e.memset(kTz[1][0:64, :, :], 0.0), w=["kTd"])
            G(lambda e: e.memset(kiTz[0][64:128, :], 0.0), w=["kiTd"])
            G(lambda e: e.memset(kiTz[1][0:64, :], 0.0), w=["kiTd"])

            for b in range(NSEQ):
                for n in range(2):
                    T(lambda e, n=n: e.matmul(banks[n][:, :], lhsT=sel[0:NSEQ, b, :],
                                              rhs=gmod[0:NSEQ, 0, n * 512:(n + 1) * 512], start=True, stop=True),
                      r=["sel", "gmod0"], w=[bkey[n]])
                    V(lambda e, n=n: e.tensor_copy(out=G1row[:, n * 512:(n + 1) * 512], in_=banks[n][:, :]),
                      r=[bkey[n]], w=["G1row"])

                with contextlib.ExitStack() as pst:
                    psb = mk_sb(pst)
                    xt = [psb("xt%d" % i, [128, D]) for i in range(2)]
                    xn = [psb("xn%d" % i, [128, D]) for i in range(2)]
                    hT = [psb("hT%d" % i, [128, 8, 128], BF16) for i in range(2)]
                    ssx = psb("ssx", [128, 2])
                    rsx = psb("rsx", [128, 2])
                    zu = [psb("zu%d" % i, [128, 512], BF16) for i in range(2)]
                    zv = psb("zv", [128, 512], BF16)
                    vn = [psb("vn%d" % i, [128, 512], BF16) for i in range(2)]
                    ssv = psb("ssv", [128, 4])
                    rsv = psb("rsv", [128, 4])
                    ya = psb("ya", [128, 512])
                    ssa = psb("ssa", [128, 1])
                    rsa = psb("rsa", [128, 1])
                    ma = psb("ma", [128, 512], BF16)
                    q_tok = [psb("q_tok%d" % i, [128, 8, 64], BF16) for i in range(2)]
                    qi_tok = [psb("qi_tok%d" % i, [128, 8, 64], BF16) for i in range(2)]
                    kd = [psb("kd%d" % i, [128, 2, 2, 64], BF16) for i in range(2)]
                    kid = [psb("kid%d" % i, [128, 2, 64], BF16) for i in range(2)]
                    rt = [psb("rt%d" % i, [128, 8, 8]) for i in range(4)]

                    def s1_load(i):
                        it = b * NT + i
                        P.dma(xt[i % 2][:], x_d[it * 128:(it + 1) * 128, :], writes=["xt%d" % (i % 2)])

                    def s1_pre(i):
                        j = i % 2
                        A(lambda e: e.activation(out=junkA[:], in_=xt[j][:], func=AF.Square,
                                                 accum_out=ssx[:, j:j + 1]), r=["xt%d" % j], w=["junkA", "ssx%d" % j])
                        rstd(rsx[:, j:j + 1], ssx[:, j:j + 1], 1, 1.0 / D, ["ssx%d" % j], ["rsx%d" % j])
                        V(lambda e: e.tensor_scalar(out=xn[j][:], in0=xt[j][:], scalar1=rsx[:, j:j + 1], scalar2=None,
                                                    op0=ALU.mult), r=["xt%d" % j, "rsx%d" % j], w=["xn%d" % j])

                    def s1_post(i):
                        j = i % 2
                        for k in range(8):
                            T(lambda e, k=k: e.transpose(out=banks[k // 4][:, (k % 4) * 128:(k % 4 + 1) * 128],
                                                         in_=xn[j][:, k * 128:(k + 1) * 128], identity=ident[:]),
                              r=["xn%d" % j, "ident"], w=[bkey[k // 4]])
                        for k in range(8):
                            if k % 2 == 0:
                                A(lambda e, k=k: e.activation(out=hT[j][:, k, :],
                                                              in_=banks[k // 4][:, (k % 4) * 128:(k % 4 + 1) * 128],
                                                              func=AF.Identity, scale=S1T[:, k, b:b + 1],
                                                              bias=sh1T[:, k, b:b + 1]),
                                  r=[bkey[k // 4], "S1T", "sh1T"], w=["hT%d" % j])
                            else:
                                V(lambda e, k=k: e.tensor_scalar(out=hT[j][:, k, :],
                                                                 in0=banks[k // 4][:, (k % 4) * 128:(k % 4 + 1) * 128],
                                                                 scalar1=S1T[:, k, b:b + 1], scalar2=sh1T[:, k, b:b + 1],
                                                                 op0=ALU.mult, op1=ALU.add),
                                  r=[bkey[k // 4], "S1T", "sh1T"], w=["hT%d" % j])

                    GROUPS = [(0, 512, 2), (512, 512, 3), (1024, 512, 4), (1536, 328, 5), (1864, 512, 6)]

                    def rope(src3, src_key, dst3, dst_key, H, it):
                        c = cs[:, it, :].unsqueeze(1).broadcast_to([128, H, 8])
                        s_ = sn[:, it, :].unsqueeze(1).broadcast_to([128, H, 8])
                        x1 = src3[:, :, 0:8]
                        x2 = src3[:, :, 8:16]
                        t = [r_[:, 0:H, :] for r_ in rt]
                        V(lambda e: e.tensor_tensor(out=t[0], in0=x1, in1=c, op=ALU.mult), r=[src_key, "cs"], w=["rt0"])
                        V(lambda e: e.tensor_tensor(out=t[1], in0=x2, in1=s_, op=ALU.mult), r=[src_key, "sn"], w=["rt1"])
                        V(lambda e: e.tensor_tensor(out=dst3[:, :, 0:8], in0=t[0], in1=t[1], op=ALU.subtract),
                          r=["rt0", "rt1"], w=[dst_key])
                        V(lambda e: e.tensor_tensor(out=t[2], in0=x2, in1=c, op=ALU.mult), r=[src_key, "cs"], w=["rt2"])
                        V(lambda e: e.tensor_tensor(out=t[3], in0=x1, in1=s_, op=ALU.mult), r=[src_key, "sn"], w=["rt3"])
                        V(lambda e: e.tensor_tensor(out=dst3[:, :, 8:16], in0=t[2], in1=t[3], op=ALU.add),
                          r=["rt2", "rt3"], w=[dst_key])
                        A(lambda e: e.activation(out=dst3[:, :, 16:64], in_=src3[:, :, 16:64], func=AF.Copy),
                          r=[src_key], w=[dst_key])

                    def s2(i):
                        j = i % 2
                        it = b * NT + i
                        for (c0, n, bk) in GROUPS:
                            for k in range(8):
                                T(lambda e, k=k, c0=c0, n=n, bk=bk: e.matmul(banks[bk][:, 0:n], lhsT=hT[j][:, k, :],
                                                                             rhs=Win[:, k, c0:c0 + n], start=(k == 0),
                                                                             stop=(k == 7)),
                                  r=["hT%d" % j, "Win"], w=[bkey[bk]])
                        A(lambda e: e.activation(out=zu[j][:], in_=banks[2][:, :], func=AF.Gelu_apprx_tanh),
                          r=[bkey[2]], w=["zu%d" % j])
                        A(lambda e: e.activation(out=zv[:], in_=banks[3][:, :], func=AF.Gelu_apprx_tanh),
                          r=[bkey[3]], w=["zv"])
                        rope(banks[4][:, :].rearrange("p (h d) -> p h d", d=64), bkey[4], q_tok[j][:], "q_tok%d" % j, 8, it)
                        rope(banks[5][:, 0:128].rearrange("p (h d) -> p h d", d=64), bkey[5], kd[j][:, :, 0, :],
                             "kd%d" % j, 2, it)
                        V(lambda e: e.tensor_copy(out=kd[j][:, :, 1, :], in_=kd[j][:, :, 0, :]), r=["kd%d" % j],
                          w=["kd%d" % j])
                        V(lambda e: e.tensor_copy(out=v_aug[:, i, :, 0:64],
                                                  in_=banks[5][:, 128:256].rearrange("p (h d) -> p h d", d=64)),
                          r=[bkey[5]], w=["v_aug"])
                        rope(banks[5][:, 256:320].rearrange("p (h d) -> p h d", d=64), bkey[5], kid[j][:, 0:1, :],
                             "kid%d" % j, 1, it)
                        V(lambda e: e.tensor_copy(out=kid[j][:, 1:2, :], in_=kid[j][:, 0:1, :]), r=["kid%d" % j],
                          w=["kid%d" % j])
                        V(lambda e: e.tensor_copy(out=w_tok[:, i, :], in_=banks[5][:, 320:328]), r=[bkey[5]],
                          w=["w_tok"])
                        rope(banks[6][:, :].rearrange("p (h d) -> p h d", d=64), bkey[6], qi_tok[j][:], "qi_tok%d" % j, 8, it)
                        for g in range(4):
                            A(lambda e, g=g: e.activation(out=junkA[:, 0:128], in_=zv[:, g * 128:(g + 1) * 128],
                                                          func=AF.Square, accum_out=ssv[:, g:g + 1]),
                              r=["zv"], w=["junkA", "ssv"])
                        rstd(rsv[:, 0:4], ssv[:, 0:4], 4, 1.0 / 128, ["ssv"], ["rsv"])
                        for g in range(4):
                            V(lambda e, g=g: e.scalar_tensor_tensor(out=vn[j][:, g * 128:(g + 1) * 128],
                                                                    in0=zv[:, g * 128:(g + 1) * 128],
                                                                    scalar=rsv[:, g:g + 1],
                                                                    in1=gv_row[:, g * 128:(g + 1) * 128],
                                                                    op0=ALU.mult, op1=ALU.mult),
                              r=["zv", "rsv", "gv_row"], w=["vn%d" % j])

                    def s3a(i):
                        j = i % 2
                        ts = slice(i * 128, (i + 1) * 128)
                        qf = q_tok[j][:].rearrange("p h d -> p (h d)")
                        for c in range(4):
                            T(lambda e, c=c: e.transpose(out=bbf[0][:, c * 128:(c + 1) * 128],
                                                         in_=qf[:, c * 128:(c + 1) * 128], identity=identb[:]),
                              r=["q_tok%d" % j, "identb"], w=[bkey[0]])
                        for kv in range(2):
                            T(lambda e, kv=kv: e.transpose(out=bbf[0][:, 512 + kv * 128:512 + (kv + 1) * 128],
                                                           in_=kd[j][:, kv, :, :].rearrange("p a d -> p (a d)"),
                                                           identity=identb[:]),
                              r=["kd%d" % j, "identb"], w=[bkey[0]])
                        T(lambda e: e.transpose(out=bbf[0][:, 768:896], in_=kid[j][:].rearrange("p a d -> p (a d)"),
                                                identity=identb[:]), r=["kid%d" % j, "identb"], w=[bkey[0]])
                        V(lambda e: e.tensor_copy(out=qT2[:, :, ts],
                                                  in_=bbf[0][:, 0:512].rearrange("p (c t) -> p c t", t=128)),
                          r=[bkey[0]], w=["qT2"])
                        for par in range(2):
                            ps = slice(64 * par, 64 * par + 64)
                            V(lambda e, par=par, ps=ps: e.tensor_copy(
                                out=kTz[par][ps, :, ts],
                                in_=bbf[0][ps, 512:768].rearrange("p (c t) -> p c t", t=128)),
                              r=[bkey[0]], w=["kTd"])
                            V(lambda e, par=par, ps=ps: e.tensor_copy(out=kiTz[par][ps, ts], in_=bbf[0][ps, 768:896]),
                              r=[bkey[0]], w=["kiTd"])
                        qif = qi_tok[j][:].rearrange("p h d -> p (h d)")
                        for c in range(4):
                            T(lambda e, c=c: e.transpose(out=bbf[1][:, c * 128:(c + 1) * 128],
                                                         in_=qif[:, c * 128:(c + 1) * 128], identity=identb[:]),
                              r=["qi_tok%d" % j, "identb"], w=[bkey[1]])
                        A(lambda e: e.activation(out=qiT2[:, i * 4:(i + 1) * 4, :, :],
                                                 in_=bbf[1][:, 0:512].rearrange("p (c g t) -> p g c t", c=4, g=4),
                                                 func=AF.Copy), r=[bkey[1]], w=["qiT2"])
                        for g in range(4):
                            T(lambda e, g=g: e.matmul(banks[7][:, g * 128:(g + 1) * 128], lhsT=WmT[:, g, :],
                                                      rhs=vn[j][:, g * 128:(g + 1) * 128], start=True, stop=True),
                              r=["WmT", "vn%d" % j], w=[bkey[7]])
                        for g in range(4):
                            V(lambda e, g=g: e.scalar_tensor_tensor(out=ya[:, g * 128:(g + 1) * 128],
                                                                    in0=banks[7][:, g * 128:(g + 1) * 128],
                                                                    scalar=bcol[:, g:g + 1],
                                                                    in1=zu[j][:, g * 128:(g + 1) * 128],
                                                                    op0=ALU.add, op1=ALU.mult),
                              r=[bkey[7], "bcol", "zu%d" % j], w=["ya"])
                        A(lambda e: e.activation(out=junkA[:, 0:512], in_=ya[:], func=AF.Square, accum_out=ssa[:]),
                          r=["ya"], w=["junkA", "ssa"])
                        rstd(rsa[:], ssa[:], 1, 1.0 / 512, ["ssa"], ["rsa"])
                        V(lambda e: e.scalar_tensor_tensor(out=ma[:], in0=ya[:], scalar=rsa[:, 0:1], in1=goa_row[:],
                                                           op0=ALU.mult, op1=ALU.mult),
                          r=["ya", "rsa", "goa_row"], w=["ma"])
                        if b == 0 and i == 0:
                            tap("ya", ya[:], ["ya"])

                    def s3b(i):
                        ts = slice(i * 128, (i + 1) * 128)
                        for c in range(4):
                            T(lambda e, c=c: e.transpose(out=bbf[7][:, c * 128:(c + 1) * 128],
                                                         in_=ma[:, c * 128:(c + 1) * 128], identity=identb[:]),
                              r=["ma", "identb"], w=[bkey[7]])
                        A(lambda e: e.activation(out=mTa[:, :, ts],
                                                 in_=bbf[7][:, 0:512].rearrange("p (c t) -> p c t", t=128),
                                                 func=AF.Copy), r=[bkey[7]], w=["mTa"])

                    s1_load(0)
                    if NT > 1:
                        s1_load(1)
                    s1_pre(0)
                    s1_post(0)
                    if NT > 2:
                        s1_load(2)
                    if NT > 1:
                        s1_pre(1)
                        s1_post(1)
                    for n in range(NT + 2):
                        if n + 2 < NT:
                            s1_pre(n + 2)
                            if n + 3 < NT:
                                s1_load(n + 3)
                        if n < NT:
                            s2(n)
                        if 0 <= n - 2 < NT:
                            s3b(n - 2)
                        if 0 <= n - 1 < NT:
                            s3a(n - 1)
                        if n + 2 < NT:
                            s1_post(n + 2)
                    P.barrier()
                    if stop == "proj":
                        P.finish()
                        return

                with contextlib.ExitStack() as tst:
                    tsb = mk_sb(tst)
                    score = tsb("score", [128, S])
                    cmax = tsb("cmax", [128, 4])
                    mask = tsb("mask", [128, S], BF16)
                    maskT = tsb("maskT", [128, NT, 128], BF16)
                    rl = [tsb("rl%d" % i, [128, 512], BF16) for i in range(4)]
                    pT = [tsb("pT%d" % i, [128, 512], BF16) for i in range(4)]
                    Wsel = [tsb("Wsel%d" % i, [128, 8, 128], BF16) for i in range(2)]
                    wrep = tsb("wrep", [128, 2, 128])
                    wcol = tsb("wcol", [128, 8])
                    lo0 = tsb("lo0", [128, 1])
                    hi0 = tsb("hi0", [128, 1])
                    w0 = tsb("w0", [128, 1])
                    wh = tsb("wh", [128, NIT + 1])
                    cbias = tsb("cbias", [128, NT])
                    mid = tsb("mid", [128, 1])
                    cnt = tsb("cnt", [128, 1])
                    btmp = tsb("btmp", [128, 1])
                    rden = tsb("rden", [128, 8])
                    yb = tsb("yb", [128, 512])
                    ssb_ = tsb("ssb_", [128, 1])
                    rsb = tsb("rsb", [128, 1])
                    mb = tsb("mb", [128, 512], BF16)
                    mbT = tsb("mbT", [128, 4, 128], BF16)
                    sso = tsb("sso", [128, 2])
                    rso = tsb("rso", [128, 1])
                    ot = tsb("ot", [128, D])
                    xres = [tsb("xres%d" % i, [128, D]) for i in range(2)]
                    for i in range(2):
                        G(lambda e, i=i: e.memset(Wsel[i][:], 0.0), w=["Wsel%d" % i])
                    for qq in range(NT):
                        G(lambda e, qq=qq: e.memset(cbias[:, qq:qq + 1], float((qq + 1) * 128 - 2 * TOPK) + 0.5),
                          w=["cbias"])
                    rl_i = [0]
                    pT_i = [0]
                    D_i = [0]

                    def wsel_build(qb):
                        wi = qb % 2
                        wk = "Wsel%d" % wi
                        w2v = w_tok[:, qb, :].rearrange("p (i two) -> p i two", two=2)
                        for par in range(2):
                            V(lambda e, par=par: e.tensor_tensor(
                                out=wrep[:, par, :].rearrange("p (i t) -> p i t", t=32),
                                in0=w2v[:, :, par].unsqueeze(2).broadcast_to([128, 4, 32]),
                                in1=D32[:].unsqueeze(1).broadcast_to([128, 4, 32]), op=ALU.mult),
                              r=["w_tok", "D32"], w=["wrep"])
                        for par in range(2):
                            T(lambda e, par=par: e.matmul(banks[0][:, par * 4:(par + 1) * 4], lhsT=wrep[:, par, :],
                                                          rhs=G4[:], start=True, stop=True),
                              r=["wrep", "G4"], w=[bkey[0]])
                        V(lambda e: e.tensor_scalar(out=wcol[:], in0=banks[0][:, 0:8], scalar1=IDX_SCALE, scalar2=None,
                                                    op0=ALU.mult), r=[bkey[0]], w=["wcol"])
                        for par in range(2):
                            for g in range(4):
                                V(lambda e, par=par, g=g: e.tensor_scalar(
                                    out=Wsel[wi][:, par * 4 + g, 32 * g:32 * g + 32], in0=D32[:],
                                    scalar1=wcol[:, par * 4 + g:par * 4 + g + 1], scalar2=None, op0=ALU.mult),
                                  r=["D32", "wcol"], w=[wk])

                    def indexer(qb):
                        N = (qb + 1) * 128
                        nch = (N + 511) // 512
                        wi = qb % 2
                        wk = "Wsel%d" % wi
                        units = []
                        for c in range(nch):
                            n = min(512, N - c * 512)
                            for g in range(4):
                                for par in range(2):
                                    units.append((c, n, g, par))

                        DBK = [0, 1, 3]

                        def dots(u):
                            c, n, g, par = u
                            ri = rl_i[0] % 4
                            dbk = DBK[D_i[0] % 3]
                            D_i[0] += 1
                            rl_i[0] += 1
                            T(lambda e: e.matmul(banks[dbk][:, 0:n],
                                                 lhsT=qiT2[:, qb * 4 + g, :, :].rearrange("p c t -> p (c t)"),
                                                 rhs=kiTz[par][:, c * 512:c * 512 + n], start=True, stop=True),
                              r=["qiT2", "kiTd"], w=[bkey[dbk]])
                            if par == 0:
                                A(lambda e: e.activation(out=rl[ri][:, 0:n], in_=banks[dbk][:, 0:n], func=AF.Relu),
                                  r=[bkey[dbk]], w=["rl%d" % ri])
                            else:
                                V(lambda e: e.tensor_scalar(out=rl[ri][:, 0:n], in0=banks[dbk][:, 0:n], scalar1=0.0,
                                                            scalar2=None, op0=ALU.max), r=[bkey[dbk]], w=["rl%d" % ri])
                            return ri

                        def selmm(u, ri):
                            c, n, g, par = u
                            sbk = 2
                            first = (g == 0 and par == 0)
                            last = (g == 3 and par == 1)
                            T(lambda e: e.matmul(banks[sbk][:, 0:n], lhsT=Wsel[wi][:, par * 4 + g, :],
                                                 rhs=rl[ri][:, 0:n], start=first, stop=last),
                              r=[wk, "rl%d" % ri], w=[bkey[sbk]])
                            if last:
                                V(lambda e: e.tensor_scalar(out=score[:, c * 512:c * 512 + n], in0=banks[sbk][:, 0:n],
                                                            scalar1=1.0, scalar2=None, op0=ALU.mult, op1=ALU.max,
                                                            accum_out=cmax[:, c:c + 1]),
                                  r=[bkey[sbk]], w=["score", "cmax"])

                        ris = {}
                        LOOK = 3
                        for i_ in range(min(LOOK, len(units))):
                            ris[i_] = dots(units[i_])
                        for i_ in range(len(units)):
                            if i_ + LOOK < len(units):
                                ris[i_ + LOOK] = dots(units[i_ + LOOK])
                            selmm(units[i_], ris[i_])

                    def topk_iter(qb):
                        N = (qb + 1) * 128
                        nch = (N + 511) // 512
                        V(lambda e: e.tensor_reduce(out=lo0[:], in_=score[:, 0:N], axis=AX.X, op=ALU.min),
                          r=["score"], w=["lo0"])
                        V(lambda e: e.tensor_reduce(out=hi0[:], in_=cmax[:, 0:nch], axis=AX.X, op=ALU.max),
                          r=["cmax"], w=["hi0"])
                        V(lambda e: e.tensor_tensor(out=score[:, qb * 128:N], in0=score[:, qb * 128:N], in1=NEGM[:],
                                                    op=ALU.add), r=["score", "NEGM"], w=["score"])
                        V(lambda e: e.tensor_tensor(out=w0[:], in0=lo0[:], in1=hi0[:], op=ALU.subtract),
                          r=["hi0", "lo0"], w=["w0"])
                        V(lambda e: e.tensor_scalar(out=wh[:], in0=P2[:], scalar1=w0[:, 0:1], scalar2=None,
                                                    op0=ALU.mult), r=["P2", "w0"], w=["wh"])
                        V(lambda e: e.tensor_scalar(out=mid[:], in0=lo0[:], scalar1=-1.0, scalar2=wh[:, 0:1],
                                                    op0=ALU.mult, op1=ALU.add), r=["lo0", "wh"], w=["mid"])
                        for i in range(NIT):
                            A(lambda e: e.activation(out=mask[:, 0:N], in_=score[:, 0:N], func=AF.Sign,
                                                     bias=mid[:, 0:1], accum_out=cnt[:]),
                              r=["score", "mid"], w=["mask", "cnt"])
                            V(lambda e, i=i: e.scalar_tensor_tensor(out=btmp[:], in0=cnt[:],
                                                                    scalar=float(2 * TOPK - N) - 0.5,
                                                                    in1=wh[:, i:i + 1], op0=ALU.is_ge, op1=ALU.mult),
                              r=["cnt", "wh"], w=["btmp"])
                            V(lambda e, i=i: e.scalar_tensor_tensor(out=mid[:], in0=mid[:], scalar=wh[:, i + 1:i + 2],
                                                                    in1=btmp[:], op0=ALU.subtract, op1=ALU.add),
                              r=["mid", "wh", "btmp"], w=["mid"])
                            yield
                        V(lambda e: e.tensor_scalar(out=mid[:], in0=mid[:], scalar1=-1.0, scalar2=wh[:, NIT:NIT + 1],
                                                    op0=ALU.mult, op1=ALU.add), r=["mid", "wh"], w=["mid"])

                    def topk_finish(qb):
                        N = (qb + 1) * 128
                        if qb < KB:
                            for jj in range(qb + 1):
                                src = TRIU if jj == qb else ONESB
                                G(lambda e, jj=jj, src=src: e.tensor_copy(out=maskT[:, jj, :], in_=src[:]),
                                  r=["TRIU", "ONESB"], w=["maskT"])
                            return
                        V(lambda e: e.tensor_scalar(out=mask[:, 0:N], in0=score[:, 0:N], scalar1=mid[:, 0:1],
                                                    scalar2=None, op0=ALU.is_ge), r=["score", "mid"], w=["mask"])
                        if b == 0 and qb == NT - 1:
                            tap("score", score[:, 0:N], ["score"])
                            tap("thr", mid[:], ["mid"])
                        for jj in range(qb + 1):
                            lb = 4 + jj // 8
                            T(lambda e, jj=jj, lb=lb: e.transpose(out=bbf[lb][:, (jj % 8) * 128:(jj % 8 + 1) * 128],
                                                                  in_=mask[:, jj * 128:(jj + 1) * 128],
                                                                  identity=identb[:]),
                              r=["mask", "identb"], w=[bkey[lb]])
                        for lb in range(4, 4 + (qb + 8) // 8):
                            j0 = (lb - 4) * 8
                            j1 = min(qb + 1, j0 + 8)
                            nj = j1 - j0
                            V(lambda e, lb=lb, j0=j0, j1=j1, nj=nj: e.tensor_copy(
                                out=maskT[:, j0:j1, :],
                                in_=bbf[lb][:, 0:nj * 128].rearrange("p (j t) -> p j t", t=128)),
                              r=[bkey[lb]], w=["maskT"])

                    def attention(qb):
                        qs = slice(qb * 128, (qb + 1) * 128)
                        for kv in range(2):
                            T(lambda e, kv=kv: e.matmul(banks[6 + kv][:, 0:260], lhsT=zerob[:, 0:128],
                                                        rhs=zerob[:, 0:260], start=True, stop=False,
                                                        skip_group_check=True), r=["zerob"], w=[bkey[6 + kv]])

                        def Lstage(jj):
                            ks = slice(jj * 128, (jj + 1) * 128)
                            pis = []
                            for par in range(2):
                                ps = slice(64 * par, 64 * par + 64)
                                lb = 4 + par
                                pi = pT_i[0] % 4
                                pT_i[0] += 1
                                pis.append(pi)
                                for kv in range(2):
                                    T(lambda e, ps=ps, lb=lb, kv=kv, par=par: e.matmul(
                                        banks[lb][:, kv * 256:(kv + 1) * 256], lhsT=kTz[par][:, kv, ks],
                                        rhs=qT2[:, 2 * kv:2 * kv + 2, qs], start=True, stop=True),
                                      r=["kTd", "qT2"], w=[bkey[lb]])
                                A(lambda e, lb=lb, pi=pi: e.activation(out=pT[pi][:], in_=banks[lb][:, :], func=AF.Exp,
                                                                       scale=0.125), r=[bkey[lb]], w=["pT%d" % pi])
                                V(lambda e, pi=pi: e.tensor_tensor(
                                    out=pT[pi][:].rearrange("p (h t) -> p h t", t=128),
                                    in0=pT[pi][:].rearrange("p (h t) -> p h t", t=128),
                                    in1=maskT[:, jj, :].unsqueeze(1).broadcast_to([128, 4, 128]), op=ALU.mult),
                                  r=["pT%d" % pi, "maskT"], w=["pT%d" % pi])
                            return pis

                        def PVstage(jj, pis):
                            for par in range(2):
                                pi = pis[par]
                                for kv in range(2):
                                    for ii in range(2):
                                        hl = 2 * ii + par
                                        T(lambda e, ii=ii, hl=hl, kv=kv, pi=pi: e.matmul(
                                            banks[6 + kv][:, hl * 65:hl * 65 + 65],
                                            lhsT=pT[pi][:, (kv * 2 + ii) * 128:(kv * 2 + ii + 1) * 128],
                                            rhs=v_aug[:, jj, kv, :], start=False, stop=(jj == qb),
                                            skip_group_check=True),
                                          r=["pT%d" % pi, "v_aug"], w=[bkey[6 + kv]])

                        nxt = Lstage(0)
                        for jj in range(qb + 1):
                            cur = nxt
                            if jj + 1 <= qb:
                                nxt = Lstage(jj + 1)
                            PVstage(jj, cur)
                            yield

                    def post_a(qb):
                        for kv in range(2):
                            ov = banks[6 + kv][:, 0:260].rearrange("p (h d) -> p h d", d=65)
                            V(lambda e, kv=kv, ov=ov: e.reciprocal(out=rden[:, kv * 4:(kv + 1) * 4], in_=ov[:, :, 64]),
                              r=[bkey[6 + kv]], w=["rden"])
                            V(lambda e, kv=kv, ov=ov: e.tensor_tensor(
                                out=yb[:, kv * 256:(kv + 1) * 256].rearrange("p (h d) -> p h d", d=64),
                                in0=ov[:, :, 0:64],
                                in1=rden[:, kv * 4:(kv + 1) * 4].unsqueeze(2).broadcast_to([128, 4, 64]),
                                op=ALU.mult), r=[bkey[6 + kv], "rden"], w=["yb"])
                        if b == 0 and qb == NT - 1:
                            tap("yb", yb[:], ["yb"])

                    def post_b1(qb):
                        yield
                        A(lambda e: e.activation(out=junkA[:, 0:512], in_=yb[:], func=AF.Square, accum_out=ssb_[:]),
                          r=["yb"], w=["junkA", "ssb_"])
                        yield
                        rstd(rsb[:], ssb_[:], 1, 1.0 / 512, ["ssb_"], ["rsb"])
                        yield
                        V(lambda e: e.scalar_tensor_tensor(out=mb[:], in0=yb[:], scalar=rsb[:, 0:1], in1=gob_row[:],
                                                           op0=ALU.mult, op1=ALU.mult),
                          r=["yb", "rsb", "gob_row"], w=["mb"])

                    def post_b2(qb):
                        it = b * NT + qb
                        xj = qb % 2
                        for c in range(4):
                            T(lambda e, c=c: e.transpose(out=bbf[2][:, c * 128:(c + 1) * 128],
                                                         in_=mb[:, c * 128:(c + 1) * 128], identity=identb[:]),
                              r=["mb", "identb"], w=[bkey[2]])
                        A(lambda e: e.activation(out=mbT[:], in_=bbf[2][:, 0:512].rearrange("p (c t) -> p c t", t=128),
                                                 func=AF.Copy), r=[bkey[2]], w=["mbT"])
                        yield
                        for n in range(2):
                            for k in range(8):
                                lhs = mTa[:, k, qb * 128:(qb + 1) * 128] if k < 4 else mbT[:, k - 4, :]
                                T(lambda e, n=n, k=k, lhs=lhs: e.matmul(banks[2 + n][:, :], lhsT=lhs,
                                                                        rhs=Wout[:, k, n * 512:(n + 1) * 512],
                                                                        start=(k == 0), stop=(k == 7)),
                                  r=["mTa", "mbT", "Wout"], w=[bkey[2 + n]])
                        for n in range(2):
                            A(lambda e, n=n: e.activation(out=junkA[:, 0:512], in_=banks[2 + n][:, :], func=AF.Square,
                                                          accum_out=sso[:, n:n + 1]),
                              r=[bkey[2 + n]], w=["junkA", "sso%d" % n])
                        yield
                        rstd(rso[:], sso[:, 0:1], 1, 1.0 / D, ["sso0", "sso1"], ["rso"], ss2_ap=sso[:, 1:2])
                        yield
                        for n in range(2):
                            V(lambda e, n=n: e.scalar_tensor_tensor(out=ot[:, n * 512:(n + 1) * 512],
                                                                    in0=banks[2 + n][:, :], scalar=rso[:, 0:1],
                                                                    in1=G1row[:, n * 512:(n + 1) * 512],
                                                                    op0=ALU.mult, op1=ALU.mult),
                              r=[bkey[2 + n], "rso", "G1row"], w=["ot"])
                        G(lambda e: e.tensor_tensor(out=ot[:], in0=ot[:], in1=xres[xj][:], op=ALU.add),
                          r=["ot", "xres%d" % xj], w=["ot"])
                        P.dma(x1s_d[it * 128:(it + 1) * 128, :], ot[:], reads=["ot"],
                              writes=["x1s_%d" % it])

                    def step(g_):
                        if g_ is None:
                            return False
                        try:
                            next(g_)
                            return True
                        except StopIteration:
                            return False

                    def interleave(g1, g2):
                        a1, a2 = g1 is not None, g2 is not None
                        while a1:
                            a1 = step(g1)
                            if a2:
                                a2 = step(g2)
                        return a2

                    def drain(g_):
                        while step(g_):
                            pass

                    if 0 >= KB:
                        wsel_build(0)
                        indexer(0)
                        drain(topk_iter(0))
                    topk_finish(0)
                    if 1 < NT and 1 >= KB:
                        wsel_build(1)
                    pb1 = pb2 = None
                    for qb in range(NT):
                        it = b * NT + qb
                        P.dma(xres[qb % 2][:], x_d[it * 128:(it + 1) * 128, :], writes=["xres%d" % (qb % 2)])
                        tk = None
                        if qb + 1 < NT and qb + 1 >= KB:
                            indexer(qb + 1)
                            tk = topk_iter(qb + 1)
                        if qb + 2 < NT and qb + 2 >= KB:
                            wsel_build(qb + 2)
                        att = attention(qb)
                        a_att, a_tk = True, tk is not None
                        a_p1, a_p2 = pb1 is not None, pb2 is not None
                        nstep = 0
                        while a_att:
                            a_att = step(att)
                            nstep += 1
                            if a_tk and (nstep >= 2 or qb + 1 < 3):
                                a_tk = step(tk)
                            if a_p1:
                                a_p1 = step(pb1)
                            elif a_p2:
                                a_p2 = step(pb2)
                        if a_p1:
                            drain(pb1)
                        if a_p2:
                            drain(pb2)
                        post_a(qb)
                        pb1 = post_b1(qb)
                        pb2 = post_b2(qb)
                        a_p1 = True
                        while a_tk:
                            a_tk = step(tk)
                            if a_p1:
                                a_p1 = step(pb1)
                        if qb + 1 < NT:
                            topk_finish(qb + 1)
                    drain(pb1)
                    drain(pb2)
                    P.barrier()
                    if stop == "attn":
                        P.finish()
                        return
        with contextlib.ExitStack() as bst:
            bsb = mk_sb(bst)
            W1 = bsb("W1", [128, 8, DFF], BF16)
            W2 = bsb("W2", [128, 32, D], BF16)
            wst = [bsb("wst%d" % i, [128, 2048]) for i in range(2)]
            G2row = bsb("G2row", [128, D])
            xg = [bsb("xg%d" % i, [128, D]) for i in range(4)]
            xn2 = [bsb("xn2_%d" % i, [128, D]) for i in range(1)]
            h2T = [bsb("h2T%d" % i, [128, 8, 256], BF16) for i in range(2)]
            rr = [bsb("rr%d" % i, [128, 256], BF16) for i in range(3)]
            fT = [bsb("fT%d" % i, [128, 256], BF16) for i in range(3)]
            junkB = bsb("junkB", [128, D], BF16)
            ss2 = bsb("ss2", [128, 4])
            rs2 = bsb("rs2", [128, 4])
            ssf = bsb("ssf", [128, 4])
            rsf = bsb("rsf", [128, 2])
            of = [bsb("of%d" % i, [128, D]) for i in range(2)]

            cast_engs = ["dve", "act"]
            ci = 0
            wi_ = 0
            for k in range(8):
                for hf in range(2):
                    st_ = wst[wi_ % 2]
                    sk = "wst%d" % (wi_ % 2)
                    wi_ += 1
                    P.dma(st_[:], w1_d[k * 128:(k + 1) * 128, hf * 2048:(hf + 1) * 2048], writes=[sk])
                    for q2 in range(2):
                        eng = cast_engs[ci % 2]
                        ci += 1
                        sl = slice(q2 * 1024, (q2 + 1) * 1024)
                        dl = slice(hf * 2048 + q2 * 1024, hf * 2048 + (q2 + 1) * 1024)
                        if eng == "act":
                            A(lambda e, k=k, st_=st_, sl=sl, dl=dl: e.activation(out=W1[:, k, dl], in_=st_[:, sl],
                                                                              func=AF.Copy), r=[sk], w=["W1"])
                        else:
                            P.op(eng, lambda e, k=k, st_=st_, sl=sl, dl=dl: e.tensor_copy(out=W1[:, k, dl],
                                                                                       in_=st_[:, sl]), [sk], ["W1"])
            for c2 in range(16):
                st_ = wst[wi_ % 2]
                sk = "wst%d" % (wi_ % 2)
                wi_ += 1
                P.dma(st_[:].rearrange("p (c n) -> p c n", n=D),
                      w2_d[c2 * 256:(c2 + 1) * 256, :].rearrange("(c p) n -> p c n", p=128), writes=[sk])
                for q2 in range(2):
                    eng = cast_engs[ci % 2]
                    ci += 1
                    sl = slice(q2 * 1024, (q2 + 1) * 1024)
                    if eng == "act":
                        A(lambda e, c2=c2, q2=q2, st_=st_, sl=sl: e.activation(out=W2[:, c2 * 2 + q2, :], in_=st_[:, sl],
                                                                          func=AF.Copy), r=[sk], w=["W2"])
                    else:
                        P.op(eng, lambda e, c2=c2, q2=q2, st_=st_, sl=sl: e.tensor_copy(out=W2[:, c2 * 2 + q2, :],
                                                                                   in_=st_[:, sl]), [sk], ["W2"])

            NG = NTOK // 256

            def b_load(g):
                for t in range(2):
                    it = g * 2 + t
                    xi = (g % 2) * 2 + t
                    P.dma(xg[xi][:], x1s_d[it * 128:(it + 1) * 128, :], reads=["x1s_%d" % it], writes=["xg%d" % xi])

            def b_prep_stats(g, t):
                xi = (g % 2) * 2 + t
                A(lambda e: e.activation(out=junkB[:], in_=xg[xi][:], func=AF.Square, accum_out=ss2[:, t:t + 1]),
                  r=["xg%d" % xi], w=["junkB", "ss2_%d" % t])
                rstd(rs2[:, t:t + 1], ss2[:, t:t + 1], 1, 1.0 / D, ["ss2_%d" % t], ["rs2_%d" % t])
                V(lambda e: e.tensor_scalar(out=xn2[0][:], in0=xg[xi][:], scalar1=rs2[:, t:t + 1], scalar2=None,
                                            op0=ALU.mult), r=["xg%d" % xi, "rs2_%d" % t], w=["xn2_0"])

            def b_prep_pe(g, t):
                hj = g % 2
                b = (g * 256) // S
                for k in range(8):
                    T(lambda e, k=k: e.transpose(out=banks[6 + k // 4][:, (k % 4) * 128:(k % 4 + 1) * 128],
                                                 in_=xn2[0][:, k * 128:(k + 1) * 128], identity=ident[:]),
                      r=["xn2_0", "ident"], w=[bkey[6 + k // 4]])
                for k in range(8):
                    A(lambda e, k=k: e.activation(out=h2T[hj][:, k, t * 128:(t + 1) * 128],
                                                  in_=banks[6 + k // 4][:, (k % 4) * 128:(k % 4 + 1) * 128],
                                                  func=AF.Identity, scale=S2T[:, k, b:b + 1],
                                                  bias=sh2T[:, k, b:b + 1]),
                      r=[bkey[6 + k // 4], "S2T", "sh2T"], w=["h2T%d" % hj])

            def b_prep(g):
                for t in range(2):
                    b_prep_stats(g, t)
                    b_prep_pe(g, t)

            f_i = [0]

            def b_main(g):
                hj = g % 2
                b = (g * 256) // S
                if (g * 256) % S == 0:
                    for n in range(2):
                        T(lambda e, n=n: e.matmul(banks[4 + n][:, :], lhsT=sel[0:NSEQ, b, :],
                                                  rhs=gmod[0:NSEQ, 1, n * 512:(n + 1) * 512], start=True, stop=True),
                          r=["sel", "gmod1"], w=[bkey[4 + n]])
                        V(lambda e, n=n: e.tensor_copy(out=G2row[:, n * 512:(n + 1) * 512], in_=banks[4 + n][:, :]),
                          r=[bkey[4 + n]], w=["G2row"])
                def Fst(c):
                    fb = 4 + (c % 2)
                    fi = c % 3
                    for k in range(8):
                        T(lambda e, k=k: e.matmul(banks[fb][:, 0:256], lhsT=W1[:, k, c * 128:(c + 1) * 128],
                                                  rhs=h2T[hj][:, k, :], start=(k == 0), stop=(k == 7)),
                          r=["W1", "h2T%d" % hj], w=[bkey[fb]])
                    A(lambda e: e.activation(out=rr[fi][:], in_=banks[fb][:, 0:256], func=AF.Relu),
                      r=[bkey[fb]], w=["rr%d" % fi])
                    V(lambda e: e.scalar_tensor_tensor(out=fT[fi][:], in0=banks[fb][:, 0:256], scalar=0.0,
                                                       in1=rr[fi][:], op0=ALU.max, op1=ALU.mult),
                      r=[bkey[fb], "rr%d" % fi], w=["fT%d" % fi])

                def P2st(c):
                    fi = c % 3
                    for t in range(2):
                        for n in range(2):
                            ob = t * 2 + n
                            T(lambda e, t=t, n=n, ob=ob: e.matmul(
                                banks[ob][:, :], lhsT=fT[fi][:, t * 128:(t + 1) * 128],
                                rhs=W2[:, c, n * 512:(n + 1) * 512], start=(c == 0), stop=(c == 31)),
                              r=["fT%d" % fi, "W2"], w=[bkey[ob]])

                Fst(0)
                for c in range(32):
                    if c + 1 < 32:
                        Fst(c + 1)
                    P2st(c)
                    if g + 1 < NG:
                        if c == 6:
                            b_prep_stats(g + 1, 0)
                        elif c == 13:
                            b_prep_pe(g + 1, 0)
                        elif c == 15:
                            b_prep_stats(g + 1, 1)
                        elif c == 22:
                            b_prep_pe(g + 1, 1)
                for t in range(2):
                    for n in range(2):
                        A(lambda e, t=t, n=n: e.activation(out=junkB[:, 0:512], in_=banks[t * 2 + n][:, :],
                                                           func=AF.Square, accum_out=ssf[:, t * 2 + n:t * 2 + n + 1]),
                          r=[bkey[t * 2 + n]], w=["junkB", "ssf%d" % (t * 2 + n)])
                for t in range(2):
                    rstd(rsf[:, t:t + 1], ssf[:, t * 2:t * 2 + 1], 1, 1.0 / D, ["ssf%d" % (t * 2), "ssf%d" % (t * 2 + 1)],
                         ["rsf%d" % t], ss2_ap=ssf[:, t * 2 + 1:t * 2 + 2])
                for t in range(2):
                    for n in range(2):
                        V(lambda e, t=t, n=n: e.scalar_tensor_tensor(out=of[t][:, n * 512:(n + 1) * 512],
                                                                     in0=banks[t * 2 + n][:, :], scalar=rsf[:, t:t + 1],
                                                                     in1=G2row[:, n * 512:(n + 1) * 512],
                                                                     op0=ALU.mult, op1=ALU.mult),
                          r=[bkey[t * 2 + n], "rsf%d" % t, "G2row"], w=["of%d" % t])
                for t in range(2):
                    it = g * 2 + t
                    xi = (g % 2) * 2 + t
                    G(lambda e, t=t, xi=xi: e.tensor_tensor(out=of[t][:], in0=of[t][:], in1=xg[xi][:], op=ALU.add),
                      r=["of%d" % t, "xg%d" % xi], w=["of%d" % t])
                    P.dma(out_d[it * 128:(it + 1) * 128, :], of[t][:], reads=["of%d" % t])
                if g + 2 < NG:
                    b_load(g + 2)

            b_load(0)
            if NG > 1:
                b_load(1)
            b_prep(0)
            for g in range(NG):
                b_main(g)
            P.finish()
        print("program built: instrs=%d waits=%d" % (P.ninstr, P.nwaits), flush=True)


def make_core_inputs(ci, NSEQ, S, x, c, positions, w_ada, b_ada, g_pre_mix, w_in, g_sgu_v, w_spatial, b_spatial,
                     g_out_sgu, g_out_attn, w_out, g_post_mix, g_pre_ffn, w_ff1, w_ff2, g_post_ffn):
    f32 = np.float32
    bs = slice(ci * NSEQ, (ci + 1) * NSEQ)
    NT = S // 128
    xc = np.ascontiguousarray(x[bs]).reshape(NSEQ * S, D).astype(f32, copy=False)
    cc = np.asarray(c[bs], dtype=f32)
    cT = np.ascontiguousarray(cc.T.reshape(8, 128, NSEQ).transpose(1, 0, 2))
    pos = np.ascontiguousarray(np.asarray(positions[bs]).reshape(NSEQ * NT, 128).T.astype(np.int32))
    wi = np.asarray(w_in[0], dtype=f32)
    perm = np.concatenate([np.arange(0, 1792), np.arange(2304, 2376), np.arange(1792, 2304)])
    wi_p = np.ascontiguousarray(wi[:, perm])
    return {
        "x": xc, "cT": cT, "pos": pos,
        "w_ada": np.ascontiguousarray(w_ada[0], dtype=f32),
        "b_ada": np.ascontiguousarray(b_ada[0:1], dtype=f32),
        "w_in": wi_p,
        "gpre": np.ascontiguousarray(np.asarray(g_pre_mix[0], dtype=f32).reshape(8, 128).T),
        "gpre2": np.ascontiguousarray(np.asarray(g_pre_ffn[0], dtype=f32).reshape(8, 128).T),
        "gv": np.ascontiguousarray(g_sgu_v[0:1], dtype=f32),
        "ws": np.ascontiguousarray(np.asarray(w_spatial[0], dtype=f32).transpose(1, 0, 2)),
        "bs": np.ascontiguousarray(np.asarray(b_spatial[0], dtype=f32).T),
        "goa": np.ascontiguousarray(g_out_sgu[0:1], dtype=f32),
        "gob": np.ascontiguousarray(g_out_attn[0:1], dtype=f32),
        "w_out": np.ascontiguousarray(w_out[0], dtype=f32),
        "gpost": np.ascontiguousarray(g_post_mix[0:1], dtype=f32),
        "w1": np.ascontiguousarray(w_ff1[0], dtype=f32),
        "w2": np.ascontiguousarray(w_ff2[0], dtype=f32),
        "gpost2": np.ascontiguousarray(g_post_ffn[0:1], dtype=f32),
    }


def run(inputs, n_cores, NSEQ, S, taps=None, trace=False, stop=None):
    nc = bass.Bass("TRN2", target_bir_lowering=False)
    try:
        build_program(nc, NSEQ=NSEQ, S=S, taps=taps, stop=stop)
    except StopBuild:
        pass
    in_maps = [make_core_inputs(ci, NSEQ, S, **inputs) for ci in range(n_cores)]
    res = run_bass_kernel_spmd(nc, in_maps, core_ids=list(range(n_cores)), trace=trace)
    return res


def kernel(**inputs):
    inputs = {k: np.asarray(v) for k, v in inputs.items()}
    B, S, _ = inputs["x"].shape
    NSEQ = B // NCORES
    res = run(inputs, NCORES, NSEQ, S)
    outs = [np.asarray(r["out"]).reshape(NSEQ, S, D) for r in res.results]
    return np.concatenate(outs, axis=0).astype(np.float32, copy=False)
```

```python
import contextlib
import math
import numpy as np
import concourse.bass as bass
import concourse.mybir as mybir
from concourse.bass_utils import run_bass_kernel_spmd

F32 = mybir.dt.float32
BF16 = mybir.dt.bfloat16
I32 = mybir.dt.int32
AF = mybir.ActivationFunctionType
ALU = mybir.AluOpType
AX = mybir.AxisListType

D = 1024
DIN = 2376
DFF = 4096
NCORES = 8
EPS = 1e-6
NIT = 16
IDX_SCALE = (64 ** -0.5) * (8 ** -0.5)
TWO_PI = 2.0 * math.pi


class StopBuild(Exception):
    pass


class Prog:
    NDMA = 32

    def __init__(self, nc, stack):
        self.nc = nc
        self.eng = {"pe": nc.tensor, "act": nc.scalar, "dve": nc.vector,
                    "pool": nc.gpsimd, "sp": nc.sync}
        self.sem = {k: stack.enter_context(nc.semaphore("c_" + k)) for k in self.eng}
        self.cnt = {k: 0 for k in self.eng}
        self.dsem = [stack.enter_context(nc.semaphore("d%d" % i)) for i in range(self.NDMA)]
        self.dval = [0] * self.NDMA
        self.dnext = 0
        self.seen = {k: {} for k in self.eng}
        self.res = {}
        self.nwaits = 0
        self.ninstr = 0

    def _wait(self, eng, dep):
        kind, key, val = dep
        if kind == "e":
            if key == "pe" and eng == "pe":
                return
            sem = self.sem[key]
            skey = key
        else:
            sem = self.dsem[key]
            skey = ("d", key)
        if self.seen[eng].get(skey, 0) >= val:
            return
        self.seen[eng][skey] = val
        self.eng[eng].wait_ge(sem, val)
        self.nwaits += 1

    def _deps(self, eng, reads, writes):
        deps = []
        for r in reads:
            st = self.res.get(r)
            if st and st["w"]:
                deps.append(st["w"])
        for w in writes:
            st = self.res.get(w)
            if st:
                if st["w"]:
                    deps.append(st["w"])
                deps.extend(st["r"])
        for d in deps:
            self._wait(eng, d)

    def _record(self, token, reads, writes):
        for r in reads:
            st = self.res.setdefault(r, {"w": None, "r": []})
            st["r"].append(token)
            if len(st["r"]) > 48:
                best = {}
                for t in st["r"]:
                    k = (t[0], t[1])
                    if k not in best or best[k][2] < t[2]:
                        best[k] = t
                st["r"] = list(best.values())
        for w in writes:
            self.res[w] = {"w": token, "r": []}

    def op(self, eng, fn, reads=(), writes=()):
        self._deps(eng, reads, writes)
        ins = fn(self.eng[eng])
        self.cnt[eng] += 1
        ins.then_inc(self.sem[eng], 1)
        token = ("e", eng, self.cnt[eng])
        self._record(token, reads, writes)
        self.ninstr += 1
        return token

    def dma(self, out, in_, reads=(), writes=(), q="sp", **kw):
        self._deps(q, reads, writes)
        i = self.dnext
        self.dnext = (self.dnext + 1) % self.NDMA
        if self.dval[i] > 0:
            self._wait(q, ("d", i, self.dval[i]))
        ins = self.eng[q].dma_start(out=out, in_=in_, **kw)
        self.dval[i] += 16
        ins.then_inc(self.dsem[i], 16)
        token = ("d", i, self.dval[i])
        self._record(token, reads, writes)
        self.ninstr += 1
        return token

    def barrier(self):
        for e in self.eng:
            for f in self.eng:
                if f != e and self.cnt[f] > 0:
                    self._wait(e, ("e", f, self.cnt[f]))
            for i in range(self.NDMA):
                if self.dval[i] > 0:
                    self._wait(e, ("d", i, self.dval[i]))
        self.res = {}

    def finish(self):
        for i in range(self.NDMA):
            if self.dval[i] > 0:
                self._wait("sp", ("d", i, self.dval[i]))
        for f in self.eng:
            if f != "sp" and self.cnt[f] > 0:
                self._wait("sp", ("e", f, self.cnt[f]))


def build_program(nc, NSEQ=4, S=2048, taps=None, stop=None):
    NT = S // 128
    NTT = NSEQ * NT
    NTOK = NSEQ * S
    TOPK = min(256, S // 4)
    KB = TOPK // 128
    taps = taps or {}

    def din(name, shape, dt=F32):
        return nc.dram_tensor(name, list(shape), dt, kind="ExternalInput").ap()

    x_d = din("x", [NTOK, D])
    cT_d = din("cT", [128, 8, NSEQ])
    pos_d = din("pos", [128, NTT], I32)
    wada_d = din("w_ada", [D, 6 * D])
    bada_d = din("b_ada", [1, 6 * D])
    win_d = din("w_in", [D, DIN])
    gpre_d = din("gpre", [128, 8])
    gpre2_d = din("gpre2", [128, 8])
    gv_d = din("gv", [1, 512])
    ws_d = din("ws", [128, 4, 128])
    bs_d = din("bs", [128, 4])
    goa_d = din("goa", [1, 512])
    gob_d = din("gob", [1, 512])
    wout_d = din("w_out", [D, D])
    gpost_d = din("gpost", [1, D])
    w1_d = din("w1", [D, DFF])
    w2_d = din("w2", [DFF, D])
    gpost2_d = din("gpost2", [1, D])
    out_d = nc.dram_tensor("out", [NTOK, D], F32, kind="ExternalOutput").ap()
    x1s_d = out_d
    tap_d = {k: nc.dram_tensor("tap_" + k, list(shp), F32, kind="ExternalOutput").ap()
             for k, shp in taps.items()}

    with contextlib.ExitStack() as gst:
        P = Prog(nc, gst)

        uid = [0]

        def mk_sb(stack):
            def sb(name, shape, dt=F32):
                uid[0] += 1
                return stack.enter_context(nc.sbuf_tensor("s%d_%s" % (uid[0], name), list(shape), dt))
            return sb

        gsb = mk_sb(gst)
        banks = [gst.enter_context(nc.psum_tensor("bank%d" % i, [128, 512], F32)) for i in range(8)]
        bkey = ["b%d" % i for i in range(8)]
        bbf = [b[:].bitcast(BF16) for b in banks]

        def V(fn, r=(), w=()):
            return P.op("dve", fn, r, w)

        def A(fn, r=(), w=()):
            return P.op("act", fn, r, w)

        def G(fn, r=(), w=()):
            return P.op("pool", fn, r, w)

        def T(fn, r=(), w=()):
            return P.op("pe", fn, r, w)

        ident = gsb("ident", [128, 128])
        identb = gsb("identb", [128, 128], BF16)
        ones_f = gsb("ones_f", [128, 128])
        zeros_f = gsb("zeros_f", [128, 128])
        NEGM = gsb("NEGM", [128, 128])
        TRIU = gsb("TRIU", [128, 128], BF16)
        ONESB = gsb("ONESB", [128, 128], BF16)
        D32 = gsb("D32", [128, 32])
        G4 = gsb("G4", [128, 4])
        zerob = gsb("zerob", [128, 260], BF16)
        P2 = gsb("P2", [128, NIT + 1])
        mhalf = gsb("mhalf", [128, 16])
        iot = gsb("iot", [128, 8], I32)
        iof = gsb("iof", [128, 8])
        invf = gsb("invf", [128, 8])
        rs_tmp = gsb("rs_tmp", [128, 16])

        G(lambda e: e.memset(ones_f[:], 1.0), w=["ones_f"])
        G(lambda e: e.memset(zeros_f[:], 0.0), w=["zeros_f"])
        G(lambda e: e.affine_select(out=ident[:], in_=ones_f[:], pattern=[[-1, 128]], compare_op=ALU.is_equal,
                                    fill=0.0, base=0, channel_multiplier=1), r=["ones_f"], w=["ident"])
        V(lambda e: e.tensor_copy(out=identb[:], in_=ident[:]), r=["ident"], w=["identb"])
        G(lambda e: e.affine_select(out=NEGM[:], in_=zeros_f[:], pattern=[[-1, 128]], compare_op=ALU.is_ge,
                                    fill=-1.0e30, base=0, channel_multiplier=1), r=["zeros_f"], w=["NEGM"])
        G(lambda e: e.affine_select(out=TRIU[:], in_=ones_f[:], pattern=[[1, 128]], compare_op=ALU.is_ge,
                                    fill=0.0, base=0, channel_multiplier=-1), r=["ones_f"], w=["TRIU"])
        V(lambda e: e.tensor_copy(out=ONESB[:], in_=ones_f[:]), r=["ones_f"], w=["ONESB"])
        for m in range(4):
            G(lambda e, m=m: e.affine_select(out=D32[32 * m:32 * m + 32, :], in_=ones_f[32 * m:32 * m + 32, 0:32],
                                             pattern=[[-1, 32]], compare_op=ALU.is_equal, fill=0.0, base=0,
                                             channel_multiplier=1), r=["ones_f"], w=["D32"])
        G(lambda e: e.memset(G4[:], 0.0), w=["G4"])
        for g in range(4):
            G(lambda e, g=g: e.memset(G4[32 * g:32 * g + 32, g:g + 1], 1.0), r=["G4"], w=["G4"])
        G(lambda e: e.memset(zerob[:], 0.0), w=["zerob"])
        for i in range(NIT + 1):
            G(lambda e, i=i: e.memset(P2[:, i:i + 1], 2.0 ** -(i + 1)), w=["P2"])
        G(lambda e: e.memset(mhalf[:], -0.5), w=["mhalf"])
        G(lambda e: e.iota(iot[:], pattern=[[1, 8]], base=0, channel_multiplier=0), w=["iot"])
        V(lambda e: e.tensor_copy(out=iof[:], in_=iot[:]), r=["iot"], w=["iof"])
        A(lambda e: e.activation(out=invf[:], in_=iof[:], func=AF.Exp, scale=-math.log(500000.0) / 8.0),
          r=["iof"], w=["invf"])

        def rstd(out_ap, ss_ap, n, inv_n, rk, wk, ss2_ap=None):
            tmp = rs_tmp[:, 0:n]
            if ss2_ap is not None:
                G(lambda e: e.tensor_tensor(out=tmp, in0=ss_ap, in1=ss2_ap, op=ALU.add), r=rk, w=["rs_tmp"])
                G(lambda e: e.tensor_scalar(out=tmp, in0=tmp, scalar1=inv_n, scalar2=EPS, op0=ALU.mult,
                                            op1=ALU.add), r=["rs_tmp"], w=["rs_tmp"])
            else:
                G(lambda e: e.tensor_scalar(out=tmp, in0=ss_ap, scalar1=inv_n, scalar2=EPS, op0=ALU.mult,
                                            op1=ALU.add), r=rk, w=["rs_tmp"])
            G(lambda e: e.tensor_tensor(out=out_ap, in0=tmp, in1=mhalf[:, 0:n], op=ALU.pow),
              r=["rs_tmp", "mhalf"], w=wk)

        def ck(name):
            if stop == name:
                P.finish()
                raise StopBuild()

        def tap(name, ap, rk, rows=None):
            if name in tap_d:
                dst = tap_d[name]
                P.dma(dst if rows is None else dst[rows], ap, reads=rk)

        S1T = gsb("S1T", [128, 8, NSEQ])
        sh1T = gsb("sh1T", [128, 8, NSEQ])
        S2T = gsb("S2T", [128, 8, NSEQ])
        sh2T = gsb("sh2T", [128, 8, NSEQ])
        gmod = gsb("gmod", [NSEQ, 2, D])
        sel = gsb("sel", [NSEQ, NSEQ, 128])
        gpre = gsb("gpre", [128, 8])
        gpre2 = gsb("gpre2", [128, 8])
        P.dma(gpre[:], gpre_d[:, :], writes=["gpre"])
        P.dma(gpre2[:], gpre2_d[:, :], writes=["gpre2"])

        G(lambda e: e.affine_select(out=sel[:], in_=ones_f[0:NSEQ, :].unsqueeze(1).broadcast_to([NSEQ, NSEQ, 128]),
                                    pattern=[[-1, NSEQ], [0, 128]], compare_op=ALU.is_equal, fill=0.0, base=0,
                                    channel_multiplier=1), r=["ones_f"], w=["sel"])

        with contextlib.ExitStack() as ast:
            asb = mk_sb(ast)
            Win = asb("Win", [128, 8, DIN], BF16)
            Wout = asb("Wout", [128, 8, D], BF16)
            cs = asb("cs", [128, NTT, 8])
            sn = asb("sn", [128, NTT, 8])
            gv_row = asb("gv_row", [128, 512])
            goa_row = asb("goa_row", [128, 512])
            gob_row = asb("gob_row", [128, 512])
            bcol = asb("bcol", [128, 4])
            WmT = asb("WmT", [128, 4, 128], BF16)
            P.dma(gv_row[:], gv_d[0:1, :].partition_broadcast(128), writes=["gv_row"])
            P.dma(goa_row[:], goa_d[0:1, :].partition_broadcast(128), writes=["goa_row"])
            P.dma(gob_row[:], gob_d[0:1, :].partition_broadcast(128), writes=["gob_row"])
            P.dma(bcol[:], bs_d[:, :], writes=["bcol"])

            with contextlib.ExitStack() as sst:
                ssb = mk_sb(sst)
                cTs = ssb("cTs", [128, 8, NSEQ])
                scs = ssb("scs", [128, 8, NSEQ])
                bada4 = ssb("bada4", [NSEQ, 6 * D])
                modrow = ssb("modrow", [NSEQ, 6 * D])
                gpost4 = ssb("gpost4", [NSEQ, 2, D])
                wada_st = [ssb("wada_st%d" % i, [128, 8, 512]) for i in range(2)]
                win_st = [ssb("win_st%d" % i, [128, DIN]) for i in range(2)]
                ws_sb = ssb("ws_sb", [128, 4, 128])
                wsm = ssb("wsm", [128, 4, 128])
                posi = ssb("posi", [128, NTT], I32)
                posf = ssb("posf", [128, NTT])
                ang = ssb("ang", [128, NTT * 8])
                angk = ssb("angk", [128, NTT * 8], I32)
                angf = ssb("angf", [128, NTT * 8])
                angm = ssb("angm", [128, NTT * 8])
                ang2 = ssb("ang2", [128, NTT * 8])

                P.dma(cTs[:], cT_d[:, :, :], writes=["cTs"])
                P.dma(bada4[:], bada_d[0:1, :].partition_broadcast(NSEQ), writes=["bada4"])
                P.dma(gpost4[:, 0, :], gpost_d[0:1, :].partition_broadcast(NSEQ), writes=["gpost4a"])
                P.dma(gpost4[:, 1, :], gpost2_d[0:1, :].partition_broadcast(NSEQ), writes=["gpost4b"])
                P.dma(posi[:], pos_d[:, :], writes=["posi"])
                P.dma(ws_sb[:], ws_d[:, :, :], writes=["ws_sb"])
                A(lambda e: e.activation(out=scs[:], in_=cTs[:], func=AF.Silu), r=["cTs"], w=["scs"])

                order = [2, 3, 0, 1] + list(range(4, 12))
                for n_, cb in enumerate(order):
                    st_ = wada_st[n_ % 2]
                    sk = "wada_st%d" % (n_ % 2)
                    P.dma(st_[:], wada_d[:, cb * 512:(cb + 1) * 512].rearrange("(k p) n -> p k n", p=128),
                          writes=[sk])
                    bk = n_ % 2
                    for k in range(8):
                        T(lambda e, k=k, st_=st_, bk=bk: e.matmul(banks[bk][0:NSEQ, :], lhsT=scs[:, k, :],
                                                                  rhs=st_[:, k, :], start=(k == 0), stop=(k == 7)),
                          r=["scs", sk], w=[bkey[bk]])
                    V(lambda e, bk=bk, cb=cb: e.tensor_tensor(out=modrow[:, cb * 512:(cb + 1) * 512],
                                                              in0=banks[bk][0:NSEQ, :],
                                                              in1=bada4[:, cb * 512:(cb + 1) * 512], op=ALU.add),
                      r=[bkey[bk], "bada4"], w=["modrow%d" % cb])
                allmod = ["modrow%d" % cb for cb in range(12)]
                for si, sp_ in enumerate([0, 1, 3, 4]):
                    for k in range(8):
                        c0 = (si * 8 + k) * NSEQ
                        T(lambda e, sp_=sp_, k=k, c0=c0: e.transpose(
                            out=banks[2][:, c0:c0 + NSEQ], in_=modrow[0:NSEQ, sp_ * D + k * 128:sp_ * D + (k + 1) * 128],
                            identity=ident[0:NSEQ, 0:NSEQ]), r=allmod + ["ident"], w=[bkey[2]])

                def mview(si):
                    return banks[2][:, si * 8 * NSEQ:(si + 1) * 8 * NSEQ].rearrange("p (k b) -> p k b", b=NSEQ)

                V(lambda e: e.tensor_copy(out=sh1T[:], in_=mview(0)), r=[bkey[2]], w=["sh1T"])
                V(lambda e: e.scalar_tensor_tensor(out=S1T[:], in0=mview(1), scalar=1.0,
                                                   in1=gpre[:].unsqueeze(2).broadcast_to([128, 8, NSEQ]),
                                                   op0=ALU.add, op1=ALU.mult), r=[bkey[2], "gpre"], w=["S1T"])
                V(lambda e: e.tensor_copy(out=sh2T[:], in_=mview(2)), r=[bkey[2]], w=["sh2T"])
                V(lambda e: e.scalar_tensor_tensor(out=S2T[:], in0=mview(3), scalar=1.0,
                                                   in1=gpre2[:].unsqueeze(2).broadcast_to([128, 8, NSEQ]),
                                                   op0=ALU.add, op1=ALU.mult), r=[bkey[2], "gpre2"], w=["S2T"])
                V(lambda e: e.tensor_tensor(out=gmod[:, 0, :], in0=modrow[:, 2 * D:3 * D], in1=gpost4[:, 0, :],
                                            op=ALU.mult), r=allmod + ["gpost4a"], w=["gmod0"])
                V(lambda e: e.tensor_tensor(out=gmod[:, 1, :], in0=modrow[:, 5 * D:6 * D], in1=gpost4[:, 1, :],
                                            op=ALU.mult), r=allmod + ["gpost4b"], w=["gmod1"])

                cast_engs = ["dve", "act", "pool"]
                ci = 0
                for k in range(8):
                    st_ = win_st[k % 2]
                    sk = "win_st%d" % (k % 2)
                    P.dma(st_[:], win_d[k * 128:(k + 1) * 128, :], writes=[sk])
                    for h0, h1 in ((0, 1188), (1188, DIN)):
                        eng = cast_engs[ci % 3]
                        ci += 1
                        if eng == "act":
                            A(lambda e, k=k, st_=st_, h0=h0, h1=h1: e.activation(out=Win[:, k, h0:h1], in_=st_[:, h0:h1],
                                                                              func=AF.Copy), r=[sk], w=["Win"])
                        else:
                            P.op(eng, lambda e, k=k, st_=st_, h0=h0, h1=h1: e.tensor_copy(out=Win[:, k, h0:h1],
                                                                                       in_=st_[:, h0:h1]),
                                 [sk], ["Win"])
                for k in range(8):
                    st_ = win_st[k % 2]
                    sk = "win_st%d" % (k % 2)
                    P.dma(st_[:, 0:D], wout_d[k * 128:(k + 1) * 128, :], writes=[sk])
                    eng = cast_engs[ci % 3]
                    ci += 1
                    if eng == "act":
                        A(lambda e, k=k, st_=st_: e.activation(out=Wout[:, k, :], in_=st_[:, 0:D], func=AF.Copy),
                          r=[sk], w=["Wout"])
                    else:
                        P.op(eng, lambda e, k=k, st_=st_: e.tensor_copy(out=Wout[:, k, :], in_=st_[:, 0:D]),
                             [sk], ["Wout"])
                for g in range(4):
                    G(lambda e, g=g: e.affine_select(out=wsm[:, g, :], in_=ws_sb[:, g, :], pattern=[[-1, 128]],
                                                     compare_op=ALU.is_ge, fill=0.0, base=0, channel_multiplier=1),
                      r=["ws_sb"], w=["wsm"])
                for g in range(4):
                    T(lambda e, g=g: e.transpose(out=banks[3][:, g * 128:(g + 1) * 128], in_=wsm[:, g, :],
                                                 identity=ident[:]), r=["wsm", "ident"], w=[bkey[3]])
                V(lambda e: e.tensor_copy(out=WmT[:], in_=banks[3][:, :].rearrange("p (g t) -> p g t", g=4)),
                  r=[bkey[3]], w=["WmT"])

                NA = NTT * 8
                V(lambda e: e.tensor_copy(out=posf[:], in_=posi[:]), r=["posi"], w=["posf"])
                V(lambda e: e.tensor_tensor(out=ang[:].rearrange("p (t f) -> p t f", f=8),
                                            in0=posf[:].unsqueeze(2).broadcast_to([128, NTT, 8]),
                                            in1=invf[:].unsqueeze(1).broadcast_to([128, NTT, 8]), op=ALU.mult),
                  r=["posf", "invf"], w=["ang"])

                def reduce_sin(dst, src_key, shift):
                    V(lambda e: e.tensor_scalar(out=ang2[:], in0=ang[:], scalar1=shift, scalar2=None, op0=ALU.add),
                      r=["ang"], w=["ang2"])
                    V(lambda e: e.tensor_scalar(out=angk[:], in0=ang2[:], scalar1=1.0 / TWO_PI, scalar2=None,
                                                op0=ALU.mult), r=["ang2"], w=["angk"])
                    V(lambda e: e.tensor_copy(out=angf[:], in_=angk[:]), r=["angk"], w=["angf"])
                    V(lambda e: e.scalar_tensor_tensor(out=ang2[:], in0=angf[:], scalar=-TWO_PI, in1=ang2[:],
                                                       op0=ALU.mult, op1=ALU.add), r=["angf", "ang2"], w=["ang2"])
                    V(lambda e: e.tensor_scalar(out=angm[:], in0=ang2[:], scalar1=math.pi, scalar2=-TWO_PI,
                                                op0=ALU.is_gt, op1=ALU.mult), r=["ang2"], w=["angm"])
                    V(lambda e: e.tensor_tensor(out=ang2[:], in0=ang2[:], in1=angm[:], op=ALU.add),
                      r=["ang2", "angm"], w=["ang2"])
                    V(lambda e: e.tensor_scalar(out=angm[:], in0=ang2[:], scalar1=-math.pi, scalar2=TWO_PI,
                                                op0=ALU.is_lt, op1=ALU.mult), r=["ang2"], w=["angm"])
                    V(lambda e: e.tensor_tensor(out=ang2[:], in0=ang2[:], in1=angm[:], op=ALU.add),
                      r=["ang2", "angm"], w=["ang2"])
                    V(lambda e: e.tensor_scalar(out=ang2[:], in0=ang2[:], scalar1=-3.1415925, scalar2=3.1415925,
                                                op0=ALU.max, op1=ALU.min), r=["ang2"], w=["ang2"])
                    A(lambda e: e.activation(out=dst[:].rearrange("p t f -> p (t f)"), in_=ang2[:], func=AF.Sin),
                      r=["ang2"], w=[src_key])

                reduce_sin(sn, "sn", 0.0)
                reduce_sin(cs, "cs", math.pi / 2.0)
                P.barrier()

            if stop == "setup":
                P.finish()
                return
            tap("S1T", S1T[:].rearrange("p k b -> p (k b)"), ["S1T"])
            tap("gmod", gmod[:].rearrange("b g d -> b (g d)"), ["gmod0", "gmod1"])
            tap("cs", cs[:].rearrange("p t f -> p (t f)"), ["cs"])
            tap("sn", sn[:].rearrange("p t f -> p (t f)"), ["sn"])

            qT2 = asb("qT2", [128, 4, S], BF16)
            kTz = [asb("kTz%d" % i, [128, 2, S], BF16) for i in range(2)]
            kiTz = [asb("kiTz%d" % i, [128, S], BF16) for i in range(2)]
            qiT2 = asb("qiT2", [128, S // 32, 4, 32], BF16)
            v_aug = asb("v_aug", [128, NT, 2, 65], BF16)
            w_tok = asb("w_tok", [128, NT, 8])
            mTa = asb("mTa", [128, 4, S], BF16)
            G1row = asb("G1row", [128, D])
            junkA = asb("junkA", [128, D], BF16)
            G(lambda e: e.memset(v_aug[:].rearrange("p a b c -> p (a b) c")[:, :, 64:65], 1.0), w=["v_aug"])
            G(lambda e: e.memset(kTz[0][64:128, :, :], 0.0), w=["kTd"])
            G(lambda e: e.memset(kTz[1][0:64, :, :], 0.0), w=["kTd"])
            G(lambda e: e.memset(kiTz[0][64:128, :], 0.0), w=["kiTd"])
            G(lambda e: e.memset(kiTz[1][0:64, :], 0.0), w=["kiTd"])

            for b in range(NSEQ):
                for n in range(2):
                    T(lambda e, n=n: e.matmul(banks[n][:, :], lhsT=sel[0:NSEQ, b, :],
                                              rhs=gmod[0:NSEQ, 0, n * 512:(n + 1) * 512], start=True, stop=True),
                      r=["sel", "gmod0"], w=[bkey[n]])
                    V(lambda e, n=n: e.tensor_copy(out=G1row[:, n * 512:(n + 1) * 512], in_=banks[n][:, :]),
                      r=[bkey[n]], w=["G1row"])

                with contextlib.ExitStack() as pst:
                    psb = mk_sb(pst)
                    xt = [psb("xt%d" % i, [128, D]) for i in range(2)]
                    xn = [psb("xn%d" % i, [128, D]) for i in range(2)]
                    hT = [psb("hT%d" % i, [128, 8, 128], BF16) for i in range(2)]
                    ssx = psb("ssx", [128, 2])
                    rsx = psb("rsx", [128, 2])
                    zu = [psb("zu%d" % i, [128, 512], BF16) for i in range(2)]
                    zv = psb("zv", [128, 512], BF16)
                    vn = [psb("vn%d" % i, [128, 512], BF16) for i in range(2)]
                    ssv = psb("ssv", [128, 4])
                    rsv = psb("rsv", [128, 4])
                    ya = psb("ya", [128, 512])
                    ssa = psb("ssa", [128, 1])
                    rsa = psb("rsa", [128, 1])
                    ma = psb("ma", [128, 512], BF16)
                    q_tok = [psb("q_tok%d" % i, [128, 8, 64], BF16) for i in range(2)]
                    qi_tok = [psb("qi_tok%d" % i, [128, 8, 64], BF16) for i in range(2)]
                    kd = [psb("kd%d" % i, [128, 2, 2, 64], BF16) for i in range(2)]
                    kid = [psb("kid%d" % i, [128, 2, 64], BF16) for i in range(2)]
                    rt = [psb("rt%d" % i, [128, 8, 8]) for i in range(4)]

                    def s1_load(i):
                        it = b * NT + i
                        P.dma(xt[i % 2][:], x_d[it * 128:(it + 1) * 128, :], writes=["xt%d" % (i % 2)])

                    def s1_pre(i):
                        j = i % 2
                        A(lambda e: e.activation(out=junkA[:], in_=xt[j][:], func=AF.Square,
                                                 accum_out=ssx[:, j:j + 1]), r=["xt%d" % j], w=["junkA", "ssx%d" % j])
                        rstd(rsx[:, j:j + 1], ssx[:, j:j + 1], 1, 1.0 / D, ["ssx%d" % j], ["rsx%d" % j])
                        V(lambda e: e.tensor_scalar(out=xn[j][:], in0=xt[j][:], scalar1=rsx[:, j:j + 1], scalar2=None,
                                                    op0=ALU.mult), r=["xt%d" % j, "rsx%d" % j], w=["xn%d" % j])

                    def s1_post(i):
                        j = i % 2
                        for k in range(8):
                            T(lambda e, k=k: e.transpose(out=banks[k // 4][:, (k % 4) * 128:(k % 4 + 1) * 128],
                                                         in_=xn[j][:, k * 128:(k + 1) * 128], identity=ident[:]),
                              r=["xn%d" % j, "ident"], w=[bkey[k // 4]])
                        for k in range(8):
                            A(lambda e, k=k: e.activation(out=hT[j][:, k, :],
                                                          in_=banks[k // 4][:, (k % 4) * 128:(k % 4 + 1) * 128],
                                                          func=AF.Identity, scale=S1T[:, k, b:b + 1],
                                                          bias=sh1T[:, k, b:b + 1]),
                              r=[bkey[k // 4], "S1T", "sh1T"], w=["hT%d" % j])

                    GROUPS = [(0, 512, 2), (512, 512, 3), (1024, 512, 4), (1536, 328, 5), (1864, 512, 6)]

                    def rope(src3, src_key, dst3, dst_key, H, it):
                        c = cs[:, it, :].unsqueeze(1).broadcast_to([128, H, 8])
                        s_ = sn[:, it, :].unsqueeze(1).broadcast_to([128, H, 8])
                        x1 = src3[:, :, 0:8]
                        x2 = src3[:, :, 8:16]
                        t = [r_[:, 0:H, :] for r_ in rt]
                        V(lambda e: e.tensor_tensor(out=t[0], in0=x1, in1=c, op=ALU.mult), r=[src_key, "cs"], w=["rt0"])
                        V(lambda e: e.tensor_tensor(out=t[1], in0=x2, in1=s_, op=ALU.mult), r=[src_key, "sn"], w=["rt1"])
                        V(lambda e: e.tensor_tensor(out=dst3[:, :, 0:8], in0=t[0], in1=t[1], op=ALU.subtract),
                          r=["rt0", "rt1"], w=[dst_key])
                        V(lambda e: e.tensor_tensor(out=t[2], in0=x2, in1=c, op=ALU.mult), r=[src_key, "cs"], w=["rt2"])
                        V(lambda e: e.tensor_tensor(out=t[3], in0=x1, in1=s_, op=ALU.mult), r=[src_key, "sn"], w=["rt3"])
                        V(lambda e: e.tensor_tensor(out=dst3[:, :, 8:16], in0=t[2], in1=t[3], op=ALU.add),
                          r=["rt2", "rt3"], w=[dst_key])
                        A(lambda e: e.activation(out=dst3[:, :, 16:64], in_=src3[:, :, 16:64], func=AF.Copy),
                          r=[src_key], w=[dst_key])

                    def s2(i):
                        j = i % 2
                        it = b * NT + i
                        for (c0, n, bk) in GROUPS:
                            for k in range(8):
                                T(lambda e, k=k, c0=c0, n=n, bk=bk: e.matmul(banks[bk][:, 0:n], lhsT=hT[j][:, k, :],
                                                                             rhs=Win[:, k, c0:c0 + n], start=(k == 0),
                                                                             stop=(k == 7)),
                                  r=["hT%d" % j, "Win"], w=[bkey[bk]])
                        A(lambda e: e.activation(out=zu[j][:], in_=banks[2][:, :], func=AF.Gelu_apprx_tanh),
                          r=[bkey[2]], w=["zu%d" % j])
                        A(lambda e: e.activation(out=zv[:], in_=banks[3][:, :], func=AF.Gelu_apprx_tanh),
                          r=[bkey[3]], w=["zv"])
                        rope(banks[4][:, :].rearrange("p (h d) -> p h d", d=64), bkey[4], q_tok[j][:], "q_tok%d" % j, 8, it)
                        rope(banks[5][:, 0:128].rearrange("p (h d) -> p h d", d=64), bkey[5], kd[j][:, :, 0, :],
                             "kd%d" % j, 2, it)
                        V(lambda e: e.tensor_copy(out=kd[j][:, :, 1, :], in_=kd[j][:, :, 0, :]), r=["kd%d" % j],
                          w=["kd%d" % j])
                        V(lambda e: e.tensor_copy(out=v_aug[:, i, :, 0:64],
                                                  in_=banks[5][:, 128:256].rearrange("p (h d) -> p h d", d=64)),
                          r=[bkey[5]], w=["v_aug"])
                        rope(banks[5][:, 256:320].rearrange("p (h d) -> p h d", d=64), bkey[5], kid[j][:, 0:1, :],
                             "kid%d" % j, 1, it)
                        V(lambda e: e.tensor_copy(out=kid[j][:, 1:2, :], in_=kid[j][:, 0:1, :]), r=["kid%d" % j],
                          w=["kid%d" % j])
                        V(lambda e: e.tensor_copy(out=w_tok[:, i, :], in_=banks[5][:, 320:328]), r=[bkey[5]],
                          w=["w_tok"])
                        rope(banks[6][:, :].rearrange("p (h d) -> p h d", d=64), bkey[6], qi_tok[j][:], "qi_tok%d" % j, 8, it)
                        for g in range(4):
                            A(lambda e, g=g: e.activation(out=junkA[:, 0:128], in_=zv[:, g * 128:(g + 1) * 128],
                                                          func=AF.Square, accum_out=ssv[:, g:g + 1]),
                              r=["zv"], w=["junkA", "ssv"])
                        rstd(rsv[:, 0:4], ssv[:, 0:4], 4, 1.0 / 128, ["ssv"], ["rsv"])
                        for g in range(4):
                            V(lambda e, g=g: e.scalar_tensor_tensor(out=vn[j][:, g * 128:(g + 1) * 128],
                                                                    in0=zv[:, g * 128:(g + 1) * 128],
                                                                    scalar=rsv[:, g:g + 1],
                                                                    in1=gv_row[:, g * 128:(g + 1) * 128],
                                                                    op0=ALU.mult, op1=ALU.mult),
                              r=["zv", "rsv", "gv_row"], w=["vn%d" % j])

                    def s3a(i):
                        j = i % 2
                        ts = slice(i * 128, (i + 1) * 128)
                        qf = q_tok[j][:].rearrange("p h d -> p (h d)")
                        for c in range(4):
                            T(lambda e, c=c: e.transpose(out=bbf[0][:, c * 128:(c + 1) * 128],
                                                         in_=qf[:, c * 128:(c + 1) * 128], identity=identb[:]),
                              r=["q_tok%d" % j, "identb"], w=[bkey[0]])
                        for kv in range(2):
                            T(lambda e, kv=kv: e.transpose(out=bbf[0][:, 512 + kv * 128:512 + (kv + 1) * 128],
                                                           in_=kd[j][:, kv, :, :].rearrange("p a d -> p (a d)"),
                                                           identity=identb[:]),
                              r=["kd%d" % j, "identb"], w=[bkey[0]])
                        T(lambda e: e.transpose(out=bbf[0][:, 768:896], in_=kid[j][:].rearrange("p a d -> p (a d)"),
                                                identity=identb[:]), r=["kid%d" % j, "identb"], w=[bkey[0]])
                        V(lambda e: e.tensor_copy(out=qT2[:, :, ts],
                                                  in_=bbf[0][:, 0:512].rearrange("p (c t) -> p c t", t=128)),
                          r=[bkey[0]], w=["qT2"])
                        for par in range(2):
                            ps = slice(64 * par, 64 * par + 64)
                            V(lambda e, par=par, ps=ps: e.tensor_copy(
                                out=kTz[par][ps, :, ts],
                                in_=bbf[0][ps, 512:768].rearrange("p (c t) -> p c t", t=128)),
                              r=[bkey[0]], w=["kTd"])
                            V(lambda e, par=par, ps=ps: e.tensor_copy(out=kiTz[par][ps, ts], in_=bbf[0][ps, 768:896]),
                              r=[bkey[0]], w=["kiTd"])
                        qif = qi_tok[j][:].rearrange("p h d -> p (h d)")
                        for c in range(4):
                            T(lambda e, c=c: e.transpose(out=bbf[1][:, c * 128:(c + 1) * 128],
                                                         in_=qif[:, c * 128:(c + 1) * 128], identity=identb[:]),
                              r=["qi_tok%d" % j, "identb"], w=[bkey[1]])
                        A(lambda e: e.activation(out=qiT2[:, i * 4:(i + 1) * 4, :, :],
                                                 in_=bbf[1][:, 0:512].rearrange("p (c g t) -> p g c t", c=4, g=4),
                                                 func=AF.Copy), r=[bkey[1]], w=["qiT2"])
                        for g in range(4):
                            T(lambda e, g=g: e.matmul(banks[7][:, g * 128:(g + 1) * 128], lhsT=WmT[:, g, :],
                                                      rhs=vn[j][:, g * 128:(g + 1) * 128], start=True, stop=True),
                              r=["WmT", "vn%d" % j], w=[bkey[7]])
                        for g in range(4):
                            V(lambda e, g=g: e.scalar_tensor_tensor(out=ya[:, g * 128:(g + 1) * 128],
                                                                    in0=banks[7][:, g * 128:(g + 1) * 128],
                                                                    scalar=bcol[:, g:g + 1],
                                                                    in1=zu[j][:, g * 128:(g + 1) * 128],
                                                                    op0=ALU.add, op1=ALU.mult),
                              r=[bkey[7], "bcol", "zu%d" % j], w=["ya"])
                        A(lambda e: e.activation(out=junkA[:, 0:512], in_=ya[:], func=AF.Square, accum_out=ssa[:]),
                          r=["ya"], w=["junkA", "ssa"])
                        rstd(rsa[:], ssa[:], 1, 1.0 / 512, ["ssa"], ["rsa"])
                        V(lambda e: e.scalar_tensor_tensor(out=ma[:], in0=ya[:], scalar=rsa[:, 0:1], in1=goa_row[:],
                                                           op0=ALU.mult, op1=ALU.mult),
                          r=["ya", "rsa", "goa_row"], w=["ma"])
                        if b == 0 and i == 0:
                            tap("ya", ya[:], ["ya"])

                    def s3b(i):
                        ts = slice(i * 128, (i + 1) * 128)
                        for c in range(4):
                            T(lambda e, c=c: e.transpose(out=bbf[7][:, c * 128:(c + 1) * 128],
                                                         in_=ma[:, c * 128:(c + 1) * 128], identity=identb[:]),
                              r=["ma", "identb"], w=[bkey[7]])
                        A(lambda e: e.activation(out=mTa[:, :, ts],
                                                 in_=bbf[7][:, 0:512].rearrange("p (c t) -> p c t", t=128),
                                                 func=AF.Copy), r=[bkey[7]], w=["mTa"])

                    s1_load(0)
                    if NT > 1:
                        s1_load(1)
                    s1_pre(0)
                    s1_post(0)
                    if NT > 2:
                        s1_load(2)
                    if NT > 1:
                        s1_pre(1)
                        s1_post(1)
                    for n in range(NT + 2):
                        if n + 2 < NT:
                            s1_pre(n + 2)
                            if n + 3 < NT:
                                s1_load(n + 3)
                        if n < NT:
                            s2(n)
                        if 0 <= n - 2 < NT:
                            s3b(n - 2)
                        if 0 <= n - 1 < NT:
                            s3a(n - 1)
                        if n + 2 < NT:
                            s1_post(n + 2)
                    P.barrier()
                    if stop == "proj":
                        P.finish()
                        return

                with contextlib.ExitStack() as tst:
                    tsb = mk_sb(tst)
                    score = tsb("score", [128, S])
                    cmax = tsb("cmax", [128, 4])
                    mask = tsb("mask", [128, S], BF16)
                    maskT = tsb("maskT", [128, NT, 128], BF16)
                    rl = [tsb("rl%d" % i, [128, 512], BF16) for i in range(4)]
                    pT = [tsb("pT%d" % i, [128, 512], BF16) for i in range(4)]
                    Wsel = [tsb("Wsel%d" % i, [128, 8, 128], BF16) for i in range(2)]
                    wrep = tsb("wrep", [128, 2, 128])
                    wcol = tsb("wcol", [128, 8])
                    lo0 = tsb("lo0", [128, 1])
                    hi0 = tsb("hi0", [128, 1])
                    w0 = tsb("w0", [128, 1])
                    wh = tsb("wh", [128, NIT + 1])
                    cbias = tsb("cbias", [128, NT])
                    mid = tsb("mid", [128, 1])
                    cnt = tsb("cnt", [128, 1])
                    btmp = tsb("btmp", [128, 1])
                    rden = tsb("rden", [128, 8])
                    yb = tsb("yb", [128, 512])
                    ssb_ = tsb("ssb_", [128, 1])
                    rsb = tsb("rsb", [128, 1])
                    mb = tsb("mb", [128, 512], BF16)
                    mbT = tsb("mbT", [128, 4, 128], BF16)
                    sso = tsb("sso", [128, 2])
                    rso = tsb("rso", [128, 1])
                    ot = tsb("ot", [128, D])
                    xres = [tsb("xres%d" % i, [128, D]) for i in range(2)]
                    for i in range(2):
                        G(lambda e, i=i: e.memset(Wsel[i][:], 0.0), w=["Wsel%d" % i])
                    for qq in range(NT):
                        G(lambda e, qq=qq: e.memset(cbias[:, qq:qq + 1], float((qq + 1) * 128 - 2 * TOPK) + 0.5),
                          w=["cbias"])
                    rl_i = [0]
                    pT_i = [0]
                    D_i = [0]

                    def wsel_build(qb):
                        wi = qb % 2
                        wk = "Wsel%d" % wi
                        w2v = w_tok[:, qb, :].rearrange("p (i two) -> p i two", two=2)
                        for par in range(2):
                            V(lambda e, par=par: e.tensor_tensor(
                                out=wrep[:, par, :].rearrange("p (i t) -> p i t", t=32),
                                in0=w2v[:, :, par].unsqueeze(2).broadcast_to([128, 4, 32]),
                                in1=D32[:].unsqueeze(1).broadcast_to([128, 4, 32]), op=ALU.mult),
                              r=["w_tok", "D32"], w=["wrep"])
                        for par in range(2):
                            T(lambda e, par=par: e.matmul(banks[0][:, par * 4:(par + 1) * 4], lhsT=wrep[:, par, :],
                                                          rhs=G4[:], start=True, stop=True),
                              r=["wrep", "G4"], w=[bkey[0]])
                        V(lambda e: e.tensor_scalar(out=wcol[:], in0=banks[0][:, 0:8], scalar1=IDX_SCALE, scalar2=None,
                                                    op0=ALU.mult), r=[bkey[0]], w=["wcol"])
                        for par in range(2):
                            for g in range(4):
                                V(lambda e, par=par, g=g: e.tensor_scalar(
                                    out=Wsel[wi][:, par * 4 + g, 32 * g:32 * g + 32], in0=D32[:],
                                    scalar1=wcol[:, par * 4 + g:par * 4 + g + 1], scalar2=None, op0=ALU.mult),
                                  r=["D32", "wcol"], w=[wk])

                    def indexer(qb):
                        N = (qb + 1) * 128
                        nch = (N + 511) // 512
                        wi = qb % 2
                        wk = "Wsel%d" % wi
                        units = []
                        for c in range(nch):
                            n = min(512, N - c * 512)
                            for g in range(4):
                                for par in range(2):
                                    units.append((c, n, g, par))

                        DBK = [0, 1, 3]

                        def dots(u):
                            c, n, g, par = u
                            ri = rl_i[0] % 4
                            dbk = DBK[D_i[0] % 3]
                            D_i[0] += 1
                            rl_i[0] += 1
                            T(lambda e: e.matmul(banks[dbk][:, 0:n],
                                                 lhsT=qiT2[:, qb * 4 + g, :, :].rearrange("p c t -> p (c t)"),
                                                 rhs=kiTz[par][:, c * 512:c * 512 + n], start=True, stop=True),
                              r=["qiT2", "kiTd"], w=[bkey[dbk]])
                            if par == 0:
                                A(lambda e: e.activation(out=rl[ri][:, 0:n], in_=banks[dbk][:, 0:n], func=AF.Relu),
                                  r=[bkey[dbk]], w=["rl%d" % ri])
                            else:
                                V(lambda e: e.tensor_scalar(out=rl[ri][:, 0:n], in0=banks[dbk][:, 0:n], scalar1=0.0,
                                                            scalar2=None, op0=ALU.max), r=[bkey[dbk]], w=["rl%d" % ri])
                            return ri

                        def selmm(u, ri):
                            c, n, g, par = u
                            sbk = 2
                            first = (g == 0 and par == 0)
                            last = (g == 3 and par == 1)
                            T(lambda e: e.matmul(banks[sbk][:, 0:n], lhsT=Wsel[wi][:, par * 4 + g, :],
                                                 rhs=rl[ri][:, 0:n], start=first, stop=last),
                              r=[wk, "rl%d" % ri], w=[bkey[sbk]])
                            if last:
                                V(lambda e: e.tensor_scalar(out=score[:, c * 512:c * 512 + n], in0=banks[sbk][:, 0:n],
                                                            scalar1=1.0, scalar2=None, op0=ALU.mult, op1=ALU.max,
                                                            accum_out=cmax[:, c:c + 1]),
                                  r=[bkey[sbk]], w=["score", "cmax"])

                        ris = {}
                        LOOK = 3
                        for i_ in range(min(LOOK, len(units))):
                            ris[i_] = dots(units[i_])
                        for i_ in range(len(units)):
                            if i_ + LOOK < len(units):
                                ris[i_ + LOOK] = dots(units[i_ + LOOK])
                            selmm(units[i_], ris[i_])

                    def topk_iter(qb):
                        N = (qb + 1) * 128
                        nch = (N + 511) // 512
                        V(lambda e: e.tensor_reduce(out=lo0[:], in_=score[:, 0:N], axis=AX.X, op=ALU.min),
                          r=["score"], w=["lo0"])
                        V(lambda e: e.tensor_reduce(out=hi0[:], in_=cmax[:, 0:nch], axis=AX.X, op=ALU.max),
                          r=["cmax"], w=["hi0"])
                        V(lambda e: e.tensor_tensor(out=score[:, qb * 128:N], in0=score[:, qb * 128:N], in1=NEGM[:],
                                                    op=ALU.add), r=["score", "NEGM"], w=["score"])
                        V(lambda e: e.tensor_tensor(out=w0[:], in0=lo0[:], in1=hi0[:], op=ALU.subtract),
                          r=["hi0", "lo0"], w=["w0"])
                        V(lambda e: e.tensor_scalar(out=wh[:], in0=P2[:], scalar1=w0[:, 0:1], scalar2=None,
                                                    op0=ALU.mult), r=["P2", "w0"], w=["wh"])
                        V(lambda e: e.tensor_scalar(out=mid[:], in0=lo0[:], scalar1=-1.0, scalar2=wh[:, 0:1],
                                                    op0=ALU.mult, op1=ALU.add), r=["lo0", "wh"], w=["mid"])
                        for i in range(NIT):
                            A(lambda e: e.activation(out=mask[:, 0:N], in_=score[:, 0:N], func=AF.Sign,
                                                     bias=mid[:, 0:1], accum_out=cnt[:]),
                              r=["score", "mid"], w=["mask", "cnt"])
                            V(lambda e, i=i: e.scalar_tensor_tensor(out=btmp[:], in0=cnt[:],
                                                                    scalar=float(2 * TOPK - N) - 0.5,
                                                                    in1=wh[:, i:i + 1], op0=ALU.is_ge, op1=ALU.mult),
                              r=["cnt", "wh"], w=["btmp"])
                            V(lambda e, i=i: e.scalar_tensor_tensor(out=mid[:], in0=mid[:], scalar=wh[:, i + 1:i + 2],
                                                                    in1=btmp[:], op0=ALU.subtract, op1=ALU.add),
                              r=["mid", "wh", "btmp"], w=["mid"])
                            yield
                        V(lambda e: e.tensor_scalar(out=mid[:], in0=mid[:], scalar1=-1.0, scalar2=wh[:, NIT:NIT + 1],
                                                    op0=ALU.mult, op1=ALU.add), r=["mid", "wh"], w=["mid"])

                    def topk_finish(qb):
                        N = (qb + 1) * 128
                        if qb < KB:
                            for jj in range(qb + 1):
                                src = TRIU if jj == qb else ONESB
                                G(lambda e, jj=jj, src=src: e.tensor_copy(out=maskT[:, jj, :], in_=src[:]),
                                  r=["TRIU", "ONESB"], w=["maskT"])
                            return
                        V(lambda e: e.tensor_scalar(out=mask[:, 0:N], in0=score[:, 0:N], scalar1=mid[:, 0:1],
                                                    scalar2=None, op0=ALU.is_ge), r=["score", "mid"], w=["mask"])
                        if b == 0 and qb == NT - 1:
                            tap("score", score[:, 0:N], ["score"])
                            tap("thr", mid[:], ["mid"])
                        for jj in range(qb + 1):
                            lb = 4 + jj // 8
                            T(lambda e, jj=jj, lb=lb: e.transpose(out=bbf[lb][:, (jj % 8) * 128:(jj % 8 + 1) * 128],
                                                                  in_=mask[:, jj * 128:(jj + 1) * 128],
                                                                  identity=identb[:]),
                              r=["mask", "identb"], w=[bkey[lb]])
                        for lb in range(4, 4 + (qb + 8) // 8):
                            j0 = (lb - 4) * 8
                            j1 = min(qb + 1, j0 + 8)
                            nj = j1 - j0
                            V(lambda e, lb=lb, j0=j0, j1=j1, nj=nj: e.tensor_copy(
                                out=maskT[:, j0:j1, :],
                                in_=bbf[lb][:, 0:nj * 128].rearrange("p (j t) -> p j t", t=128)),
                              r=[bkey[lb]], w=["maskT"])

                    def attention(qb):
                        qs = slice(qb * 128, (qb + 1) * 128)
                        for kv in range(2):
                            T(lambda e, kv=kv: e.matmul(banks[6 + kv][:, 0:260], lhsT=zerob[:, 0:128],
                                                        rhs=zerob[:, 0:260], start=True, stop=False,
                                                        skip_group_check=True), r=["zerob"], w=[bkey[6 + kv]])

                        def Lstage(jj):
                            ks = slice(jj * 128, (jj + 1) * 128)
                            pis = []
                            for par in range(2):
                                ps = slice(64 * par, 64 * par + 64)
                                lb = 4 + par
                                pi = pT_i[0] % 4
                                pT_i[0] += 1
                                pis.append(pi)
                                for kv in range(2):
                                    T(lambda e, ps=ps, lb=lb, kv=kv, par=par: e.matmul(
                                        banks[lb][:, kv * 256:(kv + 1) * 256], lhsT=kTz[par][:, kv, ks],
                                        rhs=qT2[:, 2 * kv:2 * kv + 2, qs], start=True, stop=True),
                                      r=["kTd", "qT2"], w=[bkey[lb]])
                                A(lambda e, lb=lb, pi=pi: e.activation(out=pT[pi][:], in_=banks[lb][:, :], func=AF.Exp,
                                                                       scale=0.125), r=[bkey[lb]], w=["pT%d" % pi])
                                V(lambda e, pi=pi: e.tensor_tensor(
                                    out=pT[pi][:].rearrange("p (h t) -> p h t", t=128),
                                    in0=pT[pi][:].rearrange("p (h t) -> p h t", t=128),
                                    in1=maskT[:, jj, :].unsqueeze(1).broadcast_to([128, 4, 128]), op=ALU.mult),
                                  r=["pT%d" % pi, "maskT"], w=["pT%d" % pi])
                            return pis

                        def PVstage(jj, pis):
                            for par in range(2):
                                pi = pis[par]
                                for kv in range(2):
                                    for ii in range(2):
                                        hl = 2 * ii + par
                                        T(lambda e, ii=ii, hl=hl, kv=kv, pi=pi: e.matmul(
                                            banks[6 + kv][:, hl * 65:hl * 65 + 65],
                                            lhsT=pT[pi][:, (kv * 2 + ii) * 128:(kv * 2 + ii + 1) * 128],
                                            rhs=v_aug[:, jj, kv, :], start=False, stop=(jj == qb),
                                            skip_group_check=True),
                                          r=["pT%d" % pi, "v_aug"], w=[bkey[6 + kv]])

                        nxt = Lstage(0)
                        for jj in range(qb + 1):
                            cur = nxt
                            if jj + 1 <= qb:
                                nxt = Lstage(jj + 1)
                            PVstage(jj, cur)
                            yield

                    def post_a(qb):
                        for kv in range(2):
                            ov = banks[6 + kv][:, 0:260].rearrange("p (h d) -> p h d", d=65)
                            V(lambda e, kv=kv, ov=ov: e.reciprocal(out=rden[:, kv * 4:(kv + 1) * 4], in_=ov[:, :, 64]),
                              r=[bkey[6 + kv]], w=["rden"])
                            V(lambda e, kv=kv, ov=ov: e.tensor_tensor(
                                out=yb[:, kv * 256:(kv + 1) * 256].rearrange("p (h d) -> p h d", d=64),
                                in0=ov[:, :, 0:64],
                                in1=rden[:, kv * 4:(kv + 1) * 4].unsqueeze(2).broadcast_to([128, 4, 64]),
                                op=ALU.mult), r=[bkey[6 + kv], "rden"], w=["yb"])
                        if b == 0 and qb == NT - 1:
                            tap("yb", yb[:], ["yb"])

                    def post_b1(qb):
                        yield
                        A(lambda e: e.activation(out=junkA[:, 0:512], in_=yb[:], func=AF.Square, accum_out=ssb_[:]),
                          r=["yb"], w=["junkA", "ssb_"])
                        yield
                        rstd(rsb[:], ssb_[:], 1, 1.0 / 512, ["ssb_"], ["rsb"])
                        yield
                        V(lambda e: e.scalar_tensor_tensor(out=mb[:], in0=yb[:], scalar=rsb[:, 0:1], in1=gob_row[:],
                                                           op0=ALU.mult, op1=ALU.mult),
                          r=["yb", "rsb", "gob_row"], w=["mb"])

                    def post_b2(qb):
                        it = b * NT + qb
                        xj = qb % 2
                        for c in range(4):
                            T(lambda e, c=c: e.transpose(out=bbf[2][:, c * 128:(c + 1) * 128],
                                                         in_=mb[:, c * 128:(c + 1) * 128], identity=identb[:]),
                              r=["mb", "identb"], w=[bkey[2]])
                        A(lambda e: e.activation(out=mbT[:], in_=bbf[2][:, 0:512].rearrange("p (c t) -> p c t", t=128),
                                                 func=AF.Copy), r=[bkey[2]], w=["mbT"])
                        yield
                        for n in range(2):
                            for k in range(8):
                                lhs = mTa[:, k, qb * 128:(qb + 1) * 128] if k < 4 else mbT[:, k - 4, :]
                                T(lambda e, n=n, k=k, lhs=lhs: e.matmul(banks[2 + n][:, :], lhsT=lhs,
                                                                        rhs=Wout[:, k, n * 512:(n + 1) * 512],
                                                                        start=(k == 0), stop=(k == 7)),
                                  r=["mTa", "mbT", "Wout"], w=[bkey[2 + n]])
                        for n in range(2):
                            A(lambda e, n=n: e.activation(out=junkA[:, 0:512], in_=banks[2 + n][:, :], func=AF.Square,
                                                          accum_out=sso[:, n:n + 1]),
                              r=[bkey[2 + n]], w=["junkA", "sso%d" % n])
                        yield
                        rstd(rso[:], sso[:, 0:1], 1, 1.0 / D, ["sso0", "sso1"], ["rso"], ss2_ap=sso[:, 1:2])
                        yield
                        for n in range(2):
                            V(lambda e, n=n: e.scalar_tensor_tensor(out=ot[:, n * 512:(n + 1) * 512],
                                                                    in0=banks[2 + n][:, :], scalar=rso[:, 0:1],
                                                                    in1=G1row[:, n * 512:(n + 1) * 512],
                                                                    op0=ALU.mult, op1=ALU.mult),
                              r=[bkey[2 + n], "rso", "G1row"], w=["ot"])
                        G(lambda e: e.tensor_tensor(out=ot[:], in0=ot[:], in1=xres[xj][:], op=ALU.add),
                          r=["ot", "xres%d" % xj], w=["ot"])
                        P.dma(x1s_d[it * 128:(it + 1) * 128, :], ot[:], reads=["ot"],
                              writes=["x1s_%d" % it])

                    def step(g_):
                        if g_ is None:
                            return False
                        try:
                            next(g_)
                            return True
                        except StopIteration:
                            return False

                    def interleave(g1, g2):
                        a1, a2 = g1 is not None, g2 is not None
                        while a1:
                            a1 = step(g1)
                            if a2:
                                a2 = step(g2)
                        return a2

                    def drain(g_):
                        while step(g_):
                            pass

                    if 0 >= KB:
                        wsel_build(0)
                        indexer(0)
                        drain(topk_iter(0))
                    topk_finish(0)
                    if 1 < NT and 1 >= KB:
                        wsel_build(1)
                    pb1 = pb2 = None
                    for qb in range(NT):
                        it = b * NT + qb
                        P.dma(xres[qb % 2][:], x_d[it * 128:(it + 1) * 128, :], writes=["xres%d" % (qb % 2)])
                        tk = None
                        if qb + 1 < NT and qb + 1 >= KB:
                            indexer(qb + 1)
                            tk = topk_iter(qb + 1)
                        if qb + 2 < NT and qb + 2 >= KB:
                            wsel_build(qb + 2)
                        att = attention(qb)
                        a_att, a_tk = True, tk is not None
                        a_p1, a_p2 = pb1 is not None, pb2 is not None
                        nstep = 0
                        while a_att:
                            a_att = step(att)
                            nstep += 1
                            if a_tk and (nstep >= 2 or qb + 1 < 3):
                                a_tk = step(tk)
                            if a_p1:
                                a_p1 = step(pb1)
                            elif a_p2:
                                a_p2 = step(pb2)
                        if a_p1:
                            drain(pb1)
                        if a_p2:
                            drain(pb2)
                        post_a(qb)
                        pb1 = post_b1(qb)
                        pb2 = post_b2(qb)
                        a_p1 = True
                        while a_tk:
                            a_tk = step(tk)
                            if a_p1:
                                a_p1 = step(pb1)
                        if qb + 1 < NT:
                            topk_finish(qb + 1)
                    drain(pb1)
                    drain(pb2)
                    P.barrier()
                    if stop == "attn":
                        P.finish()
                        return
        with contextlib.ExitStack() as bst:
            bsb = mk_sb(bst)
            W1 = bsb("W1", [128, 8, DFF], BF16)
            W2 = bsb("W2", [128, 32, D], BF16)
            wst = [bsb("wst%d" % i, [128, 2048]) for i in range(2)]
            G2row = bsb("G2row", [128, D])
            xg = [bsb("xg%d" % i, [128, D]) for i in range(4)]
            xn2 = [bsb("xn2_%d" % i, [128, D]) for i in range(1)]
            h2T = [bsb("h2T%d" % i, [128, 8, 256], BF16) for i in range(2)]
            rr = [bsb("rr%d" % i, [128, 256], BF16) for i in range(4)]
            fT = [bsb("fT%d" % i, [128, 256], BF16) for i in range(4)]
            junkB = bsb("junkB", [128, D], BF16)
            ss2 = bsb("ss2", [128, 4])
            rs2 = bsb("rs2", [128, 4])
            ssf = bsb("ssf", [128, 4])
            rsf = bsb("rsf", [128, 2])
            of = [bsb("of%d" % i, [128, D]) for i in range(2)]

            cast_engs = ["dve", "act"]
            ci = 0
            wi_ = 0
            for k in range(8):
                for hf in range(2):
                    st_ = wst[wi_ % 2]
                    sk = "wst%d" % (wi_ % 2)
                    wi_ += 1
                    P.dma(st_[:], w1_d[k * 128:(k + 1) * 128, hf * 2048:(hf + 1) * 2048], writes=[sk])
                    for q2 in range(2):
                        eng = cast_engs[ci % 2]
                        ci += 1
                        sl = slice(q2 * 1024, (q2 + 1) * 1024)
                        dl = slice(hf * 2048 + q2 * 1024, hf * 2048 + (q2 + 1) * 1024)
                        if eng == "act":
                            A(lambda e, k=k, st_=st_, sl=sl, dl=dl: e.activation(out=W1[:, k, dl], in_=st_[:, sl],
                                                                              func=AF.Copy), r=[sk], w=["W1"])
                        else:
                            P.op(eng, lambda e, k=k, st_=st_, sl=sl, dl=dl: e.tensor_copy(out=W1[:, k, dl],
                                                                                       in_=st_[:, sl]), [sk], ["W1"])
            for c2 in range(16):
                st_ = wst[wi_ % 2]
                sk = "wst%d" % (wi_ % 2)
                wi_ += 1
                P.dma(st_[:].rearrange("p (c n) -> p c n", n=D),
                      w2_d[c2 * 256:(c2 + 1) * 256, :].rearrange("(c p) n -> p c n", p=128), writes=[sk])
                for q2 in range(2):
                    eng = cast_engs[ci % 2]
                    ci += 1
                    sl = slice(q2 * 1024, (q2 + 1) * 1024)
                    if eng == "act":
                        A(lambda e, c2=c2, q2=q2, st_=st_, sl=sl: e.activation(out=W2[:, c2 * 2 + q2, :], in_=st_[:, sl],
                                                                          func=AF.Copy), r=[sk], w=["W2"])
                    else:
                        P.op(eng, lambda e, c2=c2, q2=q2, st_=st_, sl=sl: e.tensor_copy(out=W2[:, c2 * 2 + q2, :],
                                                                                   in_=st_[:, sl]), [sk], ["W2"])

            NG = NTOK // 256

            def b_load(g):
                for t in range(2):
                    it = g * 2 + t
                    xi = (g % 2) * 2 + t
                    P.dma(xg[xi][:], x1s_d[it * 128:(it + 1) * 128, :], reads=["x1s_%d" % it], writes=["xg%d" % xi])

            def b_prep_stats(g, t):
                xi = (g % 2) * 2 + t
                A(lambda e: e.activation(out=junkB[:], in_=xg[xi][:], func=AF.Square, accum_out=ss2[:, t:t + 1]),
                  r=["xg%d" % xi], w=["junkB", "ss2_%d" % t])
                rstd(rs2[:, t:t + 1], ss2[:, t:t + 1], 1, 1.0 / D, ["ss2_%d" % t], ["rs2_%d" % t])
                V(lambda e: e.tensor_scalar(out=xn2[0][:], in0=xg[xi][:], scalar1=rs2[:, t:t + 1], scalar2=None,
                                            op0=ALU.mult), r=["xg%d" % xi, "rs2_%d" % t], w=["xn2_0"])

            def b_prep_pe(g, t):
                hj = g % 2
                b = (g * 256) // S
                for half in range(2):
                    for k in range(4 * half, 4 * half + 4):
                        T(lambda e, k=k: e.transpose(out=banks[7][:, (k % 4) * 128:(k % 4 + 1) * 128],
                                                     in_=xn2[0][:, k * 128:(k + 1) * 128], identity=ident[:]),
                          r=["xn2_0", "ident"], w=[bkey[7]])
                    for k in range(4 * half, 4 * half + 4):
                        A(lambda e, k=k: e.activation(out=h2T[hj][:, k, t * 128:(t + 1) * 128],
                                                      in_=banks[7][:, (k % 4) * 128:(k % 4 + 1) * 128],
                                                      func=AF.Identity, scale=S2T[:, k, b:b + 1],
                                                      bias=sh2T[:, k, b:b + 1]),
                          r=[bkey[7], "S2T", "sh2T"], w=["h2T%d" % hj])

            def b_prep(g):
                for t in range(2):
                    b_prep_stats(g, t)
                    b_prep_pe(g, t)

            f_i = [0]

            def b_main(g):
                hj = g % 2
                b = (g * 256) // S
                if (g * 256) % S == 0:
                    for n in range(2):
                        T(lambda e, n=n: e.matmul(banks[4 + n][:, :], lhsT=sel[0:NSEQ, b, :],
                                                  rhs=gmod[0:NSEQ, 1, n * 512:(n + 1) * 512], start=True, stop=True),
                          r=["sel", "gmod1"], w=[bkey[4 + n]])
                        V(lambda e, n=n: e.tensor_copy(out=G2row[:, n * 512:(n + 1) * 512], in_=banks[4 + n][:, :]),
                          r=[bkey[4 + n]], w=["G2row"])
                def Fst(c):
                    fb = 4 + (c % 3)
                    fi = c % 4
                    for k in range(8):
                        T(lambda e, k=k: e.matmul(banks[fb][:, 0:256], lhsT=W1[:, k, c * 128:(c + 1) * 128],
                                                  rhs=h2T[hj][:, k, :], start=(k == 0), stop=(k == 7)),
                          r=["W1", "h2T%d" % hj], w=[bkey[fb]])
                    A(lambda e: e.activation(out=rr[fi][:], in_=banks[fb][:, 0:256], func=AF.Relu),
                      r=[bkey[fb]], w=["rr%d" % fi])
                    V(lambda e: e.scalar_tensor_tensor(out=fT[fi][:], in0=banks[fb][:, 0:256], scalar=0.0,
                                                       in1=rr[fi][:], op0=ALU.max, op1=ALU.mult),
                      r=[bkey[fb], "rr%d" % fi], w=["fT%d" % fi])

                def P2st(c):
                    fi = c % 4
                    for t in range(2):
                        for n in range(2):
                            ob = t * 2 + n
                            T(lambda e, t=t, n=n, ob=ob: e.matmul(
                                banks[ob][:, :], lhsT=fT[fi][:, t * 128:(t + 1) * 128],
                                rhs=W2[:, c, n * 512:(n + 1) * 512], start=(c == 0), stop=(c == 31)),
                              r=["fT%d" % fi, "W2"], w=[bkey[ob]])

                Fst(0)
                Fst(1)
                for c in range(32):
                    if c + 2 < 32:
                        Fst(c + 2)
                    P2st(c)
                    if g + 1 < NG:
                        if c == 6:
                            b_prep_stats(g + 1, 0)
                        elif c == 13:
                            b_prep_pe(g + 1, 0)
                        elif c == 15:
                            b_prep_stats(g + 1, 1)
                        elif c == 22:
                            b_prep_pe(g + 1, 1)
                for t in range(2):
                    for n in range(2):
                        A(lambda e, t=t, n=n: e.activation(out=junkB[:, 0:512], in_=banks[t * 2 + n][:, :],
                                                           func=AF.Square, accum_out=ssf[:, t * 2 + n:t * 2 + n + 1]),
                          r=[bkey[t * 2 + n]], w=["junkB", "ssf%d" % (t * 2 + n)])
                for t in range(2):
                    rstd(rsf[:, t:t + 1], ssf[:, t * 2:t * 2 + 1], 1, 1.0 / D, ["ssf%d" % (t * 2), "ssf%d" % (t * 2 + 1)],
                         ["rsf%d" % t], ss2_ap=ssf[:, t * 2 + 1:t * 2 + 2])
                for t in range(2):
                    for n in range(2):
                        V(lambda e, t=t, n=n: e.scalar_tensor_tensor(out=of[t][:, n * 512:(n + 1) * 512],
                                                                     in0=banks[t * 2 + n][:, :], scalar=rsf[:, t:t + 1],
                                                                     in1=G2row[:, n * 512:(n + 1) * 512],
                                                                     op0=ALU.mult, op1=ALU.mult),
                          r=[bkey[t * 2 + n], "rsf%d" % t, "G2row"], w=["of%d" % t])
                for t in range(2):
                    it = g * 2 + t
                    xi = (g % 2) * 2 + t
                    G(lambda e, t=t, xi=xi: e.tensor_tensor(out=of[t][:], in0=of[t][:], in1=xg[xi][:], op=ALU.add),
                      r=["of%d" % t, "xg%d" % xi], w=["of%d" % t])
                    P.dma(out_d[it * 128:(it + 1) * 128, :], of[t][:], reads=["of%d" % t])
                if g + 2 < NG:
                    b_load(g + 2)

            b_load(0)
            if NG > 1:
                b_load(1)
            b_prep(0)
            for g in range(NG):
                b_main(g)
            P.finish()
        print("program built: instrs=%d waits=%d" % (P.ninstr, P.nwaits), flush=True)


def make_core_inputs(ci, NSEQ, S, x, c, positions, w_ada, b_ada, g_pre_mix, w_in, g_sgu_v, w_spatial, b_spatial,
                     g_out_sgu, g_out_attn, w_out, g_post_mix, g_pre_ffn, w_ff1, w_ff2, g_post_ffn):
    f32 = np.float32
    bs = slice(ci * NSEQ, (ci + 1) * NSEQ)
    NT = S // 128
    xc = np.ascontiguousarray(x[bs]).reshape(NSEQ * S, D).astype(f32, copy=False)
    cc = np.asarray(c[bs], dtype=f32)
    cT = np.ascontiguousarray(cc.T.reshape(8, 128, NSEQ).transpose(1, 0, 2))
    pos = np.ascontiguousarray(np.asarray(positions[bs]).reshape(NSEQ * NT, 128).T.astype(np.int32))
    wi = np.asarray(w_in[0], dtype=f32)
    perm = np.concatenate([np.arange(0, 1792), np.arange(2304, 2376), np.arange(1792, 2304)])
    wi_p = np.ascontiguousarray(wi[:, perm])
    return {
        "x": xc, "cT": cT, "pos": pos,
        "w_ada": np.ascontiguousarray(w_ada[0], dtype=f32),
        "b_ada": np.ascontiguousarray(b_ada[0:1], dtype=f32),
        "w_in": wi_p,
        "gpre": np.ascontiguousarray(np.asarray(g_pre_mix[0], dtype=f32).reshape(8, 128).T),
        "gpre2": np.ascontiguousarray(np.asarray(g_pre_ffn[0], dtype=f32).reshape(8, 128).T),
        "gv": np.ascontiguousarray(g_sgu_v[0:1], dtype=f32),
        "ws": np.ascontiguousarray(np.asarray(w_spatial[0], dtype=f32).transpose(1, 0, 2)),
        "bs": np.ascontiguousarray(np.asarray(b_spatial[0], dtype=f32).T),
        "goa": np.ascontiguousarray(g_out_sgu[0:1], dtype=f32),
        "gob": np.ascontiguousarray(g_out_attn[0:1], dtype=f32),
        "w_out": np.ascontiguousarray(w_out[0], dtype=f32),
        "gpost": np.ascontiguousarray(g_post_mix[0:1], dtype=f32),
        "w1": np.ascontiguousarray(w_ff1[0], dtype=f32),
        "w2": np.ascontiguousarray(w_ff2[0], dtype=f32),
        "gpost2": np.ascontiguousarray(g_post_ffn[0:1], dtype=f32),
    }


def run(inputs, n_cores, NSEQ, S, taps=None, trace=False, stop=None):
    nc = bass.Bass("TRN2", target_bir_lowering=False)
    try:
        build_program(nc, NSEQ=NSEQ, S=S, taps=taps, stop=stop)
    except StopBuild:
        pass
    in_maps = [make_core_inputs(ci, NSEQ, S, **inputs) for ci in range(n_cores)]
    res = run_bass_kernel_spmd(nc, in_maps, core_ids=list(range(n_cores)), trace=trace)
    return res


def kernel(**inputs):
    inputs = {k: np.asarray(v) for k, v in inputs.items()}
    B, S, _ = inputs["x"].shape
    NSEQ = B // NCORES
    res = run(inputs, NCORES, NSEQ, S)
    outs = [np.asarray(r["out"]).reshape(NSEQ, S, D) for r in res.results]
    return np.concatenate(outs, axis=0).astype(np.float32, copy=False)
```
